# Optimizing a Trainium2 kernel written in Bass

```python
import jax, jax.numpy as jnp
from jax import lax
import numpy as np

D_MODEL = 1024
BATCH = 2
SEQ = 8192
DEPTH = 1

CHUNK = 64
MIX_WIDTH = D_MODEL
LRU_WIDTH = MIX_WIDTH // 2
LRU_BLOCKS = 8
LRU_BLOCK = LRU_WIDTH // LRU_BLOCKS
CONV_WIDTH = 4
LRU_C = 8.0
ATT_WIDTH = MIX_WIDTH - LRU_WIDTH
ATT_HEADS = 8
ATT_HEAD_DIM = ATT_WIDTH // ATT_HEADS
LEFT_CHUNKS = 8
BAND_CHUNKS = LEFT_CHUNKS + 1
REL_CLIP = 128
MEM_LEN = 256
MEM_HEADS = 4
MEM_HEAD_DIM = D_MODEL // MEM_HEADS
PEER_HEADS = 8
N_KEYS = 128
N_EXPERTS = N_KEYS * N_KEYS
PEER_TOPK = 16
PEER_KEY_DIM = 128
PEER_QUERY_DIM = 2 * PEER_KEY_DIM
PEER_BLOCK = 128
IN_WIDTH = 2 * LRU_WIDTH + 3 * ATT_WIDTH
EPS = 1e-6
NEG_INF = -1e30

kernel_name = "hybrid_rglru_chunkattn_peer_block"


def _rmsnorm(x, g):
    xf = x.astype(jnp.float32)
    y = xf * lax.rsqrt(jnp.mean(xf * xf, axis=-1, keepdims=True) + EPS)
    return (y * g.astype(jnp.float32)).astype(x.dtype)


def _rg_lru(xr, conv_w, conv_b, gate_a_w, gate_a_b, gate_x_w, gate_x_b, lru_lambda):
    B, S, _ = xr.shape
    xc = lax.conv_general_dilated(
        xr, conv_w[:, None, :], window_strides=(1,), padding=[(CONV_WIDTH - 1, 0)],
        dimension_numbers=('NWC', 'WIO', 'NWC'), feature_group_count=LRU_WIDTH) + conv_b
    xb = xc.reshape(B, S, LRU_BLOCKS, LRU_BLOCK)
    r = jax.nn.sigmoid((jnp.einsum('bshi,hij->bshj', xb, gate_a_w).reshape(B, S, LRU_WIDTH)
                        + gate_a_b).astype(jnp.float32))
    i = jax.nn.sigmoid((jnp.einsum('bshi,hij->bshj', xb, gate_x_w).reshape(B, S, LRU_WIDTH)
                        + gate_x_b).astype(jnp.float32))
    log_a = -LRU_C * r * jax.nn.softplus(-lru_lambda.astype(jnp.float32))
    a = jnp.exp(log_a)
    mult = jnp.sqrt(jnp.maximum(-jnp.expm1(2.0 * log_a), 0.0))
    b = mult * i * xc.astype(jnp.float32)

    def combine(left, right):
        a1, b1 = left
        a2, b2 = right
        return a1 * a2, a2 * b1 + b2

    _, h = lax.associative_scan(combine, (a, b), axis=1)
    return h.astype(xr.dtype)


def _chunk_attention(q, k, v, rel_bias):
    B, S, _ = q.shape
    nc = S // CHUNK
    band = BAND_CHUNKS * CHUNK

    def split(t):
        return t.reshape(B, nc, CHUNK, ATT_HEADS, ATT_HEAD_DIM)

    q, k, v = split(q), split(k), split(v)
    pad = ((0, 0), (LEFT_CHUNKS, 0), (0, 0), (0, 0), (0, 0))
    kp, vp = jnp.pad(k, pad), jnp.pad(v, pad)
    k_band = jnp.concatenate([kp[:, j:j + nc] for j in range(BAND_CHUNKS)], axis=2)
    v_band = jnp.concatenate([vp[:, j:j + nc] for j in range(BAND_CHUNKS)], axis=2)
    s = jnp.einsum('bnqhd,bnkhd->bnhqk', q, k_band).astype(jnp.float32) * (ATT_HEAD_DIM ** -0.5)
    rel = LEFT_CHUNKS * CHUNK + np.arange(CHUNK)[:, None] - np.arange(band)[None, :]
    rel_idx = np.clip(rel, -REL_CLIP, REL_CLIP) + REL_CLIP
    bias = rel_bias[:, rel_idx].astype(jnp.float32)
    key_chunk = (np.arange(nc)[:, None] - LEFT_CHUNKS
                 + (np.arange(band) // CHUNK)[None, :])
    valid = key_chunk >= 0
    s = jnp.where(valid[None, :, None, None, :], s + bias[None, None], NEG_INF)
    p = jax.nn.softmax(s, axis=-1).astype(v.dtype)
    o = jnp.einsum('bnhqk,bnkhd->bnqhd', p, v_band)
    return o.reshape(B, S, ATT_WIDTH)


def _memory_cross_attention(h, mem_n, w_q, w_kv, w_o):
    B, S, D = h.shape
    M = mem_n.shape[1]
    q = (h @ w_q).reshape(B, S, MEM_HEADS, MEM_HEAD_DIM)
    kv = (mem_n @ w_kv).reshape(B, M, 2, MEM_HEADS, MEM_HEAD_DIM)
    k, v = kv[:, :, 0], kv[:, :, 1]
    s = jnp.einsum('bshd,bmhd->bhsm', q, k).astype(jnp.float32) * (MEM_HEAD_DIM ** -0.5)
    p = jax.nn.softmax(s, axis=-1).astype(h.dtype)
    o = jnp.einsum('bhsm,bmhd->bshd', p, v).reshape(B, S, D)
    return o @ w_o


def _peer(h, w_query, sub_keys, expert_u, expert_v):
    B, S, D = h.shape
    q = (h @ w_query).reshape(B, S, PEER_HEADS, 2, PEER_KEY_DIM)
    sc = jnp.einsum('bshpk,hpnk->bshpn', q, sub_keys).astype(jnp.float32)
    v1, i1 = lax.top_k(sc[..., 0, :], PEER_TOPK)
    v2, i2 = lax.top_k(sc[..., 1, :], PEER_TOPK)
    n_cand = PEER_TOPK * PEER_TOPK
    cand = (v1[..., :, None] + v2[..., None, :]).reshape(B, S, PEER_HEADS, n_cand)
    cand_idx = (i1[..., :, None] * N_KEYS + i2[..., None, :]).reshape(B, S, PEER_HEADS, n_cand)
    top_s, pos = lax.top_k(cand, PEER_TOPK)
    idx = jnp.take_along_axis(cand_idx, pos, axis=-1)
    g = jax.nn.softmax(top_s, axis=-1).astype(h.dtype)
    n_sel = PEER_HEADS * PEER_TOPK
    nb = (B * S) // PEER_BLOCK
    hb = h.reshape(nb, PEER_BLOCK, D)
    ib = idx.reshape(nb, PEER_BLOCK, n_sel)
    gb = g.reshape(nb, PEER_BLOCK, n_sel)

    def block(args):
        xt, it, gt = args
        u = expert_u[it]
        act = jax.nn.gelu(jnp.einsum('td,tkd->tk', xt, u))
        return jnp.einsum('tk,tkd->td', gt * act, expert_v[it])

    out = lax.map(block, (hb, ib, gb))
    return out.reshape(B, S, D)


def setup_inputs(seed: int = 0) -> dict:
    key = jax.random.key(seed)
    ks = jax.random.split(key, 32)
    f32 = jnp.float32
    L = DEPTH

    def nrm(k, shape, scale):
        return jax.random.normal(k, shape, f32) * scale

    def gain(k, n):
        return 1.0 + 0.02 * jax.random.normal(k, (L, n), f32)

    a8 = jax.random.uniform(ks[10], (L, LRU_WIDTH), f32, 0.9, 0.999)
    s = a8 ** (1.0 / LRU_C)
    lru_lambda = jnp.log(s) - jnp.log1p(-s)
    return {
        "x": nrm(ks[0], (BATCH, SEQ, D_MODEL), 1.0),
        "mem": nrm(ks[1], (BATCH, MEM_LEN, D_MODEL), 1.0),
        "norm_mix": gain(ks[2], D_MODEL),
        "w_in": nrm(ks[3], (L, D_MODEL, IN_WIDTH), D_MODEL ** -0.5),
        "conv_w": nrm(ks[4], (L, CONV_WIDTH, LRU_WIDTH), CONV_WIDTH ** -0.5),
        "conv_b": nrm(ks[5], (L, LRU_WIDTH), 0.01),
        "gate_a_w": nrm(ks[6], (L, LRU_BLOCKS, LRU_BLOCK, LRU_BLOCK), LRU_BLOCK ** -0.5),
        "gate_a_b": nrm(ks[7], (L, LRU_WIDTH), 0.01),
        "gate_x_w": nrm(ks[8], (L, LRU_BLOCKS, LRU_BLOCK, LRU_BLOCK), LRU_BLOCK ** -0.5),
        "gate_x_b": nrm(ks[9], (L, LRU_WIDTH), 0.01),
        "lru_lambda": lru_lambda,
        "rel_bias": nrm(ks[11], (L, ATT_HEADS, 2 * REL_CLIP + 1), 0.5),
        "norm_grp_a": gain(ks[12], LRU_WIDTH),
        "norm_grp_b": gain(ks[13], ATT_WIDTH),
        "w_out": nrm(ks[14], (L, MIX_WIDTH, D_MODEL), MIX_WIDTH ** -0.5),
        "norm_cross": gain(ks[15], D_MODEL),
        "norm_mem": gain(ks[16], D_MODEL),
        "w_q_mem": nrm(ks[17], (L, D_MODEL, D_MODEL), D_MODEL ** -0.5),
        "w_kv_mem": nrm(ks[18], (L, D_MODEL, 2 * D_MODEL), D_MODEL ** -0.5),
        "w_o_mem": nrm(ks[19], (L, D_MODEL, D_MODEL), D_MODEL ** -0.5),
        "norm_ffn": gain(ks[20], D_MODEL),
        "w_query": nrm(ks[21], (L, D_MODEL, PEER_HEADS * PEER_QUERY_DIM), D_MODEL ** -0.5),
        "sub_keys": nrm(ks[22], (L, PEER_HEADS, 2, N_KEYS, PEER_KEY_DIM), PEER_KEY_DIM ** -0.5),
        "expert_u": nrm(ks[23], (L, N_EXPERTS, D_MODEL), D_MODEL ** -0.5),
        "expert_v": nrm(ks[24], (L, N_EXPERTS, D_MODEL), PEER_HEADS ** -0.5),
        "norm_final": 1.0 + 0.02 * jax.random.normal(ks[25], (D_MODEL,), f32),
    }


def reference(x, mem, norm_mix, w_in, conv_w, conv_b, gate_a_w, gate_a_b, gate_x_w, gate_x_b,
              lru_lambda, rel_bias, norm_grp_a, norm_grp_b, w_out, norm_cross, norm_mem,
              w_q_mem, w_kv_mem, w_o_mem, norm_ffn, w_query, sub_keys, expert_u, expert_v,
              norm_final):
    splits = [LRU_WIDTH, 2 * LRU_WIDTH, 2 * LRU_WIDTH + ATT_WIDTH, 2 * LRU_WIDTH + 2 * ATT_WIDTH]
    for l in range(DEPTH):
        hn = _rmsnorm(x, norm_mix[l])
        z = hn @ w_in[l]
        x_lru, gate, q, k, v = jnp.split(z, splits, axis=-1)
        y_a = _rg_lru(x_lru, conv_w[l], conv_b[l], gate_a_w[l], gate_a_b[l],
                      gate_x_w[l], gate_x_b[l], lru_lambda[l]) * jax.nn.gelu(gate)
        y_b = _chunk_attention(q, k, v, rel_bias[l])
        y = jnp.concatenate([_rmsnorm(y_a, norm_grp_a[l]), _rmsnorm(y_b, norm_grp_b[l])], axis=-1)
        x = x + y @ w_out[l]
        x = x + _memory_cross_attention(_rmsnorm(x, norm_cross[l]), _rmsnorm(mem, norm_mem[l]),
                                        w_q_mem[l], w_kv_mem[l], w_o_mem[l])
        x = x + _peer(_rmsnorm(x, norm_ffn[l]), w_query[l], sub_keys[l], expert_u[l], expert_v[l])
    return _rmsnorm(x, norm_final)
```

```python
import numpy as np
from contextlib import ExitStack
import concourse.bass as bass
import concourse.mybir as mybir
from concourse.bass_utils import run_bass_kernel_spmd

F32 = mybir.dt.float32
BF16 = mybir.dt.bfloat16
U32 = mybir.dt.uint32
AF = mybir.ActivationFunctionType
ALU = mybir.AluOpType
AX = mybir.AxisListType

D = 1024
EPS = 1e-6
NEG = -1e30
N_CORES = 8
SAME_ENG_SYNC = True

SM_MIX, SM_CROSS, SM_MEM, SM_FFN = 0, 8, 16, 24
SM_GA, SM_GB, SM_CB, SM_BA, SM_BX, SM_LAM, SM_CW = 32, 36, 40, 44, 48, 52, 56
NSM = 72


class Res:
    __slots__ = ("name", "w", "r", "ds")

    def __init__(self, name):
        self.name = name
        self.w = None
        self.r = {}
        self.ds = None


class KB:
    def __init__(self, nc, es):
        self.nc = nc
        self.es = es
        self.E = {}
        for nm, e in (("pe", nc.tensor), ("act", nc.scalar), ("dve", nc.vector),
                      ("pool", nc.gpsimd), ("sp", nc.sync)):
            self.E[nm] = dict(e=e, sem=es.enter_context(nc.semaphore("e_" + nm)), cnt=0, waited={}, nm=nm)
        self.free_ds = []
        self.all_ds = []
        self.n_ins = 0

    def _collect(self, r, w):
        deps = {}
        for x in r:
            if x.w is not None:
                s, v = x.w
                if deps.get(s, 0) < v:
                    deps[s] = v
        for x in w:
            if x.w is not None:
                s, v = x.w
                if deps.get(s, 0) < v:
                    deps[s] = v
            for s, v in x.r.items():
                if deps.get(s, 0) < v:
                    deps[s] = v
        return deps

    def _waits(self, E, deps, skip_own):
        for s, v in deps.items():
            if skip_own and s is E["sem"]:
                continue
            if E["waited"].get(s, 0) >= v:
                continue
            E["e"].wait_ge(s, v)
            E["waited"][s] = v

    def op(self, en, fn, r=(), w=()):
        E = self.E[en]
        skip_own = (en == "pe") or (not SAME_ENG_SYNC)
        self._waits(E, self._collect(r, w), skip_own)
        ins = fn(E["e"])
        E["cnt"] += 1
        self.n_ins += 1
        ins.then_inc(E["sem"], 1)
        tag = (E["sem"], E["cnt"])
        for x in w:
            x.w = tag
            x.r = {}
        for x in r:
            if x not in w:
                x.r[E["sem"]] = E["cnt"]
        return ins

    def dma(self, qn, out, in_, r, w, sres, **kw):
        E = self.E[qn]
        self._waits(E, self._collect(r, w), False)
        if sres.ds is None:
            if qn != "pool" and self.free_ds:
                sres.ds = self.free_ds.pop()
            else:
                sres.ds = [self.es.enter_context(self.nc.semaphore("d%d" % len(self.all_ds))), 0, qn]
                self.all_ds.append(sres.ds)
        ins = E["e"].dma_start(out=out, in_=in_, **kw)
        self.n_ins += 1
        sres.ds[1] += 1
        ins.then_inc(sres.ds[0], 16)
        val = sres.ds[1] * 16
        for x in w:
            x.w = (sres.ds[0], val)
            x.r = {}
        for x in r:
            if x not in w:
                x.r[sres.ds[0]] = val

    def barrier(self, release=()):
        deps = {}
        for nm in ("pe", "act", "dve", "pool"):
            E = self.E[nm]
            if E["cnt"] > 0:
                deps[E["sem"]] = E["cnt"]
        for ds in self.all_ds:
            if ds[1] > 0:
                deps[ds[0]] = ds[1] * 16
        for nm in ("pe", "act", "dve", "pool", "sp"):
            self._waits(self.E[nm], deps, True)
        for x in release:
            if x.ds is not None:
                if x.ds[2] != "pool":
                    self.free_ds.append(x.ds)
                x.ds = None


class Tn:
    def __init__(self, h, name, nres=1):
        self.h = h
        self.res = Res(name)
        self.rs = [Res("%s_%d" % (name, i)) for i in range(nres)] if nres > 1 else [self.res]

    def __getitem__(self, k):
        return self.h[k]


def build(NT=2048, debug=False):
    NPRE = 3 * NT
    NTILE = NT // 128
    nc = bass.Bass("TRN2", target_bir_lowering=False)
    dt_in = lambda n, s, d=F32: nc.dram_tensor(n, list(s), d, kind="ExternalInput").ap()
    xown = dt_in("xown", [NT, D])
    xprev = dt_in("xprev", [NPRE, D])
    pflag_d = dt_in("pflag", [128, NPRE // 512])
    halob_d = dt_in("halob", [128, 512])
    mem_d = dt_in("mem", [256, D])
    w_in_d = dt_in("w_in", [D, 2560])
    w_out_d = dt_in("w_out", [D, D])
    w_q_d = dt_in("w_q", [D, D])
    w_kv_d = dt_in("w_kv", [D, 2048])
    w_o_d = dt_in("w_o", [D, D])
    w_qry_d = dt_in("w_qry", [D, 2048])
    smalls_d = dt_in("smalls", [128, NSM])
    gbd_d = dt_in("gbd", [128, 8 * 128])
    abias_d = dt_in("abias", [128, 8 * 640])
    skT_d = dt_in("skT", [128, 16 * 128])
    uT_d = dt_in("uT", [16384, D])
    ev_d = dt_in("ev", [16384, D])
    gfin_d = dt_in("gfin", [128, D])
    ident_d = dt_in("ident", [128, 128])
    iota_d = dt_in("iota", [128, 128])
    y_d = nc.dram_tensor("y", [NT, D], F32, kind="ExternalOutput").ap()
    wd_d = nc.dram_tensor("wd_scratch", [NTILE, 128, 16384], BF16, kind="Internal").ap()
    dbg = {}
    if debug:
        for nm in ("dbg1", "dbg2"):
            dbg[nm] = nc.dram_tensor(nm, [NT, D], F32, kind="ExternalOutput").ap()

    with ExitStack() as es:
        k = KB(nc, es)

        def sb(ctx, name, shape, dt, nres=1):
            return Tn(ctx.enter_context(nc.sbuf_tensor("sb_" + name, list(shape), dt)), name, nres)

        def ps(ctx, name, shape, dt):
            return Tn(ctx.enter_context(nc.psum_tensor("ps_" + name, list(shape), dt)), name)

        x_res = sb(es, "x_res", [128, NTILE, D], F32, nres=NTILE)
        ident_f = sb(es, "ident_f", [128, 128], F32)
        ident_b = sb(es, "ident_b", [128, 128], BF16)
        iota_f = sb(es, "iota_f", [128, 128], F32)
        ones_b = sb(es, "ones_b", [128, 128], BF16)
        smalls = sb(es, "smalls", [128, NSM], F32)
        cst = sb(es, "cst", [128, 4], F32)
        gB = sb(es, "gB", [128, 8, 128], F32)
        nrm = [dict(ss=sb(es, "n_ss%d" % i, [128, 1], F32), sd=sb(es, "n_sd%d" % i, [128, 1], F32),
                    rstd=sb(es, "n_rstd%d" % i, [128, 1], F32), xs=sb(es, "n_xs%d" % i, [128, D], BF16),
                    junk=sb(es, "n_junk%d" % i, [128, D], BF16)) for i in range(2)]
        nrm_i = [0]

        k.dma("sp", ident_f[:], ident_d[:, :], r=[], w=[ident_f.res], sres=ident_f.res)
        k.dma("sp", iota_f[:], iota_d[:, :], r=[], w=[iota_f.res], sres=iota_f.res)
        k.dma("sp", smalls[:], smalls_d[:, :], r=[], w=[smalls.res], sres=smalls.res)
        k.op("dve", lambda e: e.tensor_copy(out=ident_b[:], in_=ident_f[:]), r=[ident_f.res], w=[ident_b.res])
        k.op("pool", lambda e: e.memset(ones_b[:], 1.0), w=[ones_b.res])
        k.op("pool", lambda e: e.memset(cst[:, 0:1], EPS), w=[cst.res])
        k.op("pool", lambda e: e.memset(cst[:, 1:2], 1.0), w=[cst.res])
        k.op("pool", lambda e: e.memset(cst[:, 2:3], 0.0), w=[cst.res])

        def set_gain(col):
            k.op("dve", lambda e: e.tensor_copy(
                out=gB[:], in_=smalls[:, col:col + 8].unsqueeze(2).to_broadcast([128, 8, 128])),
                r=[smalls.res], w=[gB.res])

        def norm_T(x_ap, x_r, pT, hT_ap, hT_r):
            n = nrm[nrm_i[0] % 2]
            nrm_i[0] += 1
            k.op("dve", lambda e: e.scalar_tensor_tensor(out=n["junk"][:], in0=x_ap, scalar=1.0, in1=x_ap,
                                                         op0=ALU.mult, op1=ALU.mult, accum_out=n["ss"][:]),
                 r=[x_r], w=[n["junk"].res, n["ss"].res])
            k.op("act", lambda e: e.activation(out=n["sd"][:], in_=n["ss"][:], func=AF.Sqrt,
                                               bias=cst[:, 0:1], scale=1.0 / D),
                 r=[n["ss"].res, cst.res], w=[n["sd"].res])
            k.op("dve", lambda e: e.reciprocal(out=n["rstd"][:], in_=n["sd"][:]), r=[n["sd"].res], w=[n["rstd"].res])
            k.op("dve", lambda e: e.tensor_scalar(out=n["xs"][:], in0=x_ap, scalar1=n["rstd"][:, 0:1], scalar2=None,
                                                  op0=ALU.mult), r=[x_r, n["rstd"].res], w=[n["xs"].res])
            for c in range(8):
                k.op("pe", lambda e, c=c: e.transpose(out=pT[:, c, :], in_=n["xs"][:, c * 128:(c + 1) * 128],
                                                      identity=ident_b[:]),
                     r=[n["xs"].res, ident_b.res], w=[pT.res])
            k.op("dve", lambda e: e.tensor_tensor(out=hT_ap, in0=pT[:], in1=gB[:], op=ALU.mult),
                 r=[pT.res, gB.res], w=[hT_r])

        def mm_group(out_ap, pairs, r, w):
            n = len(pairs)
            for i, (l, rh) in enumerate(pairs):
                k.op("pe", lambda e, l=l, rh=rh, i=i: e.matmul(out_ap, lhsT=l, rhs=rh, start=(i == 0), stop=(i == n - 1)),
                     r=r, w=w)

        def load_w_bf16(dst, src_ap, ncols):
            for c in range(8):
                k.dma("pool", dst[:, c, :], src_ap[c * 128:(c + 1) * 128, :], r=[], w=[dst.res], sres=dst.res,
                      max_dma_last_dim=4096)

        with ExitStack() as p1:
            with ExitStack() as pa:
                BA = 512
                w_inA = sb(pa, "w_inA", [128, 8, 1024], BF16)
                load_w_bf16(w_inA, w_in_d[:, 0:1024], 1024)
                w_outA = sb(pa, "w_outA", [128, 4, D], BF16)
                for c in range(4):
                    k.dma("pool", w_outA[:, c, :], w_out_d[c * 128:(c + 1) * 128, :], r=[], w=[w_outA.res], sres=w_outA.res,
                          max_dma_last_dim=4096)
                yan = sb(pa, "yan", [128, 4, 512], BF16)
                gbd_f = sb(pa, "gbd_f", [128, 8, 128], F32)
                gbd = sb(pa, "gbd", [128, 8, 128], BF16)
                k.dma("sp", gbd_f[:], gbd_d.rearrange("p (c j) -> p c j", c=8), r=[], w=[gbd_f.res], sres=gbd_f.res)
                k.op("dve", lambda e: e.tensor_copy(out=gbd[:], in_=gbd_f[:]), r=[gbd_f.res], w=[gbd.res])
                pflag = sb(pa, "pflag", [128, NPRE // 512], F32)
                k.dma("sp", pflag[:], pflag_d[:, :], r=[], w=[pflag.res], sres=pflag.res)
                cL = sb(pa, "cL", [128, 4], F32)
                tmp4 = sb(pa, "tmp4", [128, 4], F32)
                k.op("act", lambda e: e.activation(out=tmp4[:], in_=smalls[:, SM_LAM:SM_LAM + 4], func=AF.Exp, scale=-1.0),
                     r=[smalls.res], w=[tmp4.res])
                k.op("act", lambda e: e.activation(out=cL[:], in_=tmp4[:], func=AF.Ln, bias=cst[:, 1:2], scale=1.0),
                     r=[tmp4.res, cst.res], w=[cL.res])
                k.op("dve", lambda e: e.tensor_scalar(out=cL[:], in0=cL[:], scalar1=-8.0, scalar2=None, op0=ALU.mult),
                     r=[cL.res], w=[cL.res])
                set_gain(SM_MIX)
                xtmp = [sb(pa, "xtmp%d" % i, [128, D], F32) for i in range(2)]
                hT = [sb(pa, "hTa%d" % i, [128, 8, BA], BF16) for i in range(2)]
                xl = sb(pa, "xl", [128, 4, 3 + BA], F32, nres=4)
                gg = sb(pa, "gg", [128, 4, BA], F32, nres=4)
                hh = sb(pa, "hh", [128, 4, BA], F32, nres=4)
                hst = sb(pa, "hst", [128, 4], F32, nres=4)
                yaT = sb(pa, "yaT", [128, 4, BA], F32, nres=4)
                LT = [{nm: sb(pa, "l%s%d" % (nm, i), [128, BA], BF16 if nm == "xcb" else F32)
                       for nm in ("xc", "xc2", "xcb", "rr", "ii", "aa", "tt", "bb")} for i in range(2)]
                sq = sb(pa, "sq", [128, BA], BF16)
                rsn = sb(pa, "rsn", [128, BA], F32)
                pj = [ps(pa, "pj%d" % i, [128, 512], F32) for i in range(2)]
                pT = [ps(pa, "pT%d" % i, [128, 8, 128], BF16) for i in range(1)]
                pg = [ps(pa, "pg%d" % i, [128, 512], F32) for i in range(4)]
                pn = ps(pa, "pn", [128, 512], F32)
                k.op("pool", lambda e: e.memset(xl[:], 0.0), w=xl.rs)
                k.op("pool", lambda e: e.memset(hst[:], 0.0), w=hst.rs)

                nblk_pre = NPRE // BA
                nblk_own = NT // BA
                tcount = 0
                pjc = 0
                for b in range(nblk_pre + nblk_own):
                    own = b >= nblk_pre
                    ob = b - nblk_pre
                    h = hT[b % 2]
                    for t in range(4):
                        if own:
                            tile = ob * 4 + t
                            xa = x_res[:, tile, :]
                            xr = x_res.rs[tile]
                            k.dma("sp", xa, xown[tile * 128:(tile + 1) * 128, :], r=[], w=[xr], sres=xr)
                        else:
                            xt_ = xtmp[tcount % 2]
                            xa = xt_[:]
                            xr = xt_.res
                            r0 = b * BA + t * 128
                            k.dma("sp", xa, xprev[r0:r0 + 128, :], r=[], w=[xr], sres=xr)
                        norm_T(xa, xr, pT[0], h[:, :, t * 128:(t + 1) * 128], h.res)
                        tcount += 1
                    for cc in range(8 if own else 4):
                        pp = pj[pjc % 2]
                        pjc += 1
                        mm_group(pp[:], [(w_inA[:, kc, cc * 128:(cc + 1) * 128], h[:, kc, :]) for kc in range(8)],
                                 r=[w_inA.res, h.res], w=[pp.res])
                        if cc < 4:
                            k.op("act", lambda e, pp=pp, cc=cc: e.activation(out=xl[:, cc, 3:3 + BA], in_=pp[:], func=AF.Copy),
                                 r=[pp.res], w=[xl.rs[cc]])
                        else:
                            k.op("act", lambda e, pp=pp, cc=cc: e.activation(out=gg[:, cc - 4, :], in_=pp[:], func=AF.Gelu_apprx_tanh),
                                 r=[pp.res], w=[gg.rs[cc - 4]])
                    def lru_chain(cc, L, pga, pgb, own=own, b=b):
                            cw = lambda j, cc=cc: smalls[:, SM_CW + cc * 4 + j:SM_CW + cc * 4 + j + 1]
                            k.op("dve", lambda e, cc=cc, L=L, cw=cw: e.tensor_scalar(
                                out=L["xc"][:], in0=xl[:, cc, 3:3 + BA], scalar1=cw(3), scalar2=smalls[:, SM_CB + cc:SM_CB + cc + 1],
                                op0=ALU.mult, op1=ALU.add), r=[xl.rs[cc], smalls.res], w=[L["xc"].res])
                            src, dst = "xc", "xc2"
                            for j in range(3):
                                yield
                                k.op("dve", lambda e, cc=cc, L=L, cw=cw, j=j, src=src, dst=dst: e.scalar_tensor_tensor(
                                    out=L[dst][:], in0=xl[:, cc, j:j + BA], scalar=cw(j), in1=L[src][:], op0=ALU.mult, op1=ALU.add),
                                    r=[xl.rs[cc], smalls.res, L[src].res], w=[L[dst].res])
                                src, dst = dst, src
                            xc = L[src]
                            yield
                            k.op("pool", lambda e, cc=cc: e.tensor_copy(out=xl[:, cc, 0:3], in_=xl[:, cc, BA:BA + 3]),
                                 r=[xl.rs[cc]], w=[xl.rs[cc]])
                            yield
                            k.op("act", lambda e, L=L, xc=xc: e.activation(out=L["xcb"][:], in_=xc[:], func=AF.Copy),
                                 r=[xc.res], w=[L["xcb"].res])
                            yield
                            k.op("pe", lambda e, cc=cc, L=L: e.matmul(pga[:], lhsT=gbd[:, cc, :], rhs=L["xcb"][:], start=True, stop=True),
                                 r=[gbd.res, L["xcb"].res], w=[pga.res])
                            yield
                            k.op("pe", lambda e, cc=cc, L=L: e.matmul(pgb[:], lhsT=gbd[:, 4 + cc, :], rhs=L["xcb"][:], start=True, stop=True),
                                 r=[gbd.res, L["xcb"].res], w=[pgb.res])
                            yield
                            k.op("act", lambda e, cc=cc, L=L: e.activation(out=L["rr"][:], in_=pga[:], func=AF.Sigmoid,
                                                                           bias=smalls[:, SM_BA + cc:SM_BA + cc + 1], scale=1.0),
                                 r=[pga.res, smalls.res], w=[L["rr"].res])
                            yield
                            k.op("act", lambda e, cc=cc, L=L: e.activation(out=L["ii"][:], in_=pgb[:], func=AF.Sigmoid,
                                                                           bias=smalls[:, SM_BX + cc:SM_BX + cc + 1], scale=1.0),
                                 r=[pgb.res, smalls.res], w=[L["ii"].res])
                            yield
                            k.op("act", lambda e, cc=cc, L=L: e.activation(out=L["aa"][:], in_=L["rr"][:], func=AF.Exp,
                                                                           scale=cL[:, cc:cc + 1]),
                                 r=[L["rr"].res, cL.res], w=[L["aa"].res])
                            yield
                            k.op("dve", lambda e, L=L: e.tensor_tensor(out=L["tt"][:], in0=L["aa"][:], in1=L["aa"][:], op=ALU.mult),
                                 r=[L["aa"].res], w=[L["tt"].res])
                            yield
                            k.op("act", lambda e, L=L: e.activation(out=L["tt"][:], in_=L["tt"][:], func=AF.Sqrt,
                                                                    bias=cst[:, 1:2], scale=-1.0),
                                 r=[L["tt"].res, cst.res], w=[L["tt"].res])
                            yield
                            k.op("dve", lambda e, L=L: e.tensor_tensor(out=L["bb"][:], in0=L["tt"][:], in1=L["ii"][:], op=ALU.mult),
                                 r=[L["tt"].res, L["ii"].res], w=[L["bb"].res])
                            yield
                            k.op("dve", lambda e, L=L, xc=xc: e.tensor_tensor(out=L["rr"][:], in0=L["bb"][:], in1=xc[:], op=ALU.mult),
                                 r=[L["bb"].res, xc.res], w=[L["rr"].res])
                            yield
                            k.op("dve", lambda e, cc=cc, L=L: e.tensor_tensor_scan(
                                out=hh[:, cc, :], data0=L["aa"][:], data1=L["rr"][:], initial=hst[:, cc:cc + 1],
                                op0=ALU.mult, op1=ALU.add), r=[L["aa"].res, L["rr"].res, hst.rs[cc]], w=[hh.rs[cc]])
                            if own:
                                yield
                                k.op("dve", lambda e, cc=cc: e.tensor_copy(out=hst[:, cc:cc + 1], in_=hh[:, cc, BA - 1:BA]),
                                     r=[hh.rs[cc]], w=[hst.rs[cc]])
                                yield
                                k.op("dve", lambda e, cc=cc: e.tensor_tensor(out=yaT[:, cc, :], in0=hh[:, cc, :], in1=gg[:, cc, :], op=ALU.mult),
                                     r=[hh.rs[cc], gg.rs[cc]], w=[yaT.rs[cc]])
                            else:
                                yield
                                k.op("dve", lambda e, cc=cc, b=b: e.tensor_tensor(out=hst[:, cc:cc + 1], in0=hh[:, cc, BA - 1:BA],
                                                                                  in1=pflag[:, b:b + 1], op=ALU.mult),
                                     r=[hh.rs[cc], pflag.res], w=[hst.rs[cc]])

                    for pair in range(2):
                        gens = [lru_chain(2 * pair + i_, LT[i_], pg[2 * i_], pg[2 * i_ + 1]) for i_ in range(2)]
                        while gens:
                            for g_ in list(gens):
                                try:
                                    next(g_)
                                except StopIteration:
                                    gens.remove(g_)
                    if own:
                        for cc in range(4):
                            k.op("dve", lambda e, cc=cc: e.tensor_tensor(out=sq[:], in0=yaT[:, cc, :], in1=yaT[:, cc, :], op=ALU.mult),
                                 r=[yaT.rs[cc]], w=[sq.res])
                            k.op("pe", lambda e, cc=cc: e.matmul(pn[:], lhsT=ones_b[:], rhs=sq[:], start=(cc == 0), stop=(cc == 3)),
                                 r=[ones_b.res, sq.res], w=[pn.res])
                        k.op("act", lambda e: e.activation(out=rsn[:], in_=pn[:], func=AF.Sqrt, bias=cst[:, 0:1], scale=1.0 / 512),
                             r=[pn.res, cst.res], w=[rsn.res])
                        k.op("dve", lambda e: e.reciprocal(out=rsn[:], in_=rsn[:]), r=[rsn.res], w=[rsn.res])
                        for cc in range(4):
                            k.op("dve", lambda e, cc=cc, ob=ob: e.scalar_tensor_tensor(
                                out=yan[:, cc, :], in0=yaT[:, cc, :],
                                scalar=smalls[:, SM_GA + cc:SM_GA + cc + 1], in1=rsn[:], op0=ALU.mult, op1=ALU.mult),
                                r=[yaT.rs[cc], smalls.res, rsn.res], w=[yan.res])
                        for t in range(4):
                            tile = ob * 4 + t
                            for half in range(2):
                                pp = pj[pjc % 2]
                                pjc += 1
                                mm_group(pp[:], [(yan[:, c, t * 128:(t + 1) * 128], w_outA[:, c, half * 512:(half + 1) * 512]) for c in range(4)],
                                         r=[yan.res, w_outA.res], w=[pp.res])
                                k.op("dve", lambda e, pp=pp, tile=tile, half=half: e.tensor_tensor(
                                    out=x_res[:, tile, half * 512:(half + 1) * 512], in0=pp[:], in1=x_res[:, tile, half * 512:(half + 1) * 512],
                                    op=ALU.add), r=[pp.res, x_res.rs[tile]], w=[x_res.rs[tile]])
                k.barrier(release=[w_inA.res, w_outA.res, gbd_f.res, pflag.res] + [t_.res for t_ in xtmp])

            with ExitStack() as pb:
                BB = 256
                w_inB = sb(pb, "w_inB", [128, 8, 1536], BF16)
                load_w_bf16(w_inB, w_in_d[:, 1024:2560], 1536)
                w_out = sb(pb, "w_outB", [128, 4, D], BF16)
                for c in range(4):
                    k.dma("pool", w_out[:, c, :], w_out_d[512 + c * 128:512 + (c + 1) * 128, :], r=[], w=[w_out.res], sres=w_out.res,
                          max_dma_last_dim=4096)
                abias = sb(pb, "abias", [128, 8, 640], F32)
                k.dma("sp", abias[:], abias_d.rearrange("p (h c) -> p h c", h=8), r=[], w=[abias.res], sres=abias.res)
                halob = sb(pb, "halob", [128, 512], F32)
                k.dma("sp", halob[:], halob_d[:, :], r=[], w=[halob.res], sres=halob.res)
                xtmp = [sb(pb, "xtmpb%d" % i, [128, D], F32) for i in range(2)]
                hT = [sb(pb, "hTb%d" % i, [128, 8, BB], BF16) for i in range(2)]
                qA_l = [sb(pb, "qA%d" % i, [128, 4, BB], BF16) for i in range(2)]
                qB_l = [sb(pb, "qB%d" % i, [128, 4, BB], BF16) for i in range(2)]
                kT = [sb(pb, "kT%d" % i, [128, 4, BB], BF16) for i in range(4)]
                vpad = [sb(pb, "vpad%d" % i, [128, 8, 128], BF16) for i in range(8)]
                ybT = sb(pb, "ybT", [128, 4, BB], F32)
                ybn = sb(pb, "ybn", [128, 4, BB], BF16)
                sbuf_s = [sb(pb, "sbs%d" % i, [128, 640], F32) for i in range(2)]
                Pm = [sb(pb, "Pm%d" % i, [128, 640], BF16) for i in range(2)]
                Pn = [sb(pb, "Pn%d" % i, [128, 640], BF16) for i in range(2)]
                PT = [sb(pb, "PT%d" % i, [128, 5, 128], BF16) for i in range(2)]
                st = [dict(mx=sb(pb, "a_mx%d" % i, [128, 1], F32), rs=sb(pb, "a_rs%d" % i, [128, 1], F32),
                           ri=sb(pb, "a_ri%d" % i, [128, 1], F32)) for i in range(2)]
                sq = sb(pb, "sqb", [128, BB], BF16)
                rsn = sb(pb, "rsnb", [128, BB], F32)
                pj = [ps(pb, "pjb%d" % i, [128, 512], F32) for i in range(2)]
                pT = [ps(pb, "pTb%d" % i, [128, 8, 128], BF16) for i in range(1)]
                pSm = [ps(pb, "pSm%d" % i, [128, 512], F32) for i in range(2)]
                pSr = Tn(pb.enter_context(nc.psum_tensor("ps_pSr", [128, 4, 128], F32)), "pSr", nres=4)
                pPT = ps(pb, "pPT", [128, 5, 128], BF16)
                pO = ps(pb, "pO", [128, 4, 128], F32)
                pn = pj[0]
                for v_ in vpad:
                    k.op("pool", lambda e, v_=v_: e.memset(v_[:], 0.0), w=[v_.res])
                for q_ in qA_l + qB_l:
                    k.op("pool", lambda e, q_=q_: e.memset(q_[:], 0.0), w=[q_.res])
                set_gain(SM_MIX)

                nhalo = 512 // BB
                nown = NT // BB
                tcount = 0
                pjc = 0
                ac = 0
                def prep(b):
                    nonlocal tcount, pjc
                    own = b >= nhalo
                    ob = b - nhalo
                    h = hT[b % 2]
                    kcur = kT[b % 4]
                    qA, qB = qA_l[b % 2], qB_l[b % 2]
                    for t in range(2):
                        xt_ = xtmp[tcount % 2]
                        xa = xt_[:]
                        xr = xt_.res
                        if own:
                            r0 = ob * BB + t * 128
                            k.dma("sp", xa, xown[r0:r0 + 128, :], r=[], w=[xr], sres=xr)
                        else:
                            r0 = NPRE - 512 + b * BB + t * 128
                            k.dma("sp", xa, xprev[r0:r0 + 128, :], r=[], w=[xr], sres=xr)
                        norm_T(xa, xr, pT[0], h[:, :, t * 128:(t + 1) * 128], h.res)
                        tcount += 1
                        yield
                    for m in range(4):
                        pp = pj[pjc % 2]
                        pjc += 1
                        mm_group(pp[:, 0:BB], [(w_inB[:, kc, 512 + m * 128:512 + (m + 1) * 128], h[:, kc, :]) for kc in range(8)],
                                 r=[w_inB.res, h.res], w=[pp.res])
                        k.op("act", lambda e, pp=pp, m=m, kcur=kcur: e.activation(out=kcur[:, m, :], in_=pp[:, 0:BB], func=AF.Copy),
                             r=[pp.res], w=[kcur.res])
                    yield
                    for t in range(2):
                        yield
                        gt = b * 2 + t
                        vp = vpad[gt % 8]
                        pp = pj[pjc % 2]
                        pjc += 1
                        mm_group(pp[:], [(h[:, kc, t * 128:(t + 1) * 128], w_inB[:, kc, 1024:1536]) for kc in range(8)],
                                 r=[w_inB.res, h.res], w=[pp.res])
                        ppv = pp[:].rearrange("p (h d) -> p h d", h=8)
                        k.op("act", lambda e, vp=vp, ppv=ppv: e.activation(out=vp[:, 0:8:2, 0:64], in_=ppv[:, 0:8:2, :], func=AF.Copy),
                             r=[pp.res], w=[vp.res])
                        k.op("act", lambda e, vp=vp, ppv=ppv: e.activation(out=vp[:, 1:8:2, 64:128], in_=ppv[:, 1:8:2, :], func=AF.Copy),
                             r=[pp.res], w=[vp.res])
                    if not own:
                        return
                    yield
                    for m in range(4):
                        if m == 2:
                            yield
                        pp = pj[pjc % 2]
                        pjc += 1
                        mm_group(pp[:, 0:BB], [(w_inB[:, kc, m * 128:(m + 1) * 128], h[:, kc, :]) for kc in range(8)],
                                 r=[w_inB.res, h.res], w=[pp.res])
                        k.op("act", lambda e, pp=pp, m=m: e.activation(out=qA[0:64, m, :], in_=pp[0:64, 0:BB], func=AF.Copy),
                             r=[pp.res], w=[qA.res])
                        k.op("act", lambda e, pp=pp, m=m: e.activation(out=qB[64:128, m, :], in_=pp[64:128, 0:BB], func=AF.Copy),
                             r=[pp.res], w=[qB.res])
                def attend(b, fillers):
                    nonlocal pjc, ac
                    ob = b - nhalo
                    qA, qB = qA_l[b % 2], qB_l[b % 2]
                    units = []
                    for p in range(2):
                        pieces = []
                        oc = 0
                        remaining = 640
                        pos = 128 * p
                        while remaining > 0:
                            bi = pos // BB
                            c0 = pos % BB
                            n = min(BB - c0, remaining)
                            if oc < 512 and oc + n > 512:
                                n = 512 - oc
                            pieces.append((kT[(b - 2 + bi) % 4], c0, n, oc))
                            oc += n
                            pos += n
                            remaining -= n
                        for hd in range(8):
                            units.append((p, hd, pieces))

                    def S1(u):
                        p, hd, pieces = units[u]
                        m = hd // 2
                        qs = (qA if hd % 2 == 0 else qB)
                        gi = ac0 + u
                        pm, pr = pSm[gi % 2], pSr
                        for (kt_, c0, n, oc) in pieces:
                            if oc < 512:
                                oap = pm[:, oc:oc + n]
                                ores = pm.res
                            else:
                                oap = pr[:, gi % 4, oc - 512:oc - 512 + n]
                                ores = pr.res
                            k.op("pe", lambda e, kt_=kt_, c0=c0, n=n, oap=oap: e.matmul(
                                oap, lhsT=qs[:, m, p * 128:(p + 1) * 128], rhs=kt_[:, m, c0:c0 + n],
                                start=True, stop=True), r=[qs.res, kt_.res], w=[ores])

                    def S2(u, part):
                        p, hd, pieces = units[u]
                        gi = ac0 + u
                        i2 = gi % 2
                        pm, pr = pSm[gi % 2], pSr
                        S, PP, PN, s_ = sbuf_s[i2], Pm[i2], Pn[i2], st[i2]
                        if part == 1:
                            k.op("dve", lambda e: e.reciprocal(out=s_["ri"][:], in_=s_["rs"][:]), r=[s_["rs"].res], w=[s_["ri"].res])
                            k.op("dve", lambda e: e.tensor_scalar(out=PN[:], in0=PP[:], scalar1=s_["ri"][:, 0:1],
                                                                  scalar2=None, op0=ALU.mult),
                                 r=[PP.res, s_["ri"].res], w=[PN.res])
                            return
                        k.op("dve", lambda e: e.scalar_tensor_tensor(
                            out=S[:, 0:512], in0=pm[:], scalar=0.125, in1=abias[:, hd, 0:512], op0=ALU.mult, op1=ALU.add),
                            r=[pm.res, abias.res], w=[S.res])
                        k.op("dve", lambda e: e.scalar_tensor_tensor(
                            out=S[:, 512:640], in0=pr[:, gi % 4, :], scalar=0.125, in1=abias[:, hd, 512:640], op0=ALU.mult, op1=ALU.add),
                            r=[pr.res, abias.res], w=[S.res])
                        cnt = 512 - ob * BB - 128 * p
                        if cnt > 0:
                            hb0 = ob * BB + 128 * p
                            k.op("dve", lambda e: e.tensor_tensor(
                                out=S[:, 0:cnt], in0=S[:, 0:cnt], in1=halob[:, hb0:512], op=ALU.add),
                                r=[S.res, halob.res], w=[S.res])
                        k.op("dve", lambda e: e.tensor_reduce(out=s_["mx"][:], in_=S[:], axis=AX.X, op=ALU.max, negate=True),
                             r=[S.res], w=[s_["mx"].res])
                        k.op("act", lambda e: e.activation(out=PP[:], in_=S[:], func=AF.Exp, bias=s_["mx"][:, 0:1],
                                                           scale=1.0, accum_out=s_["rs"][:]),
                             r=[S.res, s_["mx"].res], w=[PP.res, s_["rs"].res])

                    def S3(u):
                        p, hd, pieces = units[u]
                        m = hd // 2
                        gi = ac0 + u
                        i2 = gi % 2
                        PN, PTs = Pn[i2], PT[i2]
                        for kc in range(5):
                            k.op("pe", lambda e, kc=kc: e.transpose(out=pPT[:, kc, :], in_=PN[:, kc * 128:(kc + 1) * 128],
                                                                    identity=ident_b[:]),
                                 r=[PN.res, ident_b.res], w=[pPT.res])
                        k.op("act", lambda e: e.activation(out=PTs[:], in_=pPT[:], func=AF.Copy), r=[pPT.res], w=[PTs.res])
                        g0 = (b * 2 + p) - 4
                        for kc in range(5):
                            vp = vpad[(g0 + kc) % 8]
                            k.op("pe", lambda e, vp=vp, kc=kc: e.matmul(
                                pO[:, m, :], lhsT=vp[:, hd, :], rhs=PTs[:, kc, :],
                                start=(hd % 2 == 0 and kc == 0), stop=(hd % 2 == 1 and kc == 4)),
                                r=[vp.res, PTs.res], w=[pO.res])
                        if hd == 7:
                            k.op("act", lambda e: e.activation(out=ybT[:, :, p * 128:(p + 1) * 128], in_=pO[:], func=AF.Copy),
                                 r=[pO.res], w=[ybT.res])

                    def advance():
                        while fillers:
                            try:
                                next(fillers[0])
                                return
                            except StopIteration:
                                fillers.pop(0)

                    ac0 = ac
                    S1(0)
                    S2(0, 0)
                    for u in range(len(units)):
                        if u + 1 < len(units):
                            S1(u + 1)
                            S2(u + 1, 0)
                        S2(u, 1)
                        S3(u)
                        advance()
                    while fillers:
                        advance()
                    ac += len(units)

                def tail(b):
                    nonlocal pjc
                    ob = b - nhalo
                    for cc in range(4):
                        k.op("dve", lambda e, cc=cc: e.tensor_tensor(out=sq[:], in0=ybT[:, cc, :], in1=ybT[:, cc, :], op=ALU.mult),
                             r=[ybT.res], w=[sq.res])
                        k.op("pe", lambda e, cc=cc: e.matmul(pn[:, 0:BB], lhsT=ones_b[:], rhs=sq[:], start=(cc == 0), stop=(cc == 3)),
                             r=[ones_b.res, sq.res], w=[pn.res])
                    k.op("act", lambda e: e.activation(out=rsn[:], in_=pn[:, 0:BB], func=AF.Sqrt, bias=cst[:, 0:1], scale=1.0 / 512),
                         r=[pn.res, cst.res], w=[rsn.res])
                    k.op("dve", lambda e: e.reciprocal(out=rsn[:], in_=rsn[:]), r=[rsn.res], w=[rsn.res])
                    for cc in range(4):
                        k.op("dve", lambda e, cc=cc: e.scalar_tensor_tensor(
                            out=ybn[:, cc, :], in0=ybT[:, cc, :], scalar=smalls[:, SM_GB + cc:SM_GB + cc + 1], in1=rsn[:],
                            op0=ALU.mult, op1=ALU.mult), r=[ybT.res, smalls.res, rsn.res], w=[ybn.res])
                    yield
                    for t in range(2):
                        tile = ob * 2 + t
                        tok0 = ob * BB + t * 128
                        for half in range(2):
                            pp = pj[pjc % 2]
                            pjc += 1
                            pairs = [(ybn[:, c, t * 128:(t + 1) * 128], w_out[:, c, half * 512:(half + 1) * 512]) for c in range(4)]
                            mm_group(pp[:], pairs, r=[ybn.res, w_out.res], w=[pp.res])
                            k.op("dve", lambda e, pp=pp, tile=tile, half=half: e.tensor_tensor(
                                out=x_res[:, tile, half * 512:(half + 1) * 512], in0=pp[:], in1=x_res[:, tile, half * 512:(half + 1) * 512],
                                op=ALU.add), r=[pp.res, x_res.rs[tile]], w=[x_res.rs[tile]])
                        yield
                nb_ = nhalo + nown
                for b0 in range(nhalo + 1):
                    for _ in prep(b0):
                        pass
                for b in range(nhalo, nb_):
                    fl = []
                    if b - 1 >= nhalo:
                        fl.append(tail(b - 1))
                    if b + 1 < nb_:
                        fl.append(prep(b + 1))
                    attend(b, fl)
                for _ in tail(nb_ - 1):
                    pass
                k.barrier(release=[w_inB.res, w_out.res, abias.res, halob.res] + [t_.res for t_ in xtmp])

        if debug:
            for tile in range(NTILE):
                k.dma("sp", dbg["dbg1"][tile * 128:(tile + 1) * 128, :], x_res[:, tile, :], r=[x_res.rs[tile]], w=[], sres=x_res.rs[tile])

        with ExitStack() as p2:
            B2 = 512
            w_q = sb(p2, "w_q", [128, 8, D], BF16)
            load_w_bf16(w_q, w_q_d[:, :], D)
            w_o = sb(p2, "w_o", [128, 8, D], BF16)
            load_w_bf16(w_o, w_o_d[:, :], D)
            kmT = sb(p2, "kmT", [128, 8, 256], BF16)
            vmem = sb(p2, "vmem", [128, 2, D], BF16)
            pj = [ps(p2, "pj2%d" % i, [128, 512], F32) for i in range(2)]
            pT = [ps(p2, "pT2%d" % i, [128, 8, 128], BF16) for i in range(1)]
            pS2 = [ps(p2, "pS2%d" % i, [128, 4, 256], F32) for i in range(2)]
            pPT2 = ps(p2, "pPT2", [128, 8, 128], BF16)
            pjc = 0
            with ExitStack() as p2s:
                w_kv = sb(p2s, "w_kv", [128, 8, 2048], BF16)
                load_w_bf16(w_kv, w_kv_d[:, :], 2048)
                memt = [sb(p2s, "memt%d" % i, [128, D], F32) for i in range(2)]
                memT = sb(p2s, "memT", [128, 8, 256], BF16)
                set_gain(SM_MEM)
                for t in range(2):
                    k.dma("sp", memt[t][:], mem_d[t * 128:(t + 1) * 128, :], r=[], w=[memt[t].res], sres=memt[t].res)
                    norm_T(memt[t][:], memt[t].res, pT[0], memT[:, :, t * 128:(t + 1) * 128], memT.res)
                for oc in range(8):
                    pp = pj[pjc % 2]
                    pjc += 1
                    mm_group(pp[:, 0:256], [(w_kv[:, kc, oc * 128:(oc + 1) * 128], memT[:, kc, :]) for kc in range(8)],
                             r=[w_kv.res, memT.res], w=[pp.res])
                    k.op("act", lambda e, pp=pp, oc=oc: e.activation(out=kmT[:, oc, :], in_=pp[:, 0:256], func=AF.Copy),
                         r=[pp.res], w=[kmT.res])
                for t in range(2):
                    for half in range(2):
                        pp = pj[pjc % 2]
                        pjc += 1
                        mm_group(pp[:], [(memT[:, kc, t * 128:(t + 1) * 128], w_kv[:, kc, 1024 + half * 512:1024 + (half + 1) * 512])
                                         for kc in range(8)], r=[w_kv.res, memT.res], w=[pp.res])
                        k.op("act", lambda e, pp=pp, t=t, half=half: e.activation(out=vmem[:, t, half * 512:(half + 1) * 512], in_=pp[:], func=AF.Copy),
                             r=[pp.res], w=[vmem.res])
                k.barrier(release=[w_kv.res] + [t_.res for t_ in memt])
            hT = [sb(p2, "hT2%d" % i, [128, 8, B2], BF16) for i in range(2)]
            qT = sb(p2, "qT2", [128, 8, B2], BF16)
            P2 = [sb(p2, "P2%d" % i, [128, 4, 256], BF16) for i in range(2)]
            P2n = [sb(p2, "P2n%d" % i, [128, 4, 256], BF16) for i in range(2)]
            PT2 = sb(p2, "PT2", [128, 4, 2, B2], BF16)
            oT = sb(p2, "oT2", [128, 8, B2], BF16)
            st2 = [dict(mx=sb(p2, "c_mx%d" % i, [128, 4], F32), rs=sb(p2, "c_rs%d" % i, [128, 4], F32),
                        ri=sb(p2, "c_ri%d" % i, [128, 4], F32)) for i in range(2)]
            set_gain(SM_CROSS)
            tc2 = 0
            for b in range(NT // B2):
                h = hT[b % 2]
                for t in range(4):
                    tile = b * 4 + t
                    norm_T(x_res[:, tile, :], x_res.rs[tile], pT[0], h[:, :, t * 128:(t + 1) * 128], h.res)
                for oc in range(8):
                    pp = pj[pjc % 2]
                    pjc += 1
                    mm_group(pp[:], [(w_q[:, kc, oc * 128:(oc + 1) * 128], h[:, kc, :]) for kc in range(8)],
                             r=[w_q.res, h.res], w=[pp.res])
                    k.op("act", lambda e, pp=pp, oc=oc: e.activation(out=qT[:, oc, :], in_=pp[:], func=AF.Copy), r=[pp.res], w=[qT.res])
                def c_S1(t, i2):
                    pS_ = pS2[i2]
                    for hh_ in range(4):
                        mm_group(pS_[:, hh_, :], [(qT[:, 2 * hh_ + j, t * 128:(t + 1) * 128], kmT[:, 2 * hh_ + j, :]) for j in range(2)],
                                 r=[qT.res, kmT.res], w=[pS_.res])

                def c_S2(t, i2):
                    pS_, P_, Pn_, s_ = pS2[i2], P2[i2], P2n[i2], st2[i2]
                    k.op("dve", lambda e: e.tensor_reduce(out=s_["mx"][:], in_=pS_[:], axis=AX.X, op=ALU.max, negate=True),
                         r=[pS_.res], w=[s_["mx"].res])
                    k.op("dve", lambda e: e.tensor_scalar(out=s_["mx"][:], in0=s_["mx"][:], scalar1=1.0 / 16, scalar2=None, op0=ALU.mult),
                         r=[s_["mx"].res], w=[s_["mx"].res])
                    for hh_ in range(4):
                        k.op("act", lambda e, hh_=hh_: e.activation(
                            out=P_[:, hh_, :], in_=pS_[:, hh_, :], func=AF.Exp, bias=s_["mx"][:, hh_:hh_ + 1], scale=1.0 / 16,
                            accum_out=s_["rs"][:, hh_:hh_ + 1]), r=[pS_.res, s_["mx"].res], w=[P_.res, s_["rs"].res])
                    k.op("dve", lambda e: e.reciprocal(out=s_["ri"][:], in_=s_["rs"][:]), r=[s_["rs"].res], w=[s_["ri"].res])
                    k.op("dve", lambda e: e.tensor_tensor(
                        out=Pn_[:], in0=P_[:], in1=s_["ri"][:, :].unsqueeze(2).to_broadcast([128, 4, 256]), op=ALU.mult),
                        r=[P_.res, s_["ri"].res], w=[Pn_.res])

                def c_S3(t, i2):
                    Pn_ = P2n[i2]
                    for hh_ in range(4):
                        for mc in range(2):
                            k.op("pe", lambda e, hh_=hh_, mc=mc: e.transpose(
                                out=pPT2[:, hh_ * 2 + mc, :], in_=Pn_[:, hh_, mc * 128:(mc + 1) * 128], identity=ident_b[:]),
                                r=[Pn_.res, ident_b.res], w=[pPT2.res])
                    k.op("act", lambda e: e.activation(out=PT2[:, :, :, t * 128:(t + 1) * 128],
                                                       in_=pPT2[:].rearrange("p (h m) t -> p h m t", h=4), func=AF.Copy),
                         r=[pPT2.res], w=[PT2.res])

                c_S1(0, tc2 % 2)
                for t in range(4):
                    if t + 1 < 4:
                        c_S1(t + 1, (tc2 + 1) % 2)
                    c_S2(t, tc2 % 2)
                    c_S3(t, tc2 % 2)
                    tc2 += 1
                for oc in range(8):
                    pp = pj[pjc % 2]
                    pjc += 1
                    mm_group(pp[:], [(vmem[:, mc, oc * 128:(oc + 1) * 128], PT2[:, oc // 2, mc, :]) for mc in range(2)],
                             r=[vmem.res, PT2.res], w=[pp.res])
                    k.op("act", lambda e, pp=pp, oc=oc: e.activation(out=oT[:, oc, :], in_=pp[:], func=AF.Copy), r=[pp.res], w=[oT.res])
                for t in range(4):
                    tile = b * 4 + t
                    for half in range(2):
                        pp = pj[pjc % 2]
                        pjc += 1
                        mm_group(pp[:], [(oT[:, c, t * 128:(t + 1) * 128], w_o[:, c, half * 512:(half + 1) * 512]) for c in range(8)],
                                 r=[oT.res, w_o.res], w=[pp.res])
                        k.op("dve", lambda e, pp=pp, tile=tile, half=half: e.tensor_tensor(
                            out=x_res[:, tile, half * 512:(half + 1) * 512], in0=pp[:], in1=x_res[:, tile, half * 512:(half + 1) * 512],
                            op=ALU.add), r=[pp.res, x_res.rs[tile]], w=[x_res.rs[tile]])
            k.barrier(release=[w_q.res, w_o.res])

        if debug:
            for tile in range(NTILE):
                k.dma("sp", dbg["dbg2"][tile * 128:(tile + 1) * 128, :], x_res[:, tile, :], r=[x_res.rs[tile]], w=[], sres=x_res.rs[tile])

        with ExitStack() as p3:
            p3s = p3.enter_context(ExitStack())
            slotI = sb(p3s, "slotI", [128, NT], F32)
            slotJ = sb(p3s, "slotJ", [128, NT], F32)
            slotG = sb(p3s, "slotG", [128, NT], F32)
            set_gain(SM_FFN)
            with ExitStack() as pa:
                B3 = 256
                w_qry = sb(pa, "w_qry", [128, 8, 2048], BF16)
                load_w_bf16(w_qry, w_qry_d[:, :], 2048)
                skb = sb(pa, "skb", [128, 16, 128], BF16)
                k.dma("pool", skb[:], skT_d.rearrange("p (o n) -> p o n", o=16), r=[], w=[skb.res], sres=skb.res)
                hT = [sb(pa, "hT3%d" % i, [128, 8, B3], BF16) for i in range(2)]
                qpT = sb(pa, "qpT", [128, 16, B3], BF16)
                sc = sb(pa, "sc", [128, 16, 128], F32, nres=4)
                sc2 = sb(pa, "sc2", [128, 16, 128], F32, nres=16)
                tv = sb(pa, "tv", [128, 16, 16], F32, nres=16)
                tiu = sb(pa, "tiu", [128, 16, 16], U32, nres=16)
                tif = sb(pa, "tif", [128, 16, 16], F32)
                cand = sb(pa, "cand", [128, 8, 256], F32)
                cand2 = sb(pa, "cand2", [128, 8, 256], F32, nres=8)
                ts = sb(pa, "ts", [128, 8, 16], F32, nres=8)
                posu = sb(pa, "posu", [128, 8, 16], U32, nres=8)
                au = sb(pa, "au", [128, 8, 16], U32)
                bu = sb(pa, "bu", [128, 8, 16], U32)
                af = sb(pa, "af", [128, 8, 16], F32)
                bf = sb(pa, "bf", [128, 8, 16], F32)
                eq = sb(pa, "eq", [128, 8, 16, 16], F32)
                isel = sb(pa, "isel", [128, 8, 16], F32)
                jsel = sb(pa, "jsel", [128, 8, 16], F32)
                gsel = sb(pa, "gsel", [128, 8, 16], F32)
                sm = sb(pa, "sm", [128, 8], F32)
                pj = [ps(pa, "pj3%d" % i, [128, 512], F32) for i in range(2)]
                pT = [ps(pa, "pT3%d" % i, [128, 8, 128], BF16) for i in range(1)]
                psc = [ps(pa, "psc%d" % i, [128, 4, 128], F32) for i in range(2)]
                pTs = ps(pa, "pTs", [128, 3, 128], F32)
                pjc = 0
                scc = 0
                iota16 = iota_f[:, 0:16]
                for b in range(NT // B3):
                    h = hT[b % 2]
                    for t in range(2):
                        tile = b * 2 + t
                        norm_T(x_res[:, tile, :], x_res.rs[tile], pT[0], h[:, :, t * 128:(t + 1) * 128], h.res)
                    for oc in range(16):
                        pp = pj[pjc % 2]
                        pjc += 1
                        mm_group(pp[:, 0:B3], [(w_qry[:, kc, oc * 128:(oc + 1) * 128], h[:, kc, :]) for kc in range(8)],
                                 r=[w_qry.res, h.res], w=[pp.res])
                        k.op("act", lambda e, pp=pp, oc=oc: e.activation(out=qpT[:, oc, :], in_=pp[:, 0:B3], func=AF.Copy),
                             r=[pp.res], w=[qpT.res])
                    for t in range(2):
                        tile = b * 2 + t
                        for g4 in range(4):
                            pq = psc[scc % 2]
                            scc += 1
                            for j in range(4):
                                oc = g4 * 4 + j
                                k.op("pe", lambda e, pq=pq, j=j, oc=oc, t=t: e.matmul(
                                    pq[:, j, :], lhsT=qpT[:, oc, t * 128:(t + 1) * 128], rhs=skb[:, oc, :], start=True, stop=True),
                                    r=[qpT.res, skb.res], w=[pq.res])
                            k.op("act", lambda e, pq=pq, g4=g4: e.activation(out=sc[:, g4 * 4:(g4 + 1) * 4, :], in_=pq[:], func=AF.Copy),
                                 r=[pq.res], w=[sc.rs[g4]])
                        for oc in range(16):
                            k.op("dve", lambda e, oc=oc: e.max(out=tv[:, oc, 0:8], in_=sc[:, oc, :]), r=[sc.rs[oc // 4]], w=[tv.rs[oc]])
                        for oc in range(16):
                            k.op("dve", lambda e, oc=oc: e.max_index(out=tiu[:, oc, 0:8], in_max=tv[:, oc, 0:8], in_values=sc[:, oc, :]),
                                 r=[sc.rs[oc // 4], tv.rs[oc]], w=[tiu.rs[oc]])
                        for oc in range(16):
                            k.op("dve", lambda e, oc=oc: e.match_replace(out=sc2[:, oc, :], in_to_replace=tv[:, oc, 0:8], in_values=sc[:, oc, :],
                                                                        imm_value=NEG), r=[sc.rs[oc // 4], tv.rs[oc]], w=[sc2.rs[oc]])
                        for oc in range(16):
                            k.op("dve", lambda e, oc=oc: e.max(out=tv[:, oc, 8:16], in_=sc2[:, oc, :]), r=[sc2.rs[oc]], w=[tv.rs[oc]])
                        for oc in range(16):
                            k.op("dve", lambda e, oc=oc: e.max_index(out=tiu[:, oc, 8:16], in_max=tv[:, oc, 8:16], in_values=sc2[:, oc, :]),
                                 r=[sc2.rs[oc], tv.rs[oc]], w=[tiu.rs[oc]])
                        k.op("dve", lambda e: e.tensor_copy(out=tif[:], in_=tiu[:]), r=tiu.rs, w=[tif.res])
                        tv4 = tv[:].rearrange("p (h two) a -> p h two a", two=2)
                        tif4 = tif[:].rearrange("p (h two) a -> p h two a", two=2)
                        k.op("dve", lambda e, tv4=tv4: e.tensor_tensor(
                            out=cand[:].rearrange("p h (a b) -> p h a b", a=16),
                            in0=tv4[:, :, 0, :].unsqueeze(3).to_broadcast([128, 8, 16, 16]),
                            in1=tv4[:, :, 1, :].unsqueeze(2).to_broadcast([128, 8, 16, 16]), op=ALU.add),
                            r=tv.rs, w=[cand.res])
                        for hd in range(8):
                            k.op("dve", lambda e, hd=hd: e.max(out=ts[:, hd, 0:8], in_=cand[:, hd, :]), r=[cand.res], w=[ts.rs[hd]])
                        for hd in range(8):
                            k.op("dve", lambda e, hd=hd: e.max_index(out=posu[:, hd, 0:8], in_max=ts[:, hd, 0:8], in_values=cand[:, hd, :]),
                                 r=[cand.res, ts.rs[hd]], w=[posu.rs[hd]])
                        for hd in range(8):
                            k.op("dve", lambda e, hd=hd: e.match_replace(out=cand2[:, hd, :], in_to_replace=ts[:, hd, 0:8], in_values=cand[:, hd, :],
                                                                        imm_value=NEG), r=[cand.res, ts.rs[hd]], w=[cand2.rs[hd]])
                        for hd in range(8):
                            k.op("dve", lambda e, hd=hd: e.max(out=ts[:, hd, 8:16], in_=cand2[:, hd, :]), r=[cand2.rs[hd]], w=[ts.rs[hd]])
                        for hd in range(8):
                            k.op("dve", lambda e, hd=hd: e.max_index(out=posu[:, hd, 8:16], in_max=ts[:, hd, 8:16], in_values=cand2[:, hd, :]),
                                 r=[cand2.rs[hd], ts.rs[hd]], w=[posu.rs[hd]])
                        k.op("dve", lambda e: e.tensor_scalar(out=au[:], in0=posu[:], scalar1=4, scalar2=None, op0=ALU.logical_shift_right),
                             r=posu.rs, w=[au.res])
                        k.op("dve", lambda e: e.tensor_scalar(out=bu[:], in0=posu[:], scalar1=15, scalar2=None, op0=ALU.bitwise_and),
                             r=posu.rs, w=[bu.res])
                        k.op("dve", lambda e: e.tensor_copy(out=af[:], in_=au[:]), r=[au.res], w=[af.res])
                        k.op("dve", lambda e: e.tensor_copy(out=bf[:], in_=bu[:]), r=[bu.res], w=[bf.res])
                        for (sel, rk, which) in ((isel, af, 0), (jsel, bf, 1)):
                            k.op("dve", lambda e, rk=rk: e.tensor_tensor(
                                out=eq[:], in0=rk[:].unsqueeze(3).to_broadcast([128, 8, 16, 16]),
                                in1=iota16.unsqueeze(1).unsqueeze(1).to_broadcast([128, 8, 16, 16]), op=ALU.is_equal),
                                r=[rk.res, iota_f.res], w=[eq.res])
                            k.op("dve", lambda e, which=which, tif4=tif4: e.tensor_tensor(
                                out=eq[:], in0=eq[:], in1=tif4[:, :, which, :].unsqueeze(2).to_broadcast([128, 8, 16, 16]), op=ALU.mult),
                                r=[eq.res, tif.res], w=[eq.res])
                            k.op("dve", lambda e, sel=sel: e.tensor_reduce(out=sel[:], in_=eq[:], axis=AX.X, op=ALU.add),
                                 r=[eq.res], w=[sel.res])
                        k.op("dve", lambda e: e.tensor_tensor(out=gsel[:], in0=ts[:], in1=ts[:, :, 0:1].to_broadcast([128, 8, 16]), op=ALU.subtract),
                             r=ts.rs, w=[gsel.res])
                        k.op("act", lambda e: e.activation(out=gsel[:], in_=gsel[:], func=AF.Exp), r=[gsel.res], w=[gsel.res])
                        k.op("dve", lambda e: e.tensor_reduce(out=sm[:], in_=gsel[:], axis=AX.X, op=ALU.add), r=[gsel.res], w=[sm.res])
                        k.op("dve", lambda e: e.reciprocal(out=sm[:], in_=sm[:]), r=[sm.res], w=[sm.res])
                        k.op("dve", lambda e: e.tensor_tensor(out=gsel[:], in0=gsel[:], in1=sm[:, :].unsqueeze(2).to_broadcast([128, 8, 16]), op=ALU.mult),
                             r=[gsel.res, sm.res], w=[gsel.res])
                        for i3, (src, dst) in enumerate(((isel, slotI), (jsel, slotJ), (gsel, slotG))):
                            k.op("pe", lambda e, i3=i3, src=src: e.transpose(out=pTs[:, i3, :], in_=src[:].rearrange("p h r -> p (h r)"),
                                                                            identity=ident_f[:]),
                                 r=[src.res, ident_f.res], w=[pTs.res])
                        for i3, (src, dst) in enumerate(((isel, slotI), (jsel, slotJ), (gsel, slotG))):
                            k.op("act", lambda e, i3=i3, dst=dst, tile=tile: e.activation(out=dst[:, tile * 128:(tile + 1) * 128], in_=pTs[:, i3, :], func=AF.Copy),
                                 r=[pTs.res], w=[dst.res])
                k.barrier(release=[w_qry.res, skb.res])

            with ExitStack() as pb:
                TCH = 8
                ohj = [sb(pb, "ohj%d" % i, [128, TCH, 128], BF16) for i in range(4)]
                eqi = [sb(pb, "eqi%d" % i, [128, TCH, 128], BF16) for i in range(4)]
                rig = [sb(pb, "rig%d" % i, [128, TCH, 128], BF16) for i in range(4)]
                Wst = [sb(pb, "Wst%d" % i, [128, 128, 128], BF16, nres=32) for i in range(2)]
                pW = [ps(pb, "pW%d" % i, [128, 4, 128], F32) for i in range(4)]
                wc = 0
                chc = 0
                iota_b3 = iota_f[:, :].unsqueeze(1).to_broadcast([128, TCH, 128])
                for tile in range(NTILE):
                    W_ = Wst[tile % 2]
                    for ch in range(128 // TCH):
                        oj, ei, rg = ohj[chc % 4], eqi[chc % 4], rig[chc % 4]
                        chc += 1
                        tok0 = tile * 128 + ch * TCH
                        k.op("dve", lambda e, oj=oj, tok0=tok0: e.tensor_tensor(
                            out=oj[:], in0=iota_b3, in1=slotJ[:, tok0:tok0 + TCH].unsqueeze(2).to_broadcast([128, TCH, 128]), op=ALU.is_equal),
                            r=[iota_f.res, slotJ.res], w=[oj.res])
                        k.op("dve", lambda e, ei=ei, tok0=tok0: e.tensor_tensor(
                            out=ei[:], in0=iota_b3, in1=slotI[:, tok0:tok0 + TCH].unsqueeze(2).to_broadcast([128, TCH, 128]), op=ALU.is_equal),
                            r=[iota_f.res, slotI.res], w=[ei.res])
                        k.op("pool", lambda e, ei=ei, rg=rg, tok0=tok0: e.tensor_tensor(
                            out=rg[:], in0=ei[:], in1=slotG[:, tok0:tok0 + TCH].unsqueeze(2).to_broadcast([128, TCH, 128]), op=ALU.mult),
                            r=[ei.res, slotG.res], w=[rg.res])
                        for q4 in range(TCH // 4):
                            pw = pW[wc % 4]
                            for j in range(4):
                                tt_ = q4 * 4 + j
                                k.op("pe", lambda e, pw=pw, j=j, oj=oj, rg=rg, tt_=tt_: e.matmul(
                                    pw[:, j, :], lhsT=oj[:, tt_, :], rhs=rg[:, tt_, :], start=True, stop=True),
                                    r=[oj.res, rg.res], w=[pw.res])
                            t0 = ch * TCH + q4 * 4
                            if True:
                                k.op("act", lambda e, pw=pw, W_=W_, t0=t0: e.activation(
                                    out=W_[:, :, t0:t0 + 4], in_=pw[:].rearrange("p t i -> p i t"), func=AF.Copy),
                                    r=[pw.res], w=[W_.rs[t0 // 4]])
                            else:
                                k.op("dve", lambda e, pw=pw, W_=W_, t0=t0: e.tensor_copy(
                                    out=W_[:, :, t0:t0 + 4], in_=pw[:].rearrange("p t i -> p i t")),
                                    r=[pw.res], w=[W_.rs[t0 // 4]])
                            wc += 1
                    k.dma("sp", wd_d[tile], W_[:].rearrange("p i t -> p (i t)"), r=W_.rs, w=[], sres=W_.res)
                k.barrier(release=[w_.res for w_ in Wst])
            p3s.close()

            with ExitStack() as pc:
                T3 = 256
                GS = 8
                NG = 128 // GS
                hfT = sb(pc, "hfT", [128, 8, NT], BF16)
                pT = [ps(pc, "pT4%d" % i, [128, 8, 128], BF16) for i in range(1)]
                pA = [ps(pc, "pA%d" % i, [128, 512], F32) for i in range(3)]
                pO = [ps(pc, "pO4%d" % i, [128, 512], F32) for i in range(4)]
                for tile in range(NTILE):
                    norm_T(x_res[:, tile, :], x_res.rs[tile], pT[0], hfT[:, :, tile * 128:(tile + 1) * 128], hfT.res)
                ut = [sb(pc, "ut%d" % i, [128, GS, 8, 128], BF16) for i in range(2)]
                vt = [sb(pc, "vt%d" % i, [128, GS, D], BF16) for i in range(2)]
                wsl = [sb(pc, "wsl%d" % i, [128, 2, GS, 128], BF16) for i in range(2)]
                ge = [sb(pc, "ge%d" % i, [128, T3], F32) for i in range(3)]
                gw = [sb(pc, "gw%d" % i, [128, T3], BF16) for i in range(3)]
                uT_v = uT_d.rearrange("(i p) (c e) -> p i c e", p=128, c=8)
                ev_v = ev_d.rearrange("(i e) d -> e i d", e=128)
                wd_v = wd_d.rearrange("t j (i x) -> j t i x", i=128)
                items = [(g, tb, i) for g in range(NG) for tb in range(NT // T3) for i in range(GS)]
                state = {}

                def emitA(n):
                    g, tb, i = items[n]
                    u_, v_ = ut[g % 2], vt[g % 2]
                    if tb == 0 and i == 0:
                        for i_ in range(GS):
                            k.dma("pool", u_[:, i_, :, :], uT_v[:, g * GS + i_, :, :], r=[], w=[u_.res], sres=u_.res)
                            k.dma("pool", v_[:, i_, :], ev_v[:, g * GS + i_, :], r=[], w=[v_.res], sres=v_.res, max_dma_last_dim=4096)
                    if i == 0:
                        ws_ = wsl[(g * (NT // T3) + tb) % 2]
                        k.dma("sp", ws_[:], wd_v[:, 2 * tb:2 * tb + 2, g * GS:(g + 1) * GS, :], r=[], w=[ws_.res], sres=ws_.res)
                    ws_ = wsl[(g * (NT // T3) + tb) % 2]
                    pa_ = pA[n % 3]
                    ge_, gw_ = ge[n % 3], gw[n % 3]
                    mm_group(pa_[:, 0:T3], [(u_[:, i, c, :], hfT[:, c, tb * T3:(tb + 1) * T3]) for c in range(8)],
                             r=[u_.res, hfT.res], w=[pa_.res])
                    k.op("act", lambda e: e.activation(out=ge_[:], in_=pa_[:, 0:T3], func=AF.Gelu_apprx_tanh),
                         r=[pa_.res], w=[ge_.res])
                    k.op("dve", lambda e: e.tensor_tensor(
                        out=gw_[:].rearrange("p (a t) -> p a t", a=2), in0=ge_[:].rearrange("p (a t) -> p a t", a=2),
                        in1=ws_[:, :, i, :], op=ALU.mult), r=[ge_.res, ws_.res], w=[gw_.res])

                def emitV(n):
                    g, tb, i = items[n]
                    v_ = vt[g % 2]
                    gw_ = gw[n % 3]
                    for t in range(2):
                        for half in range(2):
                            po = pO[t * 2 + half]
                            k.op("pe", lambda e, po=po, t=t, half=half: e.matmul(
                                po[:], lhsT=gw_[:, t * 128:(t + 1) * 128], rhs=v_[:, i, half * 512:(half + 1) * 512],
                                start=(i == 0), stop=(i == GS - 1)), r=[gw_.res, v_.res], w=[po.res])
                    if i == GS - 1:
                        for t in range(2):
                            tile = tb * 2 + t
                            for half in range(2):
                                po = pO[t * 2 + half]
                                k.op("dve", lambda e, po=po, tile=tile, half=half: e.tensor_tensor(
                                    out=x_res[:, tile, half * 512:(half + 1) * 512], in0=po[:], in1=x_res[:, tile, half * 512:(half + 1) * 512],
                                    op=ALU.add), r=[po.res, x_res.rs[tile]], w=[x_res.rs[tile]])

                emitA(0)
                emitA(1)
                for n in range(len(items)):
                    if n + 2 < len(items):
                        emitA(n + 2)
                    emitV(n)
                k.barrier(release=[t_.res for t_ in ut + vt + wsl])

        with ExitStack() as p4:
            gfin = sb(p4, "gfin", [128, D], F32)
            k.dma("sp", gfin[:], gfin_d[:, :], r=[], w=[gfin.res], sres=gfin.res)
            ot = [sb(p4, "ot%d" % i, [128, D], F32) for i in range(2)]
            for tile in range(NTILE):
                n = nrm[tile % 2]
                o_ = ot[tile % 2]
                xa = x_res[:, tile, :]
                xr = x_res.rs[tile]
                k.op("dve", lambda e, n=n, xa=xa: e.scalar_tensor_tensor(out=n["junk"][:], in0=xa, scalar=1.0, in1=xa,
                                                                         op0=ALU.mult, op1=ALU.mult, accum_out=n["ss"][:]),
                     r=[xr], w=[n["junk"].res, n["ss"].res])
                k.op("act", lambda e, n=n: e.activation(out=n["sd"][:], in_=n["ss"][:], func=AF.Sqrt, bias=cst[:, 0:1], scale=1.0 / D),
                     r=[n["ss"].res, cst.res], w=[n["sd"].res])
                k.op("dve", lambda e, n=n: e.reciprocal(out=n["rstd"][:], in_=n["sd"][:]), r=[n["sd"].res], w=[n["rstd"].res])
                k.op("dve", lambda e, n=n, xa=xa, o_=o_: e.scalar_tensor_tensor(out=o_[:], in0=xa, scalar=n["rstd"][:, 0:1], in1=gfin[:],
                                                                                op0=ALU.mult, op1=ALU.mult),
                     r=[xr, n["rstd"].res, gfin.res], w=[o_.res])
                k.dma("sp", y_d[tile * 128:(tile + 1) * 128, :], o_[:], r=[o_.res], w=[], sres=o_.res)
            k.barrier()
    return nc, k.n_ins


def prep_shared(inp):
    f = np.float32
    g = lambda a: np.ascontiguousarray(np.asarray(a, dtype=f))
    sm = np.zeros((128, NSM), f)

    def put(col, vec, nch):
        sm[:, col:col + nch] = np.asarray(vec, f).reshape(nch, 128).T

    put(SM_MIX, inp["norm_mix"][0], 8)
    put(SM_CROSS, inp["norm_cross"][0], 8)
    put(SM_MEM, inp["norm_mem"][0], 8)
    put(SM_FFN, inp["norm_ffn"][0], 8)
    put(SM_GA, inp["norm_grp_a"][0], 4)
    put(SM_GB, inp["norm_grp_b"][0], 4)
    put(SM_CB, inp["conv_b"][0], 4)
    put(SM_BA, inp["gate_a_b"][0], 4)
    put(SM_BX, inp["gate_x_b"][0], 4)
    put(SM_LAM, inp["lru_lambda"][0], 4)
    cw = np.asarray(inp["conv_w"][0], f)
    for cc in range(4):
        for j in range(4):
            sm[:, SM_CW + cc * 4 + j] = cw[j, cc * 128:(cc + 1) * 128]
    gbd = np.zeros((128, 8, 128), f)
    for gi, key in enumerate(("gate_a_w", "gate_x_w")):
        w = np.asarray(inp[key][0], f)
        for cc in range(4):
            gbd[0:64, gi * 4 + cc, 0:64] = w[2 * cc]
            gbd[64:128, gi * 4 + cc, 64:128] = w[2 * cc + 1]
    rb = np.asarray(inp["rel_bias"][0], f)
    qi = np.arange(128)[:, None]
    kj = np.arange(640)[None, :]
    idx = np.clip(512 + qi - kj, -128, 128) + 128
    ab = rb[:, idx]
    valid = np.where(qi < 64, kj < 576, kj >= 64)
    ab = np.where(valid[None], ab, f(NEG)).astype(f)
    abias = np.ascontiguousarray(ab.transpose(1, 0, 2)).reshape(128, 8 * 640)
    sk = np.asarray(inp["sub_keys"][0], f)
    skT = np.ascontiguousarray(sk.reshape(16, 128, 128).transpose(2, 0, 1)).reshape(128, 16 * 128)
    u = np.asarray(inp["expert_u"][0], f)
    uT = np.ascontiguousarray(u.reshape(128, 128, 8, 128).transpose(0, 3, 2, 1)).reshape(16384, D)
    shared = {
        "w_in": g(inp["w_in"][0]), "w_out": g(inp["w_out"][0]), "w_q": g(inp["w_q_mem"][0]),
        "w_kv": g(inp["w_kv_mem"][0]), "w_o": g(inp["w_o_mem"][0]), "w_qry": g(inp["w_query"][0]),
        "smalls": sm, "gbd": gbd.reshape(128, 8 * 128), "abias": abias, "skT": skT, "uT": uT,
        "ev": g(inp["expert_v"][0]),
        "gfin": np.ascontiguousarray(np.broadcast_to(np.asarray(inp["norm_final"], f)[None, :], (128, D))),
        "ident": np.eye(128, dtype=f),
        "iota": np.ascontiguousarray(np.broadcast_to(np.arange(128, dtype=f)[None, :], (128, 128))),
    }
    return shared


def make_in_maps(inp, NT):
    x = np.asarray(inp["x"], np.float32)
    mem = np.asarray(inp["mem"], np.float32)
    B, S, _ = x.shape
    per_seq = S // NT
    NPRE = 3 * NT
    shared = prep_shared(inp)
    maps = []
    for c in range(B * per_seq):
        b, q = divmod(c, per_seq)
        xprev = np.zeros((NPRE, D), np.float32)
        if q > 0:
            xprev[NPRE - q * NT:] = x[b, 0:q * NT]
        pflag = np.zeros((128, NPRE // 512), np.float32)
        for j in range(NPRE // 512):
            if j * 512 >= NPRE - q * NT:
                pflag[:, j] = 1.0
        halob = np.full((128, 512), 0.0 if q > 0 else NEG, np.float32)
        m = dict(shared)
        m.update({"xown": np.ascontiguousarray(x[b, q * NT:(q + 1) * NT]), "xprev": xprev, "pflag": pflag,
                  "halob": halob, "mem": np.ascontiguousarray(mem[b])})
        maps.append(m)
    return maps


_CACHE = {}


def kernel(**inputs):
    NT = 2048
    if NT not in _CACHE:
        _CACHE[NT] = build(NT)[0]
    nc = _CACHE[NT]
    maps = make_in_maps(inputs, NT)
    res = run_bass_kernel_spmd(nc, maps, core_ids=list(range(N_CORES)))
    x = np.asarray(inputs["x"])
    B, S, _ = x.shape
    out = np.empty((B, S, D), np.float32)
    per_seq = S // NT
    for c in range(N_CORES):
        b, q = divmod(c, per_seq)
        out[b, q * NT:(q + 1) * NT] = res.results[c]["y"]
    return out
```

```python
import numpy as np
from contextlib import ExitStack
import concourse.bass as bass
import concourse.mybir as mybir
from concourse.bass_utils import run_bass_kernel_spmd

F32 = mybir.dt.float32
BF16 = mybir.dt.bfloat16
U32 = mybir.dt.uint32
AF = mybir.ActivationFunctionType
ALU = mybir.AluOpType
AX = mybir.AxisListType

D = 1024
EPS = 1e-6
NEG = -1e30
N_CORES = 8
SAME_ENG_SYNC = True

SM_MIX, SM_CROSS, SM_MEM, SM_FFN = 0, 8, 16, 24
SM_GA, SM_GB, SM_CB, SM_BA, SM_BX, SM_LAM, SM_CW = 32, 36, 40, 44, 48, 52, 56
NSM = 72


class Res:
    __slots__ = ("name", "w", "r", "ds")

    def __init__(self, name):
        self.name = name
        self.w = None
        self.r = {}
        self.ds = None


class KB:
    def __init__(self, nc, es):
        self.nc = nc
        self.es = es
        self.E = {}
        for nm, e in (("pe", nc.tensor), ("act", nc.scalar), ("dve", nc.vector),
                      ("pool", nc.gpsimd), ("sp", nc.sync)):
            self.E[nm] = dict(e=e, sem=es.enter_context(nc.semaphore("e_" + nm)), cnt=0, waited={}, nm=nm)
        self.free_ds = []
        self.all_ds = []
        self.n_ins = 0

    def _collect(self, r, w):
        deps = {}
        for x in r:
            if x.w is not None:
                s, v = x.w
                if deps.get(s, 0) < v:
                    deps[s] = v
        for x in w:
            if x.w is not None:
                s, v = x.w
                if deps.get(s, 0) < v:
                    deps[s] = v
            for s, v in x.r.items():
                if deps.get(s, 0) < v:
                    deps[s] = v
        return deps

    def _waits(self, E, deps, skip_own):
        for s, v in deps.items():
            if skip_own and s is E["sem"]:
                continue
            if E["waited"].get(s, 0) >= v:
                continue
            E["e"].wait_ge(s, v)
            E["waited"][s] = v

    def op(self, en, fn, r=(), w=()):
        E = self.E[en]
        skip_own = (en == "pe") or (not SAME_ENG_SYNC)
        self._waits(E, self._collect(r, w), skip_own)
        ins = fn(E["e"])
        E["cnt"] += 1
        self.n_ins += 1
        ins.then_inc(E["sem"], 1)
        tag = (E["sem"], E["cnt"])
        for x in w:
            x.w = tag
            x.r = {}
        for x in r:
            if x not in w:
                x.r[E["sem"]] = E["cnt"]
        return ins

    def dma(self, qn, out, in_, r, w, sres, **kw):
        E = self.E[qn]
        self._waits(E, self._collect(r, w), False)
        if sres.ds is None:
            if qn != "pool" and self.free_ds:
                sres.ds = self.free_ds.pop()
            else:
                sres.ds = [self.es.enter_context(self.nc.semaphore("d%d" % len(self.all_ds))), 0, qn]
                self.all_ds.append(sres.ds)
        ins = E["e"].dma_start(out=out, in_=in_, **kw)
        self.n_ins += 1
        sres.ds[1] += 1
        ins.then_inc(sres.ds[0], 16)
        val = sres.ds[1] * 16
        for x in w:
            x.w = (sres.ds[0], val)
            x.r = {}
        for x in r:
            if x not in w:
                x.r[sres.ds[0]] = val

    def barrier(self, release=()):
        deps = {}
        for nm in ("pe", "act", "dve", "pool"):
            E = self.E[nm]
            if E["cnt"] > 0:
                deps[E["sem"]] = E["cnt"]
        for ds in self.all_ds:
            if ds[1] > 0:
                deps[ds[0]] = ds[1] * 16
        for nm in ("pe", "act", "dve", "pool", "sp"):
            self._waits(self.E[nm], deps, True)
        for x in release:
            if x.ds is not None:
                if x.ds[2] != "pool":
                    self.free_ds.append(x.ds)
                x.ds = None


class Tn:
    def __init__(self, h, name, nres=1):
        self.h = h
        self.res = Res(name)
        self.rs = [Res("%s_%d" % (name, i)) for i in range(nres)] if nres > 1 else [self.res]

    def __getitem__(self, k):
        return self.h[k]


def build(NT=2048, debug=False):
    NPRE = 3 * NT
    NTILE = NT // 128
    nc = bass.Bass("TRN2", target_bir_lowering=False)
    dt_in = lambda n, s, d=F32: nc.dram_tensor(n, list(s), d, kind="ExternalInput").ap()
    xown = dt_in("xown", [NT, D])
    xprev = dt_in("xprev", [NPRE, D])
    pflag_d = dt_in("pflag", [128, NPRE // 512])
    halob_d = dt_in("halob", [128, 512])
    mem_d = dt_in("mem", [256, D])
    w_in_d = dt_in("w_in", [D, 2560])
    w_out_d = dt_in("w_out", [D, D])
    w_q_d = dt_in("w_q", [D, D])
    w_kv_d = dt_in("w_kv", [D, 2048])
    w_o_d = dt_in("w_o", [D, D])
    w_qry_d = dt_in("w_qry", [D, 2048])
    smalls_d = dt_in("smalls", [128, NSM])
    gbd_d = dt_in("gbd", [128, 8 * 128])
    abias_d = dt_in("abias", [128, 8 * 640])
    skT_d = dt_in("skT", [128, 16 * 128])
    uT_d = dt_in("uT", [16384, D])
    ev_d = dt_in("ev", [16384, D])
    gfin_d = dt_in("gfin", [128, D])
    ident_d = dt_in("ident", [128, 128])
    iota_d = dt_in("iota", [128, 128])
    y_d = nc.dram_tensor("y", [NT, D], F32, kind="ExternalOutput").ap()
    wd_d = nc.dram_tensor("wd_scratch", [NTILE, 128, 16384], BF16, kind="Internal").ap()
    dbg = {}
    if debug:
        for nm in ("dbg1", "dbg2"):
            dbg[nm] = nc.dram_tensor(nm, [NT, D], F32, kind="ExternalOutput").ap()

    with ExitStack() as es:
        k = KB(nc, es)

        def sb(ctx, name, shape, dt, nres=1):
            return Tn(ctx.enter_context(nc.sbuf_tensor("sb_" + name, list(shape), dt)), name, nres)

        def ps(ctx, name, shape, dt):
            return Tn(ctx.enter_context(nc.psum_tensor("ps_" + name, list(shape), dt)), name)

        x_res = sb(es, "x_res", [128, NTILE, D], F32, nres=NTILE)
        ident_f = sb(es, "ident_f", [128, 128], F32)
        ident_b = sb(es, "ident_b", [128, 128], BF16)
        iota_f = sb(es, "iota_f", [128, 128], F32)
        ones_b = sb(es, "ones_b", [128, 128], BF16)
        smalls = sb(es, "smalls", [128, NSM], F32)
        cst = sb(es, "cst", [128, 4], F32)
        gB = sb(es, "gB", [128, 8, 128], F32)
        nrm = [dict(ss=sb(es, "n_ss%d" % i, [128, 1], F32), sd=sb(es, "n_sd%d" % i, [128, 1], F32),
                    rstd=sb(es, "n_rstd%d" % i, [128, 1], F32), xs=sb(es, "n_xs%d" % i, [128, D], BF16),
                    junk=sb(es, "n_junk%d" % i, [128, D], BF16)) for i in range(2)]
        nrm_i = [0]

        k.dma("sp", ident_f[:], ident_d[:, :], r=[], w=[ident_f.res], sres=ident_f.res)
        k.dma("sp", iota_f[:], iota_d[:, :], r=[], w=[iota_f.res], sres=iota_f.res)
        k.dma("sp", smalls[:], smalls_d[:, :], r=[], w=[smalls.res], sres=smalls.res)
        k.op("dve", lambda e: e.tensor_copy(out=ident_b[:], in_=ident_f[:]), r=[ident_f.res], w=[ident_b.res])
        k.op("pool", lambda e: e.memset(ones_b[:], 1.0), w=[ones_b.res])
        k.op("pool", lambda e: e.memset(cst[:, 0:1], EPS), w=[cst.res])
        k.op("pool", lambda e: e.memset(cst[:, 1:2], 1.0), w=[cst.res])
        k.op("pool", lambda e: e.memset(cst[:, 2:3], 0.0), w=[cst.res])

        def set_gain(col):
            k.op("dve", lambda e: e.tensor_copy(
                out=gB[:], in_=smalls[:, col:col + 8].unsqueeze(2).to_broadcast([128, 8, 128])),
                r=[smalls.res], w=[gB.res])

        def norm_T(x_ap, x_r, pT, hT_ap, hT_r):
            n = nrm[nrm_i[0] % 2]
            nrm_i[0] += 1
            k.op("dve", lambda e: e.scalar_tensor_tensor(out=n["junk"][:], in0=x_ap, scalar=1.0, in1=x_ap,
                                                         op0=ALU.mult, op1=ALU.mult, accum_out=n["ss"][:]),
                 r=[x_r], w=[n["junk"].res, n["ss"].res])
            k.op("act", lambda e: e.activation(out=n["sd"][:], in_=n["ss"][:], func=AF.Sqrt,
                                               bias=cst[:, 0:1], scale=1.0 / D),
                 r=[n["ss"].res, cst.res], w=[n["sd"].res])
            k.op("dve", lambda e: e.reciprocal(out=n["rstd"][:], in_=n["sd"][:]), r=[n["sd"].res], w=[n["rstd"].res])
            k.op("dve", lambda e: e.tensor_scalar(out=n["xs"][:], in0=x_ap, scalar1=n["rstd"][:, 0:1], scalar2=None,
                                                  op0=ALU.mult), r=[x_r, n["rstd"].res], w=[n["xs"].res])
            for c in range(8):
                k.op("pe", lambda e, c=c: e.transpose(out=pT[:, c, :], in_=n["xs"][:, c * 128:(c + 1) * 128],
                                                      identity=ident_b[:]),
                     r=[n["xs"].res, ident_b.res], w=[pT.res])
            k.op("dve", lambda e: e.tensor_tensor(out=hT_ap, in0=pT[:], in1=gB[:], op=ALU.mult),
                 r=[pT.res, gB.res], w=[hT_r])

        def mm_group(out_ap, pairs, r, w):
            n = len(pairs)
            for i, (l, rh) in enumerate(pairs):
                k.op("pe", lambda e, l=l, rh=rh, i=i: e.matmul(out_ap, lhsT=l, rhs=rh, start=(i == 0), stop=(i == n - 1)),
                     r=r, w=w)

        def load_w_bf16(dst, src_ap, ncols):
            for c in range(8):
                k.dma("pool", dst[:, c, :], src_ap[c * 128:(c + 1) * 128, :], r=[], w=[dst.res], sres=dst.res,
                      max_dma_last_dim=4096)

        with ExitStack() as p1:
            with ExitStack() as pa:
                BA = 512
                w_inA = sb(pa, "w_inA", [128, 8, 1024], BF16)
                load_w_bf16(w_inA, w_in_d[:, 0:1024], 1024)
                w_outA = sb(pa, "w_outA", [128, 4, D], BF16)
                for c in range(4):
                    k.dma("pool", w_outA[:, c, :], w_out_d[c * 128:(c + 1) * 128, :], r=[], w=[w_outA.res], sres=w_outA.res,
                          max_dma_last_dim=4096)
                yan = sb(pa, "yan", [128, 4, 512], BF16)
                gbd_f = sb(pa, "gbd_f", [128, 8, 128], F32)
                gbd = sb(pa, "gbd", [128, 8, 128], BF16)
                k.dma("sp", gbd_f[:], gbd_d.rearrange("p (c j) -> p c j", c=8), r=[], w=[gbd_f.res], sres=gbd_f.res)
                k.op("dve", lambda e: e.tensor_copy(out=gbd[:], in_=gbd_f[:]), r=[gbd_f.res], w=[gbd.res])
                pflag = sb(pa, "pflag", [128, NPRE // 512], F32)
                k.dma("sp", pflag[:], pflag_d[:, :], r=[], w=[pflag.res], sres=pflag.res)
                cL = sb(pa, "cL", [128, 4], F32)
                tmp4 = sb(pa, "tmp4", [128, 4], F32)
                k.op("act", lambda e: e.activation(out=tmp4[:], in_=smalls[:, SM_LAM:SM_LAM + 4], func=AF.Exp, scale=-1.0),
                     r=[smalls.res], w=[tmp4.res])
                k.op("act", lambda e: e.activation(out=cL[:], in_=tmp4[:], func=AF.Ln, bias=cst[:, 1:2], scale=1.0),
                     r=[tmp4.res, cst.res], w=[cL.res])
                k.op("dve", lambda e: e.tensor_scalar(out=cL[:], in0=cL[:], scalar1=-8.0, scalar2=None, op0=ALU.mult),
                     r=[cL.res], w=[cL.res])
                set_gain(SM_MIX)
                xtmp = [sb(pa, "xtmp%d" % i, [128, D], F32) for i in range(2)]
                hT = [sb(pa, "hTa%d" % i, [128, 8, BA], BF16) for i in range(2)]
                xl = sb(pa, "xl", [128, 4, 3 + BA], F32, nres=4)
                gg = sb(pa, "gg", [128, 4, BA], F32, nres=4)
                hh = sb(pa, "hh", [128, 4, BA], F32, nres=4)
                hst = sb(pa, "hst", [128, 4], F32, nres=4)
                yaT = sb(pa, "yaT", [128, 4, BA], F32, nres=4)
                LT = [{nm: sb(pa, "l%s%d" % (nm, i), [128, BA], BF16 if nm == "xcb" else F32)
                       for nm in ("xc", "xc2", "xcb", "rr", "ii", "aa", "tt", "bb")} for i in range(2)]
                sq = sb(pa, "sq", [128, BA], BF16)
                rsn = sb(pa, "rsn", [128, BA], F32)
                pj = [ps(pa, "pj%d" % i, [128, 512], F32) for i in range(2)]
                pT = [ps(pa, "pT%d" % i, [128, 8, 128], BF16) for i in range(1)]
                pg = [ps(pa, "pg%d" % i, [128, 512], F32) for i in range(4)]
                pn = ps(pa, "pn", [128, 512], F32)
                k.op("pool", lambda e: e.memset(xl[:], 0.0), w=xl.rs)
                k.op("pool", lambda e: e.memset(hst[:], 0.0), w=hst.rs)

                nblk_pre = NPRE // BA
                nblk_own = NT // BA
                tcount = 0
                pjc = 0
                for b in range(nblk_pre + nblk_own):
                    own = b >= nblk_pre
                    ob = b - nblk_pre
                    h = hT[b % 2]
                    for t in range(4):
                        if own:
                            tile = ob * 4 + t
                            xa = x_res[:, tile, :]
                            xr = x_res.rs[tile]
                            k.dma("sp", xa, xown[tile * 128:(tile + 1) * 128, :], r=[], w=[xr], sres=xr)
                        else:
                            xt_ = xtmp[tcount % 2]
                            xa = xt_[:]
                            xr = xt_.res
                            r0 = b * BA + t * 128
                            k.dma("sp", xa, xprev[r0:r0 + 128, :], r=[], w=[xr], sres=xr)
                        norm_T(xa, xr, pT[0], h[:, :, t * 128:(t + 1) * 128], h.res)
                        tcount += 1
                    for cc in range(8 if own else 4):
                        pp = pj[pjc % 2]
                        pjc += 1
                        mm_group(pp[:], [(w_inA[:, kc, cc * 128:(cc + 1) * 128], h[:, kc, :]) for kc in range(8)],
                                 r=[w_inA.res, h.res], w=[pp.res])
                        if cc < 4:
                            k.op("act", lambda e, pp=pp, cc=cc: e.activation(out=xl[:, cc, 3:3 + BA], in_=pp[:], func=AF.Copy),
                                 r=[pp.res], w=[xl.rs[cc]])
                        else:
                            k.op("act", lambda e, pp=pp, cc=cc: e.activation(out=gg[:, cc - 4, :], in_=pp[:], func=AF.Gelu_apprx_tanh),
                                 r=[pp.res], w=[gg.rs[cc - 4]])
                    def lru_chain(cc, L, pga, pgb, own=own, b=b):
                            cw = lambda j, cc=cc: smalls[:, SM_CW + cc * 4 + j:SM_CW + cc * 4 + j + 1]
                            k.op("dve", lambda e, cc=cc, L=L, cw=cw: e.tensor_scalar(
                                out=L["xc"][:], in0=xl[:, cc, 3:3 + BA], scalar1=cw(3), scalar2=smalls[:, SM_CB + cc:SM_CB + cc + 1],
                                op0=ALU.mult, op1=ALU.add), r=[xl.rs[cc], smalls.res], w=[L["xc"].res])
                            src, dst = "xc", "xc2"
                            for j in range(3):
                                yield
                                k.op("dve", lambda e, cc=cc, L=L, cw=cw, j=j, src=src, dst=dst: e.scalar_tensor_tensor(
                                    out=L[dst][:], in0=xl[:, cc, j:j + BA], scalar=cw(j), in1=L[src][:], op0=ALU.mult, op1=ALU.add),
                                    r=[xl.rs[cc], smalls.res, L[src].res], w=[L[dst].res])
                                src, dst = dst, src
                            xc = L[src]
                            yield
                            k.op("pool", lambda e, cc=cc: e.tensor_copy(out=xl[:, cc, 0:3], in_=xl[:, cc, BA:BA + 3]),
                                 r=[xl.rs[cc]], w=[xl.rs[cc]])
                            yield
                            k.op("act", lambda e, L=L, xc=xc: e.activation(out=L["xcb"][:], in_=xc[:], func=AF.Copy),
                                 r=[xc.res], w=[L["xcb"].res])
                            yield
                            k.op("pe", lambda e, cc=cc, L=L: e.matmul(pga[:], lhsT=gbd[:, cc, :], rhs=L["xcb"][:], start=True, stop=True),
                                 r=[gbd.res, L["xcb"].res], w=[pga.res])
                            yield
                            k.op("pe", lambda e, cc=cc, L=L: e.matmul(pgb[:], lhsT=gbd[:, 4 + cc, :], rhs=L["xcb"][:], start=True, stop=True),
                                 r=[gbd.res, L["xcb"].res], w=[pgb.res])
                            yield
                            k.op("act", lambda e, cc=cc, L=L: e.activation(out=L["rr"][:], in_=pga[:], func=AF.Sigmoid,
                                                                           bias=smalls[:, SM_BA + cc:SM_BA + cc + 1], scale=1.0),
                                 r=[pga.res, smalls.res], w=[L["rr"].res])
                            yield
                            k.op("act", lambda e, cc=cc, L=L: e.activation(out=L["ii"][:], in_=pgb[:], func=AF.Sigmoid,
                                                                           bias=smalls[:, SM_BX + cc:SM_BX + cc + 1], scale=1.0),
                                 r=[pgb.res, smalls.res], w=[L["ii"].res])
                            yield
                            k.op("act", lambda e, cc=cc, L=L: e.activation(out=L["aa"][:], in_=L["rr"][:], func=AF.Exp,
                                                                           scale=cL[:, cc:cc + 1]),
                                 r=[L["rr"].res, cL.res], w=[L["aa"].res])
                            yield
                            k.op("dve", lambda e, L=L: e.tensor_tensor(out=L["tt"][:], in0=L["aa"][:], in1=L["aa"][:], op=ALU.mult),
                                 r=[L["aa"].res], w=[L["tt"].res])
                            yield
                            k.op("act", lambda e, L=L: e.activation(out=L["tt"][:], in_=L["tt"][:], func=AF.Sqrt,
                                                                    bias=cst[:, 1:2], scale=-1.0),
                                 r=[L["tt"].res, cst.res], w=[L["tt"].res])
                            yield
                            k.op("dve", lambda e, L=L: e.tensor_tensor(out=L["bb"][:], in0=L["tt"][:], in1=L["ii"][:], op=ALU.mult),
                                 r=[L["tt"].res, L["ii"].res], w=[L["bb"].res])
                            yield
                            k.op("dve", lambda e, L=L, xc=xc: e.tensor_tensor(out=L["rr"][:], in0=L["bb"][:], in1=xc[:], op=ALU.mult),
                                 r=[L["bb"].res, xc.res], w=[L["rr"].res])
                            yield
                            k.op("dve", lambda e, cc=cc, L=L: e.tensor_tensor_scan(
                                out=hh[:, cc, :], data0=L["aa"][:], data1=L["rr"][:], initial=hst[:, cc:cc + 1],
                                op0=ALU.mult, op1=ALU.add), r=[L["aa"].res, L["rr"].res, hst.rs[cc]], w=[hh.rs[cc]])
                            if own:
                                yield
                                k.op("dve", lambda e, cc=cc: e.tensor_copy(out=hst[:, cc:cc + 1], in_=hh[:, cc, BA - 1:BA]),
                                     r=[hh.rs[cc]], w=[hst.rs[cc]])
                                yield
                                k.op("dve", lambda e, cc=cc: e.tensor_tensor(out=yaT[:, cc, :], in0=hh[:, cc, :], in1=gg[:, cc, :], op=ALU.mult),
                                     r=[hh.rs[cc], gg.rs[cc]], w=[yaT.rs[cc]])
                            else:
                                yield
                                k.op("dve", lambda e, cc=cc, b=b: e.tensor_tensor(out=hst[:, cc:cc + 1], in0=hh[:, cc, BA - 1:BA],
                                                                                  in1=pflag[:, b:b + 1], op=ALU.mult),
                                     r=[hh.rs[cc], pflag.res], w=[hst.rs[cc]])

                    for pair in range(2):
                        gens = [lru_chain(2 * pair + i_, LT[i_], pg[2 * i_], pg[2 * i_ + 1]) for i_ in range(2)]
                        while gens:
                            for g_ in list(gens):
                                try:
                                    next(g_)
                                except StopIteration:
                                    gens.remove(g_)
                    if own:
                        for cc in range(4):
                            k.op("dve", lambda e, cc=cc: e.tensor_tensor(out=sq[:], in0=yaT[:, cc, :], in1=yaT[:, cc, :], op=ALU.mult),
                                 r=[yaT.rs[cc]], w=[sq.res])
                            k.op("pe", lambda e, cc=cc: e.matmul(pn[:], lhsT=ones_b[:], rhs=sq[:], start=(cc == 0), stop=(cc == 3)),
                                 r=[ones_b.res, sq.res], w=[pn.res])
                        k.op("act", lambda e: e.activation(out=rsn[:], in_=pn[:], func=AF.Sqrt, bias=cst[:, 0:1], scale=1.0 / 512),
                             r=[pn.res, cst.res], w=[rsn.res])
                        k.op("dve", lambda e: e.reciprocal(out=rsn[:], in_=rsn[:]), r=[rsn.res], w=[rsn.res])
                        for cc in range(4):
                            k.op("dve", lambda e, cc=cc, ob=ob: e.scalar_tensor_tensor(
                                out=yan[:, cc, :], in0=yaT[:, cc, :],
                                scalar=smalls[:, SM_GA + cc:SM_GA + cc + 1], in1=rsn[:], op0=ALU.mult, op1=ALU.mult),
                                r=[yaT.rs[cc], smalls.res, rsn.res], w=[yan.res])
                        for t in range(4):
                            tile = ob * 4 + t
                            for half in range(2):
                                pp = pj[pjc % 2]
                                pjc += 1
                                mm_group(pp[:], [(yan[:, c, t * 128:(t + 1) * 128], w_outA[:, c, half * 512:(half + 1) * 512]) for c in range(4)],
                                         r=[yan.res, w_outA.res], w=[pp.res])
                                k.op("dve", lambda e, pp=pp, tile=tile, half=half: e.tensor_tensor(
                                    out=x_res[:, tile, half * 512:(half + 1) * 512], in0=pp[:], in1=x_res[:, tile, half * 512:(half + 1) * 512],
                                    op=ALU.add), r=[pp.res, x_res.rs[tile]], w=[x_res.rs[tile]])
                k.barrier(release=[w_inA.res, w_outA.res, gbd_f.res, pflag.res] + [t_.res for t_ in xtmp])

            with ExitStack() as pb:
                BB = 256
                w_inB = sb(pb, "w_inB", [128, 8, 1536], BF16)
                load_w_bf16(w_inB, w_in_d[:, 1024:2560], 1536)
                w_out = sb(pb, "w_outB", [128, 4, D], BF16)
                for c in range(4):
                    k.dma("pool", w_out[:, c, :], w_out_d[512 + c * 128:512 + (c + 1) * 128, :], r=[], w=[w_out.res], sres=w_out.res,
                          max_dma_last_dim=4096)
                abias = sb(pb, "abias", [128, 8, 640], F32)
                k.dma("sp", abias[:], abias_d.rearrange("p (h c) -> p h c", h=8), r=[], w=[abias.res], sres=abias.res)
                halob = sb(pb, "halob", [128, 512], F32)
                k.dma("sp", halob[:], halob_d[:, :], r=[], w=[halob.res], sres=halob.res)
                xtmp = [sb(pb, "xtmpb%d" % i, [128, D], F32) for i in range(2)]
                hT = [sb(pb, "hTb%d" % i, [128, 8, BB], BF16) for i in range(2)]
                qA_l = [sb(pb, "qA%d" % i, [128, 4, BB], BF16) for i in range(2)]
                qB_l = [sb(pb, "qB%d" % i, [128, 4, BB], BF16) for i in range(2)]
                kT = [sb(pb, "kT%d" % i, [128, 4, BB], BF16) for i in range(4)]
                vpad = [sb(pb, "vpad%d" % i, [128, 8, 128], BF16) for i in range(8)]
                ybT = sb(pb, "ybT", [128, 4, BB], F32)
                ybn = sb(pb, "ybn", [128, 4, BB], BF16)
                sbuf_s = [sb(pb, "sbs%d" % i, [128, 640], F32) for i in range(2)]
                Pm = [sb(pb, "Pm%d" % i, [128, 640], BF16) for i in range(2)]
                Pn = [sb(pb, "Pn%d" % i, [128, 640], BF16) for i in range(2)]
                PT = [sb(pb, "PT%d" % i, [128, 5, 128], BF16) for i in range(2)]
                st = [dict(mx=sb(pb, "a_mx%d" % i, [128, 1], F32), rs=sb(pb, "a_rs%d" % i, [128, 1], F32),
                           ri=sb(pb, "a_ri%d" % i, [128, 1], F32)) for i in range(2)]
                sq = sb(pb, "sqb", [128, BB], BF16)
                rsn = sb(pb, "rsnb", [128, BB], F32)
                pj = [ps(pb, "pjb%d" % i, [128, 512], F32) for i in range(2)]
                pT = [ps(pb, "pTb%d" % i, [128, 8, 128], BF16) for i in range(1)]
                pSm = [ps(pb, "pSm%d" % i, [128, 512], F32) for i in range(2)]
                pSr = Tn(pb.enter_context(nc.psum_tensor("ps_pSr", [128, 4, 128], F32)), "pSr", nres=4)
                pPT = ps(pb, "pPT", [128, 5, 128], BF16)
                pO = ps(pb, "pO", [128, 4, 128], F32)
                pn = pj[0]
                for v_ in vpad:
                    k.op("pool", lambda e, v_=v_: e.memset(v_[:], 0.0), w=[v_.res])
                for q_ in qA_l + qB_l:
                    k.op("pool", lambda e, q_=q_: e.memset(q_[:], 0.0), w=[q_.res])
                set_gain(SM_MIX)

                nhalo = 512 // BB
                nown = NT // BB
                tcount = 0
                pjc = 0
                ac = 0
                def prep(b):
                    nonlocal tcount, pjc
                    own = b >= nhalo
                    ob = b - nhalo
                    h = hT[b % 2]
                    kcur = kT[b % 4]
                    qA, qB = qA_l[b % 2], qB_l[b % 2]
                    for t in range(2):
                        xt_ = xtmp[tcount % 2]
                        xa = xt_[:]
                        xr = xt_.res
                        if own:
                            r0 = ob * BB + t * 128
                            k.dma("sp", xa, xown[r0:r0 + 128, :], r=[], w=[xr], sres=xr)
                        else:
                            r0 = NPRE - 512 + b * BB + t * 128
                            k.dma("sp", xa, xprev[r0:r0 + 128, :], r=[], w=[xr], sres=xr)
                        norm_T(xa, xr, pT[0], h[:, :, t * 128:(t + 1) * 128], h.res)
                        tcount += 1
                        yield
                    for m in range(4):
                        pp = pj[pjc % 2]
                        pjc += 1
                        mm_group(pp[:, 0:BB], [(w_inB[:, kc, 512 + m * 128:512 + (m + 1) * 128], h[:, kc, :]) for kc in range(8)],
                                 r=[w_inB.res, h.res], w=[pp.res])
                        k.op("act", lambda e, pp=pp, m=m, kcur=kcur: e.activation(out=kcur[:, m, :], in_=pp[:, 0:BB], func=AF.Copy),
                             r=[pp.res], w=[kcur.res])
                    yield
                    for t in range(2):
                        yield
                        gt = b * 2 + t
                        vp = vpad[gt % 8]
                        pp = pj[pjc % 2]
                        pjc += 1
                        mm_group(pp[:], [(h[:, kc, t * 128:(t + 1) * 128], w_inB[:, kc, 1024:1536]) for kc in range(8)],
                                 r=[w_inB.res, h.res], w=[pp.res])
                        ppv = pp[:].rearrange("p (h d) -> p h d", h=8)
                        k.op("act", lambda e, vp=vp, ppv=ppv: e.activation(out=vp[:, 0:8:2, 0:64], in_=ppv[:, 0:8:2, :], func=AF.Copy),
                             r=[pp.res], w=[vp.res])
                        k.op("act", lambda e, vp=vp, ppv=ppv: e.activation(out=vp[:, 1:8:2, 64:128], in_=ppv[:, 1:8:2, :], func=AF.Copy),
                             r=[pp.res], w=[vp.res])
                    if not own:
                        return
                    yield
                    for m in range(4):
                        if m == 2:
                            yield
                        pp = pj[pjc % 2]
                        pjc += 1
                        mm_group(pp[:, 0:BB], [(w_inB[:, kc, m * 128:(m + 1) * 128], h[:, kc, :]) for kc in range(8)],
                                 r=[w_inB.res, h.res], w=[pp.res])
                        k.op("act", lambda e, pp=pp, m=m: e.activation(out=qA[0:64, m, :], in_=pp[0:64, 0:BB], func=AF.Copy),
                             r=[pp.res], w=[qA.res])
                        k.op("act", lambda e, pp=pp, m=m: e.activation(out=qB[64:128, m, :], in_=pp[64:128, 0:BB], func=AF.Copy),
                             r=[pp.res], w=[qB.res])
                def attend(b, fillers):
                    nonlocal pjc, ac
                    ob = b - nhalo
                    qA, qB = qA_l[b % 2], qB_l[b % 2]
                    units = []
                    for p in range(2):
                        pieces = []
                        oc = 0
                        remaining = 640
                        pos = 128 * p
                        while remaining > 0:
                            bi = pos // BB
                            c0 = pos % BB
                            n = min(BB - c0, remaining)
                            if oc < 512 and oc + n > 512:
                                n = 512 - oc
                            pieces.append((kT[(b - 2 + bi) % 4], c0, n, oc))
                            oc += n
                            pos += n
                            remaining -= n
                        for hd in range(8):
                            units.append((p, hd, pieces))

                    def S1(u):
                        p, hd, pieces = units[u]
                        m = hd // 2
                        qs = (qA if hd % 2 == 0 else qB)
                        gi = ac0 + u
                        pm, pr = pSm[gi % 2], pSr
                        for (kt_, c0, n, oc) in pieces:
                            if oc < 512:
                                oap = pm[:, oc:oc + n]
                                ores = pm.res
                            else:
                                oap = pr[:, gi % 4, oc - 512:oc - 512 + n]
                                ores = pr.res
                            k.op("pe", lambda e, kt_=kt_, c0=c0, n=n, oap=oap: e.matmul(
                                oap, lhsT=qs[:, m, p * 128:(p + 1) * 128], rhs=kt_[:, m, c0:c0 + n],
                                start=True, stop=True), r=[qs.res, kt_.res], w=[ores])

                    def S2(u, part):
                        p, hd, pieces = units[u]
                        gi = ac0 + u
                        i2 = gi % 2
                        pm, pr = pSm[gi % 2], pSr
                        S, PP, PN, s_ = sbuf_s[i2], Pm[i2], Pn[i2], st[i2]
                        if part == 1:
                            k.op("dve", lambda e: e.reciprocal(out=s_["ri"][:], in_=s_["rs"][:]), r=[s_["rs"].res], w=[s_["ri"].res])
                            k.op("dve", lambda e: e.tensor_scalar(out=PN[:], in0=PP[:], scalar1=s_["ri"][:, 0:1],
                                                                  scalar2=None, op0=ALU.mult),
                                 r=[PP.res, s_["ri"].res], w=[PN.res])
                            return
                        k.op("dve", lambda e: e.scalar_tensor_tensor(
                            out=S[:, 0:512], in0=pm[:], scalar=0.125, in1=abias[:, hd, 0:512], op0=ALU.mult, op1=ALU.add),
                            r=[pm.res, abias.res], w=[S.res])
                        k.op("dve", lambda e: e.scalar_tensor_tensor(
                            out=S[:, 512:640], in0=pr[:, gi % 4, :], scalar=0.125, in1=abias[:, hd, 512:640], op0=ALU.mult, op1=ALU.add),
                            r=[pr.res, abias.res], w=[S.res])
                        cnt = 512 - ob * BB - 128 * p
                        if cnt > 0:
                            hb0 = ob * BB + 128 * p
                            k.op("dve", lambda e: e.tensor_tensor(
                                out=S[:, 0:cnt], in0=S[:, 0:cnt], in1=halob[:, hb0:512], op=ALU.add),
                                r=[S.res, halob.res], w=[S.res])
                        k.op("dve", lambda e: e.tensor_reduce(out=s_["mx"][:], in_=S[:], axis=AX.X, op=ALU.max, negate=True),
                             r=[S.res], w=[s_["mx"].res])
                        k.op("act", lambda e: e.activation(out=PP[:], in_=S[:], func=AF.Exp, bias=s_["mx"][:, 0:1],
                                                           scale=1.0, accum_out=s_["rs"][:]),
                             r=[S.res, s_["mx"].res], w=[PP.res, s_["rs"].res])

                    def S3(u):
                        p, hd, pieces = units[u]
                        m = hd // 2
                        gi = ac0 + u
                        i2 = gi % 2
                        PN, PTs = Pn[i2], PT[i2]
                        for kc in range(5):
                            k.op("pe", lambda e, kc=kc: e.transpose(out=pPT[:, kc, :], in_=PN[:, kc * 128:(kc + 1) * 128],
                                                                    identity=ident_b[:]),
                                 r=[PN.res, ident_b.res], w=[pPT.res])
                        k.op("act", lambda e: e.activation(out=PTs[:], in_=pPT[:], func=AF.Copy), r=[pPT.res], w=[PTs.res])
                        g0 = (b * 2 + p) - 4
                        for kc in range(5):
                            vp = vpad[(g0 + kc) % 8]
                            k.op("pe", lambda e, vp=vp, kc=kc: e.matmul(
                                pO[:, m, :], lhsT=vp[:, hd, :], rhs=PTs[:, kc, :],
                                start=(hd % 2 == 0 and kc == 0), stop=(hd % 2 == 1 and kc == 4)),
                                r=[vp.res, PTs.res], w=[pO.res])
                        if hd == 7:
                            k.op("act", lambda e: e.activation(out=ybT[:, :, p * 128:(p + 1) * 128], in_=pO[:], func=AF.Copy),
                                 r=[pO.res], w=[ybT.res])

                    def advance():
                        while fillers:
                            try:
                                next(fillers[0])
                                return
                            except StopIteration:
                                fillers.pop(0)

                    ac0 = ac
                    S1(0)
                    S1(1)
                    S2(0, 0)
                    for u in range(len(units)):
                        if u + 1 < len(units):
                            S2(u + 1, 0)
                        S2(u, 1)
                        if u + 2 < len(units):
                            S1(u + 2)
                        S3(u)
                        advance()
                    while fillers:
                        advance()
                    ac += len(units)

                def tail(b):
                    nonlocal pjc
                    ob = b - nhalo
                    for cc in range(4):
                        k.op("dve", lambda e, cc=cc: e.tensor_tensor(out=sq[:], in0=ybT[:, cc, :], in1=ybT[:, cc, :], op=ALU.mult),
                             r=[ybT.res], w=[sq.res])
                        k.op("pe", lambda e, cc=cc: e.matmul(pn[:, 0:BB], lhsT=ones_b[:], rhs=sq[:], start=(cc == 0), stop=(cc == 3)),
                             r=[ones_b.res, sq.res], w=[pn.res])
                    k.op("act", lambda e: e.activation(out=rsn[:], in_=pn[:, 0:BB], func=AF.Sqrt, bias=cst[:, 0:1], scale=1.0 / 512),
                         r=[pn.res, cst.res], w=[rsn.res])
                    k.op("dve", lambda e: e.reciprocal(out=rsn[:], in_=rsn[:]), r=[rsn.res], w=[rsn.res])
                    for cc in range(4):
                        k.op("dve", lambda e, cc=cc: e.scalar_tensor_tensor(
                            out=ybn[:, cc, :], in0=ybT[:, cc, :], scalar=smalls[:, SM_GB + cc:SM_GB + cc + 1], in1=rsn[:],
                            op0=ALU.mult, op1=ALU.mult), r=[ybT.res, smalls.res, rsn.res], w=[ybn.res])
                    yield
                    for t in range(2):
                        tile = ob * 2 + t
                        tok0 = ob * BB + t * 128
                        for half in range(2):
                            pp = pj[pjc % 2]
                            pjc += 1
                            pairs = [(ybn[:, c, t * 128:(t + 1) * 128], w_out[:, c, half * 512:(half + 1) * 512]) for c in range(4)]
                            mm_group(pp[:], pairs, r=[ybn.res, w_out.res], w=[pp.res])
                            k.op("dve", lambda e, pp=pp, tile=tile, half=half: e.tensor_tensor(
                                out=x_res[:, tile, half * 512:(half + 1) * 512], in0=pp[:], in1=x_res[:, tile, half * 512:(half + 1) * 512],
                                op=ALU.add), r=[pp.res, x_res.rs[tile]], w=[x_res.rs[tile]])
                        yield
                nb_ = nhalo + nown
                for b0 in range(nhalo + 1):
                    for _ in prep(b0):
                        pass
                for b in range(nhalo, nb_):
                    fl = []
                    if b - 1 >= nhalo:
                        fl.append(tail(b - 1))
                    if b + 1 < nb_:
                        fl.append(prep(b + 1))
                    attend(b, fl)
                for _ in tail(nb_ - 1):
                    pass
                k.barrier(release=[w_inB.res, w_out.res, abias.res, halob.res] + [t_.res for t_ in xtmp])

        if debug:
            for tile in range(NTILE):
                k.dma("sp", dbg["dbg1"][tile * 128:(tile + 1) * 128, :], x_res[:, tile, :], r=[x_res.rs[tile]], w=[], sres=x_res.rs[tile])

        with ExitStack() as p2:
            B2 = 512
            w_q = sb(p2, "w_q", [128, 8, D], BF16)
            load_w_bf16(w_q, w_q_d[:, :], D)
            w_o = sb(p2, "w_o", [128, 8, D], BF16)
            load_w_bf16(w_o, w_o_d[:, :], D)
            kmT = sb(p2, "kmT", [128, 8, 256], BF16)
            vmem = sb(p2, "vmem", [128, 2, D], BF16)
            pj = [ps(p2, "pj2%d" % i, [128, 512], F32) for i in range(2)]
            pT = [ps(p2, "pT2%d" % i, [128, 8, 128], BF16) for i in range(1)]
            pS2 = [ps(p2, "pS2%d" % i, [128, 4, 256], F32) for i in range(2)]
            pPT2 = ps(p2, "pPT2", [128, 8, 128], BF16)
            pjc = 0
            with ExitStack() as p2s:
                w_kv = sb(p2s, "w_kv", [128, 8, 2048], BF16)
                load_w_bf16(w_kv, w_kv_d[:, :], 2048)
                memt = [sb(p2s, "memt%d" % i, [128, D], F32) for i in range(2)]
                memT = sb(p2s, "memT", [128, 8, 256], BF16)
                set_gain(SM_MEM)
                for t in range(2):
                    k.dma("sp", memt[t][:], mem_d[t * 128:(t + 1) * 128, :], r=[], w=[memt[t].res], sres=memt[t].res)
                    norm_T(memt[t][:], memt[t].res, pT[0], memT[:, :, t * 128:(t + 1) * 128], memT.res)
                for oc in range(8):
                    pp = pj[pjc % 2]
                    pjc += 1
                    mm_group(pp[:, 0:256], [(w_kv[:, kc, oc * 128:(oc + 1) * 128], memT[:, kc, :]) for kc in range(8)],
                             r=[w_kv.res, memT.res], w=[pp.res])
                    k.op("act", lambda e, pp=pp, oc=oc: e.activation(out=kmT[:, oc, :], in_=pp[:, 0:256], func=AF.Copy),
                         r=[pp.res], w=[kmT.res])
                for t in range(2):
                    for half in range(2):
                        pp = pj[pjc % 2]
                        pjc += 1
                        mm_group(pp[:], [(memT[:, kc, t * 128:(t + 1) * 128], w_kv[:, kc, 1024 + half * 512:1024 + (half + 1) * 512])
                                         for kc in range(8)], r=[w_kv.res, memT.res], w=[pp.res])
                        k.op("act", lambda e, pp=pp, t=t, half=half: e.activation(out=vmem[:, t, half * 512:(half + 1) * 512], in_=pp[:], func=AF.Copy),
                             r=[pp.res], w=[vmem.res])
                k.barrier(release=[w_kv.res] + [t_.res for t_ in memt])
            hT = [sb(p2, "hT2%d" % i, [128, 8, B2], BF16) for i in range(2)]
            qT = sb(p2, "qT2", [128, 8, B2], BF16)
            P2 = [sb(p2, "P2%d" % i, [128, 4, 256], BF16) for i in range(2)]
            P2n = [sb(p2, "P2n%d" % i, [128, 4, 256], BF16) for i in range(2)]
            PT2 = sb(p2, "PT2", [128, 4, 2, B2], BF16)
            oT = sb(p2, "oT2", [128, 8, B2], BF16)
            st2 = [dict(mx=sb(p2, "c_mx%d" % i, [128, 4], F32), rs=sb(p2, "c_rs%d" % i, [128, 4], F32),
                        ri=sb(p2, "c_ri%d" % i, [128, 4], F32)) for i in range(2)]
            set_gain(SM_CROSS)
            tc2 = 0
            for b in range(NT // B2):
                h = hT[b % 2]
                for t in range(4):
                    tile = b * 4 + t
                    norm_T(x_res[:, tile, :], x_res.rs[tile], pT[0], h[:, :, t * 128:(t + 1) * 128], h.res)
                for oc in range(8):
                    pp = pj[pjc % 2]
                    pjc += 1
                    mm_group(pp[:], [(w_q[:, kc, oc * 128:(oc + 1) * 128], h[:, kc, :]) for kc in range(8)],
                             r=[w_q.res, h.res], w=[pp.res])
                    k.op("act", lambda e, pp=pp, oc=oc: e.activation(out=qT[:, oc, :], in_=pp[:], func=AF.Copy), r=[pp.res], w=[qT.res])
                def c_S1(t, i2):
                    pS_ = pS2[i2]
                    for hh_ in range(4):
                        mm_group(pS_[:, hh_, :], [(qT[:, 2 * hh_ + j, t * 128:(t + 1) * 128], kmT[:, 2 * hh_ + j, :]) for j in range(2)],
                                 r=[qT.res, kmT.res], w=[pS_.res])

                def c_S2(t, i2):
                    pS_, P_, Pn_, s_ = pS2[i2], P2[i2], P2n[i2], st2[i2]
                    k.op("dve", lambda e: e.tensor_reduce(out=s_["mx"][:], in_=pS_[:], axis=AX.X, op=ALU.max, negate=True),
                         r=[pS_.res], w=[s_["mx"].res])
                    k.op("dve", lambda e: e.tensor_scalar(out=s_["mx"][:], in0=s_["mx"][:], scalar1=1.0 / 16, scalar2=None, op0=ALU.mult),
                         r=[s_["mx"].res], w=[s_["mx"].res])
                    for hh_ in range(4):
                        k.op("act", lambda e, hh_=hh_: e.activation(
                            out=P_[:, hh_, :], in_=pS_[:, hh_, :], func=AF.Exp, bias=s_["mx"][:, hh_:hh_ + 1], scale=1.0 / 16,
                            accum_out=s_["rs"][:, hh_:hh_ + 1]), r=[pS_.res, s_["mx"].res], w=[P_.res, s_["rs"].res])
                    k.op("dve", lambda e: e.reciprocal(out=s_["ri"][:], in_=s_["rs"][:]), r=[s_["rs"].res], w=[s_["ri"].res])
                    k.op("dve", lambda e: e.tensor_tensor(
                        out=Pn_[:], in0=P_[:], in1=s_["ri"][:, :].unsqueeze(2).to_broadcast([128, 4, 256]), op=ALU.mult),
                        r=[P_.res, s_["ri"].res], w=[Pn_.res])

                def c_S3(t, i2):
                    Pn_ = P2n[i2]
                    for hh_ in range(4):
                        for mc in range(2):
                            k.op("pe", lambda e, hh_=hh_, mc=mc: e.transpose(
                                out=pPT2[:, hh_ * 2 + mc, :], in_=Pn_[:, hh_, mc * 128:(mc + 1) * 128], identity=ident_b[:]),
                                r=[Pn_.res, ident_b.res], w=[pPT2.res])
                    k.op("act", lambda e: e.activation(out=PT2[:, :, :, t * 128:(t + 1) * 128],
                                                       in_=pPT2[:].rearrange("p (h m) t -> p h m t", h=4), func=AF.Copy),
                         r=[pPT2.res], w=[PT2.res])

                c_S1(0, tc2 % 2)
                for t in range(4):
                    if t + 1 < 4:
                        c_S1(t + 1, (tc2 + 1) % 2)
                    c_S2(t, tc2 % 2)
                    c_S3(t, tc2 % 2)
                    tc2 += 1
                for oc in range(8):
                    pp = pj[pjc % 2]
                    pjc += 1
                    mm_group(pp[:], [(vmem[:, mc, oc * 128:(oc + 1) * 128], PT2[:, oc // 2, mc, :]) for mc in range(2)],
                             r=[vmem.res, PT2.res], w=[pp.res])
                    k.op("act", lambda e, pp=pp, oc=oc: e.activation(out=oT[:, oc, :], in_=pp[:], func=AF.Copy), r=[pp.res], w=[oT.res])
                for t in range(4):
                    tile = b * 4 + t
                    for half in range(2):
                        pp = pj[pjc % 2]
                        pjc += 1
                        mm_group(pp[:], [(oT[:, c, t * 128:(t + 1) * 128], w_o[:, c, half * 512:(half + 1) * 512]) for c in range(8)],
                                 r=[oT.res, w_o.res], w=[pp.res])
                        k.op("dve", lambda e, pp=pp, tile=tile, half=half: e.tensor_tensor(
                            out=x_res[:, tile, half * 512:(half + 1) * 512], in0=pp[:], in1=x_res[:, tile, half * 512:(half + 1) * 512],
                            op=ALU.add), r=[pp.res, x_res.rs[tile]], w=[x_res.rs[tile]])
            k.barrier(release=[w_q.res, w_o.res])

        if debug:
            for tile in range(NTILE):
                k.dma("sp", dbg["dbg2"][tile * 128:(tile + 1) * 128, :], x_res[:, tile, :], r=[x_res.rs[tile]], w=[], sres=x_res.rs[tile])

        with ExitStack() as p3:
            p3s = p3.enter_context(ExitStack())
            slotI = sb(p3s, "slotI", [128, NT], F32)
            slotJ = sb(p3s, "slotJ", [128, NT], F32)
            slotG = sb(p3s, "slotG", [128, NT], F32)
            set_gain(SM_FFN)
            with ExitStack() as pa:
                B3 = 256
                w_qry = sb(pa, "w_qry", [128, 8, 2048], BF16)
                load_w_bf16(w_qry, w_qry_d[:, :], 2048)
                skb = sb(pa, "skb", [128, 16, 128], BF16)
                k.dma("pool", skb[:], skT_d.rearrange("p (o n) -> p o n", o=16), r=[], w=[skb.res], sres=skb.res)
                hT = [sb(pa, "hT3%d" % i, [128, 8, B3], BF16) for i in range(2)]
                qpT = sb(pa, "qpT", [128, 16, B3], BF16)
                sc = sb(pa, "sc", [128, 16, 128], F32, nres=4)
                sc2 = sb(pa, "sc2", [128, 16, 128], F32, nres=16)
                tv = sb(pa, "tv", [128, 16, 16], F32, nres=16)
                tiu = sb(pa, "tiu", [128, 16, 16], U32, nres=16)
                tif = sb(pa, "tif", [128, 16, 16], F32)
                cand = sb(pa, "cand", [128, 8, 256], F32)
                cand2 = sb(pa, "cand2", [128, 8, 256], F32, nres=8)
                ts = sb(pa, "ts", [128, 8, 16], F32, nres=8)
                posu = sb(pa, "posu", [128, 8, 16], U32, nres=8)
                au = sb(pa, "au", [128, 8, 16], U32)
                bu = sb(pa, "bu", [128, 8, 16], U32)
                af = sb(pa, "af", [128, 8, 16], F32)
                bf = sb(pa, "bf", [128, 8, 16], F32)
                eq = sb(pa, "eq", [128, 8, 16, 16], F32)
                isel = sb(pa, "isel", [128, 8, 16], F32)
                jsel = sb(pa, "jsel", [128, 8, 16], F32)
                gsel = sb(pa, "gsel", [128, 8, 16], F32)
                sm = sb(pa, "sm", [128, 8], F32)
                pj = [ps(pa, "pj3%d" % i, [128, 512], F32) for i in range(2)]
                pT = [ps(pa, "pT3%d" % i, [128, 8, 128], BF16) for i in range(1)]
                psc = [ps(pa, "psc%d" % i, [128, 4, 128], F32) for i in range(2)]
                pTs = ps(pa, "pTs", [128, 3, 128], F32)
                pjc = 0
                scc = 0
                iota16 = iota_f[:, 0:16]
                for b in range(NT // B3):
                    h = hT[b % 2]
                    for t in range(2):
                        tile = b * 2 + t
                        norm_T(x_res[:, tile, :], x_res.rs[tile], pT[0], h[:, :, t * 128:(t + 1) * 128], h.res)
                    for oc in range(16):
                        pp = pj[pjc % 2]
                        pjc += 1
                        mm_group(pp[:, 0:B3], [(w_qry[:, kc, oc * 128:(oc + 1) * 128], h[:, kc, :]) for kc in range(8)],
                                 r=[w_qry.res, h.res], w=[pp.res])
                        k.op("act", lambda e, pp=pp, oc=oc: e.activation(out=qpT[:, oc, :], in_=pp[:, 0:B3], func=AF.Copy),
                             r=[pp.res], w=[qpT.res])
                    for t in range(2):
                        tile = b * 2 + t
                        for g4 in range(4):
                            pq = psc[scc % 2]
                            scc += 1
                            for j in range(4):
                                oc = g4 * 4 + j
                                k.op("pe", lambda e, pq=pq, j=j, oc=oc, t=t: e.matmul(
                                    pq[:, j, :], lhsT=qpT[:, oc, t * 128:(t + 1) * 128], rhs=skb[:, oc, :], start=True, stop=True),
                                    r=[qpT.res, skb.res], w=[pq.res])
                            k.op("act", lambda e, pq=pq, g4=g4: e.activation(out=sc[:, g4 * 4:(g4 + 1) * 4, :], in_=pq[:], func=AF.Copy),
                                 r=[pq.res], w=[sc.rs[g4]])
                        for oc in range(16):
                            k.op("dve", lambda e, oc=oc: e.max(out=tv[:, oc, 0:8], in_=sc[:, oc, :]), r=[sc.rs[oc // 4]], w=[tv.rs[oc]])
                        for oc in range(16):
                            k.op("dve", lambda e, oc=oc: e.max_index(out=tiu[:, oc, 0:8], in_max=tv[:, oc, 0:8], in_values=sc[:, oc, :]),
                                 r=[sc.rs[oc // 4], tv.rs[oc]], w=[tiu.rs[oc]])
                        for oc in range(16):
                            k.op("dve", lambda e, oc=oc: e.match_replace(out=sc2[:, oc, :], in_to_replace=tv[:, oc, 0:8], in_values=sc[:, oc, :],
                                                                        imm_value=NEG), r=[sc.rs[oc // 4], tv.rs[oc]], w=[sc2.rs[oc]])
                        for oc in range(16):
                            k.op("dve", lambda e, oc=oc: e.max(out=tv[:, oc, 8:16], in_=sc2[:, oc, :]), r=[sc2.rs[oc]], w=[tv.rs[oc]])
                        for oc in range(16):
                            k.op("dve", lambda e, oc=oc: e.max_index(out=tiu[:, oc, 8:16], in_max=tv[:, oc, 8:16], in_values=sc2[:, oc, :]),
                                 r=[sc2.rs[oc], tv.rs[oc]], w=[tiu.rs[oc]])
                        k.op("dve", lambda e: e.tensor_copy(out=tif[:], in_=tiu[:]), r=tiu.rs, w=[tif.res])
                        tv4 = tv[:].rearrange("p (h two) a -> p h two a", two=2)
                        tif4 = tif[:].rearrange("p (h two) a -> p h two a", two=2)
                        k.op("dve", lambda e, tv4=tv4: e.tensor_tensor(
                            out=cand[:].rearrange("p h (a b) -> p h a b", a=16),
                            in0=tv4[:, :, 0, :].unsqueeze(3).to_broadcast([128, 8, 16, 16]),
                            in1=tv4[:, :, 1, :].unsqueeze(2).to_broadcast([128, 8, 16, 16]), op=ALU.add),
                            r=tv.rs, w=[cand.res])
                        for hd in range(8):
                            k.op("dve", lambda e, hd=hd: e.max(out=ts[:, hd, 0:8], in_=cand[:, hd, :]), r=[cand.res], w=[ts.rs[hd]])
                        for hd in range(8):
                            k.op("dve", lambda e, hd=hd: e.max_index(out=posu[:, hd, 0:8], in_max=ts[:, hd, 0:8], in_values=cand[:, hd, :]),
                                 r=[cand.res, ts.rs[hd]], w=[posu.rs[hd]])
                        for hd in range(8):
                            k.op("dve", lambda e, hd=hd: e.match_replace(out=cand2[:, hd, :], in_to_replace=ts[:, hd, 0:8], in_values=cand[:, hd, :],
                                                                        imm_value=NEG), r=[cand.res, ts.rs[hd]], w=[cand2.rs[hd]])
                        for hd in range(8):
                            k.op("dve", lambda e, hd=hd: e.max(out=ts[:, hd, 8:16], in_=cand2[:, hd, :]), r=[cand2.rs[hd]], w=[ts.rs[hd]])
                        for hd in range(8):
                            k.op("dve", lambda e, hd=hd: e.max_index(out=posu[:, hd, 8:16], in_max=ts[:, hd, 8:16], in_values=cand2[:, hd, :]),
                                 r=[cand2.rs[hd], ts.rs[hd]], w=[posu.rs[hd]])
                        k.op("dve", lambda e: e.tensor_scalar(out=au[:], in0=posu[:], scalar1=4, scalar2=None, op0=ALU.logical_shift_right),
                             r=posu.rs, w=[au.res])
                        k.op("dve", lambda e: e.tensor_scalar(out=bu[:], in0=posu[:], scalar1=15, scalar2=None, op0=ALU.bitwise_and),
                             r=posu.rs, w=[bu.res])
                        k.op("dve", lambda e: e.tensor_copy(out=af[:], in_=au[:]), r=[au.res], w=[af.res])
                        k.op("dve", lambda e: e.tensor_copy(out=bf[:], in_=bu[:]), r=[bu.res], w=[bf.res])
                        for (sel, rk, which) in ((isel, af, 0), (jsel, bf, 1)):
                            k.op("dve", lambda e, rk=rk: e.tensor_tensor(
                                out=eq[:], in0=rk[:].unsqueeze(3).to_broadcast([128, 8, 16, 16]),
                                in1=iota16.unsqueeze(1).unsqueeze(1).to_broadcast([128, 8, 16, 16]), op=ALU.is_equal),
                                r=[rk.res, iota_f.res], w=[eq.res])
                            k.op("dve", lambda e, which=which, tif4=tif4: e.tensor_tensor(
                                out=eq[:], in0=eq[:], in1=tif4[:, :, which, :].unsqueeze(2).to_broadcast([128, 8, 16, 16]), op=ALU.mult),
                                r=[eq.res, tif.res], w=[eq.res])
                            k.op("dve", lambda e, sel=sel: e.tensor_reduce(out=sel[:], in_=eq[:], axis=AX.X, op=ALU.add),
                                 r=[eq.res], w=[sel.res])
                        k.op("dve", lambda e: e.tensor_tensor(out=gsel[:], in0=ts[:], in1=ts[:, :, 0:1].to_broadcast([128, 8, 16]), op=ALU.subtract),
                             r=ts.rs, w=[gsel.res])
                        k.op("act", lambda e: e.activation(out=gsel[:], in_=gsel[:], func=AF.Exp), r=[gsel.res], w=[gsel.res])
                        k.op("dve", lambda e: e.tensor_reduce(out=sm[:], in_=gsel[:], axis=AX.X, op=ALU.add), r=[gsel.res], w=[sm.res])
                        k.op("dve", lambda e: e.reciprocal(out=sm[:], in_=sm[:]), r=[sm.res], w=[sm.res])
                        k.op("dve", lambda e: e.tensor_tensor(out=gsel[:], in0=gsel[:], in1=sm[:, :].unsqueeze(2).to_broadcast([128, 8, 16]), op=ALU.mult),
                             r=[gsel.res, sm.res], w=[gsel.res])
                        for i3, (src, dst) in enumerate(((isel, slotI), (jsel, slotJ), (gsel, slotG))):
                            k.op("pe", lambda e, i3=i3, src=src: e.transpose(out=pTs[:, i3, :], in_=src[:].rearrange("p h r -> p (h r)"),
                                                                            identity=ident_f[:]),
                                 r=[src.res, ident_f.res], w=[pTs.res])
                        for i3, (src, dst) in enumerate(((isel, slotI), (jsel, slotJ), (gsel, slotG))):
                            k.op("act", lambda e, i3=i3, dst=dst, tile=tile: e.activation(out=dst[:, tile * 128:(tile + 1) * 128], in_=pTs[:, i3, :], func=AF.Copy),
                                 r=[pTs.res], w=[dst.res])
                k.barrier(release=[w_qry.res, skb.res])

            with ExitStack() as pb:
                TCH = 8
                ohj = [sb(pb, "ohj%d" % i, [128, TCH, 128], BF16) for i in range(4)]
                eqi = [sb(pb, "eqi%d" % i, [128, TCH, 128], BF16) for i in range(4)]
                rig = [sb(pb, "rig%d" % i, [128, TCH, 128], BF16) for i in range(4)]
                Wst = [sb(pb, "Wst%d" % i, [128, 128, 128], BF16, nres=32) for i in range(2)]
                pW = [ps(pb, "pW%d" % i, [128, 4, 128], F32) for i in range(4)]
                wc = 0
                chc = 0
                iota_b3 = iota_f[:, :].unsqueeze(1).to_broadcast([128, TCH, 128])
                for tile in range(NTILE):
                    W_ = Wst[tile % 2]
                    for ch in range(128 // TCH):
                        oj, ei, rg = ohj[chc % 4], eqi[chc % 4], rig[chc % 4]
                        chc += 1
                        tok0 = tile * 128 + ch * TCH
                        k.op("dve", lambda e, oj=oj, tok0=tok0: e.tensor_tensor(
                            out=oj[:], in0=iota_b3, in1=slotJ[:, tok0:tok0 + TCH].unsqueeze(2).to_broadcast([128, TCH, 128]), op=ALU.is_equal),
                            r=[iota_f.res, slotJ.res], w=[oj.res])
                        k.op("dve", lambda e, ei=ei, tok0=tok0: e.tensor_tensor(
                            out=ei[:], in0=iota_b3, in1=slotI[:, tok0:tok0 + TCH].unsqueeze(2).to_broadcast([128, TCH, 128]), op=ALU.is_equal),
                            r=[iota_f.res, slotI.res], w=[ei.res])
                        k.op("pool", lambda e, ei=ei, rg=rg, tok0=tok0: e.tensor_tensor(
                            out=rg[:], in0=ei[:], in1=slotG[:, tok0:tok0 + TCH].unsqueeze(2).to_broadcast([128, TCH, 128]), op=ALU.mult),
                            r=[ei.res, slotG.res], w=[rg.res])
                        for q4 in range(TCH // 4):
                            pw = pW[wc % 4]
                            for j in range(4):
                                tt_ = q4 * 4 + j
                                k.op("pe", lambda e, pw=pw, j=j, oj=oj, rg=rg, tt_=tt_: e.matmul(
                                    pw[:, j, :], lhsT=oj[:, tt_, :], rhs=rg[:, tt_, :], start=True, stop=True),
                                    r=[oj.res, rg.res], w=[pw.res])
                            t0 = ch * TCH + q4 * 4
                            if True:
                                k.op("act", lambda e, pw=pw, W_=W_, t0=t0: e.activation(
                                    out=W_[:, :, t0:t0 + 4], in_=pw[:].rearrange("p t i -> p i t"), func=AF.Copy),
                                    r=[pw.res], w=[W_.rs[t0 // 4]])
                            else:
                                k.op("dve", lambda e, pw=pw, W_=W_, t0=t0: e.tensor_copy(
                                    out=W_[:, :, t0:t0 + 4], in_=pw[:].rearrange("p t i -> p i t")),
                                    r=[pw.res], w=[W_.rs[t0 // 4]])
                            wc += 1
                    k.dma("sp", wd_d[tile], W_[:].rearrange("p i t -> p (i t)"), r=W_.rs, w=[], sres=W_.res)
                k.barrier(release=[w_.res for w_ in Wst])
            p3s.close()

            with ExitStack() as pc:
                T3 = 256
                GS = 8
                NG = 128 // GS
                hfT = sb(pc, "hfT", [128, 8, NT], BF16)
                pT = [ps(pc, "pT4%d" % i, [128, 8, 128], BF16) for i in range(1)]
                pA = [ps(pc, "pA%d" % i, [128, 512], F32) for i in range(3)]
                pO = [ps(pc, "pO4%d" % i, [128, 512], F32) for i in range(4)]
                for tile in range(NTILE):
                    norm_T(x_res[:, tile, :], x_res.rs[tile], pT[0], hfT[:, :, tile * 128:(tile + 1) * 128], hfT.res)
                ut = [sb(pc, "ut%d" % i, [128, GS, 8, 128], BF16) for i in range(2)]
                vt = [sb(pc, "vt%d" % i, [128, GS, D], BF16) for i in range(2)]
                wsl = [sb(pc, "wsl%d" % i, [128, 2, GS, 128], BF16) for i in range(2)]
                ge = [sb(pc, "ge%d" % i, [128, T3], F32) for i in range(3)]
                gw = [sb(pc, "gw%d" % i, [128, T3], BF16) for i in range(3)]
                uT_v = uT_d.rearrange("(i p) (c e) -> p i c e", p=128, c=8)
                ev_v = ev_d.rearrange("(i e) d -> e i d", e=128)
                wd_v = wd_d.rearrange("t j (i x) -> j t i x", i=128)
                items = [(g, tb, i) for g in range(NG) for tb in range(NT // T3) for i in range(GS)]
                state = {}

                def emitA(n):
                    g, tb, i = items[n]
                    u_, v_ = ut[g % 2], vt[g % 2]
                    if tb == 0 and i == 0:
                        for i_ in range(GS):
                            k.dma("pool", u_[:, i_, :, :], uT_v[:, g * GS + i_, :, :], r=[], w=[u_.res], sres=u_.res)
                            k.dma("pool", v_[:, i_, :], ev_v[:, g * GS + i_, :], r=[], w=[v_.res], sres=v_.res, max_dma_last_dim=4096)
                    if i == 0:
                        ws_ = wsl[(g * (NT // T3) + tb) % 2]
                        k.dma("sp", ws_[:], wd_v[:, 2 * tb:2 * tb + 2, g * GS:(g + 1) * GS, :], r=[], w=[ws_.res], sres=ws_.res)
                    ws_ = wsl[(g * (NT // T3) + tb) % 2]
                    pa_ = pA[n % 3]
                    ge_, gw_ = ge[n % 3], gw[n % 3]
                    mm_group(pa_[:, 0:T3], [(u_[:, i, c, :], hfT[:, c, tb * T3:(tb + 1) * T3]) for c in range(8)],
                             r=[u_.res, hfT.res], w=[pa_.res])
                    k.op("act", lambda e: e.activation(out=ge_[:], in_=pa_[:, 0:T3], func=AF.Gelu_apprx_tanh),
                         r=[pa_.res], w=[ge_.res])
                    k.op("dve", lambda e: e.tensor_tensor(
                        out=gw_[:].rearrange("p (a t) -> p a t", a=2), in0=ge_[:].rearrange("p (a t) -> p a t", a=2),
                        in1=ws_[:, :, i, :], op=ALU.mult), r=[ge_.res, ws_.res], w=[gw_.res])

                def emitV(n):
                    g, tb, i = items[n]
                    v_ = vt[g % 2]
                    gw_ = gw[n % 3]
                    for t in range(2):
                        for half in range(2):
                            po = pO[t * 2 + half]
                            k.op("pe", lambda e, po=po, t=t, half=half: e.matmul(
                                po[:], lhsT=gw_[:, t * 128:(t + 1) * 128], rhs=v_[:, i, half * 512:(half + 1) * 512],
                                start=(i == 0), stop=(i == GS - 1)), r=[gw_.res, v_.res], w=[po.res])
                    if i == GS - 1:
                        for t in range(2):
                            tile = tb * 2 + t
                            for half in range(2):
                                po = pO[t * 2 + half]
                                k.op("dve", lambda e, po=po, tile=tile, half=half: e.tensor_tensor(
                                    out=x_res[:, tile, half * 512:(half + 1) * 512], in0=po[:], in1=x_res[:, tile, half * 512:(half + 1) * 512],
                                    op=ALU.add), r=[po.res, x_res.rs[tile]], w=[x_res.rs[tile]])

                emitA(0)
                emitA(1)
                for n in range(len(items)):
                    if n + 2 < len(items):
                        emitA(n + 2)
                    emitV(n)
                k.barrier(release=[t_.res for t_ in ut + vt + wsl])

        with ExitStack() as p4:
            gfin = sb(p4, "gfin", [128, D], F32)
            k.dma("sp", gfin[:], gfin_d[:, :], r=[], w=[gfin.res], sres=gfin.res)
            ot = [sb(p4, "ot%d" % i, [128, D], F32) for i in range(2)]
            for tile in range(NTILE):
                n = nrm[tile % 2]
                o_ = ot[tile % 2]
                xa = x_res[:, tile, :]
                xr = x_res.rs[tile]
                k.op("dve", lambda e, n=n, xa=xa: e.scalar_tensor_tensor(out=n["junk"][:], in0=xa, scalar=1.0, in1=xa,
                                                                         op0=ALU.mult, op1=ALU.mult, accum_out=n["ss"][:]),
                     r=[xr], w=[n["junk"].res, n["ss"].res])
                k.op("act", lambda e, n=n: e.activation(out=n["sd"][:], in_=n["ss"][:], func=AF.Sqrt, bias=cst[:, 0:1], scale=1.0 / D),
                     r=[n["ss"].res, cst.res], w=[n["sd"].res])
                k.op("dve", lambda e, n=n: e.reciprocal(out=n["rstd"][:], in_=n["sd"][:]), r=[n["sd"].res], w=[n["rstd"].res])
                k.op("dve", lambda e, n=n, xa=xa, o_=o_: e.scalar_tensor_tensor(out=o_[:], in0=xa, scalar=n["rstd"][:, 0:1], in1=gfin[:],
                                                                                op0=ALU.mult, op1=ALU.mult),
                     r=[xr, n["rstd"].res, gfin.res], w=[o_.res])
                k.dma("sp", y_d[tile * 128:(tile + 1) * 128, :], o_[:], r=[o_.res], w=[], sres=o_.res)
            k.barrier()
    return nc, k.n_ins


def prep_shared(inp):
    f = np.float32
    g = lambda a: np.ascontiguousarray(np.asarray(a, dtype=f))
    sm = np.zeros((128, NSM), f)

    def put(col, vec, nch):
        sm[:, col:col + nch] = np.asarray(vec, f).reshape(nch, 128).T

    put(SM_MIX, inp["norm_mix"][0], 8)
    put(SM_CROSS, inp["norm_cross"][0], 8)
    put(SM_MEM, inp["norm_mem"][0], 8)
    put(SM_FFN, inp["norm_ffn"][0], 8)
    put(SM_GA, inp["norm_grp_a"][0], 4)
    put(SM_GB, inp["norm_grp_b"][0], 4)
    put(SM_CB, inp["conv_b"][0], 4)
    put(SM_BA, inp["gate_a_b"][0], 4)
    put(SM_BX, inp["gate_x_b"][0], 4)
    put(SM_LAM, inp["lru_lambda"][0], 4)
    cw = np.asarray(inp["conv_w"][0], f)
    for cc in range(4):
        for j in range(4):
            sm[:, SM_CW + cc * 4 + j] = cw[j, cc * 128:(cc + 1) * 128]
    gbd = np.zeros((128, 8, 128), f)
    for gi, key in enumerate(("gate_a_w", "gate_x_w")):
        w = np.asarray(inp[key][0], f)
        for cc in range(4):
            gbd[0:64, gi * 4 + cc, 0:64] = w[2 * cc]
            gbd[64:128, gi * 4 + cc, 64:128] = w[2 * cc + 1]
    rb = np.asarray(inp["rel_bias"][0], f)
    qi = np.arange(128)[:, None]
    kj = np.arange(640)[None, :]
    idx = np.clip(512 + qi - kj, -128, 128) + 128
    ab = rb[:, idx]
    valid = np.where(qi < 64, kj < 576, kj >= 64)
    ab = np.where(valid[None], ab, f(NEG)).astype(f)
    abias = np.ascontiguousarray(ab.transpose(1, 0, 2)).reshape(128, 8 * 640)
    sk = np.asarray(inp["sub_keys"][0], f)
    skT = np.ascontiguousarray(sk.reshape(16, 128, 128).transpose(2, 0, 1)).reshape(128, 16 * 128)
    u = np.asarray(inp["expert_u"][0], f)
    uT = np.ascontiguousarray(u.reshape(128, 128, 8, 128).transpose(0, 3, 2, 1)).reshape(16384, D)
    shared = {
        "w_in": g(inp["w_in"][0]), "w_out": g(inp["w_out"][0]), "w_q": g(inp["w_q_mem"][0]),
        "w_kv": g(inp["w_kv_mem"][0]), "w_o": g(inp["w_o_mem"][0]), "w_qry": g(inp["w_query"][0]),
        "smalls": sm, "gbd": gbd.reshape(128, 8 * 128), "abias": abias, "skT": skT, "uT": uT,
        "ev": g(inp["expert_v"][0]),
        "gfin": np.ascontiguousarray(np.broadcast_to(np.asarray(inp["norm_final"], f)[None, :], (128, D))),
        "ident": np.eye(128, dtype=f),
        "iota": np.ascontiguousarray(np.broadcast_to(np.arange(128, dtype=f)[None, :], (128, 128))),
    }
    return shared


def make_in_maps(inp, NT):
    x = np.asarray(inp["x"], np.float32)
    mem = np.asarray(inp["mem"], np.float32)
    B, S, _ = x.shape
    per_seq = S // NT
    NPRE = 3 * NT
    shared = prep_shared(inp)
    maps = []
    for c in range(B * per_seq):
        b, q = divmod(c, per_seq)
        xprev = np.zeros((NPRE, D), np.float32)
        if q > 0:
            xprev[NPRE - q * NT:] = x[b, 0:q * NT]
        pflag = np.zeros((128, NPRE // 512), np.float32)
        for j in range(NPRE // 512):
            if j * 512 >= NPRE - q * NT:
                pflag[:, j] = 1.0
        halob = np.full((128, 512), 0.0 if q > 0 else NEG, np.float32)
        m = dict(shared)
        m.update({"xown": np.ascontiguousarray(x[b, q * NT:(q + 1) * NT]), "xprev": xprev, "pflag": pflag,
                  "halob": halob, "mem": np.ascontiguousarray(mem[b])})
        maps.append(m)
    return maps


_CACHE = {}


def kernel(**inputs):
    NT = 2048
    if NT not in _CACHE:
        _CACHE[NT] = build(NT)[0]
    nc = _CACHE[NT]
    maps = make_in_maps(inputs, NT)
    res = run_bass_kernel_spmd(nc, maps, core_ids=list(range(N_CORES)))
    x = np.asarray(inputs["x"])
    B, S, _ = x.shape
    out = np.empty((B, S, D), np.float32)
    per_seq = S // NT
    for c in range(N_CORES):
        b, q = divmod(c, per_seq)
        out[b, q * NT:(q + 1) * NT] = res.results[c]["y"]
    return out
```

```python
import numpy as np
from contextlib import ExitStack
import concourse.bass as bass
import concourse.mybir as mybir
from concourse.bass_utils import run_bass_kernel_spmd

F32 = mybir.dt.float32
BF16 = mybir.dt.bfloat16
U32 = mybir.dt.uint32
AF = mybir.ActivationFunctionType
ALU = mybir.AluOpType
AX = mybir.AxisListType

D = 1024
EPS = 1e-6
NEG = -1e30
N_CORES = 8
SAME_ENG_SYNC = True

SM_MIX, SM_CROSS, SM_MEM, SM_FFN = 0, 8, 16, 24
SM_GA, SM_GB, SM_CB, SM_BA, SM_BX, SM_LAM, SM_CW = 32, 36, 40, 44, 48, 52, 56
NSM = 72


class Res:
    __slots__ = ("name", "w", "r", "ds")

    def __init__(self, name):
        self.name = name
        self.w = None
        self.r = {}
        self.ds = None


class KB:
    def __init__(self, nc, es):
        self.nc = nc
        self.es = es
        self.E = {}
        for nm, e in (("pe", nc.tensor), ("act", nc.scalar), ("dve", nc.vector),
                      ("pool", nc.gpsimd), ("sp", nc.sync)):
            self.E[nm] = dict(e=e, sem=es.enter_context(nc.semaphore("e_" + nm)), cnt=0, waited={}, nm=nm)
        self.free_ds = []
        self.all_ds = []
        self.n_ins = 0

    def _collect(self, r, w):
        deps = {}
        for x in r:
            if x.w is not None:
                s, v = x.w
                if deps.get(s, 0) < v:
                    deps[s] = v
        for x in w:
            if x.w is not None:
                s, v = x.w
                if deps.get(s, 0) < v:
                    deps[s] = v
            for s, v in x.r.items():
                if deps.get(s, 0) < v:
                    deps[s] = v
        return deps

    def _waits(self, E, deps, skip_own):
        for s, v in deps.items():
            if skip_own and s is E["sem"]:
                continue
            if E["waited"].get(s, 0) >= v:
                continue
            E["e"].wait_ge(s, v)
            E["waited"][s] = v

    def op(self, en, fn, r=(), w=()):
        E = self.E[en]
        skip_own = (en == "pe") or (not SAME_ENG_SYNC)
        self._waits(E, self._collect(r, w), skip_own)
        ins = fn(E["e"])
        E["cnt"] += 1
        self.n_ins += 1
        ins.then_inc(E["sem"], 1)
        tag = (E["sem"], E["cnt"])
        for x in w:
            x.w = tag
            x.r = {}
        for x in r:
            if x not in w:
                x.r[E["sem"]] = E["cnt"]
        return ins

    def dma(self, qn, out, in_, r, w, sres, **kw):
        E = self.E[qn]
        self._waits(E, self._collect(r, w), False)
        if sres.ds is None:
            if qn != "pool" and self.free_ds:
                sres.ds = self.free_ds.pop()
            else:
                sres.ds = [self.es.enter_context(self.nc.semaphore("d%d" % len(self.all_ds))), 0, qn]
                self.all_ds.append(sres.ds)
        ins = E["e"].dma_start(out=out, in_=in_, **kw)
        self.n_ins += 1
        sres.ds[1] += 1
        ins.then_inc(sres.ds[0], 16)
        val = sres.ds[1] * 16
        for x in w:
            x.w = (sres.ds[0], val)
            x.r = {}
        for x in r:
            if x not in w:
                x.r[sres.ds[0]] = val

    def barrier(self, release=()):
        deps = {}
        for nm in ("pe", "act", "dve", "pool"):
            E = self.E[nm]
            if E["cnt"] > 0:
                deps[E["sem"]] = E["cnt"]
        for ds in self.all_ds:
            if ds[1] > 0:
                deps[ds[0]] = ds[1] * 16
        for nm in ("pe", "act", "dve", "pool", "sp"):
            self._waits(self.E[nm], deps, True)
        for x in release:
            if x.ds is not None:
                if x.ds[2] != "pool":
                    self.free_ds.append(x.ds)
                x.ds = None


class Tn:
    def __init__(self, h, name, nres=1):
        self.h = h
        self.res = Res(name)
        self.rs = [Res("%s_%d" % (name, i)) for i in range(nres)] if nres > 1 else [self.res]

    def __getitem__(self, k):
        return self.h[k]


def build(NT=2048, debug=False):
    NPRE = 3 * NT
    NTILE = NT // 128
    nc = bass.Bass("TRN2", target_bir_lowering=False)
    dt_in = lambda n, s, d=F32: nc.dram_tensor(n, list(s), d, kind="ExternalInput").ap()
    xown = dt_in("xown", [NT, D])
    xprev = dt_in("xprev", [NPRE, D])
    pflag_d = dt_in("pflag", [128, NPRE // 512])
    halob_d = dt_in("halob", [128, 512])
    mem_d = dt_in("mem", [256, D])
    w_in_d = dt_in("w_in", [D, 2560])
    w_out_d = dt_in("w_out", [D, D])
    w_q_d = dt_in("w_q", [D, D])
    w_kv_d = dt_in("w_kv", [D, 2048])
    w_o_d = dt_in("w_o", [D, D])
    w_qry_d = dt_in("w_qry", [D, 2048])
    smalls_d = dt_in("smalls", [128, NSM])
    gbd_d = dt_in("gbd", [128, 8 * 128])
    abias_d = dt_in("abias", [128, 8 * 640])
    skT_d = dt_in("skT", [128, 16 * 128])
    uT_d = dt_in("uT", [16384, D])
    ev_d = dt_in("ev", [16384, D])
    gfin_d = dt_in("gfin", [128, D])
    ident_d = dt_in("ident", [128, 128])
    iota_d = dt_in("iota", [128, 128])
    y_d = nc.dram_tensor("y", [NT, D], F32, kind="ExternalOutput").ap()
    wd_d = nc.dram_tensor("wd_scratch", [NTILE, 128, 16384], BF16, kind="Internal").ap()
    dbg = {}
    if debug:
        for nm in ("dbg1", "dbg2"):
            dbg[nm] = nc.dram_tensor(nm, [NT, D], F32, kind="ExternalOutput").ap()

    with ExitStack() as es:
        k = KB(nc, es)

        def sb(ctx, name, shape, dt, nres=1):
            return Tn(ctx.enter_context(nc.sbuf_tensor("sb_" + name, list(shape), dt)), name, nres)

        def ps(ctx, name, shape, dt):
            return Tn(ctx.enter_context(nc.psum_tensor("ps_" + name, list(shape), dt)), name)

        x_res = sb(es, "x_res", [128, NTILE, D], F32, nres=NTILE)
        ident_f = sb(es, "ident_f", [128, 128], F32)
        ident_b = sb(es, "ident_b", [128, 128], BF16)
        iota_f = sb(es, "iota_f", [128, 128], F32)
        ones_b = sb(es, "ones_b", [128, 128], BF16)
        smalls = sb(es, "smalls", [128, NSM], F32)
        cst = sb(es, "cst", [128, 4], F32)
        gB = sb(es, "gB", [128, 8, 128], F32)
        nrm = [dict(ss=sb(es, "n_ss%d" % i, [128, 1], F32), sd=sb(es, "n_sd%d" % i, [128, 1], F32),
                    rstd=sb(es, "n_rstd%d" % i, [128, 1], F32), xs=sb(es, "n_xs%d" % i, [128, D], BF16),
                    junk=sb(es, "n_junk%d" % i, [128, D], BF16)) for i in range(2)]
        nrm_i = [0]

        k.dma("sp", ident_f[:], ident_d[:, :], r=[], w=[ident_f.res], sres=ident_f.res)
        k.dma("sp", iota_f[:], iota_d[:, :], r=[], w=[iota_f.res], sres=iota_f.res)
        k.dma("sp", smalls[:], smalls_d[:, :], r=[], w=[smalls.res], sres=smalls.res)
        k.op("dve", lambda e: e.tensor_copy(out=ident_b[:], in_=ident_f[:]), r=[ident_f.res], w=[ident_b.res])
        k.op("pool", lambda e: e.memset(ones_b[:], 1.0), w=[ones_b.res])
        k.op("pool", lambda e: e.memset(cst[:, 0:1], EPS), w=[cst.res])
        k.op("pool", lambda e: e.memset(cst[:, 1:2], 1.0), w=[cst.res])
        k.op("pool", lambda e: e.memset(cst[:, 2:3], 0.0), w=[cst.res])

        def set_gain(col):
            k.op("dve", lambda e: e.tensor_copy(
                out=gB[:], in_=smalls[:, col:col + 8].unsqueeze(2).to_broadcast([128, 8, 128])),
                r=[smalls.res], w=[gB.res])

        def norm_T_g(x_ap, x_r, pT, hT_ap, hT_r):
            n = nrm[nrm_i[0] % 2]
            nrm_i[0] += 1
            k.op("dve", lambda e: e.scalar_tensor_tensor(out=n["junk"][:], in0=x_ap, scalar=1.0, in1=x_ap,
                                                         op0=ALU.mult, op1=ALU.mult, accum_out=n["ss"][:]),
                 r=[x_r], w=[n["junk"].res, n["ss"].res])
            yield
            k.op("act", lambda e: e.activation(out=n["sd"][:], in_=n["ss"][:], func=AF.Ln,
                                               bias=cst[:, 0:1], scale=1.0 / D),
                 r=[n["ss"].res, cst.res], w=[n["sd"].res])
            k.op("act", lambda e: e.activation(out=n["rstd"][:], in_=n["sd"][:], func=AF.Exp, scale=-0.5),
                 r=[n["sd"].res], w=[n["rstd"].res])
            yield
            k.op("dve", lambda e: e.tensor_scalar(out=n["xs"][:], in0=x_ap, scalar1=n["rstd"][:, 0:1], scalar2=None,
                                                  op0=ALU.mult), r=[x_r, n["rstd"].res], w=[n["xs"].res])
            for c in range(8):
                k.op("pe", lambda e, c=c: e.transpose(out=pT[:, c, :], in_=n["xs"][:, c * 128:(c + 1) * 128],
                                                      identity=ident_b[:]),
                     r=[n["xs"].res, ident_b.res], w=[pT.res])
            yield
            k.op("dve", lambda e: e.tensor_tensor(out=hT_ap, in0=pT[:], in1=gB[:], op=ALU.mult),
                 r=[pT.res, gB.res], w=[hT_r])

        def norm_T(*a):
            for _ in norm_T_g(*a):
                pass

        def mm_group(out_ap, pairs, r, w):
            n = len(pairs)
            for i, (l, rh) in enumerate(pairs):
                k.op("pe", lambda e, l=l, rh=rh, i=i: e.matmul(out_ap, lhsT=l, rhs=rh, start=(i == 0), stop=(i == n - 1)),
                     r=r, w=w)

        def load_w_bf16(dst, src_ap, ncols):
            for c in range(8):
                k.dma("pool", dst[:, c, :], src_ap[c * 128:(c + 1) * 128, :], r=[], w=[dst.res], sres=dst.res,
                      max_dma_last_dim=4096)

        with ExitStack() as p1:
            with ExitStack() as pa:
                BA = 512
                w_inA = sb(pa, "w_inA", [128, 8, 1024], BF16)
                load_w_bf16(w_inA, w_in_d[:, 0:1024], 1024)
                w_outA = sb(pa, "w_outA", [128, 4, D], BF16)
                for c in range(4):
                    k.dma("pool", w_outA[:, c, :], w_out_d[c * 128:(c + 1) * 128, :], r=[], w=[w_outA.res], sres=w_outA.res,
                          max_dma_last_dim=4096)
                yan = sb(pa, "yan", [128, 4, 512], BF16)
                gbd_f = sb(pa, "gbd_f", [128, 8, 128], F32)
                gbd = sb(pa, "gbd", [128, 8, 128], BF16)
                k.dma("sp", gbd_f[:], gbd_d.rearrange("p (c j) -> p c j", c=8), r=[], w=[gbd_f.res], sres=gbd_f.res)
                k.op("dve", lambda e: e.tensor_copy(out=gbd[:], in_=gbd_f[:]), r=[gbd_f.res], w=[gbd.res])
                pflag = sb(pa, "pflag", [128, NPRE // 512], F32)
                k.dma("sp", pflag[:], pflag_d[:, :], r=[], w=[pflag.res], sres=pflag.res)
                cL = sb(pa, "cL", [128, 4], F32)
                tmp4 = sb(pa, "tmp4", [128, 4], F32)
                k.op("act", lambda e: e.activation(out=tmp4[:], in_=smalls[:, SM_LAM:SM_LAM + 4], func=AF.Exp, scale=-1.0),
                     r=[smalls.res], w=[tmp4.res])
                k.op("act", lambda e: e.activation(out=cL[:], in_=tmp4[:], func=AF.Ln, bias=cst[:, 1:2], scale=1.0),
                     r=[tmp4.res, cst.res], w=[cL.res])
                k.op("dve", lambda e: e.tensor_scalar(out=cL[:], in0=cL[:], scalar1=-8.0, scalar2=None, op0=ALU.mult),
                     r=[cL.res], w=[cL.res])
                set_gain(SM_MIX)
                xtmp = [sb(pa, "xtmp%d" % i, [128, D], F32) for i in range(2)]
                hT = [sb(pa, "hTa%d" % i, [128, 8, BA], BF16) for i in range(2)]
                xl = sb(pa, "xl", [128, 4, 3 + BA], F32, nres=4)
                gg = sb(pa, "gg", [128, 4, BA], F32, nres=4)
                hh = sb(pa, "hh", [128, 4, BA], F32, nres=4)
                hst = sb(pa, "hst", [128, 4], F32, nres=4)
                yaT = sb(pa, "yaT", [128, 4, BA], F32, nres=4)
                LT = [{nm: sb(pa, "l%s%d" % (nm, i), [128, BA], BF16 if nm == "xcb" else F32)
                       for nm in ("xc", "xc2", "xcb", "rr", "ii", "aa", "tt", "bb")} for i in range(2)]
                sq = sb(pa, "sq", [128, BA], BF16)
                rsn = sb(pa, "rsn", [128, BA], F32)
                pj = [ps(pa, "pj%d" % i, [128, 512], F32) for i in range(2)]
                pT = [ps(pa, "pT%d" % i, [128, 8, 128], BF16) for i in range(1)]
                pg = [ps(pa, "pg%d" % i, [128, 512], F32) for i in range(4)]
                pn = ps(pa, "pn", [128, 512], F32)
                k.op("pool", lambda e: e.memset(xl[:], 0.0), w=xl.rs)
                k.op("pool", lambda e: e.memset(hst[:], 0.0), w=hst.rs)

                nblk_pre = NPRE // BA
                nblk_own = NT // BA
                tcount = 0
                pjc = 0
                for b in range(nblk_pre + nblk_own):
                    own = b >= nblk_pre
                    ob = b - nblk_pre
                    h = hT[b % 2]
                    for t in range(4):
                        if own:
                            tile = ob * 4 + t
                            xa = x_res[:, tile, :]
                            xr = x_res.rs[tile]
                            k.dma("sp", xa, xown[tile * 128:(tile + 1) * 128, :], r=[], w=[xr], sres=xr)
                        else:
                            xt_ = xtmp[tcount % 2]
                            xa = xt_[:]
                            xr = xt_.res
                            r0 = b * BA + t * 128
                            k.dma("sp", xa, xprev[r0:r0 + 128, :], r=[], w=[xr], sres=xr)
                        norm_T(xa, xr, pT[0], h[:, :, t * 128:(t + 1) * 128], h.res)
                        tcount += 1
                    for cc in range(8 if own else 4):
                        pp = pj[pjc % 2]
                        pjc += 1
                        mm_group(pp[:], [(w_inA[:, kc, cc * 128:(cc + 1) * 128], h[:, kc, :]) for kc in range(8)],
                                 r=[w_inA.res, h.res], w=[pp.res])
                        if cc < 4:
                            k.op("act", lambda e, pp=pp, cc=cc: e.activation(out=xl[:, cc, 3:3 + BA], in_=pp[:], func=AF.Copy),
                                 r=[pp.res], w=[xl.rs[cc]])
                        else:
                            k.op("act", lambda e, pp=pp, cc=cc: e.activation(out=gg[:, cc - 4, :], in_=pp[:], func=AF.Gelu_apprx_tanh),
                                 r=[pp.res], w=[gg.rs[cc - 4]])
                    def lru_chain(cc, L, pga, pgb, own=own, b=b):
                            cw = lambda j, cc=cc: smalls[:, SM_CW + cc * 4 + j:SM_CW + cc * 4 + j + 1]
                            k.op("dve", lambda e, cc=cc, L=L, cw=cw: e.tensor_scalar(
                                out=L["xc"][:], in0=xl[:, cc, 3:3 + BA], scalar1=cw(3), scalar2=smalls[:, SM_CB + cc:SM_CB + cc + 1],
                                op0=ALU.mult, op1=ALU.add), r=[xl.rs[cc], smalls.res], w=[L["xc"].res])
                            src, dst = "xc", "xc2"
                            for j in range(3):
                                yield
                                k.op("dve", lambda e, cc=cc, L=L, cw=cw, j=j, src=src, dst=dst: e.scalar_tensor_tensor(
                                    out=L[dst][:], in0=xl[:, cc, j:j + BA], scalar=cw(j), in1=L[src][:], op0=ALU.mult, op1=ALU.add),
                                    r=[xl.rs[cc], smalls.res, L[src].res], w=[L[dst].res])
                                src, dst = dst, src
                            xc = L[src]
                            yield
                            k.op("pool", lambda e, cc=cc: e.tensor_copy(out=xl[:, cc, 0:3], in_=xl[:, cc, BA:BA + 3]),
                                 r=[xl.rs[cc]], w=[xl.rs[cc]])
                            yield
                            k.op("act", lambda e, L=L, xc=xc: e.activation(out=L["xcb"][:], in_=xc[:], func=AF.Copy),
                                 r=[xc.res], w=[L["xcb"].res])
                            yield
                            k.op("pe", lambda e, cc=cc, L=L: e.matmul(pga[:], lhsT=gbd[:, cc, :], rhs=L["xcb"][:], start=True, stop=True),
                                 r=[gbd.res, L["xcb"].res], w=[pga.res])
                            yield
                            k.op("pe", lambda e, cc=cc, L=L: e.matmul(pgb[:], lhsT=gbd[:, 4 + cc, :], rhs=L["xcb"][:], start=True, stop=True),
                                 r=[gbd.res, L["xcb"].res], w=[pgb.res])
                            yield
                            k.op("act", lambda e, cc=cc, L=L: e.activation(out=L["rr"][:], in_=pga[:], func=AF.Sigmoid,
                                                                           bias=smalls[:, SM_BA + cc:SM_BA + cc + 1], scale=1.0),
                                 r=[pga.res, smalls.res], w=[L["rr"].res])
                            yield
                            k.op("act", lambda e, cc=cc, L=L: e.activation(out=L["ii"][:], in_=pgb[:], func=AF.Sigmoid,
                                                                           bias=smalls[:, SM_BX + cc:SM_BX + cc + 1], scale=1.0),
                                 r=[pgb.res, smalls.res], w=[L["ii"].res])
                            yield
                            k.op("act", lambda e, cc=cc, L=L: e.activation(out=L["aa"][:], in_=L["rr"][:], func=AF.Exp,
                                                                           scale=cL[:, cc:cc + 1]),
                                 r=[L["rr"].res, cL.res], w=[L["aa"].res])
                            yield
                            k.op("dve", lambda e, L=L: e.tensor_tensor(out=L["tt"][:], in0=L["aa"][:], in1=L["aa"][:], op=ALU.mult),
                                 r=[L["aa"].res], w=[L["tt"].res])
                            yield
                            k.op("act", lambda e, L=L: e.activation(out=L["tt"][:], in_=L["tt"][:], func=AF.Sqrt,
                                                                    bias=cst[:, 1:2], scale=-1.0),
                                 r=[L["tt"].res, cst.res], w=[L["tt"].res])
                            yield
                            k.op("dve", lambda e, L=L: e.tensor_tensor(out=L["bb"][:], in0=L["tt"][:], in1=L["ii"][:], op=ALU.mult),
                                 r=[L["tt"].res, L["ii"].res], w=[L["bb"].res])
                            yield
                            k.op("dve", lambda e, L=L, xc=xc: e.tensor_tensor(out=L["rr"][:], in0=L["bb"][:], in1=xc[:], op=ALU.mult),
                                 r=[L["bb"].res, xc.res], w=[L["rr"].res])
                            yield
                            k.op("dve", lambda e, cc=cc, L=L: e.tensor_tensor_scan(
                                out=hh[:, cc, :], data0=L["aa"][:], data1=L["rr"][:], initial=hst[:, cc:cc + 1],
                                op0=ALU.mult, op1=ALU.add), r=[L["aa"].res, L["rr"].res, hst.rs[cc]], w=[hh.rs[cc]])
                            if own:
                                yield
                                k.op("dve", lambda e, cc=cc: e.tensor_copy(out=hst[:, cc:cc + 1], in_=hh[:, cc, BA - 1:BA]),
                                     r=[hh.rs[cc]], w=[hst.rs[cc]])
                                yield
                                k.op("dve", lambda e, cc=cc: e.tensor_tensor(out=yaT[:, cc, :], in0=hh[:, cc, :], in1=gg[:, cc, :], op=ALU.mult),
                                     r=[hh.rs[cc], gg.rs[cc]], w=[yaT.rs[cc]])
                            else:
                                yield
                                k.op("dve", lambda e, cc=cc, b=b: e.tensor_tensor(out=hst[:, cc:cc + 1], in0=hh[:, cc, BA - 1:BA],
                                                                                  in1=pflag[:, b:b + 1], op=ALU.mult),
                                     r=[hh.rs[cc], pflag.res], w=[hst.rs[cc]])

                    for pair in range(2):
                        gens = [lru_chain(2 * pair + i_, LT[i_], pg[2 * i_], pg[2 * i_ + 1]) for i_ in range(2)]
                        while gens:
                            for g_ in list(gens):
                                try:
                                    next(g_)
                                except StopIteration:
                                    gens.remove(g_)
                    if own:
                        for cc in range(4):
                            k.op("dve", lambda e, cc=cc: e.tensor_tensor(out=sq[:], in0=yaT[:, cc, :], in1=yaT[:, cc, :], op=ALU.mult),
                                 r=[yaT.rs[cc]], w=[sq.res])
                            k.op("pe", lambda e, cc=cc: e.matmul(pn[:], lhsT=ones_b[:], rhs=sq[:], start=(cc == 0), stop=(cc == 3)),
                                 r=[ones_b.res, sq.res], w=[pn.res])
                        k.op("act", lambda e: e.activation(out=rsn[:], in_=pn[:], func=AF.Ln, bias=cst[:, 0:1], scale=1.0 / 512),
                             r=[pn.res, cst.res], w=[rsn.res])
                        k.op("act", lambda e: e.activation(out=rsn[:], in_=rsn[:], func=AF.Exp, scale=-0.5), r=[rsn.res], w=[rsn.res])
                        for cc in range(4):
                            k.op("dve", lambda e, cc=cc, ob=ob: e.scalar_tensor_tensor(
                                out=yan[:, cc, :], in0=yaT[:, cc, :],
                                scalar=smalls[:, SM_GA + cc:SM_GA + cc + 1], in1=rsn[:], op0=ALU.mult, op1=ALU.mult),
                                r=[yaT.rs[cc], smalls.res, rsn.res], w=[yan.res])
                        for t in range(4):
                            tile = ob * 4 + t
                            for half in range(2):
                                pp = pj[pjc % 2]
                                pjc += 1
                                mm_group(pp[:], [(yan[:, c, t * 128:(t + 1) * 128], w_outA[:, c, half * 512:(half + 1) * 512]) for c in range(4)],
                                         r=[yan.res, w_outA.res], w=[pp.res])
                                k.op("dve", lambda e, pp=pp, tile=tile, half=half: e.tensor_tensor(
                                    out=x_res[:, tile, half * 512:(half + 1) * 512], in0=pp[:], in1=x_res[:, tile, half * 512:(half + 1) * 512],
                                    op=ALU.add), r=[pp.res, x_res.rs[tile]], w=[x_res.rs[tile]])
                k.barrier(release=[w_inA.res, w_outA.res, gbd_f.res, pflag.res] + [t_.res for t_ in xtmp])

            with ExitStack() as pb:
                BB = 256
                w_inB = sb(pb, "w_inB", [128, 8, 1536], BF16)
                load_w_bf16(w_inB, w_in_d[:, 1024:2560], 1536)
                w_out = sb(pb, "w_outB", [128, 4, D], BF16)
                for c in range(4):
                    k.dma("pool", w_out[:, c, :], w_out_d[512 + c * 128:512 + (c + 1) * 128, :], r=[], w=[w_out.res], sres=w_out.res,
                          max_dma_last_dim=4096)
                abias = sb(pb, "abias", [128, 8, 640], F32)
                k.dma("sp", abias[:], abias_d.rearrange("p (h c) -> p h c", h=8), r=[], w=[abias.res], sres=abias.res)
                halob = sb(pb, "halob", [128, 512], F32)
                k.dma("sp", halob[:], halob_d[:, :], r=[], w=[halob.res], sres=halob.res)
                xtmp = [sb(pb, "xtmpb%d" % i, [128, D], F32) for i in range(2)]
                hT = [sb(pb, "hTb%d" % i, [128, 8, BB], BF16) for i in range(2)]
                qA_l = [sb(pb, "qA%d" % i, [128, 4, BB], BF16) for i in range(2)]
                qB_l = [sb(pb, "qB%d" % i, [128, 4, BB], BF16) for i in range(2)]
                kT = [sb(pb, "kT%d" % i, [128, 4, BB], BF16) for i in range(4)]
                vpad = [sb(pb, "vpad%d" % i, [128, 8, 128], BF16) for i in range(8)]
                ybT = sb(pb, "ybT", [128, 4, BB], F32)
                ybn = sb(pb, "ybn", [128, 4, BB], BF16)
                sbuf_s = [sb(pb, "sbs%d" % i, [128, 640], F32) for i in range(2)]
                Pm = [sb(pb, "Pm%d" % i, [128, 640], BF16) for i in range(2)]
                Pn = [sb(pb, "Pn%d" % i, [128, 640], BF16) for i in range(2)]
                PT = [sb(pb, "PT%d" % i, [128, 5, 128], BF16) for i in range(2)]
                st = [dict(mx=sb(pb, "a_mx%d" % i, [128, 1], F32), rs=sb(pb, "a_rs%d" % i, [128, 1], F32),
                           ri=sb(pb, "a_ri%d" % i, [128, 1], F32)) for i in range(2)]
                sq4 = [sb(pb, "sqb%d" % i, [128, BB], BF16) for i in range(4)]
                rsn = sb(pb, "rsnb", [128, BB], F32)
                pj = [ps(pb, "pjb%d" % i, [128, 512], F32) for i in range(2)]
                pT = [ps(pb, "pTb%d" % i, [128, 8, 128], BF16) for i in range(1)]
                pSm = [ps(pb, "pSm%d" % i, [128, 512], F32) for i in range(2)]
                pSr = Tn(pb.enter_context(nc.psum_tensor("ps_pSr", [128, 4, 128], F32)), "pSr", nres=4)
                pPT = ps(pb, "pPT", [128, 5, 128], BF16)
                pO = ps(pb, "pO", [128, 4, 128], F32)
                pn = pj[0]
                for v_ in vpad:
                    k.op("pool", lambda e, v_=v_: e.memset(v_[:], 0.0), w=[v_.res])
                for q_ in qA_l + qB_l:
                    k.op("pool", lambda e, q_=q_: e.memset(q_[:], 0.0), w=[q_.res])
                set_gain(SM_MIX)

                nhalo = 512 // BB
                nown = NT // BB
                tcount = 0
                pjc = 0
                ac = 0
                def prep(b):
                    nonlocal tcount, pjc
                    own = b >= nhalo
                    ob = b - nhalo
                    h = hT[b % 2]
                    kcur = kT[b % 4]
                    qA, qB = qA_l[b % 2], qB_l[b % 2]
                    for t in range(2):
                        xt_ = xtmp[tcount % 2]
                        xa = xt_[:]
                        xr = xt_.res
                        if own:
                            r0 = ob * BB + t * 128
                            k.dma("sp", xa, xown[r0:r0 + 128, :], r=[], w=[xr], sres=xr)
                        else:
                            r0 = NPRE - 512 + b * BB + t * 128
                            k.dma("sp", xa, xprev[r0:r0 + 128, :], r=[], w=[xr], sres=xr)
                        yield from norm_T_g(xa, xr, pT[0], h[:, :, t * 128:(t + 1) * 128], h.res)
                        tcount += 1
                        yield
                    for m in range(4):
                        pp = pj[pjc % 2]
                        pjc += 1
                        mm_group(pp[:, 0:BB], [(w_inB[:, kc, 512 + m * 128:512 + (m + 1) * 128], h[:, kc, :]) for kc in range(8)],
                                 r=[w_inB.res, h.res], w=[pp.res])
                        k.op("act", lambda e, pp=pp, m=m, kcur=kcur: e.activation(out=kcur[:, m, :], in_=pp[:, 0:BB], func=AF.Copy),
                             r=[pp.res], w=[kcur.res])
                    yield
                    for t in range(2):
                        yield
                        gt = b * 2 + t
                        vp = vpad[gt % 8]
                        pp = pj[pjc % 2]
                        pjc += 1
                        mm_group(pp[:], [(h[:, kc, t * 128:(t + 1) * 128], w_inB[:, kc, 1024:1536]) for kc in range(8)],
                                 r=[w_inB.res, h.res], w=[pp.res])
                        ppv = pp[:].rearrange("p (h d) -> p h d", h=8)
                        k.op("act", lambda e, vp=vp, ppv=ppv: e.activation(out=vp[:, 0:8:2, 0:64], in_=ppv[:, 0:8:2, :], func=AF.Copy),
                             r=[pp.res], w=[vp.res])
                        k.op("act", lambda e, vp=vp, ppv=ppv: e.activation(out=vp[:, 1:8:2, 64:128], in_=ppv[:, 1:8:2, :], func=AF.Copy),
                             r=[pp.res], w=[vp.res])
                    if not own:
                        return
                    yield
                    for m in range(4):
                        if m == 2:
                            yield
                        pp = pj[pjc % 2]
                        pjc += 1
                        mm_group(pp[:, 0:BB], [(w_inB[:, kc, m * 128:(m + 1) * 128], h[:, kc, :]) for kc in range(8)],
                                 r=[w_inB.res, h.res], w=[pp.res])
                        k.op("act", lambda e, pp=pp, m=m: e.activation(out=qA[0:64, m, :], in_=pp[0:64, 0:BB], func=AF.Copy),
                             r=[pp.res], w=[qA.res])
                        k.op("act", lambda e, pp=pp, m=m: e.activation(out=qB[64:128, m, :], in_=pp[64:128, 0:BB], func=AF.Copy),
                             r=[pp.res], w=[qB.res])
                def attend(b, fillers):
                    nonlocal pjc, ac
                    ob = b - nhalo
                    qA, qB = qA_l[b % 2], qB_l[b % 2]
                    units = []
                    for p in range(2):
                        pieces = []
                        oc = 0
                        remaining = 640
                        pos = 128 * p
                        while remaining > 0:
                            bi = pos // BB
                            c0 = pos % BB
                            n = min(BB - c0, remaining)
                            if oc < 512 and oc + n > 512:
                                n = 512 - oc
                            pieces.append((kT[(b - 2 + bi) % 4], c0, n, oc))
                            oc += n
                            pos += n
                            remaining -= n
                        for hd in range(8):
                            units.append((p, hd, pieces))

                    def S1(u):
                        p, hd, pieces = units[u]
                        m = hd // 2
                        qs = (qA if hd % 2 == 0 else qB)
                        gi = ac0 + u
                        pm, pr = pSm[gi % 2], pSr
                        for (kt_, c0, n, oc) in pieces:
                            if oc < 512:
                                oap = pm[:, oc:oc + n]
                                ores = pm.res
                            else:
                                oap = pr[:, gi % 4, oc - 512:oc - 512 + n]
                                ores = pr.res
                            k.op("pe", lambda e, kt_=kt_, c0=c0, n=n, oap=oap: e.matmul(
                                oap, lhsT=qs[:, m, p * 128:(p + 1) * 128], rhs=kt_[:, m, c0:c0 + n],
                                start=True, stop=True), r=[qs.res, kt_.res], w=[ores])

                    def S2(u, part):
                        p, hd, pieces = units[u]
                        gi = ac0 + u
                        i2 = gi % 2
                        pm, pr = pSm[gi % 2], pSr
                        S, PP, PN, s_ = sbuf_s[i2], Pm[i2], Pn[i2], st[i2]
                        if part == 1:
                            k.op("dve", lambda e: e.reciprocal(out=s_["ri"][:], in_=s_["rs"][:]), r=[s_["rs"].res], w=[s_["ri"].res])
                            k.op("dve", lambda e: e.tensor_scalar(out=PN[:], in0=PP[:], scalar1=s_["ri"][:, 0:1],
                                                                  scalar2=None, op0=ALU.mult),
                                 r=[PP.res, s_["ri"].res], w=[PN.res])
                            return
                        k.op("dve", lambda e: e.scalar_tensor_tensor(
                            out=S[:, 0:512], in0=pm[:], scalar=0.125, in1=abias[:, hd, 0:512], op0=ALU.mult, op1=ALU.add),
                            r=[pm.res, abias.res], w=[S.res])
                        k.op("dve", lambda e: e.scalar_tensor_tensor(
                            out=S[:, 512:640], in0=pr[:, gi % 4, :], scalar=0.125, in1=abias[:, hd, 512:640], op0=ALU.mult, op1=ALU.add),
                            r=[pr.res, abias.res], w=[S.res])
                        cnt = 512 - ob * BB - 128 * p
                        if cnt > 0:
                            hb0 = ob * BB + 128 * p
                            k.op("dve", lambda e: e.tensor_tensor(
                                out=S[:, 0:cnt], in0=S[:, 0:cnt], in1=halob[:, hb0:512], op=ALU.add),
                                r=[S.res, halob.res], w=[S.res])
                        k.op("dve", lambda e: e.tensor_reduce(out=s_["mx"][:], in_=S[:], axis=AX.X, op=ALU.max, negate=True),
                             r=[S.res], w=[s_["mx"].res])
                        k.op("act", lambda e: e.activation(out=PP[:], in_=S[:], func=AF.Exp, bias=s_["mx"][:, 0:1],
                                                           scale=1.0, accum_out=s_["rs"][:]),
                             r=[S.res, s_["mx"].res], w=[PP.res, s_["rs"].res])

                    def S3(u):
                        p, hd, pieces = units[u]
                        m = hd // 2
                        gi = ac0 + u
                        i2 = gi % 2
                        PN, PTs = Pn[i2], PT[i2]
                        for kc in range(5):
                            k.op("pe", lambda e, kc=kc: e.transpose(out=pPT[:, kc, :], in_=PN[:, kc * 128:(kc + 1) * 128],
                                                                    identity=ident_b[:]),
                                 r=[PN.res, ident_b.res], w=[pPT.res])
                        k.op("act", lambda e: e.activation(out=PTs[:], in_=pPT[:], func=AF.Copy), r=[pPT.res], w=[PTs.res])
                        g0 = (b * 2 + p) - 4
                        for kc in range(5):
                            vp = vpad[(g0 + kc) % 8]
                            k.op("pe", lambda e, vp=vp, kc=kc: e.matmul(
                                pO[:, m, :], lhsT=vp[:, hd, :], rhs=PTs[:, kc, :],
                                start=(hd % 2 == 0 and kc == 0), stop=(hd % 2 == 1 and kc == 4)),
                                r=[vp.res, PTs.res], w=[pO.res])
                        if hd == 7:
                            k.op("act", lambda e: e.activation(out=ybT[:, :, p * 128:(p + 1) * 128], in_=pO[:], func=AF.Copy),
                                 r=[pO.res], w=[ybT.res])

                    def advance():
                        while fillers:
                            try:
                                next(fillers[0])
                                return
                            except StopIteration:
                                fillers.pop(0)

                    ac0 = ac
                    S1(0)
                    S1(1)
                    S2(0, 0)
                    for u in range(len(units)):
                        if u + 1 < len(units):
                            S2(u + 1, 0)
                        S2(u, 1)
                        if u + 2 < len(units):
                            S1(u + 2)
                        S3(u)
                        advance()
                    while fillers:
                        advance()
                    ac += len(units)

                def tail(b):
                    nonlocal pjc
                    ob = b - nhalo
                    for cc in range(4):
                        k.op("dve", lambda e, cc=cc: e.tensor_tensor(out=sq4[cc][:], in0=ybT[:, cc, :], in1=ybT[:, cc, :], op=ALU.mult),
                             r=[ybT.res], w=[sq4[cc].res])
                    for cc in range(4):
                        k.op("pe", lambda e, cc=cc: e.matmul(pn[:, 0:BB], lhsT=ones_b[:], rhs=sq4[cc][:], start=(cc == 0), stop=(cc == 3)),
                             r=[ones_b.res, sq4[cc].res], w=[pn.res])
                    yield
                    k.op("act", lambda e: e.activation(out=rsn[:], in_=pn[:, 0:BB], func=AF.Ln, bias=cst[:, 0:1], scale=1.0 / 512),
                         r=[pn.res, cst.res], w=[rsn.res])
                    k.op("act", lambda e: e.activation(out=rsn[:], in_=rsn[:], func=AF.Exp, scale=-0.5), r=[rsn.res], w=[rsn.res])
                    yield
                    for cc in range(4):
                        k.op("dve", lambda e, cc=cc: e.scalar_tensor_tensor(
                            out=ybn[:, cc, :], in0=ybT[:, cc, :], scalar=smalls[:, SM_GB + cc:SM_GB + cc + 1], in1=rsn[:],
                            op0=ALU.mult, op1=ALU.mult), r=[ybT.res, smalls.res, rsn.res], w=[ybn.res])
                    yield
                    for t in range(2):
                        tile = ob * 2 + t
                        tok0 = ob * BB + t * 128
                        for half in range(2):
                            pp = pj[pjc % 2]
                            pjc += 1
                            pairs = [(ybn[:, c, t * 128:(t + 1) * 128], w_out[:, c, half * 512:(half + 1) * 512]) for c in range(4)]
                            mm_group(pp[:], pairs, r=[ybn.res, w_out.res], w=[pp.res])
                            k.op("dve", lambda e, pp=pp, tile=tile, half=half: e.tensor_tensor(
                                out=x_res[:, tile, half * 512:(half + 1) * 512], in0=pp[:], in1=x_res[:, tile, half * 512:(half + 1) * 512],
                                op=ALU.add), r=[pp.res, x_res.rs[tile]], w=[x_res.rs[tile]])
                        yield
                nb_ = nhalo + nown
                for b0 in range(nhalo + 1):
                    for _ in prep(b0):
                        pass
                for b in range(nhalo, nb_):
                    fl = []
                    if b - 1 >= nhalo:
                        fl.append(tail(b - 1))
                    if b + 1 < nb_:
                        fl.append(prep(b + 1))
                    attend(b, fl)
                for _ in tail(nb_ - 1):
                    pass
                k.barrier(release=[w_inB.res, w_out.res, abias.res, halob.res] + [t_.res for t_ in xtmp])

        if debug:
            for tile in range(NTILE):
                k.dma("sp", dbg["dbg1"][tile * 128:(tile + 1) * 128, :], x_res[:, tile, :], r=[x_res.rs[tile]], w=[], sres=x_res.rs[tile])

        with ExitStack() as p2:
            B2 = 512
            w_q = sb(p2, "w_q", [128, 8, D], BF16)
            load_w_bf16(w_q, w_q_d[:, :], D)
            w_o = sb(p2, "w_o", [128, 8, D], BF16)
            load_w_bf16(w_o, w_o_d[:, :], D)
            kmT = sb(p2, "kmT", [128, 8, 256], BF16)
            vmem = sb(p2, "vmem", [128, 2, D], BF16)
            pj = [ps(p2, "pj2%d" % i, [128, 512], F32) for i in range(2)]
            pT = [ps(p2, "pT2%d" % i, [128, 8, 128], BF16) for i in range(1)]
            pS2 = [ps(p2, "pS2%d" % i, [128, 4, 256], F32) for i in range(2)]
            pPT2 = ps(p2, "pPT2", [128, 8, 128], BF16)
            pjc = 0
            with ExitStack() as p2s:
                w_kv = sb(p2s, "w_kv", [128, 8, 2048], BF16)
                load_w_bf16(w_kv, w_kv_d[:, :], 2048)
                memt = [sb(p2s, "memt%d" % i, [128, D], F32) for i in range(2)]
                memT = sb(p2s, "memT", [128, 8, 256], BF16)
                set_gain(SM_MEM)
                for t in range(2):
                    k.dma("sp", memt[t][:], mem_d[t * 128:(t + 1) * 128, :], r=[], w=[memt[t].res], sres=memt[t].res)
                    norm_T(memt[t][:], memt[t].res, pT[0], memT[:, :, t * 128:(t + 1) * 128], memT.res)
                for oc in range(8):
                    pp = pj[pjc % 2]
                    pjc += 1
                    mm_group(pp[:, 0:256], [(w_kv[:, kc, oc * 128:(oc + 1) * 128], memT[:, kc, :]) for kc in range(8)],
                             r=[w_kv.res, memT.res], w=[pp.res])
                    k.op("act", lambda e, pp=pp, oc=oc: e.activation(out=kmT[:, oc, :], in_=pp[:, 0:256], func=AF.Copy),
                         r=[pp.res], w=[kmT.res])
                for t in range(2):
                    for half in range(2):
                        pp = pj[pjc % 2]
                        pjc += 1
                        mm_group(pp[:], [(memT[:, kc, t * 128:(t + 1) * 128], w_kv[:, kc, 1024 + half * 512:1024 + (half + 1) * 512])
                                         for kc in range(8)], r=[w_kv.res, memT.res], w=[pp.res])
                        k.op("act", lambda e, pp=pp, t=t, half=half: e.activation(out=vmem[:, t, half * 512:(half + 1) * 512], in_=pp[:], func=AF.Copy),
                             r=[pp.res], w=[vmem.res])
                k.barrier(release=[w_kv.res] + [t_.res for t_ in memt])
            hT = [sb(p2, "hT2%d" % i, [128, 8, B2], BF16) for i in range(2)]
            qT = sb(p2, "qT2", [128, 8, B2], BF16)
            P2 = [sb(p2, "P2%d" % i, [128, 4, 256], BF16) for i in range(2)]
            P2n = [sb(p2, "P2n%d" % i, [128, 4, 256], BF16) for i in range(2)]
            PT2 = sb(p2, "PT2", [128, 4, 2, B2], BF16)
            oT = sb(p2, "oT2", [128, 8, B2], BF16)
            st2 = [dict(mx=sb(p2, "c_mx%d" % i, [128, 4], F32), rs=sb(p2, "c_rs%d" % i, [128, 4], F32),
                        ri=sb(p2, "c_ri%d" % i, [128, 4], F32)) for i in range(2)]
            set_gain(SM_CROSS)
            tc2 = 0
            for b in range(NT // B2):
                h = hT[b % 2]
                for t in range(4):
                    tile = b * 4 + t
                    norm_T(x_res[:, tile, :], x_res.rs[tile], pT[0], h[:, :, t * 128:(t + 1) * 128], h.res)
                for oc in range(8):
                    pp = pj[pjc % 2]
                    pjc += 1
                    mm_group(pp[:], [(w_q[:, kc, oc * 128:(oc + 1) * 128], h[:, kc, :]) for kc in range(8)],
                             r=[w_q.res, h.res], w=[pp.res])
                    k.op("act", lambda e, pp=pp, oc=oc: e.activation(out=qT[:, oc, :], in_=pp[:], func=AF.Copy), r=[pp.res], w=[qT.res])
                def c_S1(t, i2):
                    pS_ = pS2[i2]
                    for hh_ in range(4):
                        mm_group(pS_[:, hh_, :], [(qT[:, 2 * hh_ + j, t * 128:(t + 1) * 128], kmT[:, 2 * hh_ + j, :]) for j in range(2)],
                                 r=[qT.res, kmT.res], w=[pS_.res])

                def c_S2(t, i2):
                    pS_, P_, Pn_, s_ = pS2[i2], P2[i2], P2n[i2], st2[i2]
                    k.op("dve", lambda e: e.tensor_reduce(out=s_["mx"][:], in_=pS_[:], axis=AX.X, op=ALU.max, negate=True),
                         r=[pS_.res], w=[s_["mx"].res])
                    k.op("dve", lambda e: e.tensor_scalar(out=s_["mx"][:], in0=s_["mx"][:], scalar1=1.0 / 16, scalar2=None, op0=ALU.mult),
                         r=[s_["mx"].res], w=[s_["mx"].res])
                    for hh_ in range(4):
                        k.op("act", lambda e, hh_=hh_: e.activation(
                            out=P_[:, hh_, :], in_=pS_[:, hh_, :], func=AF.Exp, bias=s_["mx"][:, hh_:hh_ + 1], scale=1.0 / 16,
                            accum_out=s_["rs"][:, hh_:hh_ + 1]), r=[pS_.res, s_["mx"].res], w=[P_.res, s_["rs"].res])
                    k.op("dve", lambda e: e.reciprocal(out=s_["ri"][:], in_=s_["rs"][:]), r=[s_["rs"].res], w=[s_["ri"].res])
                    k.op("dve", lambda e: e.tensor_tensor(
                        out=Pn_[:], in0=P_[:], in1=s_["ri"][:, :].unsqueeze(2).to_broadcast([128, 4, 256]), op=ALU.mult),
                        r=[P_.res, s_["ri"].res], w=[Pn_.res])

                def c_S3(t, i2):
                    Pn_ = P2n[i2]
                    for hh_ in range(4):
                        for mc in range(2):
                            k.op("pe", lambda e, hh_=hh_, mc=mc: e.transpose(
                                out=pPT2[:, hh_ * 2 + mc, :], in_=Pn_[:, hh_, mc * 128:(mc + 1) * 128], identity=ident_b[:]),
                                r=[Pn_.res, ident_b.res], w=[pPT2.res])
                    k.op("act", lambda e: e.activation(out=PT2[:, :, :, t * 128:(t + 1) * 128],
                                                       in_=pPT2[:].rearrange("p (h m) t -> p h m t", h=4), func=AF.Copy),
                         r=[pPT2.res], w=[PT2.res])

                c_S1(0, tc2 % 2)
                for t in range(4):
                    if t + 1 < 4:
                        c_S1(t + 1, (tc2 + 1) % 2)
                    c_S2(t, tc2 % 2)
                    c_S3(t, tc2 % 2)
                    tc2 += 1
                for oc in range(8):
                    pp = pj[pjc % 2]
                    pjc += 1
                    mm_group(pp[:], [(vmem[:, mc, oc * 128:(oc + 1) * 128], PT2[:, oc // 2, mc, :]) for mc in range(2)],
                             r=[vmem.res, PT2.res], w=[pp.res])
                    k.op("act", lambda e, pp=pp, oc=oc: e.activation(out=oT[:, oc, :], in_=pp[:], func=AF.Copy), r=[pp.res], w=[oT.res])
                for t in range(4):
                    tile = b * 4 + t
                    for half in range(2):
                        pp = pj[pjc % 2]
                        pjc += 1
                        mm_group(pp[:], [(oT[:, c, t * 128:(t + 1) * 128], w_o[:, c, half * 512:(half + 1) * 512]) for c in range(8)],
                                 r=[oT.res, w_o.res], w=[pp.res])
                        k.op("dve", lambda e, pp=pp, tile=tile, half=half: e.tensor_tensor(
                            out=x_res[:, tile, half * 512:(half + 1) * 512], in0=pp[:], in1=x_res[:, tile, half * 512:(half + 1) * 512],
                            op=ALU.add), r=[pp.res, x_res.rs[tile]], w=[x_res.rs[tile]])
            k.barrier(release=[w_q.res, w_o.res])

        if debug:
            for tile in range(NTILE):
                k.dma("sp", dbg["dbg2"][tile * 128:(tile + 1) * 128, :], x_res[:, tile, :], r=[x_res.rs[tile]], w=[], sres=x_res.rs[tile])

        with ExitStack() as p3:
            p3s = p3.enter_context(ExitStack())
            slotI = sb(p3s, "slotI", [128, NT], F32)
            slotJ = sb(p3s, "slotJ", [128, NT], F32)
            slotG = sb(p3s, "slotG", [128, NT], F32)
            set_gain(SM_FFN)
            with ExitStack() as pa:
                B3 = 256
                w_qry = sb(pa, "w_qry", [128, 8, 2048], BF16)
                load_w_bf16(w_qry, w_qry_d[:, :], 2048)
                skb = sb(pa, "skb", [128, 16, 128], BF16)
                k.dma("pool", skb[:], skT_d.rearrange("p (o n) -> p o n", o=16), r=[], w=[skb.res], sres=skb.res)
                hT = [sb(pa, "hT3%d" % i, [128, 8, B3], BF16) for i in range(2)]
                qpT = sb(pa, "qpT", [128, 16, B3], BF16)
                sc = sb(pa, "sc", [128, 16, 128], F32, nres=4)
                sc2 = sb(pa, "sc2", [128, 16, 128], F32, nres=16)
                tv = sb(pa, "tv", [128, 16, 16], F32, nres=16)
                tiu = sb(pa, "tiu", [128, 16, 16], U32, nres=16)
                tif = sb(pa, "tif", [128, 16, 16], F32)
                cand = sb(pa, "cand", [128, 8, 256], F32)
                cand2 = sb(pa, "cand2", [128, 8, 256], F32, nres=8)
                ts = sb(pa, "ts", [128, 8, 16], F32, nres=8)
                posu = sb(pa, "posu", [128, 8, 16], U32, nres=8)
                au = sb(pa, "au", [128, 8, 16], U32)
                bu = sb(pa, "bu", [128, 8, 16], U32)
                af = sb(pa, "af", [128, 8, 16], F32)
                bf = sb(pa, "bf", [128, 8, 16], F32)
                eq = sb(pa, "eq", [128, 8, 16, 16], F32)
                isel = sb(pa, "isel", [128, 8, 16], F32)
                jsel = sb(pa, "jsel", [128, 8, 16], F32)
                gsel = sb(pa, "gsel", [128, 8, 16], F32)
                sm = sb(pa, "sm", [128, 8], F32)
                pj = [ps(pa, "pj3%d" % i, [128, 512], F32) for i in range(2)]
                pT = [ps(pa, "pT3%d" % i, [128, 8, 128], BF16) for i in range(1)]
                psc = [ps(pa, "psc%d" % i, [128, 4, 128], F32) for i in range(2)]
                pTs = ps(pa, "pTs", [128, 3, 128], F32)
                pjc = 0
                scc = 0
                iota16 = iota_f[:, 0:16]
                for b in range(NT // B3):
                    h = hT[b % 2]
                    for t in range(2):
                        tile = b * 2 + t
                        norm_T(x_res[:, tile, :], x_res.rs[tile], pT[0], h[:, :, t * 128:(t + 1) * 128], h.res)
                    for oc in range(16):
                        pp = pj[pjc % 2]
                        pjc += 1
                        mm_group(pp[:, 0:B3], [(w_qry[:, kc, oc * 128:(oc + 1) * 128], h[:, kc, :]) for kc in range(8)],
                                 r=[w_qry.res, h.res], w=[pp.res])
                        k.op("act", lambda e, pp=pp, oc=oc: e.activation(out=qpT[:, oc, :], in_=pp[:, 0:B3], func=AF.Copy),
                             r=[pp.res], w=[qpT.res])
                    for t in range(2):
                        tile = b * 2 + t
                        for g4 in range(4):
                            pq = psc[scc % 2]
                            scc += 1
                            for j in range(4):
                                oc = g4 * 4 + j
                                k.op("pe", lambda e, pq=pq, j=j, oc=oc, t=t: e.matmul(
                                    pq[:, j, :], lhsT=qpT[:, oc, t * 128:(t + 1) * 128], rhs=skb[:, oc, :], start=True, stop=True),
                                    r=[qpT.res, skb.res], w=[pq.res])
                            k.op("act", lambda e, pq=pq, g4=g4: e.activation(out=sc[:, g4 * 4:(g4 + 1) * 4, :], in_=pq[:], func=AF.Copy),
                                 r=[pq.res], w=[sc.rs[g4]])
                        for oc in range(16):
                            k.op("dve", lambda e, oc=oc: e.max(out=tv[:, oc, 0:8], in_=sc[:, oc, :]), r=[sc.rs[oc // 4]], w=[tv.rs[oc]])
                        for oc in range(16):
                            k.op("dve", lambda e, oc=oc: e.max_index(out=tiu[:, oc, 0:8], in_max=tv[:, oc, 0:8], in_values=sc[:, oc, :]),
                                 r=[sc.rs[oc // 4], tv.rs[oc]], w=[tiu.rs[oc]])
                        for oc in range(16):
                            k.op("dve", lambda e, oc=oc: e.match_replace(out=sc2[:, oc, :], in_to_replace=tv[:, oc, 0:8], in_values=sc[:, oc, :],
                                                                        imm_value=NEG), r=[sc.rs[oc // 4], tv.rs[oc]], w=[sc2.rs[oc]])
                        for oc in range(16):
                            k.op("dve", lambda e, oc=oc: e.max(out=tv[:, oc, 8:16], in_=sc2[:, oc, :]), r=[sc2.rs[oc]], w=[tv.rs[oc]])
                        for oc in range(16):
                            k.op("dve", lambda e, oc=oc: e.max_index(out=tiu[:, oc, 8:16], in_max=tv[:, oc, 8:16], in_values=sc2[:, oc, :]),
                                 r=[sc2.rs[oc], tv.rs[oc]], w=[tiu.rs[oc]])
                        k.op("dve", lambda e: e.tensor_copy(out=tif[:], in_=tiu[:]), r=tiu.rs, w=[tif.res])
                        tv4 = tv[:].rearrange("p (h two) a -> p h two a", two=2)
                        tif4 = tif[:].rearrange("p (h two) a -> p h two a", two=2)
                        k.op("dve", lambda e, tv4=tv4: e.tensor_tensor(
                            out=cand[:].rearrange("p h (a b) -> p h a b", a=16),
                            in0=tv4[:, :, 0, :].unsqueeze(3).to_broadcast([128, 8, 16, 16]),
                            in1=tv4[:, :, 1, :].unsqueeze(2).to_broadcast([128, 8, 16, 16]), op=ALU.add),
                            r=tv.rs, w=[cand.res])
                        for hd in range(8):
                            k.op("dve", lambda e, hd=hd: e.max(out=ts[:, hd, 0:8], in_=cand[:, hd, :]), r=[cand.res], w=[ts.rs[hd]])
                        for hd in range(8):
                            k.op("dve", lambda e, hd=hd: e.max_index(out=posu[:, hd, 0:8], in_max=ts[:, hd, 0:8], in_values=cand[:, hd, :]),
                                 r=[cand.res, ts.rs[hd]], w=[posu.rs[hd]])
                        for hd in range(8):
                            k.op("dve", lambda e, hd=hd: e.match_replace(out=cand2[:, hd, :], in_to_replace=ts[:, hd, 0:8], in_values=cand[:, hd, :],
                                                                        imm_value=NEG), r=[cand.res, ts.rs[hd]], w=[cand2.rs[hd]])
                        for hd in range(8):
                            k.op("dve", lambda e, hd=hd: e.max(out=ts[:, hd, 8:16], in_=cand2[:, hd, :]), r=[cand2.rs[hd]], w=[ts.rs[hd]])
                        for hd in range(8):
                            k.op("dve", lambda e, hd=hd: e.max_index(out=posu[:, hd, 8:16], in_max=ts[:, hd, 8:16], in_values=cand2[:, hd, :]),
                                 r=[cand2.rs[hd], ts.rs[hd]], w=[posu.rs[hd]])
                        k.op("dve", lambda e: e.tensor_scalar(out=au[:], in0=posu[:], scalar1=4, scalar2=None, op0=ALU.logical_shift_right),
                             r=posu.rs, w=[au.res])
                        k.op("dve", lambda e: e.tensor_scalar(out=bu[:], in0=posu[:], scalar1=15, scalar2=None, op0=ALU.bitwise_and),
                             r=posu.rs, w=[bu.res])
                        k.op("dve", lambda e: e.tensor_copy(out=af[:], in_=au[:]), r=[au.res], w=[af.res])
                        k.op("dve", lambda e: e.tensor_copy(out=bf[:], in_=bu[:]), r=[bu.res], w=[bf.res])
                        for (sel, rk, which) in ((isel, af, 0), (jsel, bf, 1)):
                            k.op("dve", lambda e, rk=rk: e.tensor_tensor(
                                out=eq[:], in0=rk[:].unsqueeze(3).to_broadcast([128, 8, 16, 16]),
                                in1=iota16.unsqueeze(1).unsqueeze(1).to_broadcast([128, 8, 16, 16]), op=ALU.is_equal),
                                r=[rk.res, iota_f.res], w=[eq.res])
                            k.op("dve", lambda e, which=which, tif4=tif4: e.tensor_tensor(
                                out=eq[:], in0=eq[:], in1=tif4[:, :, which, :].unsqueeze(2).to_broadcast([128, 8, 16, 16]), op=ALU.mult),
                                r=[eq.res, tif.res], w=[eq.res])
                            k.op("dve", lambda e, sel=sel: e.tensor_reduce(out=sel[:], in_=eq[:], axis=AX.X, op=ALU.add),
                                 r=[eq.res], w=[sel.res])
                        k.op("dve", lambda e: e.tensor_tensor(out=gsel[:], in0=ts[:], in1=ts[:, :, 0:1].to_broadcast([128, 8, 16]), op=ALU.subtract),
                             r=ts.rs, w=[gsel.res])
                        k.op("act", lambda e: e.activation(out=gsel[:], in_=gsel[:], func=AF.Exp), r=[gsel.res], w=[gsel.res])
                        k.op("dve", lambda e: e.tensor_reduce(out=sm[:], in_=gsel[:], axis=AX.X, op=ALU.add), r=[gsel.res], w=[sm.res])
                        k.op("dve", lambda e: e.reciprocal(out=sm[:], in_=sm[:]), r=[sm.res], w=[sm.res])
                        k.op("dve", lambda e: e.tensor_tensor(out=gsel[:], in0=gsel[:], in1=sm[:, :].unsqueeze(2).to_broadcast([128, 8, 16]), op=ALU.mult),
                             r=[gsel.res, sm.res], w=[gsel.res])
                        for i3, (src, dst) in enumerate(((isel, slotI), (jsel, slotJ), (gsel, slotG))):
                            k.op("pe", lambda e, i3=i3, src=src: e.transpose(out=pTs[:, i3, :], in_=src[:].rearrange("p h r -> p (h r)"),
                                                                            identity=ident_f[:]),
                                 r=[src.res, ident_f.res], w=[pTs.res])
                        for i3, (src, dst) in enumerate(((isel, slotI), (jsel, slotJ), (gsel, slotG))):
                            k.op("act", lambda e, i3=i3, dst=dst, tile=tile: e.activation(out=dst[:, tile * 128:(tile + 1) * 128], in_=pTs[:, i3, :], func=AF.Copy),
                                 r=[pTs.res], w=[dst.res])
                k.barrier(release=[w_qry.res, skb.res])

            with ExitStack() as pb:
                TCH = 8
                ohj = [sb(pb, "ohj%d" % i, [128, TCH, 128], BF16) for i in range(4)]
                eqi = [sb(pb, "eqi%d" % i, [128, TCH, 128], BF16) for i in range(4)]
                rig = [sb(pb, "rig%d" % i, [128, TCH, 128], BF16) for i in range(4)]
                Wst = [sb(pb, "Wst%d" % i, [128, 128, 128], BF16, nres=32) for i in range(2)]
                pW = [ps(pb, "pW%d" % i, [128, 4, 128], F32) for i in range(4)]
                wc = 0
                chc = 0
                iota_b3 = iota_f[:, :].unsqueeze(1).to_broadcast([128, TCH, 128])
                for tile in range(NTILE):
                    W_ = Wst[tile % 2]
                    for ch in range(128 // TCH):
                        oj, ei, rg = ohj[chc % 4], eqi[chc % 4], rig[chc % 4]
                        chc += 1
                        tok0 = tile * 128 + ch * TCH
                        k.op("dve", lambda e, oj=oj, tok0=tok0: e.tensor_tensor(
                            out=oj[:], in0=iota_b3, in1=slotJ[:, tok0:tok0 + TCH].unsqueeze(2).to_broadcast([128, TCH, 128]), op=ALU.is_equal),
                            r=[iota_f.res, slotJ.res], w=[oj.res])
                        k.op("dve", lambda e, ei=ei, tok0=tok0: e.tensor_tensor(
                            out=ei[:], in0=iota_b3, in1=slotI[:, tok0:tok0 + TCH].unsqueeze(2).to_broadcast([128, TCH, 128]), op=ALU.is_equal),
                            r=[iota_f.res, slotI.res], w=[ei.res])
                        k.op("pool", lambda e, ei=ei, rg=rg, tok0=tok0: e.tensor_tensor(
                            out=rg[:], in0=ei[:], in1=slotG[:, tok0:tok0 + TCH].unsqueeze(2).to_broadcast([128, TCH, 128]), op=ALU.mult),
                            r=[ei.res, slotG.res], w=[rg.res])
                        for q4 in range(TCH // 4):
                            pw = pW[wc % 4]
                            for j in range(4):
                                tt_ = q4 * 4 + j
                                k.op("pe", lambda e, pw=pw, j=j, oj=oj, rg=rg, tt_=tt_: e.matmul(
                                    pw[:, j, :], lhsT=oj[:, tt_, :], rhs=rg[:, tt_, :], start=True, stop=True),
                                    r=[oj.res, rg.res], w=[pw.res])
                            t0 = ch * TCH + q4 * 4
                            if True:
                                k.op("act", lambda e, pw=pw, W_=W_, t0=t0: e.activation(
                                    out=W_[:, :, t0:t0 + 4], in_=pw[:].rearrange("p t i -> p i t"), func=AF.Copy),
                                    r=[pw.res], w=[W_.rs[t0 // 4]])
                            else:
                                k.op("dve", lambda e, pw=pw, W_=W_, t0=t0: e.tensor_copy(
                                    out=W_[:, :, t0:t0 + 4], in_=pw[:].rearrange("p t i -> p i t")),
                                    r=[pw.res], w=[W_.rs[t0 // 4]])
                            wc += 1
                    k.dma("sp", wd_d[tile], W_[:].rearrange("p i t -> p (i t)"), r=W_.rs, w=[], sres=W_.res)
                k.barrier(release=[w_.res for w_ in Wst])
            p3s.close()

            with ExitStack() as pc:
                T3 = 256
                GS = 8
                NG = 128 // GS
                hfT = sb(pc, "hfT", [128, 8, NT], BF16)
                pT = [ps(pc, "pT4%d" % i, [128, 8, 128], BF16) for i in range(1)]
                pA = [ps(pc, "pA%d" % i, [128, 512], F32) for i in range(3)]
                pO = [ps(pc, "pO4%d" % i, [128, 512], F32) for i in range(4)]
                for tile in range(NTILE):
                    norm_T(x_res[:, tile, :], x_res.rs[tile], pT[0], hfT[:, :, tile * 128:(tile + 1) * 128], hfT.res)
                ut = [sb(pc, "ut%d" % i, [128, GS, 8, 128], BF16) for i in range(2)]
                vt = [sb(pc, "vt%d" % i, [128, GS, D], BF16) for i in range(2)]
                wsl = [sb(pc, "wsl%d" % i, [128, 2, GS, 128], BF16) for i in range(2)]
                ge = [sb(pc, "ge%d" % i, [128, T3], F32) for i in range(3)]
                gw = [sb(pc, "gw%d" % i, [128, T3], BF16) for i in range(3)]
                uT_v = uT_d.rearrange("(i p) (c e) -> p i c e", p=128, c=8)
                ev_v = ev_d.rearrange("(i e) d -> e i d", e=128)
                wd_v = wd_d.rearrange("t j (i x) -> j t i x", i=128)
                items = [(g, tb, i) for g in range(NG) for tb in range(NT // T3) for i in range(GS)]
                state = {}

                def emitA(n):
                    g, tb, i = items[n]
                    u_, v_ = ut[g % 2], vt[g % 2]
                    if tb == 0 and i == 0:
                        for i_ in range(GS):
                            k.dma("pool", u_[:, i_, :, :], uT_v[:, g * GS + i_, :, :], r=[], w=[u_.res], sres=u_.res)
                            k.dma("pool", v_[:, i_, :], ev_v[:, g * GS + i_, :], r=[], w=[v_.res], sres=v_.res, max_dma_last_dim=4096)
                    if i == 0:
                        ws_ = wsl[(g * (NT // T3) + tb) % 2]
                        k.dma("sp", ws_[:], wd_v[:, 2 * tb:2 * tb + 2, g * GS:(g + 1) * GS, :], r=[], w=[ws_.res], sres=ws_.res)
                    ws_ = wsl[(g * (NT // T3) + tb) % 2]
                    pa_ = pA[n % 3]
                    ge_, gw_ = ge[n % 3], gw[n % 3]
                    mm_group(pa_[:, 0:T3], [(u_[:, i, c, :], hfT[:, c, tb * T3:(tb + 1) * T3]) for c in range(8)],
                             r=[u_.res, hfT.res], w=[pa_.res])
                    k.op("act", lambda e: e.activation(out=ge_[:], in_=pa_[:, 0:T3], func=AF.Gelu_apprx_tanh),
                         r=[pa_.res], w=[ge_.res])
                    k.op("dve", lambda e: e.tensor_tensor(
                        out=gw_[:].rearrange("p (a t) -> p a t", a=2), in0=ge_[:].rearrange("p (a t) -> p a t", a=2),
                        in1=ws_[:, :, i, :], op=ALU.mult), r=[ge_.res, ws_.res], w=[gw_.res])

                def emitV(n):
                    g, tb, i = items[n]
                    v_ = vt[g % 2]
                    gw_ = gw[n % 3]
                    for t in range(2):
                        for half in range(2):
                            po = pO[t * 2 + half]
                            k.op("pe", lambda e, po=po, t=t, half=half: e.matmul(
                                po[:], lhsT=gw_[:, t * 128:(t + 1) * 128], rhs=v_[:, i, half * 512:(half + 1) * 512],
                                start=(i == 0), stop=(i == GS - 1)), r=[gw_.res, v_.res], w=[po.res])
                    if i == GS - 1:
                        for t in range(2):
                            tile = tb * 2 + t
                            for half in range(2):
                                po = pO[t * 2 + half]
                                k.op("dve", lambda e, po=po, tile=tile, half=half: e.tensor_tensor(
                                    out=x_res[:, tile, half * 512:(half + 1) * 512], in0=po[:], in1=x_res[:, tile, half * 512:(half + 1) * 512],
                                    op=ALU.add), r=[po.res, x_res.rs[tile]], w=[x_res.rs[tile]])

                emitA(0)
                emitA(1)
                for n in range(len(items)):
                    if n + 2 < len(items):
                        emitA(n + 2)
                    emitV(n)
                k.barrier(release=[t_.res for t_ in ut + vt + wsl])

        with ExitStack() as p4:
            gfin = sb(p4, "gfin", [128, D], F32)
            k.dma("sp", gfin[:], gfin_d[:, :], r=[], w=[gfin.res], sres=gfin.res)
            ot = [sb(p4, "ot%d" % i, [128, D], F32) for i in range(2)]
            for tile in range(NTILE):
                n = nrm[tile % 2]
                o_ = ot[tile % 2]
                xa = x_res[:, tile, :]
                xr = x_res.rs[tile]
                k.op("dve", lambda e, n=n, xa=xa: e.scalar_tensor_tensor(out=n["junk"][:], in0=xa, scalar=1.0, in1=xa,
                                                                         op0=ALU.mult, op1=ALU.mult, accum_out=n["ss"][:]),
                     r=[xr], w=[n["junk"].res, n["ss"].res])
                k.op("act", lambda e, n=n: e.activation(out=n["sd"][:], in_=n["ss"][:], func=AF.Ln, bias=cst[:, 0:1], scale=1.0 / D),
                     r=[n["ss"].res, cst.res], w=[n["sd"].res])
                k.op("act", lambda e, n=n: e.activation(out=n["rstd"][:], in_=n["sd"][:], func=AF.Exp, scale=-0.5),
                     r=[n["sd"].res], w=[n["rstd"].res])
                k.op("dve", lambda e, n=n, xa=xa, o_=o_: e.scalar_tensor_tensor(out=o_[:], in0=xa, scalar=n["rstd"][:, 0:1], in1=gfin[:],
                                                                                op0=ALU.mult, op1=ALU.mult),
                     r=[xr, n["rstd"].res, gfin.res], w=[o_.res])
                k.dma("sp", y_d[tile * 128:(tile + 1) * 128, :], o_[:], r=[o_.res], w=[], sres=o_.res)
            k.barrier()
    return nc, k.n_ins


def prep_shared(inp):
    f = np.float32
    g = lambda a: np.ascontiguousarray(np.asarray(a, dtype=f))
    sm = np.zeros((128, NSM), f)

    def put(col, vec, nch):
        sm[:, col:col + nch] = np.asarray(vec, f).reshape(nch, 128).T

    put(SM_MIX, inp["norm_mix"][0], 8)
    put(SM_CROSS, inp["norm_cross"][0], 8)
    put(SM_MEM, inp["norm_mem"][0], 8)
    put(SM_FFN, inp["norm_ffn"][0], 8)
    put(SM_GA, inp["norm_grp_a"][0], 4)
    put(SM_GB, inp["norm_grp_b"][0], 4)
    put(SM_CB, inp["conv_b"][0], 4)
    put(SM_BA, inp["gate_a_b"][0], 4)
    put(SM_BX, inp["gate_x_b"][0], 4)
    put(SM_LAM, inp["lru_lambda"][0], 4)
    cw = np.asarray(inp["conv_w"][0], f)
    for cc in range(4):
        for j in range(4):
            sm[:, SM_CW + cc * 4 + j] = cw[j, cc * 128:(cc + 1) * 128]
    gbd = np.zeros((128, 8, 128), f)
    for gi, key in enumerate(("gate_a_w", "gate_x_w")):
        w = np.asarray(inp[key][0], f)
        for cc in range(4):
            gbd[0:64, gi * 4 + cc, 0:64] = w[2 * cc]
            gbd[64:128, gi * 4 + cc, 64:128] = w[2 * cc + 1]
    rb = np.asarray(inp["rel_bias"][0], f)
    qi = np.arange(128)[:, None]
    kj = np.arange(640)[None, :]
    idx = np.clip(512 + qi - kj, -128, 128) + 128
    ab = rb[:, idx]
    valid = np.where(qi < 64, kj < 576, kj >= 64)
    ab = np.where(valid[None], ab, f(NEG)).astype(f)
    abias = np.ascontiguousarray(ab.transpose(1, 0, 2)).reshape(128, 8 * 640)
    sk = np.asarray(inp["sub_keys"][0], f)
    skT = np.ascontiguousarray(sk.reshape(16, 128, 128).transpose(2, 0, 1)).reshape(128, 16 * 128)
    u = np.asarray(inp["expert_u"][0], f)
    uT = np.ascontiguousarray(u.reshape(128, 128, 8, 128).transpose(0, 3, 2, 1)).reshape(16384, D)
    shared = {
        "w_in": g(inp["w_in"][0]), "w_out": g(inp["w_out"][0]), "w_q": g(inp["w_q_mem"][0]),
        "w_kv": g(inp["w_kv_mem"][0]), "w_o": g(inp["w_o_mem"][0]), "w_qry": g(inp["w_query"][0]),
        "smalls": sm, "gbd": gbd.reshape(128, 8 * 128), "abias": abias, "skT": skT, "uT": uT,
        "ev": g(inp["expert_v"][0]),
        "gfin": np.ascontiguousarray(np.broadcast_to(np.asarray(inp["norm_final"], f)[None, :], (128, D))),
        "ident": np.eye(128, dtype=f),
        "iota": np.ascontiguousarray(np.broadcast_to(np.arange(128, dtype=f)[None, :], (128, 128))),
    }
    return shared


def make_in_maps(inp, NT):
    x = np.asarray(inp["x"], np.float32)
    mem = np.asarray(inp["mem"], np.float32)
    B, S, _ = x.shape
    per_seq = S // NT
    NPRE = 3 * NT
    shared = prep_shared(inp)
    maps = []
    for c in range(B * per_seq):
        b, q = divmod(c, per_seq)
        xprev = np.zeros((NPRE, D), np.float32)
        if q > 0:
            xprev[NPRE - q * NT:] = x[b, 0:q * NT]
        pflag = np.zeros((128, NPRE // 512), np.float32)
        for j in range(NPRE // 512):
            if j * 512 >= NPRE - q * NT:
                pflag[:, j] = 1.0
        halob = np.full((128, 512), 0.0 if q > 0 else NEG, np.float32)
        m = dict(shared)
        m.update({"xown": np.ascontiguousarray(x[b, q * NT:(q + 1) * NT]), "xprev": xprev, "pflag": pflag,
                  "halob": halob, "mem": np.ascontiguousarray(mem[b])})
        maps.append(m)
    return maps


_CACHE = {}


def kernel(**inputs):
    NT = 2048
    if NT not in _CACHE:
        _CACHE[NT] = build(NT)[0]
    nc = _CACHE[NT]
    maps = make_in_maps(inputs, NT)
    res = run_bass_kernel_spmd(nc, maps, core_ids=list(range(N_CORES)))
    x = np.asarray(inputs["x"])
    B, S, _ = x.shape
    out = np.empty((B, S, D), np.float32)
    per_seq = S // NT
    for c in range(N_CORES):
        b, q = divmod(c, per_seq)
        out[b, q * NT:(q + 1) * NT] = res.results[c]["y"]
    return out
```

```python
import numpy as np
from contextlib import ExitStack
import concourse.bass as bass
import concourse.mybir as mybir
from concourse.bass_utils import run_bass_kernel_spmd

F32 = mybir.dt.float32
BF16 = mybir.dt.bfloat16
U32 = mybir.dt.uint32
AF = mybir.ActivationFunctionType
ALU = mybir.AluOpType
AX = mybir.AxisListType

D = 1024
EPS = 1e-6
NEG = -1e30
N_CORES = 8
SAME_ENG_SYNC = True

SM_MIX, SM_CROSS, SM_MEM, SM_FFN = 0, 8, 16, 24
SM_GA, SM_GB, SM_CB, SM_BA, SM_BX, SM_LAM, SM_CW = 32, 36, 40, 44, 48, 52, 56
NSM = 72


class Res:
    __slots__ = ("name", "w", "r", "ds")

    def __init__(self, name):
        self.name = name
        self.w = None
        self.r = {}
        self.ds = None


class KB:
    def __init__(self, nc, es):
        self.nc = nc
        self.es = es
        self.E = {}
        for nm, e in (("pe", nc.tensor), ("act", nc.scalar), ("dve", nc.vector),
                      ("pool", nc.gpsimd), ("sp", nc.sync)):
            self.E[nm] = dict(e=e, sem=es.enter_context(nc.semaphore("e_" + nm)), cnt=0, waited={}, nm=nm)
        self.free_ds = []
        self.all_ds = []
        self.n_ins = 0

    def _collect(self, r, w):
        deps = {}
        for x in r:
            if x.w is not None:
                s, v = x.w
                if deps.get(s, 0) < v:
                    deps[s] = v
        for x in w:
            if x.w is not None:
                s, v = x.w
                if deps.get(s, 0) < v:
                    deps[s] = v
            for s, v in x.r.items():
                if deps.get(s, 0) < v:
                    deps[s] = v
        return deps

    def _waits(self, E, deps, skip_own):
        for s, v in deps.items():
            if skip_own and s is E["sem"]:
                continue
            if E["waited"].get(s, 0) >= v:
                continue
            E["e"].wait_ge(s, v)
            E["waited"][s] = v

    def op(self, en, fn, r=(), w=()):
        E = self.E[en]
        skip_own = (en == "pe") or (not SAME_ENG_SYNC)
        self._waits(E, self._collect(r, w), skip_own)
        ins = fn(E["e"])
        E["cnt"] += 1
        self.n_ins += 1
        ins.then_inc(E["sem"], 1)
        tag = (E["sem"], E["cnt"])
        for x in w:
            x.w = tag
            x.r = {}
        for x in r:
            if x not in w:
                x.r[E["sem"]] = E["cnt"]
        return ins

    def dma(self, qn, out, in_, r, w, sres, **kw):
        E = self.E[qn]
        self._waits(E, self._collect(r, w), False)
        if sres.ds is None:
            if qn != "pool" and self.free_ds:
                sres.ds = self.free_ds.pop()
            else:
                sres.ds = [self.es.enter_context(self.nc.semaphore("d%d" % len(self.all_ds))), 0, qn]
                self.all_ds.append(sres.ds)
        ins = E["e"].dma_start(out=out, in_=in_, **kw)
        self.n_ins += 1
        sres.ds[1] += 1
        ins.then_inc(sres.ds[0], 16)
        val = sres.ds[1] * 16
        for x in w:
            x.w = (sres.ds[0], val)
            x.r = {}
        for x in r:
            if x not in w:
                x.r[sres.ds[0]] = val

    def barrier(self, release=()):
        deps = {}
        for nm in ("pe", "act", "dve", "pool"):
            E = self.E[nm]
            if E["cnt"] > 0:
                deps[E["sem"]] = E["cnt"]
        for ds in self.all_ds:
            if ds[1] > 0:
                deps[ds[0]] = ds[1] * 16
        for nm in ("pe", "act", "dve", "pool", "sp"):
            self._waits(self.E[nm], deps, True)
        for x in release:
            if x.ds is not None:
                if x.ds[2] != "pool":
                    self.free_ds.append(x.ds)
                x.ds = None


class Tn:
    def __init__(self, h, name, nres=1):
        self.h = h
        self.res = Res(name)
        self.rs = [Res("%s_%d" % (name, i)) for i in range(nres)] if nres > 1 else [self.res]

    def __getitem__(self, k):
        return self.h[k]


def build(NT=2048, debug=False):
    NPRE = 3 * NT
    NTILE = NT // 128
    nc = bass.Bass("TRN2", target_bir_lowering=False)
    dt_in = lambda n, s, d=F32: nc.dram_tensor(n, list(s), d, kind="ExternalInput").ap()
    xown = dt_in("xown", [NT, D])
    xprev = dt_in("xprev", [NPRE, D])
    pflag_d = dt_in("pflag", [128, NPRE // 512])
    halob_d = dt_in("halob", [128, 512])
    mem_d = dt_in("mem", [256, D])
    w_in_d = dt_in("w_in", [D, 2560])
    w_out_d = dt_in("w_out", [D, D])
    w_q_d = dt_in("w_q", [D, D])
    w_kv_d = dt_in("w_kv", [D, 2048])
    w_o_d = dt_in("w_o", [D, D])
    w_qry_d = dt_in("w_qry", [D, 2048])
    smalls_d = dt_in("smalls", [128, NSM])
    gbd_d = dt_in("gbd", [128, 8 * 128])
    abias_d = dt_in("abias", [128, 8 * 640])
    skT_d = dt_in("skT", [128, 16 * 128])
    uT_d = dt_in("uT", [16384, D])
    ev_d = dt_in("ev", [16384, D])
    gfin_d = dt_in("gfin", [128, D])
    ident_d = dt_in("ident", [128, 128])
    iota_d = dt_in("iota", [128, 128])
    y_d = nc.dram_tensor("y", [NT, D], F32, kind="ExternalOutput").ap()
    wd_d = nc.dram_tensor("wd_scratch", [NTILE, 128, 16384], BF16, kind="Internal").ap()
    dbg = {}
    if debug:
        for nm in ("dbg1", "dbg2"):
            dbg[nm] = nc.dram_tensor(nm, [NT, D], F32, kind="ExternalOutput").ap()

    with ExitStack() as es:
        k = KB(nc, es)

        def sb(ctx, name, shape, dt, nres=1):
            return Tn(ctx.enter_context(nc.sbuf_tensor("sb_" + name, list(shape), dt)), name, nres)

        def ps(ctx, name, shape, dt):
            return Tn(ctx.enter_context(nc.psum_tensor("ps_" + name, list(shape), dt)), name)

        x_res = sb(es, "x_res", [128, NTILE, D], F32, nres=NTILE)
        ident_f = sb(es, "ident_f", [128, 128], F32)
        ident_b = sb(es, "ident_b", [128, 128], BF16)
        iota_f = sb(es, "iota_f", [128, 128], F32)
        ones_b = sb(es, "ones_b", [128, 128], BF16)
        smalls = sb(es, "smalls", [128, NSM], F32)
        cst = sb(es, "cst", [128, 4], F32)
        gB = sb(es, "gB", [128, 8, 128], F32)
        nrm = [dict(ss=sb(es, "n_ss%d" % i, [128, 1], F32), sd=sb(es, "n_sd%d" % i, [128, 1], F32),
                    rstd=sb(es, "n_rstd%d" % i, [128, 1], F32), xs=sb(es, "n_xs%d" % i, [128, D], BF16),
                    junk=sb(es, "n_junk%d" % i, [128, D], BF16)) for i in range(2)]
        nrm_i = [0]

        k.dma("sp", ident_f[:], ident_d[:, :], r=[], w=[ident_f.res], sres=ident_f.res)
        k.dma("sp", iota_f[:], iota_d[:, :], r=[], w=[iota_f.res], sres=iota_f.res)
        k.dma("sp", smalls[:], smalls_d[:, :], r=[], w=[smalls.res], sres=smalls.res)
        k.op("dve", lambda e: e.tensor_copy(out=ident_b[:], in_=ident_f[:]), r=[ident_f.res], w=[ident_b.res])
        k.op("pool", lambda e: e.memset(ones_b[:], 1.0), w=[ones_b.res])
        k.op("pool", lambda e: e.memset(cst[:, 0:1], EPS), w=[cst.res])
        k.op("pool", lambda e: e.memset(cst[:, 1:2], 1.0), w=[cst.res])
        k.op("pool", lambda e: e.memset(cst[:, 2:3], 0.0), w=[cst.res])

        def set_gain(col):
            k.op("dve", lambda e: e.tensor_copy(
                out=gB[:], in_=smalls[:, col:col + 8].unsqueeze(2).to_broadcast([128, 8, 128])),
                r=[smalls.res], w=[gB.res])

        def norm_T_g(x_ap, x_r, pT, hT_ap, hT_r):
            n = nrm[nrm_i[0] % 2]
            nrm_i[0] += 1
            k.op("dve", lambda e: e.scalar_tensor_tensor(out=n["junk"][:], in0=x_ap, scalar=1.0, in1=x_ap,
                                                         op0=ALU.mult, op1=ALU.mult, accum_out=n["ss"][:]),
                 r=[x_r], w=[n["junk"].res, n["ss"].res])
            yield
            k.op("act", lambda e: e.activation(out=n["sd"][:], in_=n["ss"][:], func=AF.Ln,
                                               bias=cst[:, 0:1], scale=1.0 / D),
                 r=[n["ss"].res, cst.res], w=[n["sd"].res])
            k.op("act", lambda e: e.activation(out=n["rstd"][:], in_=n["sd"][:], func=AF.Exp, scale=-0.5),
                 r=[n["sd"].res], w=[n["rstd"].res])
            yield
            k.op("dve", lambda e: e.tensor_scalar(out=n["xs"][:], in0=x_ap, scalar1=n["rstd"][:, 0:1], scalar2=None,
                                                  op0=ALU.mult), r=[x_r, n["rstd"].res], w=[n["xs"].res])
            for c in range(8):
                k.op("pe", lambda e, c=c: e.transpose(out=pT[:, c, :], in_=n["xs"][:, c * 128:(c + 1) * 128],
                                                      identity=ident_b[:]),
                     r=[n["xs"].res, ident_b.res], w=[pT.res])
            yield
            k.op("dve", lambda e: e.tensor_tensor(out=hT_ap, in0=pT[:], in1=gB[:], op=ALU.mult),
                 r=[pT.res, gB.res], w=[hT_r])

        def norm_T(*a):
            for _ in norm_T_g(*a):
                pass

        def lockstep_g(gens):
            gens = list(gens)
            while gens:
                for g_ in list(gens):
                    try:
                        next(g_)
                    except StopIteration:
                        gens.remove(g_)
                yield

        def norm_pairs(args_list):
            for i_ in range(0, len(args_list), 2):
                for _ in lockstep_g([norm_T_g(*a) for a in args_list[i_:i_ + 2]]):
                    pass

        def mm_group(out_ap, pairs, r, w):
            n = len(pairs)
            for i, (l, rh) in enumerate(pairs):
                k.op("pe", lambda e, l=l, rh=rh, i=i: e.matmul(out_ap, lhsT=l, rhs=rh, start=(i == 0), stop=(i == n - 1)),
                     r=r, w=w)

        def load_w_bf16(dst, src_ap, ncols):
            for c in range(8):
                k.dma("pool", dst[:, c, :], src_ap[c * 128:(c + 1) * 128, :], r=[], w=[dst.res], sres=dst.res,
                      max_dma_last_dim=4096)

        with ExitStack() as p1:
            with ExitStack() as pa:
                BA = 512
                w_inA = sb(pa, "w_inA", [128, 8, 1024], BF16)
                load_w_bf16(w_inA, w_in_d[:, 0:1024], 1024)
                w_outA = sb(pa, "w_outA", [128, 4, D], BF16)
                for c in range(4):
                    k.dma("pool", w_outA[:, c, :], w_out_d[c * 128:(c + 1) * 128, :], r=[], w=[w_outA.res], sres=w_outA.res,
                          max_dma_last_dim=4096)
                yan = sb(pa, "yan", [128, 4, 512], BF16)
                gbd_f = sb(pa, "gbd_f", [128, 8, 128], F32)
                gbd = sb(pa, "gbd", [128, 8, 128], BF16)
                k.dma("sp", gbd_f[:], gbd_d.rearrange("p (c j) -> p c j", c=8), r=[], w=[gbd_f.res], sres=gbd_f.res)
                k.op("dve", lambda e: e.tensor_copy(out=gbd[:], in_=gbd_f[:]), r=[gbd_f.res], w=[gbd.res])
                pflag = sb(pa, "pflag", [128, NPRE // 512], F32)
                k.dma("sp", pflag[:], pflag_d[:, :], r=[], w=[pflag.res], sres=pflag.res)
                cL = sb(pa, "cL", [128, 4], F32)
                tmp4 = sb(pa, "tmp4", [128, 4], F32)
                k.op("act", lambda e: e.activation(out=tmp4[:], in_=smalls[:, SM_LAM:SM_LAM + 4], func=AF.Exp, scale=-1.0),
                     r=[smalls.res], w=[tmp4.res])
                k.op("act", lambda e: e.activation(out=cL[:], in_=tmp4[:], func=AF.Ln, bias=cst[:, 1:2], scale=1.0),
                     r=[tmp4.res, cst.res], w=[cL.res])
                k.op("dve", lambda e: e.tensor_scalar(out=cL[:], in0=cL[:], scalar1=-8.0, scalar2=None, op0=ALU.mult),
                     r=[cL.res], w=[cL.res])
                set_gain(SM_MIX)
                xtmp = [sb(pa, "xtmp%d" % i, [128, D], F32) for i in range(2)]
                hT = [sb(pa, "hTa%d" % i, [128, 8, BA], BF16) for i in range(2)]
                xl = sb(pa, "xl", [128, 4, 3 + BA], F32, nres=4)
                gg = sb(pa, "gg", [128, 4, BA], F32, nres=4)
                hh = sb(pa, "hh", [128, 4, BA], F32, nres=4)
                hst = sb(pa, "hst", [128, 4], F32, nres=4)
                yaT = sb(pa, "yaT", [128, 4, BA], F32, nres=4)
                LT = [{nm: sb(pa, "l%s%d" % (nm, i), [128, BA], BF16 if nm == "xcb" else F32)
                       for nm in ("xc", "xc2", "xcb", "rr", "ii", "aa", "tt", "bb")} for i in range(2)]
                sq = sb(pa, "sq", [128, BA], BF16)
                rsn = sb(pa, "rsn", [128, BA], F32)
                pj = [ps(pa, "pj%d" % i, [128, 512], F32) for i in range(2)]
                pT = [ps(pa, "pT%d" % i, [128, 8, 128], BF16) for i in range(2)]
                pg = [ps(pa, "pg%d" % i, [128, 512], F32) for i in range(4)]
                pn = pj[0]
                k.op("pool", lambda e: e.memset(xl[:], 0.0), w=xl.rs)
                k.op("pool", lambda e: e.memset(hst[:], 0.0), w=hst.rs)

                nblk_pre = NPRE // BA
                nblk_own = NT // BA
                tcount = 0
                pjc = 0
                def front_norm(b):
                    nonlocal tcount
                    own = b >= nblk_pre
                    ob = b - nblk_pre
                    h = hT[b % 2]

                    def tile_g(t):
                        nonlocal tcount
                        if own:
                            tile = ob * 4 + t
                            xa = x_res[:, tile, :]
                            xr = x_res.rs[tile]
                            k.dma("sp", xa, xown[tile * 128:(tile + 1) * 128, :], r=[], w=[xr], sres=xr)
                        else:
                            xt_ = xtmp[tcount % 2]
                            xa = xt_[:]
                            xr = xt_.res
                            r0 = b * BA + t * 128
                            k.dma("sp", xa, xprev[r0:r0 + 128, :], r=[], w=[xr], sres=xr)
                        tcount += 1
                        yield from norm_T_g(xa, xr, pT[t % 2], h[:, :, t * 128:(t + 1) * 128], h.res)

                    yield from lockstep_g([tile_g(0), tile_g(1)])
                    yield from lockstep_g([tile_g(2), tile_g(3)])

                def front_proj(b):
                    nonlocal pjc
                    own = b >= nblk_pre
                    h = hT[b % 2]
                    for cc in range(8 if own else 4):
                        pp = pj[pjc % 2]
                        pjc += 1
                        mm_group(pp[:], [(w_inA[:, kc, cc * 128:(cc + 1) * 128], h[:, kc, :]) for kc in range(8)],
                                 r=[w_inA.res, h.res], w=[pp.res])
                        if cc < 4:
                            k.op("act", lambda e, pp=pp, cc=cc: e.activation(out=xl[:, cc, 3:3 + BA], in_=pp[:], func=AF.Copy),
                                 r=[pp.res], w=[xl.rs[cc]])
                        else:
                            k.op("act", lambda e, pp=pp, cc=cc: e.activation(out=gg[:, cc - 4, :], in_=pp[:], func=AF.Gelu_apprx_tanh),
                                 r=[pp.res], w=[gg.rs[cc - 4]])

                for _ in front_norm(0):
                    pass
                front_proj(0)
                for b in range(nblk_pre + nblk_own):
                    own = b >= nblk_pre
                    ob = b - nblk_pre
                    fn = front_norm(b + 1) if b + 1 < nblk_pre + nblk_own else iter(())
                    def lru_chain(cc, L, pga, pgb, own=own, b=b):
                            cw = lambda j, cc=cc: smalls[:, SM_CW + cc * 4 + j:SM_CW + cc * 4 + j + 1]
                            k.op("dve", lambda e, cc=cc, L=L, cw=cw: e.tensor_scalar(
                                out=L["xc"][:], in0=xl[:, cc, 3:3 + BA], scalar1=cw(3), scalar2=smalls[:, SM_CB + cc:SM_CB + cc + 1],
                                op0=ALU.mult, op1=ALU.add), r=[xl.rs[cc], smalls.res], w=[L["xc"].res])
                            src, dst = "xc", "xc2"
                            for j in range(3):
                                yield
                                k.op("dve", lambda e, cc=cc, L=L, cw=cw, j=j, src=src, dst=dst: e.scalar_tensor_tensor(
                                    out=L[dst][:], in0=xl[:, cc, j:j + BA], scalar=cw(j), in1=L[src][:], op0=ALU.mult, op1=ALU.add),
                                    r=[xl.rs[cc], smalls.res, L[src].res], w=[L[dst].res])
                                src, dst = dst, src
                            xc = L[src]
                            yield
                            k.op("pool", lambda e, cc=cc: e.tensor_copy(out=xl[:, cc, 0:3], in_=xl[:, cc, BA:BA + 3]),
                                 r=[xl.rs[cc]], w=[xl.rs[cc]])
                            yield
                            k.op("act", lambda e, L=L, xc=xc: e.activation(out=L["xcb"][:], in_=xc[:], func=AF.Copy),
                                 r=[xc.res], w=[L["xcb"].res])
                            yield
                            k.op("pe", lambda e, cc=cc, L=L: e.matmul(pga[:], lhsT=gbd[:, cc, :], rhs=L["xcb"][:], start=True, stop=True),
                                 r=[gbd.res, L["xcb"].res], w=[pga.res])
                            yield
                            k.op("pe", lambda e, cc=cc, L=L: e.matmul(pgb[:], lhsT=gbd[:, 4 + cc, :], rhs=L["xcb"][:], start=True, stop=True),
                                 r=[gbd.res, L["xcb"].res], w=[pgb.res])
                            yield
                            k.op("act", lambda e, cc=cc, L=L: e.activation(out=L["rr"][:], in_=pga[:], func=AF.Sigmoid,
                                                                           bias=smalls[:, SM_BA + cc:SM_BA + cc + 1], scale=1.0),
                                 r=[pga.res, smalls.res], w=[L["rr"].res])
                            yield
                            k.op("act", lambda e, cc=cc, L=L: e.activation(out=L["ii"][:], in_=pgb[:], func=AF.Sigmoid,
                                                                           bias=smalls[:, SM_BX + cc:SM_BX + cc + 1], scale=1.0),
                                 r=[pgb.res, smalls.res], w=[L["ii"].res])
                            yield
                            k.op("act", lambda e, cc=cc, L=L: e.activation(out=L["aa"][:], in_=L["rr"][:], func=AF.Exp,
                                                                           scale=cL[:, cc:cc + 1]),
                                 r=[L["rr"].res, cL.res], w=[L["aa"].res])
                            yield
                            k.op("dve", lambda e, L=L: e.tensor_tensor(out=L["tt"][:], in0=L["aa"][:], in1=L["aa"][:], op=ALU.mult),
                                 r=[L["aa"].res], w=[L["tt"].res])
                            yield
                            k.op("act", lambda e, L=L: e.activation(out=L["tt"][:], in_=L["tt"][:], func=AF.Sqrt,
                                                                    bias=cst[:, 1:2], scale=-1.0),
                                 r=[L["tt"].res, cst.res], w=[L["tt"].res])
                            yield
                            k.op("dve", lambda e, L=L: e.tensor_tensor(out=L["bb"][:], in0=L["tt"][:], in1=L["ii"][:], op=ALU.mult),
                                 r=[L["tt"].res, L["ii"].res], w=[L["bb"].res])
                            yield
                            k.op("dve", lambda e, L=L, xc=xc: e.tensor_tensor(out=L["rr"][:], in0=L["bb"][:], in1=xc[:], op=ALU.mult),
                                 r=[L["bb"].res, xc.res], w=[L["rr"].res])
                            yield
                            k.op("dve", lambda e, cc=cc, L=L: e.tensor_tensor_scan(
                                out=hh[:, cc, :], data0=L["aa"][:], data1=L["rr"][:], initial=hst[:, cc:cc + 1],
                                op0=ALU.mult, op1=ALU.add), r=[L["aa"].res, L["rr"].res, hst.rs[cc]], w=[hh.rs[cc]])
                            if own:
                                yield
                                k.op("dve", lambda e, cc=cc: e.tensor_copy(out=hst[:, cc:cc + 1], in_=hh[:, cc, BA - 1:BA]),
                                     r=[hh.rs[cc]], w=[hst.rs[cc]])
                                yield
                                k.op("dve", lambda e, cc=cc: e.tensor_tensor(out=yaT[:, cc, :], in0=hh[:, cc, :], in1=gg[:, cc, :], op=ALU.mult),
                                     r=[hh.rs[cc], gg.rs[cc]], w=[yaT.rs[cc]])
                            else:
                                yield
                                k.op("dve", lambda e, cc=cc, b=b: e.tensor_tensor(out=hst[:, cc:cc + 1], in0=hh[:, cc, BA - 1:BA],
                                                                                  in1=pflag[:, b:b + 1], op=ALU.mult),
                                     r=[hh.rs[cc], pflag.res], w=[hst.rs[cc]])

                    for pair in range(2):
                        gens = [lru_chain(2 * pair + i_, LT[i_], pg[2 * i_], pg[2 * i_ + 1]) for i_ in range(2)]
                        while gens:
                            for g_ in list(gens):
                                try:
                                    next(g_)
                                except StopIteration:
                                    gens.remove(g_)
                            next(fn, None)
                    for _ in fn:
                        pass
                    if own:
                        for cc in range(4):
                            k.op("dve", lambda e, cc=cc: e.tensor_tensor(out=sq[:], in0=yaT[:, cc, :], in1=yaT[:, cc, :], op=ALU.mult),
                                 r=[yaT.rs[cc]], w=[sq.res])
                            k.op("pe", lambda e, cc=cc: e.matmul(pn[:], lhsT=ones_b[:], rhs=sq[:], start=(cc == 0), stop=(cc == 3)),
                                 r=[ones_b.res, sq.res], w=[pn.res])
                        k.op("act", lambda e: e.activation(out=rsn[:], in_=pn[:], func=AF.Ln, bias=cst[:, 0:1], scale=1.0 / 512),
                             r=[pn.res, cst.res], w=[rsn.res])
                        k.op("act", lambda e: e.activation(out=rsn[:], in_=rsn[:], func=AF.Exp, scale=-0.5), r=[rsn.res], w=[rsn.res])
                        for cc in range(4):
                            k.op("dve", lambda e, cc=cc, ob=ob: e.scalar_tensor_tensor(
                                out=yan[:, cc, :], in0=yaT[:, cc, :],
                                scalar=smalls[:, SM_GA + cc:SM_GA + cc + 1], in1=rsn[:], op0=ALU.mult, op1=ALU.mult),
                                r=[yaT.rs[cc], smalls.res, rsn.res], w=[yan.res])
                        for t in range(4):
                            tile = ob * 4 + t
                            for half in range(2):
                                pp = pj[pjc % 2]
                                pjc += 1
                                mm_group(pp[:], [(yan[:, c, t * 128:(t + 1) * 128], w_outA[:, c, half * 512:(half + 1) * 512]) for c in range(4)],
                                         r=[yan.res, w_outA.res], w=[pp.res])
                                k.op("dve", lambda e, pp=pp, tile=tile, half=half: e.tensor_tensor(
                                    out=x_res[:, tile, half * 512:(half + 1) * 512], in0=pp[:], in1=x_res[:, tile, half * 512:(half + 1) * 512],
                                    op=ALU.add), r=[pp.res, x_res.rs[tile]], w=[x_res.rs[tile]])
                    if b + 1 < nblk_pre + nblk_own:
                        front_proj(b + 1)
                k.barrier(release=[w_inA.res, w_outA.res, gbd_f.res, pflag.res] + [t_.res for t_ in xtmp])

            with ExitStack() as pb:
                BB = 256
                w_inB = sb(pb, "w_inB", [128, 8, 1536], BF16)
                load_w_bf16(w_inB, w_in_d[:, 1024:2560], 1536)
                w_out = sb(pb, "w_outB", [128, 4, D], BF16)
                for c in range(4):
                    k.dma("pool", w_out[:, c, :], w_out_d[512 + c * 128:512 + (c + 1) * 128, :], r=[], w=[w_out.res], sres=w_out.res,
                          max_dma_last_dim=4096)
                abias = sb(pb, "abias", [128, 8, 640], F32)
                k.dma("sp", abias[:], abias_d.rearrange("p (h c) -> p h c", h=8), r=[], w=[abias.res], sres=abias.res)
                halob = sb(pb, "halob", [128, 512], F32)
                k.dma("sp", halob[:], halob_d[:, :], r=[], w=[halob.res], sres=halob.res)
                xtmp = [sb(pb, "xtmpb%d" % i, [128, D], F32) for i in range(2)]
                hT = [sb(pb, "hTb%d" % i, [128, 8, BB], BF16) for i in range(2)]
                qA_l = [sb(pb, "qA%d" % i, [128, 4, BB], BF16) for i in range(2)]
                qB_l = [sb(pb, "qB%d" % i, [128, 4, BB], BF16) for i in range(2)]
                kT = [sb(pb, "kT%d" % i, [128, 4, BB], BF16) for i in range(4)]
                vpad = [sb(pb, "vpad%d" % i, [128, 8, 128], BF16) for i in range(8)]
                ybT = sb(pb, "ybT", [128, 4, BB], F32)
                ybn = sb(pb, "ybn", [128, 4, BB], BF16)
                sbuf_s = [sb(pb, "sbs%d" % i, [128, 640], F32) for i in range(2)]
                Pm = [sb(pb, "Pm%d" % i, [128, 640], BF16) for i in range(2)]
                Pn = [sb(pb, "Pn%d" % i, [128, 640], BF16) for i in range(2)]
                PT = [sb(pb, "PT%d" % i, [128, 5, 128], BF16) for i in range(2)]
                st = [dict(mx=sb(pb, "a_mx%d" % i, [128, 1], F32), rs=sb(pb, "a_rs%d" % i, [128, 1], F32),
                           ri=sb(pb, "a_ri%d" % i, [128, 1], F32)) for i in range(2)]
                sq4 = [sb(pb, "sqb%d" % i, [128, BB], BF16) for i in range(4)]
                rsn = sb(pb, "rsnb", [128, BB], F32)
                pj = [ps(pb, "pjb%d" % i, [128, 512], F32) for i in range(2)]
                pT = [ps(pb, "pTb%d" % i, [128, 8, 128], BF16) for i in range(1)]
                pSm = [ps(pb, "pSm%d" % i, [128, 512], F32) for i in range(2)]
                pSr = Tn(pb.enter_context(nc.psum_tensor("ps_pSr", [128, 4, 128], F32)), "pSr", nres=4)
                pPT = ps(pb, "pPT", [128, 5, 128], BF16)
                pO = ps(pb, "pO", [128, 4, 128], F32)
                pn = pj[0]
                for v_ in vpad:
                    k.op("pool", lambda e, v_=v_: e.memset(v_[:], 0.0), w=[v_.res])
                for q_ in qA_l + qB_l:
                    k.op("pool", lambda e, q_=q_: e.memset(q_[:], 0.0), w=[q_.res])
                set_gain(SM_MIX)

                nhalo = 512 // BB
                nown = NT // BB
                tcount = 0
                pjc = 0
                ac = 0
                def prep(b):
                    nonlocal tcount, pjc
                    own = b >= nhalo
                    ob = b - nhalo
                    h = hT[b % 2]
                    kcur = kT[b % 4]
                    qA, qB = qA_l[b % 2], qB_l[b % 2]
                    for t in range(2):
                        xt_ = xtmp[tcount % 2]
                        xa = xt_[:]
                        xr = xt_.res
                        if own:
                            r0 = ob * BB + t * 128
                            k.dma("sp", xa, xown[r0:r0 + 128, :], r=[], w=[xr], sres=xr)
                        else:
                            r0 = NPRE - 512 + b * BB + t * 128
                            k.dma("sp", xa, xprev[r0:r0 + 128, :], r=[], w=[xr], sres=xr)
                        yield from norm_T_g(xa, xr, pT[0], h[:, :, t * 128:(t + 1) * 128], h.res)
                        tcount += 1
                        yield
                    for m in range(4):
                        pp = pj[pjc % 2]
                        pjc += 1
                        mm_group(pp[:, 0:BB], [(w_inB[:, kc, 512 + m * 128:512 + (m + 1) * 128], h[:, kc, :]) for kc in range(8)],
                                 r=[w_inB.res, h.res], w=[pp.res])
                        k.op("act", lambda e, pp=pp, m=m, kcur=kcur: e.activation(out=kcur[:, m, :], in_=pp[:, 0:BB], func=AF.Copy),
                             r=[pp.res], w=[kcur.res])
                    yield
                    for t in range(2):
                        yield
                        gt = b * 2 + t
                        vp = vpad[gt % 8]
                        pp = pj[pjc % 2]
                        pjc += 1
                        mm_group(pp[:], [(h[:, kc, t * 128:(t + 1) * 128], w_inB[:, kc, 1024:1536]) for kc in range(8)],
                                 r=[w_inB.res, h.res], w=[pp.res])
                        ppv = pp[:].rearrange("p (h d) -> p h d", h=8)
                        k.op("act", lambda e, vp=vp, ppv=ppv: e.activation(out=vp[:, 0:8:2, 0:64], in_=ppv[:, 0:8:2, :], func=AF.Copy),
                             r=[pp.res], w=[vp.res])
                        k.op("act", lambda e, vp=vp, ppv=ppv: e.activation(out=vp[:, 1:8:2, 64:128], in_=ppv[:, 1:8:2, :], func=AF.Copy),
                             r=[pp.res], w=[vp.res])
                    if not own:
                        return
                    yield
                    for m in range(4):
                        if m == 2:
                            yield
                        pp = pj[pjc % 2]
                        pjc += 1
                        mm_group(pp[:, 0:BB], [(w_inB[:, kc, m * 128:(m + 1) * 128], h[:, kc, :]) for kc in range(8)],
                                 r=[w_inB.res, h.res], w=[pp.res])
                        k.op("act", lambda e, pp=pp, m=m: e.activation(out=qA[0:64, m, :], in_=pp[0:64, 0:BB], func=AF.Copy),
                             r=[pp.res], w=[qA.res])
                        k.op("act", lambda e, pp=pp, m=m: e.activation(out=qB[64:128, m, :], in_=pp[64:128, 0:BB], func=AF.Copy),
                             r=[pp.res], w=[qB.res])
                def attend(b, fillers):
                    nonlocal pjc, ac
                    ob = b - nhalo
                    qA, qB = qA_l[b % 2], qB_l[b % 2]
                    units = []
                    for p in range(2):
                        pieces = []
                        oc = 0
                        remaining = 640
                        pos = 128 * p
                        while remaining > 0:
                            bi = pos // BB
                            c0 = pos % BB
                            n = min(BB - c0, remaining)
                            if oc < 512 and oc + n > 512:
                                n = 512 - oc
                            pieces.append((kT[(b - 2 + bi) % 4], c0, n, oc))
                            oc += n
                            pos += n
                            remaining -= n
                        for hd in range(8):
                            units.append((p, hd, pieces))

                    def S1(u):
                        p, hd, pieces = units[u]
                        m = hd // 2
                        qs = (qA if hd % 2 == 0 else qB)
                        gi = ac0 + u
                        pm, pr = pSm[gi % 2], pSr
                        for (kt_, c0, n, oc) in pieces:
                            if oc < 512:
                                oap = pm[:, oc:oc + n]
                                ores = pm.res
                            else:
                                oap = pr[:, gi % 4, oc - 512:oc - 512 + n]
                                ores = pr.res
                            k.op("pe", lambda e, kt_=kt_, c0=c0, n=n, oap=oap: e.matmul(
                                oap, lhsT=qs[:, m, p * 128:(p + 1) * 128], rhs=kt_[:, m, c0:c0 + n],
                                start=True, stop=True), r=[qs.res, kt_.res], w=[ores])

                    def S2(u, part):
                        p, hd, pieces = units[u]
                        gi = ac0 + u
                        i2 = gi % 2
                        pm, pr = pSm[gi % 2], pSr
                        S, PP, PN, s_ = sbuf_s[i2], Pm[i2], Pn[i2], st[i2]
                        if part == 1:
                            k.op("dve", lambda e: e.reciprocal(out=s_["ri"][:], in_=s_["rs"][:]), r=[s_["rs"].res], w=[s_["ri"].res])
                            k.op("dve", lambda e: e.tensor_scalar(out=PN[:], in0=PP[:], scalar1=s_["ri"][:, 0:1],
                                                                  scalar2=None, op0=ALU.mult),
                                 r=[PP.res, s_["ri"].res], w=[PN.res])
                            return
                        k.op("dve", lambda e: e.scalar_tensor_tensor(
                            out=S[:, 0:512], in0=pm[:], scalar=0.125, in1=abias[:, hd, 0:512], op0=ALU.mult, op1=ALU.add),
                            r=[pm.res, abias.res], w=[S.res])
                        k.op("dve", lambda e: e.scalar_tensor_tensor(
                            out=S[:, 512:640], in0=pr[:, gi % 4, :], scalar=0.125, in1=abias[:, hd, 512:640], op0=ALU.mult, op1=ALU.add),
                            r=[pr.res, abias.res], w=[S.res])
                        cnt = 512 - ob * BB - 128 * p
                        if cnt > 0:
                            hb0 = ob * BB + 128 * p
                            k.op("dve", lambda e: e.tensor_tensor(
                                out=S[:, 0:cnt], in0=S[:, 0:cnt], in1=halob[:, hb0:512], op=ALU.add),
                                r=[S.res, halob.res], w=[S.res])
                        k.op("dve", lambda e: e.tensor_reduce(out=s_["mx"][:], in_=S[:], axis=AX.X, op=ALU.max, negate=True),
                             r=[S.res], w=[s_["mx"].res])
                        k.op("act", lambda e: e.activation(out=PP[:], in_=S[:], func=AF.Exp, bias=s_["mx"][:, 0:1],
                                                           scale=1.0, accum_out=s_["rs"][:]),
                             r=[S.res, s_["mx"].res], w=[PP.res, s_["rs"].res])

                    def S3(u):
                        p, hd, pieces = units[u]
                        m = hd // 2
                        gi = ac0 + u
                        i2 = gi % 2
                        PN, PTs = Pn[i2], PT[i2]
                        for kc in range(5):
                            k.op("pe", lambda e, kc=kc: e.transpose(out=pPT[:, kc, :], in_=PN[:, kc * 128:(kc + 1) * 128],
                                                                    identity=ident_b[:]),
                                 r=[PN.res, ident_b.res], w=[pPT.res])
                        k.op("act", lambda e: e.activation(out=PTs[:], in_=pPT[:], func=AF.Copy), r=[pPT.res], w=[PTs.res])
                        g0 = (b * 2 + p) - 4
                        for kc in range(5):
                            vp = vpad[(g0 + kc) % 8]
                            k.op("pe", lambda e, vp=vp, kc=kc: e.matmul(
                                pO[:, m, :], lhsT=vp[:, hd, :], rhs=PTs[:, kc, :],
                                start=(hd % 2 == 0 and kc == 0), stop=(hd % 2 == 1 and kc == 4)),
                                r=[vp.res, PTs.res], w=[pO.res])
                        if hd == 7:
                            k.op("act", lambda e: e.activation(out=ybT[:, :, p * 128:(p + 1) * 128], in_=pO[:], func=AF.Copy),
                                 r=[pO.res], w=[ybT.res])

                    def advance():
                        while fillers:
                            try:
                                next(fillers[0])
                                return
                            except StopIteration:
                                fillers.pop(0)

                    ac0 = ac
                    S1(0)
                    S1(1)
                    S2(0, 0)
                    for u in range(len(units)):
                        if u + 1 < len(units):
                            S2(u + 1, 0)
                        S2(u, 1)
                        if u + 2 < len(units):
                            S1(u + 2)
                        S3(u)
                        advance()
                    while fillers:
                        advance()
                    ac += len(units)

                def tail(b):
                    nonlocal pjc
                    ob = b - nhalo
                    for cc in range(4):
                        k.op("dve", lambda e, cc=cc: e.tensor_tensor(out=sq4[cc][:], in0=ybT[:, cc, :], in1=ybT[:, cc, :], op=ALU.mult),
                             r=[ybT.res], w=[sq4[cc].res])
                    for cc in range(4):
                        k.op("pe", lambda e, cc=cc: e.matmul(pn[:, 0:BB], lhsT=ones_b[:], rhs=sq4[cc][:], start=(cc == 0), stop=(cc == 3)),
                             r=[ones_b.res, sq4[cc].res], w=[pn.res])
                    yield
                    k.op("act", lambda e: e.activation(out=rsn[:], in_=pn[:, 0:BB], func=AF.Ln, bias=cst[:, 0:1], scale=1.0 / 512),
                         r=[pn.res, cst.res], w=[rsn.res])
                    k.op("act", lambda e: e.activation(out=rsn[:], in_=rsn[:], func=AF.Exp, scale=-0.5), r=[rsn.res], w=[rsn.res])
                    yield
                    for cc in range(4):
                        k.op("dve", lambda e, cc=cc: e.scalar_tensor_tensor(
                            out=ybn[:, cc, :], in0=ybT[:, cc, :], scalar=smalls[:, SM_GB + cc:SM_GB + cc + 1], in1=rsn[:],
                            op0=ALU.mult, op1=ALU.mult), r=[ybT.res, smalls.res, rsn.res], w=[ybn.res])
                    yield
                    for t in range(2):
                        tile = ob * 2 + t
                        tok0 = ob * BB + t * 128
                        for half in range(2):
                            pp = pj[pjc % 2]
                            pjc += 1
                            pairs = [(ybn[:, c, t * 128:(t + 1) * 128], w_out[:, c, half * 512:(half + 1) * 512]) for c in range(4)]
                            mm_group(pp[:], pairs, r=[ybn.res, w_out.res], w=[pp.res])
                            k.op("dve", lambda e, pp=pp, tile=tile, half=half: e.tensor_tensor(
                                out=x_res[:, tile, half * 512:(half + 1) * 512], in0=pp[:], in1=x_res[:, tile, half * 512:(half + 1) * 512],
                                op=ALU.add), r=[pp.res, x_res.rs[tile]], w=[x_res.rs[tile]])
                        yield
                nb_ = nhalo + nown
                for b0 in range(nhalo + 1):
                    for _ in prep(b0):
                        pass
                for b in range(nhalo, nb_):
                    fl = []
                    if b - 1 >= nhalo:
                        fl.append(tail(b - 1))
                    if b + 1 < nb_:
                        fl.append(prep(b + 1))
                    attend(b, fl)
                for _ in tail(nb_ - 1):
                    pass
                k.barrier(release=[w_inB.res, w_out.res, abias.res, halob.res] + [t_.res for t_ in xtmp])

        if debug:
            for tile in range(NTILE):
                k.dma("sp", dbg["dbg1"][tile * 128:(tile + 1) * 128, :], x_res[:, tile, :], r=[x_res.rs[tile]], w=[], sres=x_res.rs[tile])

        with ExitStack() as p2:
            B2 = 512
            w_q = sb(p2, "w_q", [128, 8, D], BF16)
            load_w_bf16(w_q, w_q_d[:, :], D)
            w_o = sb(p2, "w_o", [128, 8, D], BF16)
            load_w_bf16(w_o, w_o_d[:, :], D)
            kmT = sb(p2, "kmT", [128, 8, 256], BF16)
            vmem = sb(p2, "vmem", [128, 2, D], BF16)
            pj = [ps(p2, "pj2%d" % i, [128, 512], F32) for i in range(2)]
            pT = [ps(p2, "pT2%d" % i, [128, 8, 128], BF16) for i in range(1)]
            pS2 = [ps(p2, "pS2%d" % i, [128, 4, 256], F32) for i in range(2)]
            pPT2 = ps(p2, "pPT2", [128, 8, 128], BF16)
            pjc = 0
            with ExitStack() as p2s:
                w_kv = sb(p2s, "w_kv", [128, 8, 2048], BF16)
                load_w_bf16(w_kv, w_kv_d[:, :], 2048)
                memt = [sb(p2s, "memt%d" % i, [128, D], F32) for i in range(2)]
                memT = sb(p2s, "memT", [128, 8, 256], BF16)
                set_gain(SM_MEM)
                for t in range(2):
                    k.dma("sp", memt[t][:], mem_d[t * 128:(t + 1) * 128, :], r=[], w=[memt[t].res], sres=memt[t].res)
                    norm_T(memt[t][:], memt[t].res, pT[0], memT[:, :, t * 128:(t + 1) * 128], memT.res)
                for oc in range(8):
                    pp = pj[pjc % 2]
                    pjc += 1
                    mm_group(pp[:, 0:256], [(w_kv[:, kc, oc * 128:(oc + 1) * 128], memT[:, kc, :]) for kc in range(8)],
                             r=[w_kv.res, memT.res], w=[pp.res])
                    k.op("act", lambda e, pp=pp, oc=oc: e.activation(out=kmT[:, oc, :], in_=pp[:, 0:256], func=AF.Copy),
                         r=[pp.res], w=[kmT.res])
                for t in range(2):
                    for half in range(2):
                        pp = pj[pjc % 2]
                        pjc += 1
                        mm_group(pp[:], [(memT[:, kc, t * 128:(t + 1) * 128], w_kv[:, kc, 1024 + half * 512:1024 + (half + 1) * 512])
                                         for kc in range(8)], r=[w_kv.res, memT.res], w=[pp.res])
                        k.op("act", lambda e, pp=pp, t=t, half=half: e.activation(out=vmem[:, t, half * 512:(half + 1) * 512], in_=pp[:], func=AF.Copy),
                             r=[pp.res], w=[vmem.res])
                k.barrier(release=[w_kv.res] + [t_.res for t_ in memt])
            hT = [sb(p2, "hT2%d" % i, [128, 8, B2], BF16) for i in range(2)]
            qT = sb(p2, "qT2", [128, 8, B2], BF16)
            P2 = [sb(p2, "P2%d" % i, [128, 4, 256], BF16) for i in range(2)]
            P2n = [sb(p2, "P2n%d" % i, [128, 4, 256], BF16) for i in range(2)]
            PT2 = sb(p2, "PT2", [128, 4, 2, B2], BF16)
            oT = sb(p2, "oT2", [128, 8, B2], BF16)
            st2 = [dict(mx=sb(p2, "c_mx%d" % i, [128, 4], F32), rs=sb(p2, "c_rs%d" % i, [128, 4], F32),
                        ri=sb(p2, "c_ri%d" % i, [128, 4], F32)) for i in range(2)]
            set_gain(SM_CROSS)
            tc2 = 0
            for b in range(NT // B2):
                h = hT[b % 2]
                for t in range(4):
                    norm_T(x_res[:, b * 4 + t, :], x_res.rs[b * 4 + t], pT[0], h[:, :, t * 128:(t + 1) * 128], h.res)
                for oc in range(8):
                    pp = pj[pjc % 2]
                    pjc += 1
                    mm_group(pp[:], [(w_q[:, kc, oc * 128:(oc + 1) * 128], h[:, kc, :]) for kc in range(8)],
                             r=[w_q.res, h.res], w=[pp.res])
                    k.op("act", lambda e, pp=pp, oc=oc: e.activation(out=qT[:, oc, :], in_=pp[:], func=AF.Copy), r=[pp.res], w=[qT.res])
                def c_S1(t, i2):
                    pS_ = pS2[i2]
                    for hh_ in range(4):
                        mm_group(pS_[:, hh_, :], [(qT[:, 2 * hh_ + j, t * 128:(t + 1) * 128], kmT[:, 2 * hh_ + j, :]) for j in range(2)],
                                 r=[qT.res, kmT.res], w=[pS_.res])

                def c_S2(t, i2):
                    pS_, P_, Pn_, s_ = pS2[i2], P2[i2], P2n[i2], st2[i2]
                    k.op("dve", lambda e: e.tensor_reduce(out=s_["mx"][:], in_=pS_[:], axis=AX.X, op=ALU.max, negate=True),
                         r=[pS_.res], w=[s_["mx"].res])
                    k.op("dve", lambda e: e.tensor_scalar(out=s_["mx"][:], in0=s_["mx"][:], scalar1=1.0 / 16, scalar2=None, op0=ALU.mult),
                         r=[s_["mx"].res], w=[s_["mx"].res])
                    for hh_ in range(4):
                        k.op("act", lambda e, hh_=hh_: e.activation(
                            out=P_[:, hh_, :], in_=pS_[:, hh_, :], func=AF.Exp, bias=s_["mx"][:, hh_:hh_ + 1], scale=1.0 / 16,
                            accum_out=s_["rs"][:, hh_:hh_ + 1]), r=[pS_.res, s_["mx"].res], w=[P_.res, s_["rs"].res])
                    k.op("dve", lambda e: e.reciprocal(out=s_["ri"][:], in_=s_["rs"][:]), r=[s_["rs"].res], w=[s_["ri"].res])
                    k.op("dve", lambda e: e.tensor_tensor(
                        out=Pn_[:], in0=P_[:], in1=s_["ri"][:, :].unsqueeze(2).to_broadcast([128, 4, 256]), op=ALU.mult),
                        r=[P_.res, s_["ri"].res], w=[Pn_.res])

                def c_S3(t, i2):
                    Pn_ = P2n[i2]
                    for hh_ in range(4):
                        for mc in range(2):
                            k.op("pe", lambda e, hh_=hh_, mc=mc: e.transpose(
                                out=pPT2[:, hh_ * 2 + mc, :], in_=Pn_[:, hh_, mc * 128:(mc + 1) * 128], identity=ident_b[:]),
                                r=[Pn_.res, ident_b.res], w=[pPT2.res])
                    k.op("act", lambda e: e.activation(out=PT2[:, :, :, t * 128:(t + 1) * 128],
                                                       in_=pPT2[:].rearrange("p (h m) t -> p h m t", h=4), func=AF.Copy),
                         r=[pPT2.res], w=[PT2.res])

                c_S1(0, tc2 % 2)
                for t in range(4):
                    if t + 1 < 4:
                        c_S1(t + 1, (tc2 + 1) % 2)
                    c_S2(t, tc2 % 2)
                    c_S3(t, tc2 % 2)
                    tc2 += 1
                for oc in range(8):
                    pp = pj[pjc % 2]
                    pjc += 1
                    mm_group(pp[:], [(vmem[:, mc, oc * 128:(oc + 1) * 128], PT2[:, oc // 2, mc, :]) for mc in range(2)],
                             r=[vmem.res, PT2.res], w=[pp.res])
                    k.op("act", lambda e, pp=pp, oc=oc: e.activation(out=oT[:, oc, :], in_=pp[:], func=AF.Copy), r=[pp.res], w=[oT.res])
                for t in range(4):
                    tile = b * 4 + t
                    for half in range(2):
                        pp = pj[pjc % 2]
                        pjc += 1
                        mm_group(pp[:], [(oT[:, c, t * 128:(t + 1) * 128], w_o[:, c, half * 512:(half + 1) * 512]) for c in range(8)],
                                 r=[oT.res, w_o.res], w=[pp.res])
                        k.op("dve", lambda e, pp=pp, tile=tile, half=half: e.tensor_tensor(
                            out=x_res[:, tile, half * 512:(half + 1) * 512], in0=pp[:], in1=x_res[:, tile, half * 512:(half + 1) * 512],
                            op=ALU.add), r=[pp.res, x_res.rs[tile]], w=[x_res.rs[tile]])
            k.barrier(release=[w_q.res, w_o.res])

        if debug:
            for tile in range(NTILE):
                k.dma("sp", dbg["dbg2"][tile * 128:(tile + 1) * 128, :], x_res[:, tile, :], r=[x_res.rs[tile]], w=[], sres=x_res.rs[tile])

        with ExitStack() as p3:
            p3s = p3.enter_context(ExitStack())
            slotI = sb(p3s, "slotI", [128, NT], F32)
            slotJ = sb(p3s, "slotJ", [128, NT], F32)
            slotG = sb(p3s, "slotG", [128, NT], F32)
            set_gain(SM_FFN)
            with ExitStack() as pa:
                B3 = 256
                w_qry = sb(pa, "w_qry", [128, 8, 2048], BF16)
                load_w_bf16(w_qry, w_qry_d[:, :], 2048)
                skb = sb(pa, "skb", [128, 16, 128], BF16)
                k.dma("pool", skb[:], skT_d.rearrange("p (o n) -> p o n", o=16), r=[], w=[skb.res], sres=skb.res)
                hT = [sb(pa, "hT3%d" % i, [128, 8, B3], BF16) for i in range(2)]
                qpT = sb(pa, "qpT", [128, 16, B3], BF16)
                sc = sb(pa, "sc", [128, 16, 128], F32, nres=4)
                sc2 = sb(pa, "sc2", [128, 16, 128], F32, nres=16)
                tv = sb(pa, "tv", [128, 16, 16], F32, nres=16)
                tiu = sb(pa, "tiu", [128, 16, 16], U32, nres=16)
                tif = sb(pa, "tif", [128, 16, 16], F32)
                cand = sb(pa, "cand", [128, 8, 256], F32)
                cand2 = sb(pa, "cand2", [128, 8, 256], F32, nres=8)
                ts = sb(pa, "ts", [128, 8, 16], F32, nres=8)
                posu = sb(pa, "posu", [128, 8, 16], U32, nres=8)
                au = sb(pa, "au", [128, 8, 16], U32)
                bu = sb(pa, "bu", [128, 8, 16], U32)
                af = sb(pa, "af", [128, 8, 16], F32)
                bf = sb(pa, "bf", [128, 8, 16], F32)
                eq = sb(pa, "eq", [128, 8, 16, 16], F32)
                isel = sb(pa, "isel", [128, 8, 16], F32)
                jsel = sb(pa, "jsel", [128, 8, 16], F32)
                gsel = sb(pa, "gsel", [128, 8, 16], F32)
                sm = sb(pa, "sm", [128, 8], F32)
                pj = [ps(pa, "pj3%d" % i, [128, 512], F32) for i in range(2)]
                pT = [ps(pa, "pT3%d" % i, [128, 8, 128], BF16) for i in range(2)]
                psc = [ps(pa, "psc%d" % i, [128, 4, 128], F32) for i in range(2)]
                pTs = ps(pa, "pTs", [128, 3, 128], F32)
                pjc = 0
                scc = 0
                iota16 = iota_f[:, 0:16]
                for b in range(NT // B3):
                    h = hT[b % 2]
                    norm_pairs([(x_res[:, b * 2 + t, :], x_res.rs[b * 2 + t], pT[t % 2], h[:, :, t * 128:(t + 1) * 128], h.res) for t in range(2)])
                    for oc in range(16):
                        pp = pj[pjc % 2]
                        pjc += 1
                        mm_group(pp[:, 0:B3], [(w_qry[:, kc, oc * 128:(oc + 1) * 128], h[:, kc, :]) for kc in range(8)],
                                 r=[w_qry.res, h.res], w=[pp.res])
                        k.op("act", lambda e, pp=pp, oc=oc: e.activation(out=qpT[:, oc, :], in_=pp[:, 0:B3], func=AF.Copy),
                             r=[pp.res], w=[qpT.res])
                    for t in range(2):
                        tile = b * 2 + t
                        for g4 in range(4):
                            pq = psc[scc % 2]
                            scc += 1
                            for j in range(4):
                                oc = g4 * 4 + j
                                k.op("pe", lambda e, pq=pq, j=j, oc=oc, t=t: e.matmul(
                                    pq[:, j, :], lhsT=qpT[:, oc, t * 128:(t + 1) * 128], rhs=skb[:, oc, :], start=True, stop=True),
                                    r=[qpT.res, skb.res], w=[pq.res])
                            k.op("act", lambda e, pq=pq, g4=g4: e.activation(out=sc[:, g4 * 4:(g4 + 1) * 4, :], in_=pq[:], func=AF.Copy),
                                 r=[pq.res], w=[sc.rs[g4]])
                        for oc in range(16):
                            k.op("dve", lambda e, oc=oc: e.max(out=tv[:, oc, 0:8], in_=sc[:, oc, :]), r=[sc.rs[oc // 4]], w=[tv.rs[oc]])
                        for oc in range(16):
                            k.op("dve", lambda e, oc=oc: e.max_index(out=tiu[:, oc, 0:8], in_max=tv[:, oc, 0:8], in_values=sc[:, oc, :]),
                                 r=[sc.rs[oc // 4], tv.rs[oc]], w=[tiu.rs[oc]])
                        for oc in range(16):
                            k.op("dve", lambda e, oc=oc: e.match_replace(out=sc2[:, oc, :], in_to_replace=tv[:, oc, 0:8], in_values=sc[:, oc, :],
                                                                        imm_value=NEG), r=[sc.rs[oc // 4], tv.rs[oc]], w=[sc2.rs[oc]])
                        for oc in range(16):
                            k.op("dve", lambda e, oc=oc: e.max(out=tv[:, oc, 8:16], in_=sc2[:, oc, :]), r=[sc2.rs[oc]], w=[tv.rs[oc]])
                        for oc in range(16):
                            k.op("dve", lambda e, oc=oc: e.max_index(out=tiu[:, oc, 8:16], in_max=tv[:, oc, 8:16], in_values=sc2[:, oc, :]),
                                 r=[sc2.rs[oc], tv.rs[oc]], w=[tiu.rs[oc]])
                        k.op("dve", lambda e: e.tensor_copy(out=tif[:], in_=tiu[:]), r=tiu.rs, w=[tif.res])
                        tv4 = tv[:].rearrange("p (h two) a -> p h two a", two=2)
                        tif4 = tif[:].rearrange("p (h two) a -> p h two a", two=2)
                        k.op("dve", lambda e, tv4=tv4: e.tensor_tensor(
                            out=cand[:].rearrange("p h (a b) -> p h a b", a=16),
                            in0=tv4[:, :, 0, :].unsqueeze(3).to_broadcast([128, 8, 16, 16]),
                            in1=tv4[:, :, 1, :].unsqueeze(2).to_broadcast([128, 8, 16, 16]), op=ALU.add),
                            r=tv.rs, w=[cand.res])
                        for hd in range(8):
                            k.op("dve", lambda e, hd=hd: e.max(out=ts[:, hd, 0:8], in_=cand[:, hd, :]), r=[cand.res], w=[ts.rs[hd]])
                        for hd in range(8):
                            k.op("dve", lambda e, hd=hd: e.max_index(out=posu[:, hd, 0:8], in_max=ts[:, hd, 0:8], in_values=cand[:, hd, :]),
                                 r=[cand.res, ts.rs[hd]], w=[posu.rs[hd]])
                        for hd in range(8):
                            k.op("dve", lambda e, hd=hd: e.match_replace(out=cand2[:, hd, :], in_to_replace=ts[:, hd, 0:8], in_values=cand[:, hd, :],
                                                                        imm_value=NEG), r=[cand.res, ts.rs[hd]], w=[cand2.rs[hd]])
                        for hd in range(8):
                            k.op("dve", lambda e, hd=hd: e.max(out=ts[:, hd, 8:16], in_=cand2[:, hd, :]), r=[cand2.rs[hd]], w=[ts.rs[hd]])
                        for hd in range(8):
                            k.op("dve", lambda e, hd=hd: e.max_index(out=posu[:, hd, 8:16], in_max=ts[:, hd, 8:16], in_values=cand2[:, hd, :]),
                                 r=[cand2.rs[hd], ts.rs[hd]], w=[posu.rs[hd]])
                        k.op("dve", lambda e: e.tensor_scalar(out=au[:], in0=posu[:], scalar1=4, scalar2=None, op0=ALU.logical_shift_right),
                             r=posu.rs, w=[au.res])
                        k.op("dve", lambda e: e.tensor_scalar(out=bu[:], in0=posu[:], scalar1=15, scalar2=None, op0=ALU.bitwise_and),
                             r=posu.rs, w=[bu.res])
                        k.op("dve", lambda e: e.tensor_copy(out=af[:], in_=au[:]), r=[au.res], w=[af.res])
                        k.op("dve", lambda e: e.tensor_copy(out=bf[:], in_=bu[:]), r=[bu.res], w=[bf.res])
                        for (sel, rk, which) in ((isel, af, 0), (jsel, bf, 1)):
                            k.op("dve", lambda e, rk=rk: e.tensor_tensor(
                                out=eq[:], in0=rk[:].unsqueeze(3).to_broadcast([128, 8, 16, 16]),
                                in1=iota16.unsqueeze(1).unsqueeze(1).to_broadcast([128, 8, 16, 16]), op=ALU.is_equal),
                                r=[rk.res, iota_f.res], w=[eq.res])
                            k.op("dve", lambda e, which=which, tif4=tif4: e.tensor_tensor(
                                out=eq[:], in0=eq[:], in1=tif4[:, :, which, :].unsqueeze(2).to_broadcast([128, 8, 16, 16]), op=ALU.mult),
                                r=[eq.res, tif.res], w=[eq.res])
                            k.op("dve", lambda e, sel=sel: e.tensor_reduce(out=sel[:], in_=eq[:], axis=AX.X, op=ALU.add),
                                 r=[eq.res], w=[sel.res])
                        k.op("dve", lambda e: e.tensor_tensor(out=gsel[:], in0=ts[:], in1=ts[:, :, 0:1].to_broadcast([128, 8, 16]), op=ALU.subtract),
                             r=ts.rs, w=[gsel.res])
                        k.op("act", lambda e: e.activation(out=gsel[:], in_=gsel[:], func=AF.Exp), r=[gsel.res], w=[gsel.res])
                        k.op("dve", lambda e: e.tensor_reduce(out=sm[:], in_=gsel[:], axis=AX.X, op=ALU.add), r=[gsel.res], w=[sm.res])
                        k.op("dve", lambda e: e.reciprocal(out=sm[:], in_=sm[:]), r=[sm.res], w=[sm.res])
                        k.op("dve", lambda e: e.tensor_tensor(out=gsel[:], in0=gsel[:], in1=sm[:, :].unsqueeze(2).to_broadcast([128, 8, 16]), op=ALU.mult),
                             r=[gsel.res, sm.res], w=[gsel.res])
                        for i3, (src, dst) in enumerate(((isel, slotI), (jsel, slotJ), (gsel, slotG))):
                            k.op("pe", lambda e, i3=i3, src=src: e.transpose(out=pTs[:, i3, :], in_=src[:].rearrange("p h r -> p (h r)"),
                                                                            identity=ident_f[:]),
                                 r=[src.res, ident_f.res], w=[pTs.res])
                        for i3, (src, dst) in enumerate(((isel, slotI), (jsel, slotJ), (gsel, slotG))):
                            k.op("act", lambda e, i3=i3, dst=dst, tile=tile: e.activation(out=dst[:, tile * 128:(tile + 1) * 128], in_=pTs[:, i3, :], func=AF.Copy),
                                 r=[pTs.res], w=[dst.res])
                k.barrier(release=[w_qry.res, skb.res])

            with ExitStack() as pb:
                TCH = 8
                ohj = [sb(pb, "ohj%d" % i, [128, TCH, 128], BF16) for i in range(4)]
                eqi = [sb(pb, "eqi%d" % i, [128, TCH, 128], BF16) for i in range(4)]
                rig = [sb(pb, "rig%d" % i, [128, TCH, 128], BF16) for i in range(4)]
                Wst = [sb(pb, "Wst%d" % i, [128, 128, 128], BF16, nres=32) for i in range(2)]
                pW = [ps(pb, "pW%d" % i, [128, 4, 128], F32) for i in range(4)]
                wc = 0
                chc = 0
                iota_b3 = iota_f[:, :].unsqueeze(1).to_broadcast([128, TCH, 128])
                for tile in range(NTILE):
                    W_ = Wst[tile % 2]
                    for ch in range(128 // TCH):
                        oj, ei, rg = ohj[chc % 4], eqi[chc % 4], rig[chc % 4]
                        chc += 1
                        tok0 = tile * 128 + ch * TCH
                        k.op("dve", lambda e, oj=oj, tok0=tok0: e.tensor_tensor(
                            out=oj[:], in0=iota_b3, in1=slotJ[:, tok0:tok0 + TCH].unsqueeze(2).to_broadcast([128, TCH, 128]), op=ALU.is_equal),
                            r=[iota_f.res, slotJ.res], w=[oj.res])
                        k.op("dve", lambda e, ei=ei, tok0=tok0: e.tensor_tensor(
                            out=ei[:], in0=iota_b3, in1=slotI[:, tok0:tok0 + TCH].unsqueeze(2).to_broadcast([128, TCH, 128]), op=ALU.is_equal),
                            r=[iota_f.res, slotI.res], w=[ei.res])
                        k.op("pool", lambda e, ei=ei, rg=rg, tok0=tok0: e.tensor_tensor(
                            out=rg[:], in0=ei[:], in1=slotG[:, tok0:tok0 + TCH].unsqueeze(2).to_broadcast([128, TCH, 128]), op=ALU.mult),
                            r=[ei.res, slotG.res], w=[rg.res])
                        for q4 in range(TCH // 4):
                            pw = pW[wc % 4]
                            for j in range(4):
                                tt_ = q4 * 4 + j
                                k.op("pe", lambda e, pw=pw, j=j, oj=oj, rg=rg, tt_=tt_: e.matmul(
                                    pw[:, j, :], lhsT=oj[:, tt_, :], rhs=rg[:, tt_, :], start=True, stop=True),
                                    r=[oj.res, rg.res], w=[pw.res])
                            t0 = ch * TCH + q4 * 4
                            if True:
                                k.op("act", lambda e, pw=pw, W_=W_, t0=t0: e.activation(
                                    out=W_[:, :, t0:t0 + 4], in_=pw[:].rearrange("p t i -> p i t"), func=AF.Copy),
                                    r=[pw.res], w=[W_.rs[t0 // 4]])
                            else:
                                k.op("dve", lambda e, pw=pw, W_=W_, t0=t0: e.tensor_copy(
                                    out=W_[:, :, t0:t0 + 4], in_=pw[:].rearrange("p t i -> p i t")),
                                    r=[pw.res], w=[W_.rs[t0 // 4]])
                            wc += 1
                    k.dma("sp", wd_d[tile], W_[:].rearrange("p i t -> p (i t)"), r=W_.rs, w=[], sres=W_.res)
                k.barrier(release=[w_.res for w_ in Wst])
            p3s.close()

            with ExitStack() as pc:
                T3 = 256
                GS = 8
                NG = 128 // GS
                hfT = sb(pc, "hfT", [128, 8, NT], BF16)
                pT = [ps(pc, "pT4%d" % i, [128, 8, 128], BF16) for i in range(1)]
                pA = [ps(pc, "pA%d" % i, [128, 512], F32) for i in range(3)]
                pO = [ps(pc, "pO4%d" % i, [128, 512], F32) for i in range(4)]
                for tile in range(NTILE):
                    norm_T(x_res[:, tile, :], x_res.rs[tile], pT[0], hfT[:, :, tile * 128:(tile + 1) * 128], hfT.res)
                ut = [sb(pc, "ut%d" % i, [128, GS, 8, 128], BF16) for i in range(2)]
                vt = [sb(pc, "vt%d" % i, [128, GS, D], BF16) for i in range(2)]
                wsl = [sb(pc, "wsl%d" % i, [128, 2, GS, 128], BF16) for i in range(2)]
                ge = [sb(pc, "ge%d" % i, [128, T3], F32) for i in range(3)]
                gw = [sb(pc, "gw%d" % i, [128, T3], BF16) for i in range(3)]
                uT_v = uT_d.rearrange("(i p) (c e) -> p i c e", p=128, c=8)
                ev_v = ev_d.rearrange("(i e) d -> e i d", e=128)
                wd_v = wd_d.rearrange("t j (i x) -> j t i x", i=128)
                items = [(g, tb, i) for g in range(NG) for tb in range(NT // T3) for i in range(GS)]
                state = {}

                def emitA(n):
                    g, tb, i = items[n]
                    u_, v_ = ut[g % 2], vt[g % 2]
                    if tb == 0 and i == 0:
                        for i_ in range(GS):
                            k.dma("pool", u_[:, i_, :, :], uT_v[:, g * GS + i_, :, :], r=[], w=[u_.res], sres=u_.res)
                            k.dma("pool", v_[:, i_, :], ev_v[:, g * GS + i_, :], r=[], w=[v_.res], sres=v_.res, max_dma_last_dim=4096)
                    if i == 0:
                        ws_ = wsl[(g * (NT // T3) + tb) % 2]
                        k.dma("sp", ws_[:], wd_v[:, 2 * tb:2 * tb + 2, g * GS:(g + 1) * GS, :], r=[], w=[ws_.res], sres=ws_.res)
                    ws_ = wsl[(g * (NT // T3) + tb) % 2]
                    pa_ = pA[n % 3]
                    ge_, gw_ = ge[n % 3], gw[n % 3]
                    mm_group(pa_[:, 0:T3], [(u_[:, i, c, :], hfT[:, c, tb * T3:(tb + 1) * T3]) for c in range(8)],
                             r=[u_.res, hfT.res], w=[pa_.res])
                    k.op("act", lambda e: e.activation(out=ge_[:], in_=pa_[:, 0:T3], func=AF.Gelu_apprx_tanh),
                         r=[pa_.res], w=[ge_.res])
                    k.op("dve", lambda e: e.tensor_tensor(
                        out=gw_[:].rearrange("p (a t) -> p a t", a=2), in0=ge_[:].rearrange("p (a t) -> p a t", a=2),
                        in1=ws_[:, :, i, :], op=ALU.mult), r=[ge_.res, ws_.res], w=[gw_.res])

                def emitV(n):
                    g, tb, i = items[n]
                    v_ = vt[g % 2]
                    gw_ = gw[n % 3]
                    for t in range(2):
                        for half in range(2):
                            po = pO[t * 2 + half]
                            k.op("pe", lambda e, po=po, t=t, half=half: e.matmul(
                                po[:], lhsT=gw_[:, t * 128:(t + 1) * 128], rhs=v_[:, i, half * 512:(half + 1) * 512],
                                start=(i == 0), stop=(i == GS - 1)), r=[gw_.res, v_.res], w=[po.res])
                    if i == GS - 1:
                        for t in range(2):
                            tile = tb * 2 + t
                            for half in range(2):
                                po = pO[t * 2 + half]
                                k.op("dve", lambda e, po=po, tile=tile, half=half: e.tensor_tensor(
                                    out=x_res[:, tile, half * 512:(half + 1) * 512], in0=po[:], in1=x_res[:, tile, half * 512:(half + 1) * 512],
                                    op=ALU.add), r=[po.res, x_res.rs[tile]], w=[x_res.rs[tile]])

                emitA(0)
                emitA(1)
                for n in range(len(items)):
                    if n + 2 < len(items):
                        emitA(n + 2)
                    emitV(n)
                k.barrier(release=[t_.res for t_ in ut + vt + wsl])

        with ExitStack() as p4:
            gfin = sb(p4, "gfin", [128, D], F32)
            k.dma("sp", gfin[:], gfin_d[:, :], r=[], w=[gfin.res], sres=gfin.res)
            ot = [sb(p4, "ot%d" % i, [128, D], F32) for i in range(2)]
            for tile in range(NTILE):
                n = nrm[tile % 2]
                o_ = ot[tile % 2]
                xa = x_res[:, tile, :]
                xr = x_res.rs[tile]
                k.op("dve", lambda e, n=n, xa=xa: e.scalar_tensor_tensor(out=n["junk"][:], in0=xa, scalar=1.0, in1=xa,
                                                                         op0=ALU.mult, op1=ALU.mult, accum_out=n["ss"][:]),
                     r=[xr], w=[n["junk"].res, n["ss"].res])
                k.op("act", lambda e, n=n: e.activation(out=n["sd"][:], in_=n["ss"][:], func=AF.Ln, bias=cst[:, 0:1], scale=1.0 / D),
                     r=[n["ss"].res, cst.res], w=[n["sd"].res])
                k.op("act", lambda e, n=n: e.activation(out=n["rstd"][:], in_=n["sd"][:], func=AF.Exp, scale=-0.5),
                     r=[n["sd"].res], w=[n["rstd"].res])
                k.op("dve", lambda e, n=n, xa=xa, o_=o_: e.scalar_tensor_tensor(out=o_[:], in0=xa, scalar=n["rstd"][:, 0:1], in1=gfin[:],
                                                                                op0=ALU.mult, op1=ALU.mult),
                     r=[xr, n["rstd"].res, gfin.res], w=[o_.res])
                k.dma("sp", y_d[tile * 128:(tile + 1) * 128, :], o_[:], r=[o_.res], w=[], sres=o_.res)
            k.barrier()
    return nc, k.n_ins


def prep_shared(inp):
    f = np.float32
    g = lambda a: np.ascontiguousarray(np.asarray(a, dtype=f))
    sm = np.zeros((128, NSM), f)

    def put(col, vec, nch):
        sm[:, col:col + nch] = np.asarray(vec, f).reshape(nch, 128).T

    put(SM_MIX, inp["norm_mix"][0], 8)
    put(SM_CROSS, inp["norm_cross"][0], 8)
    put(SM_MEM, inp["norm_mem"][0], 8)
    put(SM_FFN, inp["norm_ffn"][0], 8)
    put(SM_GA, inp["norm_grp_a"][0], 4)
    put(SM_GB, inp["norm_grp_b"][0], 4)
    put(SM_CB, inp["conv_b"][0], 4)
    put(SM_BA, inp["gate_a_b"][0], 4)
    put(SM_BX, inp["gate_x_b"][0], 4)
    put(SM_LAM, inp["lru_lambda"][0], 4)
    cw = np.asarray(inp["conv_w"][0], f)
    for cc in range(4):
        for j in range(4):
            sm[:, SM_CW + cc * 4 + j] = cw[j, cc * 128:(cc + 1) * 128]
    gbd = np.zeros((128, 8, 128), f)
    for gi, key in enumerate(("gate_a_w", "gate_x_w")):
        w = np.asarray(inp[key][0], f)
        for cc in range(4):
            gbd[0:64, gi * 4 + cc, 0:64] = w[2 * cc]
            gbd[64:128, gi * 4 + cc, 64:128] = w[2 * cc + 1]
    rb = np.asarray(inp["rel_bias"][0], f)
    qi = np.arange(128)[:, None]
    kj = np.arange(640)[None, :]
    idx = np.clip(512 + qi - kj, -128, 128) + 128
    ab = rb[:, idx]
    valid = np.where(qi < 64, kj < 576, kj >= 64)
    ab = np.where(valid[None], ab, f(NEG)).astype(f)
    abias = np.ascontiguousarray(ab.transpose(1, 0, 2)).reshape(128, 8 * 640)
    sk = np.asarray(inp["sub_keys"][0], f)
    skT = np.ascontiguousarray(sk.reshape(16, 128, 128).transpose(2, 0, 1)).reshape(128, 16 * 128)
    u = np.asarray(inp["expert_u"][0], f)
    uT = np.ascontiguousarray(u.reshape(128, 128, 8, 128).transpose(0, 3, 2, 1)).reshape(16384, D)
    shared = {
        "w_in": g(inp["w_in"][0]), "w_out": g(inp["w_out"][0]), "w_q": g(inp["w_q_mem"][0]),
        "w_kv": g(inp["w_kv_mem"][0]), "w_o": g(inp["w_o_mem"][0]), "w_qry": g(inp["w_query"][0]),
        "smalls": sm, "gbd": gbd.reshape(128, 8 * 128), "abias": abias, "skT": skT, "uT": uT,
        "ev": g(inp["expert_v"][0]),
        "gfin": np.ascontiguousarray(np.broadcast_to(np.asarray(inp["norm_final"], f)[None, :], (128, D))),
        "ident": np.eye(128, dtype=f),
        "iota": np.ascontiguousarray(np.broadcast_to(np.arange(128, dtype=f)[None, :], (128, 128))),
    }
    return shared


def make_in_maps(inp, NT):
    x = np.asarray(inp["x"], np.float32)
    mem = np.asarray(inp["mem"], np.float32)
    B, S, _ = x.shape
    per_seq = S // NT
    NPRE = 3 * NT
    shared = prep_shared(inp)
    maps = []
    for c in range(B * per_seq):
        b, q = divmod(c, per_seq)
        xprev = np.zeros((NPRE, D), np.float32)
        if q > 0:
            xprev[NPRE - q * NT:] = x[b, 0:q * NT]
        pflag = np.zeros((128, NPRE // 512), np.float32)
        for j in range(NPRE // 512):
            if j * 512 >= NPRE - q * NT:
                pflag[:, j] = 1.0
        halob = np.full((128, 512), 0.0 if q > 0 else NEG, np.float32)
        m = dict(shared)
        m.update({"xown": np.ascontiguousarray(x[b, q * NT:(q + 1) * NT]), "xprev": xprev, "pflag": pflag,
                  "halob": halob, "mem": np.ascontiguousarray(mem[b])})
        maps.append(m)
    return maps


_CACHE = {}


def kernel(**inputs):
    NT = 2048
    if NT not in _CACHE:
        _CACHE[NT] = build(NT)[0]
    nc = _CACHE[NT]
    maps = make_in_maps(inputs, NT)
    res = run_bass_kernel_spmd(nc, maps, core_ids=list(range(N_CORES)))
    x = np.asarray(inputs["x"])
    B, S, _ = x.shape
    out = np.empty((B, S, D), np.float32)
    per_seq = S // NT
    for c in range(N_CORES):
        b, q = divmod(c, per_seq)
        out[b, q * NT:(q + 1) * NT] = res.results[c]["y"]
    return out
```

```python
import numpy as np
from contextlib import ExitStack
import concourse.bass as bass
import concourse.mybir as mybir
from concourse.bass_utils import run_bass_kernel_spmd

F32 = mybir.dt.float32
BF16 = mybir.dt.bfloat16
U32 = mybir.dt.uint32
AF = mybir.ActivationFunctionType
ALU = mybir.AluOpType
AX = mybir.AxisListType

D = 1024
EPS = 1e-6
NEG = -1e30
N_CORES = 8
SAME_ENG_SYNC = True

SM_MIX, SM_CROSS, SM_MEM, SM_FFN = 0, 8, 16, 24
SM_GA, SM_GB, SM_CB, SM_BA, SM_BX, SM_LAM, SM_CW = 32, 36, 40, 44, 48, 52, 56
NSM = 72


class Res:
    __slots__ = ("name", "w", "r", "ds")

    def __init__(self, name):
        self.name = name
        self.w = None
        self.r = {}
        self.ds = None


class KB:
    def __init__(self, nc, es):
        self.nc = nc
        self.es = es
        self.E = {}
        for nm, e in (("pe", nc.tensor), ("act", nc.scalar), ("dve", nc.vector),
                      ("pool", nc.gpsimd), ("sp", nc.sync)):
            self.E[nm] = dict(e=e, sem=es.enter_context(nc.semaphore("e_" + nm)), cnt=0, waited={}, nm=nm)
        self.free_ds = []
        self.all_ds = []
        self.n_ins = 0

    def _collect(self, r, w):
        deps = {}
        for x in r:
            if x.w is not None:
                s, v = x.w
                if deps.get(s, 0) < v:
                    deps[s] = v
        for x in w:
            if x.w is not None:
                s, v = x.w
                if deps.get(s, 0) < v:
                    deps[s] = v
            for s, v in x.r.items():
                if deps.get(s, 0) < v:
                    deps[s] = v
        return deps

    def _waits(self, E, deps, skip_own):
        for s, v in deps.items():
            if skip_own and s is E["sem"]:
                continue
            if E["waited"].get(s, 0) >= v:
                continue
            E["e"].wait_ge(s, v)
            E["waited"][s] = v

    def op(self, en, fn, r=(), w=()):
        E = self.E[en]
        skip_own = (en == "pe") or (not SAME_ENG_SYNC)
        self._waits(E, self._collect(r, w), skip_own)
        ins = fn(E["e"])
        E["cnt"] += 1
        self.n_ins += 1
        ins.then_inc(E["sem"], 1)
        tag = (E["sem"], E["cnt"])
        for x in w:
            x.w = tag
            x.r = {}
        for x in r:
            if x not in w:
                x.r[E["sem"]] = E["cnt"]
        return ins

    def dma(self, qn, out, in_, r, w, sres, **kw):
        E = self.E[qn]
        self._waits(E, self._collect(r, w), False)
        if sres.ds is None:
            if qn != "pool" and self.free_ds:
                sres.ds = self.free_ds.pop()
            else:
                sres.ds = [self.es.enter_context(self.nc.semaphore("d%d" % len(self.all_ds))), 0, qn]
                self.all_ds.append(sres.ds)
        ins = E["e"].dma_start(out=out, in_=in_, **kw)
        self.n_ins += 1
        sres.ds[1] += 1
        ins.then_inc(sres.ds[0], 16)
        val = sres.ds[1] * 16
        for x in w:
            x.w = (sres.ds[0], val)
            x.r = {}
        for x in r:
            if x not in w:
                x.r[sres.ds[0]] = val

    def barrier(self, release=()):
        deps = {}
        for nm in ("pe", "act", "dve", "pool"):
            E = self.E[nm]
            if E["cnt"] > 0:
                deps[E["sem"]] = E["cnt"]
        for ds in self.all_ds:
            if ds[1] > 0:
                deps[ds[0]] = ds[1] * 16
        for nm in ("pe", "act", "dve", "pool", "sp"):
            self._waits(self.E[nm], deps, True)
        for x in release:
            if x.ds is not None:
                if x.ds[2] != "pool":
                    self.free_ds.append(x.ds)
                x.ds = None


class Tn:
    def __init__(self, h, name, nres=1):
        self.h = h
        self.res = Res(name)
        self.rs = [Res("%s_%d" % (name, i)) for i in range(nres)] if nres > 1 else [self.res]

    def __getitem__(self, k):
        return self.h[k]


def build(NT=2048, debug=False):
    NPRE = 3 * NT
    NTILE = NT // 128
    nc = bass.Bass("TRN2", target_bir_lowering=False)
    dt_in = lambda n, s, d=F32: nc.dram_tensor(n, list(s), d, kind="ExternalInput").ap()
    xown = dt_in("xown", [NT, D])
    xprev = dt_in("xprev", [NPRE, D])
    pflag_d = dt_in("pflag", [128, NPRE // 512])
    halob_d = dt_in("halob", [128, 512])
    mem_d = dt_in("mem", [256, D])
    w_in_d = dt_in("w_in", [D, 2560])
    w_out_d = dt_in("w_out", [D, D])
    w_q_d = dt_in("w_q", [D, D])
    w_kv_d = dt_in("w_kv", [D, 2048])
    w_o_d = dt_in("w_o", [D, D])
    w_qry_d = dt_in("w_qry", [D, 2048])
    smalls_d = dt_in("smalls", [128, NSM])
    gbd_d = dt_in("gbd", [128, 8 * 128])
    abias_d = dt_in("abias", [128, 8 * 640])
    skT_d = dt_in("skT", [128, 16 * 128])
    uT_d = dt_in("uT", [16384, D])
    ev_d = dt_in("ev", [16384, D])
    gfin_d = dt_in("gfin", [128, D])
    ident_d = dt_in("ident", [128, 128])
    iota_d = dt_in("iota", [128, 128])
    y_d = nc.dram_tensor("y", [NT, D], F32, kind="ExternalOutput").ap()
    wd_d = nc.dram_tensor("wd_scratch", [NTILE, 128, 16384], BF16, kind="Internal").ap()
    dbg = {}
    if debug:
        for nm in ("dbg1", "dbg2"):
            dbg[nm] = nc.dram_tensor(nm, [NT, D], F32, kind="ExternalOutput").ap()

    with ExitStack() as es:
        k = KB(nc, es)

        def sb(ctx, name, shape, dt, nres=1):
            return Tn(ctx.enter_context(nc.sbuf_tensor("sb_" + name, list(shape), dt)), name, nres)

        def ps(ctx, name, shape, dt):
            return Tn(ctx.enter_context(nc.psum_tensor("ps_" + name, list(shape), dt)), name)

        x_res = sb(es, "x_res", [128, NTILE, D], F32, nres=NTILE)
        ident_f = sb(es, "ident_f", [128, 128], F32)
        ident_b = sb(es, "ident_b", [128, 128], BF16)
        iota_f = sb(es, "iota_f", [128, 128], F32)
        ones_b = sb(es, "ones_b", [128, 128], BF16)
        smalls = sb(es, "smalls", [128, NSM], F32)
        cst = sb(es, "cst", [128, 4], F32)
        gB = sb(es, "gB", [128, 8, 128], F32)
        nrm = [dict(ss=sb(es, "n_ss%d" % i, [128, 1], F32), sd=sb(es, "n_sd%d" % i, [128, 1], F32),
                    rstd=sb(es, "n_rstd%d" % i, [128, 1], F32), xs=sb(es, "n_xs%d" % i, [128, D], BF16),
                    junk=sb(es, "n_junk%d" % i, [128, D], BF16)) for i in range(2)]
        nrm_i = [0]

        k.dma("sp", ident_f[:], ident_d[:, :], r=[], w=[ident_f.res], sres=ident_f.res)
        k.dma("sp", iota_f[:], iota_d[:, :], r=[], w=[iota_f.res], sres=iota_f.res)
        k.dma("sp", smalls[:], smalls_d[:, :], r=[], w=[smalls.res], sres=smalls.res)
        k.op("dve", lambda e: e.tensor_copy(out=ident_b[:], in_=ident_f[:]), r=[ident_f.res], w=[ident_b.res])
        k.op("pool", lambda e: e.memset(ones_b[:], 1.0), w=[ones_b.res])
        k.op("pool", lambda e: e.memset(cst[:, 0:1], EPS), w=[cst.res])
        k.op("pool", lambda e: e.memset(cst[:, 1:2], 1.0), w=[cst.res])
        k.op("pool", lambda e: e.memset(cst[:, 2:3], 0.0), w=[cst.res])

        def set_gain(col):
            k.op("dve", lambda e: e.tensor_copy(
                out=gB[:], in_=smalls[:, col:col + 8].unsqueeze(2).to_broadcast([128, 8, 128])),
                r=[smalls.res], w=[gB.res])

        def norm_T_g(x_ap, x_r, pT, hT_ap, hT_r):
            n = nrm[nrm_i[0] % 2]
            nrm_i[0] += 1
            k.op("dve", lambda e: e.scalar_tensor_tensor(out=n["junk"][:], in0=x_ap, scalar=1.0, in1=x_ap,
                                                         op0=ALU.mult, op1=ALU.mult, accum_out=n["ss"][:]),
                 r=[x_r], w=[n["junk"].res, n["ss"].res])
            yield
            k.op("act", lambda e: e.activation(out=n["sd"][:], in_=n["ss"][:], func=AF.Ln,
                                               bias=cst[:, 0:1], scale=1.0 / D),
                 r=[n["ss"].res, cst.res], w=[n["sd"].res])
            k.op("act", lambda e: e.activation(out=n["rstd"][:], in_=n["sd"][:], func=AF.Exp, scale=-0.5),
                 r=[n["sd"].res], w=[n["rstd"].res])
            yield
            k.op("dve", lambda e: e.tensor_scalar(out=n["xs"][:], in0=x_ap, scalar1=n["rstd"][:, 0:1], scalar2=None,
                                                  op0=ALU.mult), r=[x_r, n["rstd"].res], w=[n["xs"].res])
            for c in range(8):
                k.op("pe", lambda e, c=c: e.transpose(out=pT[:, c, :], in_=n["xs"][:, c * 128:(c + 1) * 128],
                                                      identity=ident_b[:]),
                     r=[n["xs"].res, ident_b.res], w=[pT.res])
            yield
            k.op("dve", lambda e: e.tensor_tensor(out=hT_ap, in0=pT[:], in1=gB[:], op=ALU.mult),
                 r=[pT.res, gB.res], w=[hT_r])

        def norm_T(*a):
            for _ in norm_T_g(*a):
                pass

        def lockstep_g(gens):
            gens = list(gens)
            while gens:
                for g_ in list(gens):
                    try:
                        next(g_)
                    except StopIteration:
                        gens.remove(g_)
                yield

        def norm_pairs(args_list):
            for i_ in range(0, len(args_list), 2):
                for _ in lockstep_g([norm_T_g(*a) for a in args_list[i_:i_ + 2]]):
                    pass

        def mm_group(out_ap, pairs, r, w):
            n = len(pairs)
            for i, (l, rh) in enumerate(pairs):
                k.op("pe", lambda e, l=l, rh=rh, i=i: e.matmul(out_ap, lhsT=l, rhs=rh, start=(i == 0), stop=(i == n - 1)),
                     r=r, w=w)

        def load_w_bf16(dst, src_ap, ncols):
            for c in range(8):
                k.dma("pool", dst[:, c, :], src_ap[c * 128:(c + 1) * 128, :], r=[], w=[dst.res], sres=dst.res,
                      max_dma_last_dim=4096)

        with ExitStack() as p1:
            with ExitStack() as pa:
                BA = 512
                w_inA = sb(pa, "w_inA", [128, 8, 1024], BF16)
                load_w_bf16(w_inA, w_in_d[:, 0:1024], 1024)
                w_outA = sb(pa, "w_outA", [128, 4, D], BF16)
                for c in range(4):
                    k.dma("pool", w_outA[:, c, :], w_out_d[c * 128:(c + 1) * 128, :], r=[], w=[w_outA.res], sres=w_outA.res,
                          max_dma_last_dim=4096)
                yan = sb(pa, "yan", [128, 4, 512], BF16)
                gbd_f = sb(pa, "gbd_f", [128, 8, 128], F32)
                gbd = sb(pa, "gbd", [128, 8, 128], BF16)
                k.dma("sp", gbd_f[:], gbd_d.rearrange("p (c j) -> p c j", c=8), r=[], w=[gbd_f.res], sres=gbd_f.res)
                k.op("dve", lambda e: e.tensor_copy(out=gbd[:], in_=gbd_f[:]), r=[gbd_f.res], w=[gbd.res])
                pflag = sb(pa, "pflag", [128, NPRE // 512], F32)
                k.dma("sp", pflag[:], pflag_d[:, :], r=[], w=[pflag.res], sres=pflag.res)
                cL = sb(pa, "cL", [128, 4], F32)
                tmp4 = sb(pa, "tmp4", [128, 4], F32)
                k.op("act", lambda e: e.activation(out=tmp4[:], in_=smalls[:, SM_LAM:SM_LAM + 4], func=AF.Exp, scale=-1.0),
                     r=[smalls.res], w=[tmp4.res])
                k.op("act", lambda e: e.activation(out=cL[:], in_=tmp4[:], func=AF.Ln, bias=cst[:, 1:2], scale=1.0),
                     r=[tmp4.res, cst.res], w=[cL.res])
                k.op("dve", lambda e: e.tensor_scalar(out=cL[:], in0=cL[:], scalar1=-8.0, scalar2=None, op0=ALU.mult),
                     r=[cL.res], w=[cL.res])
                set_gain(SM_MIX)
                xtmp = [sb(pa, "xtmp%d" % i, [128, D], F32) for i in range(2)]
                hT = [sb(pa, "hTa%d" % i, [128, 8, BA], BF16) for i in range(2)]
                xl = sb(pa, "xl", [128, 4, 3 + BA], F32, nres=4)
                gg = sb(pa, "gg", [128, 4, BA], F32, nres=4)
                hh = sb(pa, "hh", [128, 4, BA], F32, nres=4)
                hst = sb(pa, "hst", [128, 4], F32, nres=4)
                yaT = sb(pa, "yaT", [128, 4, BA], F32, nres=4)
                LT = [{nm: sb(pa, "l%s%d" % (nm, i), [128, BA], BF16 if nm == "xcb" else F32)
                       for nm in ("xc", "xc2", "xcb", "rr", "ii", "aa", "tt", "bb")} for i in range(2)]
                sq = sb(pa, "sq", [128, BA], BF16)
                rsn = sb(pa, "rsn", [128, BA], F32)
                pj = [ps(pa, "pj%d" % i, [128, 512], F32) for i in range(2)]
                pT = [ps(pa, "pT%d" % i, [128, 8, 128], BF16) for i in range(2)]
                pg = [ps(pa, "pg%d" % i, [128, 512], F32) for i in range(4)]
                pn = pj[0]
                k.op("pool", lambda e: e.memset(xl[:], 0.0), w=xl.rs)
                k.op("pool", lambda e: e.memset(hst[:], 0.0), w=hst.rs)

                nblk_pre = NPRE // BA
                nblk_own = NT // BA
                tcount = 0
                pjc = 0
                def front_norm(b):
                    nonlocal tcount
                    own = b >= nblk_pre
                    ob = b - nblk_pre
                    h = hT[b % 2]

                    def tile_g(t):
                        nonlocal tcount
                        if own:
                            tile = ob * 4 + t
                            xa = x_res[:, tile, :]
                            xr = x_res.rs[tile]
                            k.dma("sp", xa, xown[tile * 128:(tile + 1) * 128, :], r=[], w=[xr], sres=xr)
                        else:
                            xt_ = xtmp[tcount % 2]
                            xa = xt_[:]
                            xr = xt_.res
                            r0 = b * BA + t * 128
                            k.dma("sp", xa, xprev[r0:r0 + 128, :], r=[], w=[xr], sres=xr)
                        tcount += 1
                        yield from norm_T_g(xa, xr, pT[t % 2], h[:, :, t * 128:(t + 1) * 128], h.res)

                    yield from lockstep_g([tile_g(0), tile_g(1)])
                    yield from lockstep_g([tile_g(2), tile_g(3)])

                def front_proj(b):
                    nonlocal pjc
                    own = b >= nblk_pre
                    h = hT[b % 2]
                    for cc in range(8 if own else 4):
                        pp = pj[pjc % 2]
                        pjc += 1
                        mm_group(pp[:], [(w_inA[:, kc, cc * 128:(cc + 1) * 128], h[:, kc, :]) for kc in range(8)],
                                 r=[w_inA.res, h.res], w=[pp.res])
                        if cc < 4:
                            k.op("act", lambda e, pp=pp, cc=cc: e.activation(out=xl[:, cc, 3:3 + BA], in_=pp[:], func=AF.Copy),
                                 r=[pp.res], w=[xl.rs[cc]])
                        else:
                            k.op("act", lambda e, pp=pp, cc=cc: e.activation(out=gg[:, cc - 4, :], in_=pp[:], func=AF.Gelu_apprx_tanh),
                                 r=[pp.res], w=[gg.rs[cc - 4]])

                for _ in front_norm(0):
                    pass
                front_proj(0)
                for b in range(nblk_pre + nblk_own):
                    own = b >= nblk_pre
                    ob = b - nblk_pre
                    fn = front_norm(b + 1) if b + 1 < nblk_pre + nblk_own else iter(())
                    def lru_chain(cc, L, pga, pgb, own=own, b=b):
                            cw = lambda j, cc=cc: smalls[:, SM_CW + cc * 4 + j:SM_CW + cc * 4 + j + 1]
                            k.op("dve", lambda e, cc=cc, L=L, cw=cw: e.tensor_scalar(
                                out=L["xc"][:], in0=xl[:, cc, 3:3 + BA], scalar1=cw(3), scalar2=smalls[:, SM_CB + cc:SM_CB + cc + 1],
                                op0=ALU.mult, op1=ALU.add), r=[xl.rs[cc], smalls.res], w=[L["xc"].res])
                            src, dst = "xc", "xc2"
                            for j in range(3):
                                yield
                                k.op("dve", lambda e, cc=cc, L=L, cw=cw, j=j, src=src, dst=dst: e.scalar_tensor_tensor(
                                    out=L[dst][:], in0=xl[:, cc, j:j + BA], scalar=cw(j), in1=L[src][:], op0=ALU.mult, op1=ALU.add),
                                    r=[xl.rs[cc], smalls.res, L[src].res], w=[L[dst].res])
                                src, dst = dst, src
                            xc = L[src]
                            yield
                            k.op("pool", lambda e, cc=cc: e.tensor_copy(out=xl[:, cc, 0:3], in_=xl[:, cc, BA:BA + 3]),
                                 r=[xl.rs[cc]], w=[xl.rs[cc]])
                            yield
                            k.op("act", lambda e, L=L, xc=xc: e.activation(out=L["xcb"][:], in_=xc[:], func=AF.Copy),
                                 r=[xc.res], w=[L["xcb"].res])
                            yield
                            k.op("pe", lambda e, cc=cc, L=L: e.matmul(pga[:], lhsT=gbd[:, cc, :], rhs=L["xcb"][:], start=True, stop=True),
                                 r=[gbd.res, L["xcb"].res], w=[pga.res])
                            yield
                            k.op("pe", lambda e, cc=cc, L=L: e.matmul(pgb[:], lhsT=gbd[:, 4 + cc, :], rhs=L["xcb"][:], start=True, stop=True),
                                 r=[gbd.res, L["xcb"].res], w=[pgb.res])
                            yield
                            k.op("act", lambda e, cc=cc, L=L: e.activation(out=L["rr"][:], in_=pga[:], func=AF.Sigmoid,
                                                                           bias=smalls[:, SM_BA + cc:SM_BA + cc + 1], scale=1.0),
                                 r=[pga.res, smalls.res], w=[L["rr"].res])
                            yield
                            k.op("act", lambda e, cc=cc, L=L: e.activation(out=L["ii"][:], in_=pgb[:], func=AF.Sigmoid,
                                                                           bias=smalls[:, SM_BX + cc:SM_BX + cc + 1], scale=1.0),
                                 r=[pgb.res, smalls.res], w=[L["ii"].res])
                            yield
                            k.op("act", lambda e, cc=cc, L=L: e.activation(out=L["aa"][:], in_=L["rr"][:], func=AF.Exp,
                                                                           scale=cL[:, cc:cc + 1]),
                                 r=[L["rr"].res, cL.res], w=[L["aa"].res])
                            yield
                            k.op("dve", lambda e, L=L: e.tensor_tensor(out=L["tt"][:], in0=L["aa"][:], in1=L["aa"][:], op=ALU.mult),
                                 r=[L["aa"].res], w=[L["tt"].res])
                            yield
                            k.op("act", lambda e, L=L: e.activation(out=L["tt"][:], in_=L["tt"][:], func=AF.Sqrt,
                                                                    bias=cst[:, 1:2], scale=-1.0),
                                 r=[L["tt"].res, cst.res], w=[L["tt"].res])
                            yield
                            k.op("dve", lambda e, L=L: e.tensor_tensor(out=L["bb"][:], in0=L["tt"][:], in1=L["ii"][:], op=ALU.mult),
                                 r=[L["tt"].res, L["ii"].res], w=[L["bb"].res])
                            yield
                            k.op("dve", lambda e, L=L, xc=xc: e.tensor_tensor(out=L["rr"][:], in0=L["bb"][:], in1=xc[:], op=ALU.mult),
                                 r=[L["bb"].res, xc.res], w=[L["rr"].res])
                            yield
                            k.op("dve", lambda e, cc=cc, L=L: e.tensor_tensor_scan(
                                out=hh[:, cc, :], data0=L["aa"][:], data1=L["rr"][:], initial=hst[:, cc:cc + 1],
                                op0=ALU.mult, op1=ALU.add), r=[L["aa"].res, L["rr"].res, hst.rs[cc]], w=[hh.rs[cc]])
                            if own:
                                yield
                                k.op("dve", lambda e, cc=cc: e.tensor_copy(out=hst[:, cc:cc + 1], in_=hh[:, cc, BA - 1:BA]),
                                     r=[hh.rs[cc]], w=[hst.rs[cc]])
                                yield
                                k.op("dve", lambda e, cc=cc: e.tensor_tensor(out=yaT[:, cc, :], in0=hh[:, cc, :], in1=gg[:, cc, :], op=ALU.mult),
                                     r=[hh.rs[cc], gg.rs[cc]], w=[yaT.rs[cc]])
                            else:
                                yield
                                k.op("dve", lambda e, cc=cc, b=b: e.tensor_tensor(out=hst[:, cc:cc + 1], in0=hh[:, cc, BA - 1:BA],
                                                                                  in1=pflag[:, b:b + 1], op=ALU.mult),
                                     r=[hh.rs[cc], pflag.res], w=[hst.rs[cc]])

                    for pair in range(2):
                        gens = [lru_chain(2 * pair + i_, LT[i_], pg[2 * i_], pg[2 * i_ + 1]) for i_ in range(2)]
                        while gens:
                            for g_ in list(gens):
                                try:
                                    next(g_)
                                except StopIteration:
                                    gens.remove(g_)
                            next(fn, None)
                    for _ in fn:
                        pass
                    if own:
                        for cc in range(4):
                            k.op("dve", lambda e, cc=cc: e.tensor_tensor(out=sq[:], in0=yaT[:, cc, :], in1=yaT[:, cc, :], op=ALU.mult),
                                 r=[yaT.rs[cc]], w=[sq.res])
                            k.op("pe", lambda e, cc=cc: e.matmul(pn[:], lhsT=ones_b[:], rhs=sq[:], start=(cc == 0), stop=(cc == 3)),
                                 r=[ones_b.res, sq.res], w=[pn.res])
                        k.op("act", lambda e: e.activation(out=rsn[:], in_=pn[:], func=AF.Ln, bias=cst[:, 0:1], scale=1.0 / 512),
                             r=[pn.res, cst.res], w=[rsn.res])
                        k.op("act", lambda e: e.activation(out=rsn[:], in_=rsn[:], func=AF.Exp, scale=-0.5), r=[rsn.res], w=[rsn.res])
                        for cc in range(4):
                            k.op("dve", lambda e, cc=cc, ob=ob: e.scalar_tensor_tensor(
                                out=yan[:, cc, :], in0=yaT[:, cc, :],
                                scalar=smalls[:, SM_GA + cc:SM_GA + cc + 1], in1=rsn[:], op0=ALU.mult, op1=ALU.mult),
                                r=[yaT.rs[cc], smalls.res, rsn.res], w=[yan.res])
                        for t in range(4):
                            tile = ob * 4 + t
                            for half in range(2):
                                pp = pj[pjc % 2]
                                pjc += 1
                                mm_group(pp[:], [(yan[:, c, t * 128:(t + 1) * 128], w_outA[:, c, half * 512:(half + 1) * 512]) for c in range(4)],
                                         r=[yan.res, w_outA.res], w=[pp.res])
                                k.op("dve", lambda e, pp=pp, tile=tile, half=half: e.tensor_tensor(
                                    out=x_res[:, tile, half * 512:(half + 1) * 512], in0=pp[:], in1=x_res[:, tile, half * 512:(half + 1) * 512],
                                    op=ALU.add), r=[pp.res, x_res.rs[tile]], w=[x_res.rs[tile]])
                    if b + 1 < nblk_pre + nblk_own:
                        front_proj(b + 1)
                k.barrier(release=[w_inA.res, w_outA.res, gbd_f.res, pflag.res] + [t_.res for t_ in xtmp])

            with ExitStack() as pb:
                BB = 256
                w_inB = sb(pb, "w_inB", [128, 8, 1536], BF16)
                load_w_bf16(w_inB, w_in_d[:, 1024:2560], 1536)
                w_out = sb(pb, "w_outB", [128, 4, D], BF16)
                for c in range(4):
                    k.dma("pool", w_out[:, c, :], w_out_d[512 + c * 128:512 + (c + 1) * 128, :], r=[], w=[w_out.res], sres=w_out.res,
                          max_dma_last_dim=4096)
                abias = sb(pb, "abias", [128, 8, 640], F32)
                k.dma("sp", abias[:], abias_d.rearrange("p (h c) -> p h c", h=8), r=[], w=[abias.res], sres=abias.res)
                halob = sb(pb, "halob", [128, 512], F32)
                k.dma("sp", halob[:], halob_d[:, :], r=[], w=[halob.res], sres=halob.res)
                xtmp = [sb(pb, "xtmpb%d" % i, [128, D], F32) for i in range(2)]
                hT = [sb(pb, "hTb%d" % i, [128, 8, BB], BF16) for i in range(2)]
                qA_l = [sb(pb, "qA%d" % i, [128, 4, BB], BF16) for i in range(2)]
                qB_l = [sb(pb, "qB%d" % i, [128, 4, BB], BF16) for i in range(2)]
                kT = [sb(pb, "kT%d" % i, [128, 4, BB], BF16) for i in range(4)]
                vpad = [sb(pb, "vpad%d" % i, [128, 8, 128], BF16) for i in range(8)]
                ybT = sb(pb, "ybT", [128, 4, BB], F32)
                ybn = sb(pb, "ybn", [128, 4, BB], BF16)
                sbuf_s = [sb(pb, "sbs%d" % i, [128, 640], F32) for i in range(2)]
                Pm = [sb(pb, "Pm%d" % i, [128, 640], BF16) for i in range(2)]
                Pn = [sb(pb, "Pn%d" % i, [128, 640], BF16) for i in range(2)]
                PT = [sb(pb, "PT%d" % i, [128, 5, 128], BF16) for i in range(2)]
                st = [dict(mx=sb(pb, "a_mx%d" % i, [128, 1], F32), rs=sb(pb, "a_rs%d" % i, [128, 1], F32),
                           ri=sb(pb, "a_ri%d" % i, [128, 1], F32)) for i in range(2)]
                sq4 = [sb(pb, "sqb%d" % i, [128, BB], BF16) for i in range(4)]
                rsn = sb(pb, "rsnb", [128, BB], F32)
                pj = [ps(pb, "pjb%d" % i, [128, 512], F32) for i in range(2)]
                pT = [ps(pb, "pTb%d" % i, [128, 8, 128], BF16) for i in range(1)]
                pSm = [ps(pb, "pSm%d" % i, [128, 512], F32) for i in range(2)]
                pSr = Tn(pb.enter_context(nc.psum_tensor("ps_pSr", [128, 4, 128], F32)), "pSr", nres=4)
                pPT = ps(pb, "pPT", [128, 5, 128], BF16)
                pO = ps(pb, "pO", [128, 4, 128], F32)
                pn = pj[0]
                for v_ in vpad:
                    k.op("pool", lambda e, v_=v_: e.memset(v_[:], 0.0), w=[v_.res])
                for q_ in qA_l + qB_l:
                    k.op("pool", lambda e, q_=q_: e.memset(q_[:], 0.0), w=[q_.res])
                set_gain(SM_MIX)

                nhalo = 512 // BB
                nown = NT // BB
                tcount = 0
                pjc = 0
                ac = 0
                def prep(b):
                    nonlocal tcount, pjc
                    own = b >= nhalo
                    ob = b - nhalo
                    h = hT[b % 2]
                    kcur = kT[b % 4]
                    qA, qB = qA_l[b % 2], qB_l[b % 2]
                    for t in range(2):
                        xt_ = xtmp[tcount % 2]
                        xa = xt_[:]
                        xr = xt_.res
                        if own:
                            r0 = ob * BB + t * 128
                            k.dma("sp", xa, xown[r0:r0 + 128, :], r=[], w=[xr], sres=xr)
                        else:
                            r0 = NPRE - 512 + b * BB + t * 128
                            k.dma("sp", xa, xprev[r0:r0 + 128, :], r=[], w=[xr], sres=xr)
                        yield from norm_T_g(xa, xr, pT[0], h[:, :, t * 128:(t + 1) * 128], h.res)
                        tcount += 1
                        yield
                    for m in range(4):
                        pp = pj[pjc % 2]
                        pjc += 1
                        mm_group(pp[:, 0:BB], [(w_inB[:, kc, 512 + m * 128:512 + (m + 1) * 128], h[:, kc, :]) for kc in range(8)],
                                 r=[w_inB.res, h.res], w=[pp.res])
                        k.op("act", lambda e, pp=pp, m=m, kcur=kcur: e.activation(out=kcur[:, m, :], in_=pp[:, 0:BB], func=AF.Copy),
                             r=[pp.res], w=[kcur.res])
                    yield
                    for t in range(2):
                        yield
                        gt = b * 2 + t
                        vp = vpad[gt % 8]
                        pp = pj[pjc % 2]
                        pjc += 1
                        mm_group(pp[:], [(h[:, kc, t * 128:(t + 1) * 128], w_inB[:, kc, 1024:1536]) for kc in range(8)],
                                 r=[w_inB.res, h.res], w=[pp.res])
                        ppv = pp[:].rearrange("p (h d) -> p h d", h=8)
                        k.op("act", lambda e, vp=vp, ppv=ppv: e.activation(out=vp[:, 0:8:2, 0:64], in_=ppv[:, 0:8:2, :], func=AF.Copy),
                             r=[pp.res], w=[vp.res])
                        k.op("act", lambda e, vp=vp, ppv=ppv: e.activation(out=vp[:, 1:8:2, 64:128], in_=ppv[:, 1:8:2, :], func=AF.Copy),
                             r=[pp.res], w=[vp.res])
                    if not own:
                        return
                    yield
                    for m in range(4):
                        if m == 2:
                            yield
                        pp = pj[pjc % 2]
                        pjc += 1
                        mm_group(pp[:, 0:BB], [(w_inB[:, kc, m * 128:(m + 1) * 128], h[:, kc, :]) for kc in range(8)],
                                 r=[w_inB.res, h.res], w=[pp.res])
                        k.op("act", lambda e, pp=pp, m=m: e.activation(out=qA[0:64, m, :], in_=pp[0:64, 0:BB], func=AF.Copy),
                             r=[pp.res], w=[qA.res])
                        k.op("act", lambda e, pp=pp, m=m: e.activation(out=qB[64:128, m, :], in_=pp[64:128, 0:BB], func=AF.Copy),
                             r=[pp.res], w=[qB.res])
                def attend(b, fillers):
                    nonlocal pjc, ac
                    ob = b - nhalo
                    qA, qB = qA_l[b % 2], qB_l[b % 2]
                    units = []
                    for p in range(2):
                        pieces = []
                        oc = 0
                        remaining = 640
                        pos = 128 * p
                        while remaining > 0:
                            bi = pos // BB
                            c0 = pos % BB
                            n = min(BB - c0, remaining)
                            if oc < 512 and oc + n > 512:
                                n = 512 - oc
                            pieces.append((kT[(b - 2 + bi) % 4], c0, n, oc))
                            oc += n
                            pos += n
                            remaining -= n
                        for hd in range(8):
                            units.append((p, hd, pieces))

                    def S1(u):
                        p, hd, pieces = units[u]
                        m = hd // 2
                        qs = (qA if hd % 2 == 0 else qB)
                        gi = ac0 + u
                        pm, pr = pSm[gi % 2], pSr
                        for (kt_, c0, n, oc) in pieces:
                            if oc < 512:
                                oap = pm[:, oc:oc + n]
                                ores = pm.res
                            else:
                                oap = pr[:, gi % 4, oc - 512:oc - 512 + n]
                                ores = pr.res
                            k.op("pe", lambda e, kt_=kt_, c0=c0, n=n, oap=oap: e.matmul(
                                oap, lhsT=qs[:, m, p * 128:(p + 1) * 128], rhs=kt_[:, m, c0:c0 + n],
                                start=True, stop=True), r=[qs.res, kt_.res], w=[ores])

                    def S2(u, part):
                        p, hd, pieces = units[u]
                        gi = ac0 + u
                        i2 = gi % 2
                        pm, pr = pSm[gi % 2], pSr
                        S, PP, PN, s_ = sbuf_s[i2], Pm[i2], Pn[i2], st[i2]
                        if part == 1:
                            k.op("dve", lambda e: e.reciprocal(out=s_["ri"][:], in_=s_["rs"][:]), r=[s_["rs"].res], w=[s_["ri"].res])
                            k.op("dve", lambda e: e.tensor_scalar(out=PN[:], in0=PP[:], scalar1=s_["ri"][:, 0:1],
                                                                  scalar2=None, op0=ALU.mult),
                                 r=[PP.res, s_["ri"].res], w=[PN.res])
                            return
                        k.op("dve", lambda e: e.scalar_tensor_tensor(
                            out=S[:, 0:512], in0=pm[:], scalar=0.125, in1=abias[:, hd, 0:512], op0=ALU.mult, op1=ALU.add),
                            r=[pm.res, abias.res], w=[S.res])
                        k.op("dve", lambda e: e.scalar_tensor_tensor(
                            out=S[:, 512:640], in0=pr[:, gi % 4, :], scalar=0.125, in1=abias[:, hd, 512:640], op0=ALU.mult, op1=ALU.add),
                            r=[pr.res, abias.res], w=[S.res])
                        cnt = 512 - ob * BB - 128 * p
                        if cnt > 0:
                            hb0 = ob * BB + 128 * p
                            k.op("dve", lambda e: e.tensor_tensor(
                                out=S[:, 0:cnt], in0=S[:, 0:cnt], in1=halob[:, hb0:512], op=ALU.add),
                                r=[S.res, halob.res], w=[S.res])
                        k.op("dve", lambda e: e.tensor_reduce(out=s_["mx"][:], in_=S[:], axis=AX.X, op=ALU.max, negate=True),
                             r=[S.res], w=[s_["mx"].res])
                        k.op("act", lambda e: e.activation(out=PP[:], in_=S[:], func=AF.Exp, bias=s_["mx"][:, 0:1],
                                                           scale=1.0, accum_out=s_["rs"][:]),
                             r=[S.res, s_["mx"].res], w=[PP.res, s_["rs"].res])

                    def S3(u):
                        p, hd, pieces = units[u]
                        m = hd // 2
                        gi = ac0 + u
                        i2 = gi % 2
                        PN, PTs = Pn[i2], PT[i2]
                        for kc in range(5):
                            k.op("pe", lambda e, kc=kc: e.transpose(out=pPT[:, kc, :], in_=PN[:, kc * 128:(kc + 1) * 128],
                                                                    identity=ident_b[:]),
                                 r=[PN.res, ident_b.res], w=[pPT.res])
                        k.op("act", lambda e: e.activation(out=PTs[:], in_=pPT[:], func=AF.Copy), r=[pPT.res], w=[PTs.res])
                        g0 = (b * 2 + p) - 4
                        for kc in range(5):
                            vp = vpad[(g0 + kc) % 8]
                            k.op("pe", lambda e, vp=vp, kc=kc: e.matmul(
                                pO[:, m, :], lhsT=vp[:, hd, :], rhs=PTs[:, kc, :],
                                start=(hd % 2 == 0 and kc == 0), stop=(hd % 2 == 1 and kc == 4)),
                                r=[vp.res, PTs.res], w=[pO.res])
                        if hd == 7:
                            k.op("act", lambda e: e.activation(out=ybT[:, :, p * 128:(p + 1) * 128], in_=pO[:], func=AF.Copy),
                                 r=[pO.res], w=[ybT.res])

                    def advance():
                        while fillers:
                            try:
                                next(fillers[0])
                                return
                            except StopIteration:
                                fillers.pop(0)

                    ac0 = ac
                    S1(0)
                    S1(1)
                    S2(0, 0)
                    for u in range(len(units)):
                        if u + 1 < len(units):
                            S2(u + 1, 0)
                        S2(u, 1)
                        if u + 2 < len(units):
                            S1(u + 2)
                        S3(u)
                        advance()
                    while fillers:
                        advance()
                    ac += len(units)

                def tail(b):
                    nonlocal pjc
                    ob = b - nhalo
                    for cc in range(4):
                        k.op("dve", lambda e, cc=cc: e.tensor_tensor(out=sq4[cc][:], in0=ybT[:, cc, :], in1=ybT[:, cc, :], op=ALU.mult),
                             r=[ybT.res], w=[sq4[cc].res])
                    for cc in range(4):
                        k.op("pe", lambda e, cc=cc: e.matmul(pn[:, 0:BB], lhsT=ones_b[:], rhs=sq4[cc][:], start=(cc == 0), stop=(cc == 3)),
                             r=[ones_b.res, sq4[cc].res], w=[pn.res])
                    yield
                    k.op("act", lambda e: e.activation(out=rsn[:], in_=pn[:, 0:BB], func=AF.Ln, bias=cst[:, 0:1], scale=1.0 / 512),
                         r=[pn.res, cst.res], w=[rsn.res])
                    k.op("act", lambda e: e.activation(out=rsn[:], in_=rsn[:], func=AF.Exp, scale=-0.5), r=[rsn.res], w=[rsn.res])
                    yield
                    for cc in range(4):
                        k.op("dve", lambda e, cc=cc: e.scalar_tensor_tensor(
                            out=ybn[:, cc, :], in0=ybT[:, cc, :], scalar=smalls[:, SM_GB + cc:SM_GB + cc + 1], in1=rsn[:],
                            op0=ALU.mult, op1=ALU.mult), r=[ybT.res, smalls.res, rsn.res], w=[ybn.res])
                    yield
                    for t in range(2):
                        tile = ob * 2 + t
                        tok0 = ob * BB + t * 128
                        for half in range(2):
                            pp = pj[pjc % 2]
                            pjc += 1
                            pairs = [(ybn[:, c, t * 128:(t + 1) * 128], w_out[:, c, half * 512:(half + 1) * 512]) for c in range(4)]
                            mm_group(pp[:], pairs, r=[ybn.res, w_out.res], w=[pp.res])
                            k.op("dve", lambda e, pp=pp, tile=tile, half=half: e.tensor_tensor(
                                out=x_res[:, tile, half * 512:(half + 1) * 512], in0=pp[:], in1=x_res[:, tile, half * 512:(half + 1) * 512],
                                op=ALU.add), r=[pp.res, x_res.rs[tile]], w=[x_res.rs[tile]])
                        yield
                nb_ = nhalo + nown
                for b0 in range(nhalo + 1):
                    for _ in prep(b0):
                        pass
                for b in range(nhalo, nb_):
                    fl = []
                    if b - 1 >= nhalo:
                        fl.append(tail(b - 1))
                    if b + 1 < nb_:
                        fl.append(prep(b + 1))
                    attend(b, fl)
                for _ in tail(nb_ - 1):
                    pass
                k.barrier(release=[w_inB.res, w_out.res, abias.res, halob.res] + [t_.res for t_ in xtmp])

        if debug:
            for tile in range(NTILE):
                k.dma("sp", dbg["dbg1"][tile * 128:(tile + 1) * 128, :], x_res[:, tile, :], r=[x_res.rs[tile]], w=[], sres=x_res.rs[tile])

        with ExitStack() as p2:
            B2 = 512
            w_q = sb(p2, "w_q", [128, 8, D], BF16)
            w_o = sb(p2, "w_o", [128, 8, D], BF16)
            kmT = sb(p2, "kmT", [128, 8, 256], BF16)
            vmem = sb(p2, "vmem", [128, 2, D], BF16)
            pj = [ps(p2, "pj2%d" % i, [128, 512], F32) for i in range(2)]
            pT = [ps(p2, "pT2%d" % i, [128, 8, 128], BF16) for i in range(1)]
            pS2 = [ps(p2, "pS2%d" % i, [128, 4, 256], F32) for i in range(2)]
            pPT2 = ps(p2, "pPT2", [128, 8, 128], BF16)
            pjc = 0
            with ExitStack() as p2s:
                w_kv = sb(p2s, "w_kv", [128, 8, 2048], BF16)
                load_w_bf16(w_kv, w_kv_d[:, :], 2048)
                load_w_bf16(w_q, w_q_d[:, :], D)
                load_w_bf16(w_o, w_o_d[:, :], D)
                memt = [sb(p2s, "memt%d" % i, [128, D], F32) for i in range(2)]
                memT = sb(p2s, "memT", [128, 8, 256], BF16)
                set_gain(SM_MEM)
                for t in range(2):
                    k.dma("sp", memt[t][:], mem_d[t * 128:(t + 1) * 128, :], r=[], w=[memt[t].res], sres=memt[t].res)
                    norm_T(memt[t][:], memt[t].res, pT[0], memT[:, :, t * 128:(t + 1) * 128], memT.res)
                for oc in range(8):
                    pp = pj[pjc % 2]
                    pjc += 1
                    mm_group(pp[:, 0:256], [(w_kv[:, kc, oc * 128:(oc + 1) * 128], memT[:, kc, :]) for kc in range(8)],
                             r=[w_kv.res, memT.res], w=[pp.res])
                    k.op("act", lambda e, pp=pp, oc=oc: e.activation(out=kmT[:, oc, :], in_=pp[:, 0:256], func=AF.Copy),
                         r=[pp.res], w=[kmT.res])
                for t in range(2):
                    for half in range(2):
                        pp = pj[pjc % 2]
                        pjc += 1
                        mm_group(pp[:], [(memT[:, kc, t * 128:(t + 1) * 128], w_kv[:, kc, 1024 + half * 512:1024 + (half + 1) * 512])
                                         for kc in range(8)], r=[w_kv.res, memT.res], w=[pp.res])
                        k.op("act", lambda e, pp=pp, t=t, half=half: e.activation(out=vmem[:, t, half * 512:(half + 1) * 512], in_=pp[:], func=AF.Copy),
                             r=[pp.res], w=[vmem.res])
                k.barrier(release=[w_kv.res] + [t_.res for t_ in memt])
            hT = [sb(p2, "hT2%d" % i, [128, 8, B2], BF16) for i in range(2)]
            qT = sb(p2, "qT2", [128, 8, B2], BF16)
            P2 = [sb(p2, "P2%d" % i, [128, 4, 256], BF16) for i in range(2)]
            P2n = [sb(p2, "P2n%d" % i, [128, 4, 256], BF16) for i in range(2)]
            PT2 = sb(p2, "PT2", [128, 4, 2, B2], BF16)
            oT = sb(p2, "oT2", [128, 8, B2], BF16)
            st2 = [dict(mx=sb(p2, "c_mx%d" % i, [128, 4], F32), rs=sb(p2, "c_rs%d" % i, [128, 4], F32),
                        ri=sb(p2, "c_ri%d" % i, [128, 4], F32)) for i in range(2)]
            set_gain(SM_CROSS)
            tc2 = 0
            for b in range(NT // B2):
                h = hT[b % 2]
                for t in range(4):
                    norm_T(x_res[:, b * 4 + t, :], x_res.rs[b * 4 + t], pT[0], h[:, :, t * 128:(t + 1) * 128], h.res)
                for oc in range(8):
                    pp = pj[pjc % 2]
                    pjc += 1
                    mm_group(pp[:], [(w_q[:, kc, oc * 128:(oc + 1) * 128], h[:, kc, :]) for kc in range(8)],
                             r=[w_q.res, h.res], w=[pp.res])
                    k.op("act", lambda e, pp=pp, oc=oc: e.activation(out=qT[:, oc, :], in_=pp[:], func=AF.Copy), r=[pp.res], w=[qT.res])
                def c_S1(t, i2):
                    pS_ = pS2[i2]
                    for hh_ in range(4):
                        mm_group(pS_[:, hh_, :], [(qT[:, 2 * hh_ + j, t * 128:(t + 1) * 128], kmT[:, 2 * hh_ + j, :]) for j in range(2)],
                                 r=[qT.res, kmT.res], w=[pS_.res])

                def c_S2(t, i2):
                    pS_, P_, Pn_, s_ = pS2[i2], P2[i2], P2n[i2], st2[i2]
                    k.op("dve", lambda e: e.tensor_reduce(out=s_["mx"][:], in_=pS_[:], axis=AX.X, op=ALU.max, negate=True),
                         r=[pS_.res], w=[s_["mx"].res])
                    k.op("dve", lambda e: e.tensor_scalar(out=s_["mx"][:], in0=s_["mx"][:], scalar1=1.0 / 16, scalar2=None, op0=ALU.mult),
                         r=[s_["mx"].res], w=[s_["mx"].res])
                    for hh_ in range(4):
                        k.op("act", lambda e, hh_=hh_: e.activation(
                            out=P_[:, hh_, :], in_=pS_[:, hh_, :], func=AF.Exp, bias=s_["mx"][:, hh_:hh_ + 1], scale=1.0 / 16,
                            accum_out=s_["rs"][:, hh_:hh_ + 1]), r=[pS_.res, s_["mx"].res], w=[P_.res, s_["rs"].res])
                    k.op("dve", lambda e: e.reciprocal(out=s_["ri"][:], in_=s_["rs"][:]), r=[s_["rs"].res], w=[s_["ri"].res])
                    k.op("dve", lambda e: e.tensor_tensor(
                        out=Pn_[:], in0=P_[:], in1=s_["ri"][:, :].unsqueeze(2).to_broadcast([128, 4, 256]), op=ALU.mult),
                        r=[P_.res, s_["ri"].res], w=[Pn_.res])

                def c_S3(t, i2):
                    Pn_ = P2n[i2]
                    for hh_ in range(4):
                        for mc in range(2):
                            k.op("pe", lambda e, hh_=hh_, mc=mc: e.transpose(
                                out=pPT2[:, hh_ * 2 + mc, :], in_=Pn_[:, hh_, mc * 128:(mc + 1) * 128], identity=ident_b[:]),
                                r=[Pn_.res, ident_b.res], w=[pPT2.res])
                    k.op("act", lambda e: e.activation(out=PT2[:, :, :, t * 128:(t + 1) * 128],
                                                       in_=pPT2[:].rearrange("p (h m) t -> p h m t", h=4), func=AF.Copy),
                         r=[pPT2.res], w=[PT2.res])

                c_S1(0, tc2 % 2)
                for t in range(4):
                    if t + 1 < 4:
                        c_S1(t + 1, (tc2 + 1) % 2)
                    c_S2(t, tc2 % 2)
                    c_S3(t, tc2 % 2)
                    tc2 += 1
                for oc in range(8):
                    pp = pj[pjc % 2]
                    pjc += 1
                    mm_group(pp[:], [(vmem[:, mc, oc * 128:(oc + 1) * 128], PT2[:, oc // 2, mc, :]) for mc in range(2)],
                             r=[vmem.res, PT2.res], w=[pp.res])
                    k.op("act", lambda e, pp=pp, oc=oc: e.activation(out=oT[:, oc, :], in_=pp[:], func=AF.Copy), r=[pp.res], w=[oT.res])
                for t in range(4):
                    tile = b * 4 + t
                    for half in range(2):
                        pp = pj[pjc % 2]
                        pjc += 1
                        mm_group(pp[:], [(oT[:, c, t * 128:(t + 1) * 128], w_o[:, c, half * 512:(half + 1) * 512]) for c in range(8)],
                                 r=[oT.res, w_o.res], w=[pp.res])
                        k.op("dve", lambda e, pp=pp, tile=tile, half=half: e.tensor_tensor(
                            out=x_res[:, tile, half * 512:(half + 1) * 512], in0=pp[:], in1=x_res[:, tile, half * 512:(half + 1) * 512],
                            op=ALU.add), r=[pp.res, x_res.rs[tile]], w=[x_res.rs[tile]])
            k.barrier(release=[w_q.res, w_o.res])

        if debug:
            for tile in range(NTILE):
                k.dma("sp", dbg["dbg2"][tile * 128:(tile + 1) * 128, :], x_res[:, tile, :], r=[x_res.rs[tile]], w=[], sres=x_res.rs[tile])

        with ExitStack() as p3:
            p3s = p3.enter_context(ExitStack())
            slotI = sb(p3s, "slotI", [128, NT], F32)
            slotJ = sb(p3s, "slotJ", [128, NT], F32)
            slotG = sb(p3s, "slotG", [128, NT], F32)
            set_gain(SM_FFN)
            with ExitStack() as pa:
                B3 = 256
                w_qry = sb(pa, "w_qry", [128, 8, 2048], BF16)
                load_w_bf16(w_qry, w_qry_d[:, :], 2048)
                skb = sb(pa, "skb", [128, 16, 128], BF16)
                k.dma("pool", skb[:], skT_d.rearrange("p (o n) -> p o n", o=16), r=[], w=[skb.res], sres=skb.res)
                hT = [sb(pa, "hT3%d" % i, [128, 8, B3], BF16) for i in range(2)]
                qpT = sb(pa, "qpT", [128, 16, B3], BF16)
                sc = sb(pa, "sc", [128, 16, 128], F32, nres=4)
                sc2 = sb(pa, "sc2", [128, 16, 128], F32, nres=16)
                tv = sb(pa, "tv", [128, 16, 16], F32, nres=16)
                tiu = sb(pa, "tiu", [128, 16, 16], U32, nres=16)
                tif = sb(pa, "tif", [128, 16, 16], F32)
                cand = sb(pa, "cand", [128, 8, 256], F32)
                cand2 = sb(pa, "cand2", [128, 8, 256], F32, nres=8)
                ts = sb(pa, "ts", [128, 8, 16], F32, nres=8)
                posu = sb(pa, "posu", [128, 8, 16], U32, nres=8)
                au = sb(pa, "au", [128, 8, 16], U32)
                bu = sb(pa, "bu", [128, 8, 16], U32)
                af = sb(pa, "af", [128, 8, 16], F32)
                bf = sb(pa, "bf", [128, 8, 16], F32)
                eq = sb(pa, "eq", [128, 8, 16, 16], F32)
                isel = sb(pa, "isel", [128, 8, 16], F32)
                jsel = sb(pa, "jsel", [128, 8, 16], F32)
                gsel = sb(pa, "gsel", [128, 8, 16], F32)
                sm = sb(pa, "sm", [128, 8], F32)
                pj = [ps(pa, "pj3%d" % i, [128, 512], F32) for i in range(2)]
                pT = [ps(pa, "pT3%d" % i, [128, 8, 128], BF16) for i in range(2)]
                psc = [ps(pa, "psc%d" % i, [128, 4, 128], F32) for i in range(2)]
                pTs = ps(pa, "pTs", [128, 3, 128], F32)
                pjc = 0
                scc = 0
                iota16 = iota_f[:, 0:16]
                for b in range(NT // B3):
                    h = hT[b % 2]
                    norm_pairs([(x_res[:, b * 2 + t, :], x_res.rs[b * 2 + t], pT[t % 2], h[:, :, t * 128:(t + 1) * 128], h.res) for t in range(2)])
                    for oc in range(16):
                        pp = pj[pjc % 2]
                        pjc += 1
                        mm_group(pp[:, 0:B3], [(w_qry[:, kc, oc * 128:(oc + 1) * 128], h[:, kc, :]) for kc in range(8)],
                                 r=[w_qry.res, h.res], w=[pp.res])
                        k.op("act", lambda e, pp=pp, oc=oc: e.activation(out=qpT[:, oc, :], in_=pp[:, 0:B3], func=AF.Copy),
                             r=[pp.res], w=[qpT.res])
                    for t in range(2):
                        tile = b * 2 + t
                        for g4 in range(4):
                            pq = psc[scc % 2]
                            scc += 1
                            for j in range(4):
                                oc = g4 * 4 + j
                                k.op("pe", lambda e, pq=pq, j=j, oc=oc, t=t: e.matmul(
                                    pq[:, j, :], lhsT=qpT[:, oc, t * 128:(t + 1) * 128], rhs=skb[:, oc, :], start=True, stop=True),
                                    r=[qpT.res, skb.res], w=[pq.res])
                            k.op("act", lambda e, pq=pq, g4=g4: e.activation(out=sc[:, g4 * 4:(g4 + 1) * 4, :], in_=pq[:], func=AF.Copy),
                                 r=[pq.res], w=[sc.rs[g4]])
                        for oc in range(16):
                            k.op("dve", lambda e, oc=oc: e.max(out=tv[:, oc, 0:8], in_=sc[:, oc, :]), r=[sc.rs[oc // 4]], w=[tv.rs[oc]])
                        for oc in range(16):
                            k.op("dve", lambda e, oc=oc: e.max_index(out=tiu[:, oc, 0:8], in_max=tv[:, oc, 0:8], in_values=sc[:, oc, :]),
                                 r=[sc.rs[oc // 4], tv.rs[oc]], w=[tiu.rs[oc]])
                        for oc in range(16):
                            k.op("dve", lambda e, oc=oc: e.match_replace(out=sc2[:, oc, :], in_to_replace=tv[:, oc, 0:8], in_values=sc[:, oc, :],
                                                                        imm_value=NEG), r=[sc.rs[oc // 4], tv.rs[oc]], w=[sc2.rs[oc]])
                        for oc in range(16):
                            k.op("dve", lambda e, oc=oc: e.max(out=tv[:, oc, 8:16], in_=sc2[:, oc, :]), r=[sc2.rs[oc]], w=[tv.rs[oc]])
                        for oc in range(16):
                            k.op("dve", lambda e, oc=oc: e.max_index(out=tiu[:, oc, 8:16], in_max=tv[:, oc, 8:16], in_values=sc2[:, oc, :]),
                                 r=[sc2.rs[oc], tv.rs[oc]], w=[tiu.rs[oc]])
                        k.op("dve", lambda e: e.tensor_copy(out=tif[:], in_=tiu[:]), r=tiu.rs, w=[tif.res])
                        tv4 = tv[:].rearrange("p (h two) a -> p h two a", two=2)
                        tif4 = tif[:].rearrange("p (h two) a -> p h two a", two=2)
                        k.op("dve", lambda e, tv4=tv4: e.tensor_tensor(
                            out=cand[:].rearrange("p h (a b) -> p h a b", a=16),
                            in0=tv4[:, :, 0, :].unsqueeze(3).to_broadcast([128, 8, 16, 16]),
                            in1=tv4[:, :, 1, :].unsqueeze(2).to_broadcast([128, 8, 16, 16]), op=ALU.add),
                            r=tv.rs, w=[cand.res])
                        for hd in range(8):
                            k.op("dve", lambda e, hd=hd: e.max(out=ts[:, hd, 0:8], in_=cand[:, hd, :]), r=[cand.res], w=[ts.rs[hd]])
                        for hd in range(8):
                            k.op("dve", lambda e, hd=hd: e.max_index(out=posu[:, hd, 0:8], in_max=ts[:, hd, 0:8], in_values=cand[:, hd, :]),
                                 r=[cand.res, ts.rs[hd]], w=[posu.rs[hd]])
                        for hd in range(8):
                            k.op("dve", lambda e, hd=hd: e.match_replace(out=cand2[:, hd, :], in_to_replace=ts[:, hd, 0:8], in_values=cand[:, hd, :],
                                                                        imm_value=NEG), r=[cand.res, ts.rs[hd]], w=[cand2.rs[hd]])
                        for hd in range(8):
                            k.op("dve", lambda e, hd=hd: e.max(out=ts[:, hd, 8:16], in_=cand2[:, hd, :]), r=[cand2.rs[hd]], w=[ts.rs[hd]])
                        for hd in range(8):
                            k.op("dve", lambda e, hd=hd: e.max_index(out=posu[:, hd, 8:16], in_max=ts[:, hd, 8:16], in_values=cand2[:, hd, :]),
                                 r=[cand2.rs[hd], ts.rs[hd]], w=[posu.rs[hd]])
                        k.op("dve", lambda e: e.tensor_scalar(out=au[:], in0=posu[:], scalar1=4, scalar2=None, op0=ALU.logical_shift_right),
                             r=posu.rs, w=[au.res])
                        k.op("dve", lambda e: e.tensor_scalar(out=bu[:], in0=posu[:], scalar1=15, scalar2=None, op0=ALU.bitwise_and),
                             r=posu.rs, w=[bu.res])
                        k.op("dve", lambda e: e.tensor_copy(out=af[:], in_=au[:]), r=[au.res], w=[af.res])
                        k.op("dve", lambda e: e.tensor_copy(out=bf[:], in_=bu[:]), r=[bu.res], w=[bf.res])
                        for (sel, rk, which) in ((isel, af, 0), (jsel, bf, 1)):
                            k.op("dve", lambda e, rk=rk: e.tensor_tensor(
                                out=eq[:], in0=rk[:].unsqueeze(3).to_broadcast([128, 8, 16, 16]),
                                in1=iota16.unsqueeze(1).unsqueeze(1).to_broadcast([128, 8, 16, 16]), op=ALU.is_equal),
                                r=[rk.res, iota_f.res], w=[eq.res])
                            k.op("dve", lambda e, which=which, tif4=tif4: e.tensor_tensor(
                                out=eq[:], in0=eq[:], in1=tif4[:, :, which, :].unsqueeze(2).to_broadcast([128, 8, 16, 16]), op=ALU.mult),
                                r=[eq.res, tif.res], w=[eq.res])
                            k.op("dve", lambda e, sel=sel: e.tensor_reduce(out=sel[:], in_=eq[:], axis=AX.X, op=ALU.add),
                                 r=[eq.res], w=[sel.res])
                        k.op("dve", lambda e: e.tensor_tensor(out=gsel[:], in0=ts[:], in1=ts[:, :, 0:1].to_broadcast([128, 8, 16]), op=ALU.subtract),
                             r=ts.rs, w=[gsel.res])
                        k.op("act", lambda e: e.activation(out=gsel[:], in_=gsel[:], func=AF.Exp), r=[gsel.res], w=[gsel.res])
                        k.op("dve", lambda e: e.tensor_reduce(out=sm[:], in_=gsel[:], axis=AX.X, op=ALU.add), r=[gsel.res], w=[sm.res])
                        k.op("dve", lambda e: e.reciprocal(out=sm[:], in_=sm[:]), r=[sm.res], w=[sm.res])
                        k.op("dve", lambda e: e.tensor_tensor(out=gsel[:], in0=gsel[:], in1=sm[:, :].unsqueeze(2).to_broadcast([128, 8, 16]), op=ALU.mult),
                             r=[gsel.res, sm.res], w=[gsel.res])
                        for i3, (src, dst) in enumerate(((isel, slotI), (jsel, slotJ), (gsel, slotG))):
                            k.op("pe", lambda e, i3=i3, src=src: e.transpose(out=pTs[:, i3, :], in_=src[:].rearrange("p h r -> p (h r)"),
                                                                            identity=ident_f[:]),
                                 r=[src.res, ident_f.res], w=[pTs.res])
                        for i3, (src, dst) in enumerate(((isel, slotI), (jsel, slotJ), (gsel, slotG))):
                            k.op("act", lambda e, i3=i3, dst=dst, tile=tile: e.activation(out=dst[:, tile * 128:(tile + 1) * 128], in_=pTs[:, i3, :], func=AF.Copy),
                                 r=[pTs.res], w=[dst.res])
                k.barrier(release=[w_qry.res, skb.res])

            with ExitStack() as pb:
                TCH = 8
                ohj = [sb(pb, "ohj%d" % i, [128, TCH, 128], BF16) for i in range(4)]
                eqi = [sb(pb, "eqi%d" % i, [128, TCH, 128], BF16) for i in range(4)]
                rig = [sb(pb, "rig%d" % i, [128, TCH, 128], BF16, nres=TCH) for i in range(4)]
                Wst = [sb(pb, "Wst%d" % i, [128, 128, 128], BF16, nres=32) for i in range(2)]
                pW = [ps(pb, "pW%d" % i, [128, 4, 128], F32) for i in range(4)]
                wc = 0
                chc = 0
                iota_b3 = iota_f[:, :].unsqueeze(1).to_broadcast([128, TCH, 128])
                for tile in range(NTILE):
                    W_ = Wst[tile % 2]
                    for ch in range(128 // TCH):
                        oj, ei, rg = ohj[chc % 4], eqi[chc % 4], rig[chc % 4]
                        chc += 1
                        tok0 = tile * 128 + ch * TCH
                        k.op("dve", lambda e, oj=oj, tok0=tok0: e.tensor_tensor(
                            out=oj[:], in0=iota_b3, in1=slotJ[:, tok0:tok0 + TCH].unsqueeze(2).to_broadcast([128, TCH, 128]), op=ALU.is_equal),
                            r=[iota_f.res, slotJ.res], w=[oj.res])
                        k.op("dve", lambda e, ei=ei, tok0=tok0: e.tensor_tensor(
                            out=ei[:], in0=iota_b3, in1=slotI[:, tok0:tok0 + TCH].unsqueeze(2).to_broadcast([128, TCH, 128]), op=ALU.is_equal),
                            r=[iota_f.res, slotI.res], w=[ei.res])
                        if chc % 9 < 4:
                            for tt_ in range(TCH):
                                k.op("act", lambda e, ei=ei, rg=rg, tt_=tt_, tok0=tok0: e.activation(
                                    out=rg[:, tt_, :], in_=ei[:, tt_, :], func=AF.Copy, scale=slotG[:, tok0 + tt_:tok0 + tt_ + 1]),
                                    r=[ei.res, slotG.res], w=[rg.rs[tt_]])
                        else:
                            k.op("dve", lambda e, ei=ei, rg=rg, tok0=tok0: e.tensor_tensor(
                                out=rg[:], in0=ei[:], in1=slotG[:, tok0:tok0 + TCH].unsqueeze(2).to_broadcast([128, TCH, 128]), op=ALU.mult),
                                r=[ei.res, slotG.res], w=rg.rs)
                        for q4 in range(TCH // 4):
                            pw = pW[wc % 4]
                            for j in range(4):
                                tt_ = q4 * 4 + j
                                k.op("pe", lambda e, pw=pw, j=j, oj=oj, rg=rg, tt_=tt_: e.matmul(
                                    pw[:, j, :], lhsT=oj[:, tt_, :], rhs=rg[:, tt_, :], start=True, stop=True),
                                    r=[oj.res, rg.rs[tt_]], w=[pw.res])
                            t0 = ch * TCH + q4 * 4
                            if True:
                                k.op("act", lambda e, pw=pw, W_=W_, t0=t0: e.activation(
                                    out=W_[:, :, t0:t0 + 4], in_=pw[:].rearrange("p t i -> p i t"), func=AF.Copy),
                                    r=[pw.res], w=[W_.rs[t0 // 4]])
                            else:
                                k.op("dve", lambda e, pw=pw, W_=W_, t0=t0: e.tensor_copy(
                                    out=W_[:, :, t0:t0 + 4], in_=pw[:].rearrange("p t i -> p i t")),
                                    r=[pw.res], w=[W_.rs[t0 // 4]])
                            wc += 1
                    k.dma("sp", wd_d[tile], W_[:].rearrange("p i t -> p (i t)"), r=W_.rs, w=[], sres=W_.res)
                k.barrier(release=[w_.res for w_ in Wst])
            p3s.close()

            with ExitStack() as pc:
                T3 = 256
                GS = 8
                NG = 128 // GS
                hfT = sb(pc, "hfT", [128, 8, NT], BF16)
                pT = [ps(pc, "pT4%d" % i, [128, 8, 128], BF16) for i in range(1)]
                pA = [ps(pc, "pA%d" % i, [128, 512], F32) for i in range(3)]
                pO = [ps(pc, "pO4%d" % i, [128, 512], F32) for i in range(4)]
                for tile in range(NTILE):
                    norm_T(x_res[:, tile, :], x_res.rs[tile], pT[0], hfT[:, :, tile * 128:(tile + 1) * 128], hfT.res)
                ut = [sb(pc, "ut%d" % i, [128, GS, 8, 128], BF16) for i in range(2)]
                vt = [sb(pc, "vt%d" % i, [128, GS, D], BF16) for i in range(2)]
                wsl = [sb(pc, "wsl%d" % i, [128, 2, GS, 128], BF16) for i in range(2)]
                ge = [sb(pc, "ge%d" % i, [128, T3], F32) for i in range(3)]
                gw = [sb(pc, "gw%d" % i, [128, T3], BF16) for i in range(3)]
                uT_v = uT_d.rearrange("(i p) (c e) -> p i c e", p=128, c=8)
                ev_v = ev_d.rearrange("(i e) d -> e i d", e=128)
                wd_v = wd_d.rearrange("t j (i x) -> j t i x", i=128)
                items = [(g, tb, i) for g in range(NG) for tb in range(NT // T3) for i in range(GS)]
                state = {}

                def emitA(n):
                    g, tb, i = items[n]
                    u_, v_ = ut[g % 2], vt[g % 2]
                    if tb == 0 and i == 0:
                        for i_ in range(GS):
                            k.dma("pool", u_[:, i_, :, :], uT_v[:, g * GS + i_, :, :], r=[], w=[u_.res], sres=u_.res)
                            k.dma("pool", v_[:, i_, :], ev_v[:, g * GS + i_, :], r=[], w=[v_.res], sres=v_.res, max_dma_last_dim=4096)
                    if i == 0:
                        ws_ = wsl[(g * (NT // T3) + tb) % 2]
                        k.dma("sp", ws_[:], wd_v[:, 2 * tb:2 * tb + 2, g * GS:(g + 1) * GS, :], r=[], w=[ws_.res], sres=ws_.res)
                    ws_ = wsl[(g * (NT // T3) + tb) % 2]
                    pa_ = pA[n % 3]
                    ge_, gw_ = ge[n % 3], gw[n % 3]
                    mm_group(pa_[:, 0:T3], [(u_[:, i, c, :], hfT[:, c, tb * T3:(tb + 1) * T3]) for c in range(8)],
                             r=[u_.res, hfT.res], w=[pa_.res])
                    k.op("act", lambda e: e.activation(out=ge_[:], in_=pa_[:, 0:T3], func=AF.Gelu_apprx_tanh),
                         r=[pa_.res], w=[ge_.res])
                    k.op("dve", lambda e: e.tensor_tensor(
                        out=gw_[:].rearrange("p (a t) -> p a t", a=2), in0=ge_[:].rearrange("p (a t) -> p a t", a=2),
                        in1=ws_[:, :, i, :], op=ALU.mult), r=[ge_.res, ws_.res], w=[gw_.res])

                def emitV(n):
                    g, tb, i = items[n]
                    v_ = vt[g % 2]
                    gw_ = gw[n % 3]
                    for t in range(2):
                        for half in range(2):
                            po = pO[t * 2 + half]
                            k.op("pe", lambda e, po=po, t=t, half=half: e.matmul(
                                po[:], lhsT=gw_[:, t * 128:(t + 1) * 128], rhs=v_[:, i, half * 512:(half + 1) * 512],
                                start=(i == 0), stop=(i == GS - 1)), r=[gw_.res, v_.res], w=[po.res])
                    if i == GS - 1:
                        for t in range(2):
                            tile = tb * 2 + t
                            for half in range(2):
                                po = pO[t * 2 + half]
                                k.op("dve", lambda e, po=po, tile=tile, half=half: e.tensor_tensor(
                                    out=x_res[:, tile, half * 512:(half + 1) * 512], in0=po[:], in1=x_res[:, tile, half * 512:(half + 1) * 512],
                                    op=ALU.add), r=[po.res, x_res.rs[tile]], w=[x_res.rs[tile]])

                emitA(0)
                emitA(1)
                for n in range(len(items)):
                    if n + 2 < len(items):
                        emitA(n + 2)
                    emitV(n)
                k.barrier(release=[t_.res for t_ in ut + vt + wsl])

        with ExitStack() as p4:
            gfin = sb(p4, "gfin", [128, D], F32)
            k.dma("sp", gfin[:], gfin_d[:, :], r=[], w=[gfin.res], sres=gfin.res)
            ot = [sb(p4, "ot%d" % i, [128, D], F32) for i in range(2)]
            for tile in range(NTILE):
                n = nrm[tile % 2]
                o_ = ot[tile % 2]
                xa = x_res[:, tile, :]
                xr = x_res.rs[tile]
                k.op("dve", lambda e, n=n, xa=xa: e.scalar_tensor_tensor(out=n["junk"][:], in0=xa, scalar=1.0, in1=xa,
                                                                         op0=ALU.mult, op1=ALU.mult, accum_out=n["ss"][:]),
                     r=[xr], w=[n["junk"].res, n["ss"].res])
                k.op("act", lambda e, n=n: e.activation(out=n["sd"][:], in_=n["ss"][:], func=AF.Ln, bias=cst[:, 0:1], scale=1.0 / D),
                     r=[n["ss"].res, cst.res], w=[n["sd"].res])
                k.op("act", lambda e, n=n: e.activation(out=n["rstd"][:], in_=n["sd"][:], func=AF.Exp, scale=-0.5),
                     r=[n["sd"].res], w=[n["rstd"].res])
                k.op("dve", lambda e, n=n, xa=xa, o_=o_: e.scalar_tensor_tensor(out=o_[:], in0=xa, scalar=n["rstd"][:, 0:1], in1=gfin[:],
                                                                                op0=ALU.mult, op1=ALU.mult),
                     r=[xr, n["rstd"].res, gfin.res], w=[o_.res])
                k.dma("sp", y_d[tile * 128:(tile + 1) * 128, :], o_[:], r=[o_.res], w=[], sres=o_.res)
            k.barrier()
    return nc, k.n_ins


def prep_shared(inp):
    f = np.float32
    g = lambda a: np.ascontiguousarray(np.asarray(a, dtype=f))
    sm = np.zeros((128, NSM), f)

    def put(col, vec, nch):
        sm[:, col:col + nch] = np.asarray(vec, f).reshape(nch, 128).T

    put(SM_MIX, inp["norm_mix"][0], 8)
    put(SM_CROSS, inp["norm_cross"][0], 8)
    put(SM_MEM, inp["norm_mem"][0], 8)
    put(SM_FFN, inp["norm_ffn"][0], 8)
    put(SM_GA, inp["norm_grp_a"][0], 4)
    put(SM_GB, inp["norm_grp_b"][0], 4)
    put(SM_CB, inp["conv_b"][0], 4)
    put(SM_BA, inp["gate_a_b"][0], 4)
    put(SM_BX, inp["gate_x_b"][0], 4)
    put(SM_LAM, inp["lru_lambda"][0], 4)
    cw = np.asarray(inp["conv_w"][0], f)
    for cc in range(4):
        for j in range(4):
            sm[:, SM_CW + cc * 4 + j] = cw[j, cc * 128:(cc + 1) * 128]
    gbd = np.zeros((128, 8, 128), f)
    for gi, key in enumerate(("gate_a_w", "gate_x_w")):
        w = np.asarray(inp[key][0], f)
        for cc in range(4):
            gbd[0:64, gi * 4 + cc, 0:64] = w[2 * cc]
            gbd[64:128, gi * 4 + cc, 64:128] = w[2 * cc + 1]
    rb = np.asarray(inp["rel_bias"][0], f)
    qi = np.arange(128)[:, None]
    kj = np.arange(640)[None, :]
    idx = np.clip(512 + qi - kj, -128, 128) + 128
    ab = rb[:, idx]
    valid = np.where(qi < 64, kj < 576, kj >= 64)
    ab = np.where(valid[None], ab, f(NEG)).astype(f)
    abias = np.ascontiguousarray(ab.transpose(1, 0, 2)).reshape(128, 8 * 640)
    sk = np.asarray(inp["sub_keys"][0], f)
    skT = np.ascontiguousarray(sk.reshape(16, 128, 128).transpose(2, 0, 1)).reshape(128, 16 * 128)
    u = np.asarray(inp["expert_u"][0], f)
    uT = np.ascontiguousarray(u.reshape(128, 128, 8, 128).transpose(0, 3, 2, 1)).reshape(16384, D)
    shared = {
        "w_in": g(inp["w_in"][0]), "w_out": g(inp["w_out"][0]), "w_q": g(inp["w_q_mem"][0]),
        "w_kv": g(inp["w_kv_mem"][0]), "w_o": g(inp["w_o_mem"][0]), "w_qry": g(inp["w_query"][0]),
        "smalls": sm, "gbd": gbd.reshape(128, 8 * 128), "abias": abias, "skT": skT, "uT": uT,
        "ev": g(inp["expert_v"][0]),
        "gfin": np.ascontiguousarray(np.broadcast_to(np.asarray(inp["norm_final"], f)[None, :], (128, D))),
        "ident": np.eye(128, dtype=f),
        "iota": np.ascontiguousarray(np.broadcast_to(np.arange(128, dtype=f)[None, :], (128, 128))),
    }
    return shared


def make_in_maps(inp, NT):
    x = np.asarray(inp["x"], np.float32)
    mem = np.asarray(inp["mem"], np.float32)
    B, S, _ = x.shape
    per_seq = S // NT
    NPRE = 3 * NT
    shared = prep_shared(inp)
    maps = []
    for c in range(B * per_seq):
        b, q = divmod(c, per_seq)
        xprev = np.zeros((NPRE, D), np.float32)
        if q > 0:
            xprev[NPRE - q * NT:] = x[b, 0:q * NT]
        pflag = np.zeros((128, NPRE // 512), np.float32)
        for j in range(NPRE // 512):
            if j * 512 >= NPRE - q * NT:
                pflag[:, j] = 1.0
        halob = np.full((128, 512), 0.0 if q > 0 else NEG, np.float32)
        m = dict(shared)
        m.update({"xown": np.ascontiguousarray(x[b, q * NT:(q + 1) * NT]), "xprev": xprev, "pflag": pflag,
                  "halob": halob, "mem": np.ascontiguousarray(mem[b])})
        maps.append(m)
    return maps


_CACHE = {}


def kernel(**inputs):
    NT = 2048
    if NT not in _CACHE:
        _CACHE[NT] = build(NT)[0]
    nc = _CACHE[NT]
    maps = make_in_maps(inputs, NT)
    res = run_bass_kernel_spmd(nc, maps, core_ids=list(range(N_CORES)))
    x = np.asarray(inputs["x"])
    B, S, _ = x.shape
    out = np.empty((B, S, D), np.float32)
    per_seq = S // NT
    for c in range(N_CORES):
        b, q = divmod(c, per_seq)
        out[b, q * NT:(q + 1) * NT] = res.results[c]["y"]
    return out
```

```python
import numpy as np
from contextlib import ExitStack
import concourse.bass as bass
import concourse.mybir as mybir
from concourse.bass_utils import run_bass_kernel_spmd

F32 = mybir.dt.float32
BF16 = mybir.dt.bfloat16
U32 = mybir.dt.uint32
AF = mybir.ActivationFunctionType
ALU = mybir.AluOpType
AX = mybir.AxisListType

D = 1024
EPS = 1e-6
NEG = -1e30
N_CORES = 8
SAME_ENG_SYNC = True

SM_MIX, SM_CROSS, SM_MEM, SM_FFN = 0, 8, 16, 24
SM_GA, SM_GB, SM_CB, SM_BA, SM_BX, SM_LAM, SM_CW = 32, 36, 40, 44, 48, 52, 56
NSM = 72


class Res:
    __slots__ = ("name", "w", "r", "ds")

    def __init__(self, name):
        self.name = name
        self.w = None
        self.r = {}
        self.ds = None


class KB:
    def __init__(self, nc, es):
        self.nc = nc
        self.es = es
        self.E = {}
        for nm, e in (("pe", nc.tensor), ("act", nc.scalar), ("dve", nc.vector),
                      ("pool", nc.gpsimd), ("sp", nc.sync)):
            self.E[nm] = dict(e=e, sem=es.enter_context(nc.semaphore("e_" + nm)), cnt=0, waited={}, nm=nm)
        self.free_ds = []
        self.all_ds = []
        self.n_ins = 0

    def _collect(self, r, w):
        deps = {}
        for x in r:
            if x.w is not None:
                s, v = x.w
                if deps.get(s, 0) < v:
                    deps[s] = v
        for x in w:
            if x.w is not None:
                s, v = x.w
                if deps.get(s, 0) < v:
                    deps[s] = v
            for s, v in x.r.items():
                if deps.get(s, 0) < v:
                    deps[s] = v
        return deps

    def _waits(self, E, deps, skip_own):
        for s, v in deps.items():
            if skip_own and s is E["sem"]:
                continue
            if E["waited"].get(s, 0) >= v:
                continue
            E["e"].wait_ge(s, v)
            E["waited"][s] = v

    def op(self, en, fn, r=(), w=()):
        E = self.E[en]
        skip_own = (en == "pe") or (not SAME_ENG_SYNC)
        self._waits(E, self._collect(r, w), skip_own)
        ins = fn(E["e"])
        E["cnt"] += 1
        self.n_ins += 1
        ins.then_inc(E["sem"], 1)
        tag = (E["sem"], E["cnt"])
        for x in w:
            x.w = tag
            x.r = {}
        for x in r:
            if x not in w:
                x.r[E["sem"]] = E["cnt"]
        return ins

    def dma(self, qn, out, in_, r, w, sres, **kw):
        E = self.E[qn]
        self._waits(E, self._collect(r, w), False)
        if sres.ds is None:
            if qn != "pool" and self.free_ds:
                sres.ds = self.free_ds.pop()
            else:
                sres.ds = [self.es.enter_context(self.nc.semaphore("d%d" % len(self.all_ds))), 0, qn]
                self.all_ds.append(sres.ds)
        ins = E["e"].dma_start(out=out, in_=in_, **kw)
        self.n_ins += 1
        sres.ds[1] += 1
        ins.then_inc(sres.ds[0], 16)
        val = sres.ds[1] * 16
        for x in w:
            x.w = (sres.ds[0], val)
            x.r = {}
        for x in r:
            if x not in w:
                x.r[sres.ds[0]] = val

    def barrier(self, release=()):
        deps = {}
        for nm in ("pe", "act", "dve", "pool"):
            E = self.E[nm]
            if E["cnt"] > 0:
                deps[E["sem"]] = E["cnt"]
        for ds in self.all_ds:
            if ds[1] > 0:
                deps[ds[0]] = ds[1] * 16
        for nm in ("pe", "act", "dve", "pool", "sp"):
            self._waits(self.E[nm], deps, True)
        for x in release:
            if x.ds is not None:
                if x.ds[2] != "pool":
                    self.free_ds.append(x.ds)
                x.ds = None


class Tn:
    def __init__(self, h, name, nres=1):
        self.h = h
        self.res = Res(name)
        self.rs = [Res("%s_%d" % (name, i)) for i in range(nres)] if nres > 1 else [self.res]

    def __getitem__(self, k):
        return self.h[k]


def build(NT=2048, debug=False):
    NPRE = 3 * NT
    NTILE = NT // 128
    nc = bass.Bass("TRN2", target_bir_lowering=False)
    dt_in = lambda n, s, d=F32: nc.dram_tensor(n, list(s), d, kind="ExternalInput").ap()
    xown = dt_in("xown", [NT, D])
    xprev = dt_in("xprev", [NPRE, D])
    pflag_d = dt_in("pflag", [128, NPRE // 512])
    halob_d = dt_in("halob", [128, 512])
    mem_d = dt_in("mem", [256, D])
    w_in_d = dt_in("w_in", [D, 2560])
    w_out_d = dt_in("w_out", [D, D])
    w_q_d = dt_in("w_q", [D, D])
    w_kv_d = dt_in("w_kv", [D, 2048])
    w_o_d = dt_in("w_o", [D, D])
    w_qry_d = dt_in("w_qry", [D, 2048])
    smalls_d = dt_in("smalls", [128, NSM])
    gbd_d = dt_in("gbd", [128, 8 * 128])
    abias_d = dt_in("abias", [128, 8 * 640])
    skT_d = dt_in("skT", [128, 16 * 128])
    uT_d = dt_in("uT", [16384, D])
    ev_d = dt_in("ev", [16384, D])
    gfin_d = dt_in("gfin", [128, D])
    ident_d = dt_in("ident", [128, 128])
    iota_d = dt_in("iota", [128, 128])
    y_d = nc.dram_tensor("y", [NT, D], F32, kind="ExternalOutput").ap()
    wd_d = nc.dram_tensor("wd_scratch", [NTILE, 128, 16384], BF16, kind="Internal").ap()
    dbg = {}
    if debug:
        for nm in ("dbg1", "dbg2"):
            dbg[nm] = nc.dram_tensor(nm, [NT, D], F32, kind="ExternalOutput").ap()

    with ExitStack() as es:
        k = KB(nc, es)

        def sb(ctx, name, shape, dt, nres=1):
            return Tn(ctx.enter_context(nc.sbuf_tensor("sb_" + name, list(shape), dt)), name, nres)

        def ps(ctx, name, shape, dt):
            return Tn(ctx.enter_context(nc.psum_tensor("ps_" + name, list(shape), dt)), name)

        x_res = sb(es, "x_res", [128, NTILE, D], F32, nres=NTILE)
        ident_f = sb(es, "ident_f", [128, 128], F32)
        ident_b = sb(es, "ident_b", [128, 128], BF16)
        iota_f = sb(es, "iota_f", [128, 128], F32)
        ones_b = sb(es, "ones_b", [128, 128], BF16)
        smalls = sb(es, "smalls", [128, NSM], F32)
        cst = sb(es, "cst", [128, 4], F32)
        gB = sb(es, "gB", [128, 8, 128], F32)
        nrm = [dict(ss=sb(es, "n_ss%d" % i, [128, 1], F32), sd=sb(es, "n_sd%d" % i, [128, 1], F32),
                    rstd=sb(es, "n_rstd%d" % i, [128, 1], F32), xs=sb(es, "n_xs%d" % i, [128, D], BF16),
                    junk=sb(es, "n_junk%d" % i, [128, D], BF16)) for i in range(2)]
        nrm_i = [0]

        k.dma("sp", ident_f[:], ident_d[:, :], r=[], w=[ident_f.res], sres=ident_f.res)
        k.dma("sp", iota_f[:], iota_d[:, :], r=[], w=[iota_f.res], sres=iota_f.res)
        k.dma("sp", smalls[:], smalls_d[:, :], r=[], w=[smalls.res], sres=smalls.res)
        k.op("dve", lambda e: e.tensor_copy(out=ident_b[:], in_=ident_f[:]), r=[ident_f.res], w=[ident_b.res])
        k.op("pool", lambda e: e.memset(ones_b[:], 1.0), w=[ones_b.res])
        k.op("pool", lambda e: e.memset(cst[:, 0:1], EPS), w=[cst.res])
        k.op("pool", lambda e: e.memset(cst[:, 1:2], 1.0), w=[cst.res])
        k.op("pool", lambda e: e.memset(cst[:, 2:3], 0.0), w=[cst.res])

        def set_gain(col):
            k.op("dve", lambda e: e.tensor_copy(
                out=gB[:], in_=smalls[:, col:col + 8].unsqueeze(2).to_broadcast([128, 8, 128])),
                r=[smalls.res], w=[gB.res])

        def norm_T_g(x_ap, x_r, pT, hT_ap, hT_r):
            n = nrm[nrm_i[0] % 2]
            nrm_i[0] += 1
            k.op("dve", lambda e: e.scalar_tensor_tensor(out=n["junk"][:], in0=x_ap, scalar=1.0, in1=x_ap,
                                                         op0=ALU.mult, op1=ALU.mult, accum_out=n["ss"][:]),
                 r=[x_r], w=[n["junk"].res, n["ss"].res])
            yield
            k.op("act", lambda e: e.activation(out=n["sd"][:], in_=n["ss"][:], func=AF.Ln,
                                               bias=cst[:, 0:1], scale=1.0 / D),
                 r=[n["ss"].res, cst.res], w=[n["sd"].res])
            k.op("act", lambda e: e.activation(out=n["rstd"][:], in_=n["sd"][:], func=AF.Exp, scale=-0.5),
                 r=[n["sd"].res], w=[n["rstd"].res])
            yield
            k.op("dve", lambda e: e.tensor_scalar(out=n["xs"][:], in0=x_ap, scalar1=n["rstd"][:, 0:1], scalar2=None,
                                                  op0=ALU.mult), r=[x_r, n["rstd"].res], w=[n["xs"].res])
            for c in range(8):
                k.op("pe", lambda e, c=c: e.transpose(out=pT[:, c, :], in_=n["xs"][:, c * 128:(c + 1) * 128],
                                                      identity=ident_b[:]),
                     r=[n["xs"].res, ident_b.res], w=[pT.res])
            yield
            k.op("dve", lambda e: e.tensor_tensor(out=hT_ap, in0=pT[:], in1=gB[:], op=ALU.mult),
                 r=[pT.res, gB.res], w=[hT_r])

        def norm_T(*a):
            for _ in norm_T_g(*a):
                pass

        def lockstep_g(gens):
            gens = list(gens)
            while gens:
                for g_ in list(gens):
                    try:
                        next(g_)
                    except StopIteration:
                        gens.remove(g_)
                yield

        def norm_pairs(args_list):
            for i_ in range(0, len(args_list), 2):
                for _ in lockstep_g([norm_T_g(*a) for a in args_list[i_:i_ + 2]]):
                    pass

        def mm_group(out_ap, pairs, r, w):
            n = len(pairs)
            for i, (l, rh) in enumerate(pairs):
                k.op("pe", lambda e, l=l, rh=rh, i=i: e.matmul(out_ap, lhsT=l, rhs=rh, start=(i == 0), stop=(i == n - 1)),
                     r=r, w=w)

        def load_w_bf16(dst, src_ap, ncols):
            for c in range(8):
                k.dma("pool", dst[:, c, :], src_ap[c * 128:(c + 1) * 128, :], r=[], w=[dst.res], sres=dst.res,
                      max_dma_last_dim=4096)

        with ExitStack() as p1:
            with ExitStack() as pa:
                BA = 512
                w_inA = sb(pa, "w_inA", [128, 8, 1024], BF16)
                load_w_bf16(w_inA, w_in_d[:, 0:1024], 1024)
                w_outA = sb(pa, "w_outA", [128, 4, D], BF16)
                for c in range(4):
                    k.dma("pool", w_outA[:, c, :], w_out_d[c * 128:(c + 1) * 128, :], r=[], w=[w_outA.res], sres=w_outA.res,
                          max_dma_last_dim=4096)
                yan = sb(pa, "yan", [128, 4, 512], BF16)
                gbd_f = sb(pa, "gbd_f", [128, 8, 128], F32)
                gbd = sb(pa, "gbd", [128, 8, 128], BF16)
                k.dma("sp", gbd_f[:], gbd_d.rearrange("p (c j) -> p c j", c=8), r=[], w=[gbd_f.res], sres=gbd_f.res)
                k.op("dve", lambda e: e.tensor_copy(out=gbd[:], in_=gbd_f[:]), r=[gbd_f.res], w=[gbd.res])
                pflag = sb(pa, "pflag", [128, NPRE // 512], F32)
                k.dma("sp", pflag[:], pflag_d[:, :], r=[], w=[pflag.res], sres=pflag.res)
                cL = sb(pa, "cL", [128, 4], F32)
                tmp4 = sb(pa, "tmp4", [128, 4], F32)
                k.op("act", lambda e: e.activation(out=tmp4[:], in_=smalls[:, SM_LAM:SM_LAM + 4], func=AF.Exp, scale=-1.0),
                     r=[smalls.res], w=[tmp4.res])
                k.op("act", lambda e: e.activation(out=cL[:], in_=tmp4[:], func=AF.Ln, bias=cst[:, 1:2], scale=1.0),
                     r=[tmp4.res, cst.res], w=[cL.res])
                k.op("dve", lambda e: e.tensor_scalar(out=cL[:], in0=cL[:], scalar1=-8.0, scalar2=None, op0=ALU.mult),
                     r=[cL.res], w=[cL.res])
                set_gain(SM_MIX)
                xtmp = [sb(pa, "xtmp%d" % i, [128, D], F32) for i in range(2)]
                hT = [sb(pa, "hTa%d" % i, [128, 8, BA], BF16) for i in range(2)]
                xl = sb(pa, "xl", [128, 4, 3 + BA], F32, nres=4)
                gg = sb(pa, "gg", [128, 4, BA], F32, nres=4)
                hh = sb(pa, "hh", [128, 4, BA], F32, nres=4)
                hst = sb(pa, "hst", [128, 4], F32, nres=4)
                yaT = sb(pa, "yaT", [128, 4, BA], F32, nres=4)
                LT = [{nm: sb(pa, "l%s%d" % (nm, i), [128, BA], BF16 if nm == "xcb" else F32)
                       for nm in ("xc", "xc2", "xcb", "rr", "ii", "aa", "tt", "bb")} for i in range(2)]
                sq = sb(pa, "sq", [128, BA], BF16)
                rsn = sb(pa, "rsn", [128, BA], F32)
                pj = [ps(pa, "pj%d" % i, [128, 512], F32) for i in range(2)]
                pT = [ps(pa, "pT%d" % i, [128, 8, 128], BF16) for i in range(2)]
                pg = [ps(pa, "pg%d" % i, [128, 512], F32) for i in range(4)]
                pn = pj[0]
                k.op("pool", lambda e: e.memset(xl[:], 0.0), w=xl.rs)
                k.op("pool", lambda e: e.memset(hst[:], 0.0), w=hst.rs)

                nblk_pre = NPRE // BA
                nblk_own = NT // BA
                tcount = 0
                pjc = 0
                def front_norm(b):
                    nonlocal tcount
                    own = b >= nblk_pre
                    ob = b - nblk_pre
                    h = hT[b % 2]

                    def tile_g(t):
                        nonlocal tcount
                        if own:
                            tile = ob * 4 + t
                            xa = x_res[:, tile, :]
                            xr = x_res.rs[tile]
                            k.dma("sp", xa, xown[tile * 128:(tile + 1) * 128, :], r=[], w=[xr], sres=xr)
                        else:
                            xt_ = xtmp[tcount % 2]
                            xa = xt_[:]
                            xr = xt_.res
                            r0 = b * BA + t * 128
                            k.dma("sp", xa, xprev[r0:r0 + 128, :], r=[], w=[xr], sres=xr)
                        tcount += 1
                        yield from norm_T_g(xa, xr, pT[t % 2], h[:, :, t * 128:(t + 1) * 128], h.res)

                    yield from lockstep_g([tile_g(0), tile_g(1)])
                    yield from lockstep_g([tile_g(2), tile_g(3)])

                def front_proj(b):
                    nonlocal pjc
                    own = b >= nblk_pre
                    h = hT[b % 2]
                    for cc in range(8 if own else 4):
                        pp = pj[pjc % 2]
                        pjc += 1
                        mm_group(pp[:], [(w_inA[:, kc, cc * 128:(cc + 1) * 128], h[:, kc, :]) for kc in range(8)],
                                 r=[w_inA.res, h.res], w=[pp.res])
                        if cc < 4:
                            k.op("act", lambda e, pp=pp, cc=cc: e.activation(out=xl[:, cc, 3:3 + BA], in_=pp[:], func=AF.Copy),
                                 r=[pp.res], w=[xl.rs[cc]])
                        else:
                            k.op("act", lambda e, pp=pp, cc=cc: e.activation(out=gg[:, cc - 4, :], in_=pp[:], func=AF.Gelu_apprx_tanh),
                                 r=[pp.res], w=[gg.rs[cc - 4]])

                for _ in front_norm(0):
                    pass
                front_proj(0)
                for b in range(nblk_pre + nblk_own):
                    own = b >= nblk_pre
                    ob = b - nblk_pre
                    fn = front_norm(b + 1) if b + 1 < nblk_pre + nblk_own else iter(())
                    def lru_chain(cc, L, pga, pgb, own=own, b=b):
                            cw = lambda j, cc=cc: smalls[:, SM_CW + cc * 4 + j:SM_CW + cc * 4 + j + 1]
                            k.op("dve", lambda e, cc=cc, L=L, cw=cw: e.tensor_scalar(
                                out=L["xc"][:], in0=xl[:, cc, 3:3 + BA], scalar1=cw(3), scalar2=smalls[:, SM_CB + cc:SM_CB + cc + 1],
                                op0=ALU.mult, op1=ALU.add), r=[xl.rs[cc], smalls.res], w=[L["xc"].res])
                            src, dst = "xc", "xc2"
                            for j in range(3):
                                yield
                                k.op("dve", lambda e, cc=cc, L=L, cw=cw, j=j, src=src, dst=dst: e.scalar_tensor_tensor(
                                    out=L[dst][:], in0=xl[:, cc, j:j + BA], scalar=cw(j), in1=L[src][:], op0=ALU.mult, op1=ALU.add),
                                    r=[xl.rs[cc], smalls.res, L[src].res], w=[L[dst].res])
                                src, dst = dst, src
                            xc = L[src]
                            yield
                            k.op("pool", lambda e, cc=cc: e.tensor_copy(out=xl[:, cc, 0:3], in_=xl[:, cc, BA:BA + 3]),
                                 r=[xl.rs[cc]], w=[xl.rs[cc]])
                            yield
                            k.op("act", lambda e, L=L, xc=xc: e.activation(out=L["xcb"][:], in_=xc[:], func=AF.Copy),
                                 r=[xc.res], w=[L["xcb"].res])
                            yield
                            k.op("pe", lambda e, cc=cc, L=L: e.matmul(pga[:], lhsT=gbd[:, cc, :], rhs=L["xcb"][:], start=True, stop=True),
                                 r=[gbd.res, L["xcb"].res], w=[pga.res])
                            yield
                            k.op("pe", lambda e, cc=cc, L=L: e.matmul(pgb[:], lhsT=gbd[:, 4 + cc, :], rhs=L["xcb"][:], start=True, stop=True),
                                 r=[gbd.res, L["xcb"].res], w=[pgb.res])
                            yield
                            k.op("act", lambda e, cc=cc, L=L: e.activation(out=L["rr"][:], in_=pga[:], func=AF.Sigmoid,
                                                                           bias=smalls[:, SM_BA + cc:SM_BA + cc + 1], scale=1.0),
                                 r=[pga.res, smalls.res], w=[L["rr"].res])
                            yield
                            k.op("act", lambda e, cc=cc, L=L: e.activation(out=L["ii"][:], in_=pgb[:], func=AF.Sigmoid,
                                                                           bias=smalls[:, SM_BX + cc:SM_BX + cc + 1], scale=1.0),
                                 r=[pgb.res, smalls.res], w=[L["ii"].res])
                            yield
                            k.op("act", lambda e, cc=cc, L=L: e.activation(out=L["aa"][:], in_=L["rr"][:], func=AF.Exp,
                                                                           scale=cL[:, cc:cc + 1]),
                                 r=[L["rr"].res, cL.res], w=[L["aa"].res])
                            yield
                            k.op("dve", lambda e, L=L: e.tensor_tensor(out=L["tt"][:], in0=L["aa"][:], in1=L["aa"][:], op=ALU.mult),
                                 r=[L["aa"].res], w=[L["tt"].res])
                            yield
                            k.op("act", lambda e, L=L: e.activation(out=L["tt"][:], in_=L["tt"][:], func=AF.Sqrt,
                                                                    bias=cst[:, 1:2], scale=-1.0),
                                 r=[L["tt"].res, cst.res], w=[L["tt"].res])
                            yield
                            k.op("dve", lambda e, L=L: e.tensor_tensor(out=L["bb"][:], in0=L["tt"][:], in1=L["ii"][:], op=ALU.mult),
                                 r=[L["tt"].res, L["ii"].res], w=[L["bb"].res])
                            yield
                            k.op("dve", lambda e, L=L, xc=xc: e.tensor_tensor(out=L["rr"][:], in0=L["bb"][:], in1=xc[:], op=ALU.mult),
                                 r=[L["bb"].res, xc.res], w=[L["rr"].res])
                            yield
                            k.op("dve", lambda e, cc=cc, L=L: e.tensor_tensor_scan(
                                out=hh[:, cc, :], data0=L["aa"][:], data1=L["rr"][:], initial=hst[:, cc:cc + 1],
                                op0=ALU.mult, op1=ALU.add), r=[L["aa"].res, L["rr"].res, hst.rs[cc]], w=[hh.rs[cc]])
                            if own:
                                yield
                                k.op("dve", lambda e, cc=cc: e.tensor_copy(out=hst[:, cc:cc + 1], in_=hh[:, cc, BA - 1:BA]),
                                     r=[hh.rs[cc]], w=[hst.rs[cc]])
                                yield
                                k.op("dve", lambda e, cc=cc: e.tensor_tensor(out=yaT[:, cc, :], in0=hh[:, cc, :], in1=gg[:, cc, :], op=ALU.mult),
                                     r=[hh.rs[cc], gg.rs[cc]], w=[yaT.rs[cc]])
                            else:
                                yield
                                k.op("dve", lambda e, cc=cc, b=b: e.tensor_tensor(out=hst[:, cc:cc + 1], in0=hh[:, cc, BA - 1:BA],
                                                                                  in1=pflag[:, b:b + 1], op=ALU.mult),
                                     r=[hh.rs[cc], pflag.res], w=[hst.rs[cc]])

                    for pair in range(2):
                        gens = [lru_chain(2 * pair + i_, LT[i_], pg[2 * i_], pg[2 * i_ + 1]) for i_ in range(2)]
                        while gens:
                            for g_ in list(gens):
                                try:
                                    next(g_)
                                except StopIteration:
                                    gens.remove(g_)
                            next(fn, None)
                    for _ in fn:
                        pass
                    if own:
                        for cc in range(4):
                            k.op("dve", lambda e, cc=cc: e.tensor_tensor(out=sq[:], in0=yaT[:, cc, :], in1=yaT[:, cc, :], op=ALU.mult),
                                 r=[yaT.rs[cc]], w=[sq.res])
                            k.op("pe", lambda e, cc=cc: e.matmul(pn[:], lhsT=ones_b[:], rhs=sq[:], start=(cc == 0), stop=(cc == 3)),
                                 r=[ones_b.res, sq.res], w=[pn.res])
                        k.op("act", lambda e: e.activation(out=rsn[:], in_=pn[:], func=AF.Ln, bias=cst[:, 0:1], scale=1.0 / 512),
                             r=[pn.res, cst.res], w=[rsn.res])
                        k.op("act", lambda e: e.activation(out=rsn[:], in_=rsn[:], func=AF.Exp, scale=-0.5), r=[rsn.res], w=[rsn.res])
                        for cc in range(4):
                            k.op("dve", lambda e, cc=cc, ob=ob: e.scalar_tensor_tensor(
                                out=yan[:, cc, :], in0=yaT[:, cc, :],
                                scalar=smalls[:, SM_GA + cc:SM_GA + cc + 1], in1=rsn[:], op0=ALU.mult, op1=ALU.mult),
                                r=[yaT.rs[cc], smalls.res, rsn.res], w=[yan.res])
                        for t in range(4):
                            tile = ob * 4 + t
                            for half in range(2):
                                pp = pj[pjc % 2]
                                pjc += 1
                                mm_group(pp[:], [(yan[:, c, t * 128:(t + 1) * 128], w_outA[:, c, half * 512:(half + 1) * 512]) for c in range(4)],
                                         r=[yan.res, w_outA.res], w=[pp.res])
                                k.op("dve", lambda e, pp=pp, tile=tile, half=half: e.tensor_tensor(
                                    out=x_res[:, tile, half * 512:(half + 1) * 512], in0=pp[:], in1=x_res[:, tile, half * 512:(half + 1) * 512],
                                    op=ALU.add), r=[pp.res, x_res.rs[tile]], w=[x_res.rs[tile]])
                    if b + 1 < nblk_pre + nblk_own:
                        front_proj(b + 1)
                k.barrier(release=[w_inA.res, w_outA.res, gbd_f.res, pflag.res] + [t_.res for t_ in xtmp])

            with ExitStack() as pb:
                BB = 256
                w_inB = sb(pb, "w_inB", [128, 8, 1536], BF16)
                load_w_bf16(w_inB, w_in_d[:, 1024:2560], 1536)
                w_out = sb(pb, "w_outB", [128, 4, D], BF16)
                for c in range(4):
                    k.dma("pool", w_out[:, c, :], w_out_d[512 + c * 128:512 + (c + 1) * 128, :], r=[], w=[w_out.res], sres=w_out.res,
                          max_dma_last_dim=4096)
                abias = sb(pb, "abias", [128, 8, 640], F32)
                k.dma("sp", abias[:], abias_d.rearrange("p (h c) -> p h c", h=8), r=[], w=[abias.res], sres=abias.res)
                halob = sb(pb, "halob", [128, 512], F32)
                k.dma("sp", halob[:], halob_d[:, :], r=[], w=[halob.res], sres=halob.res)
                xtmp = [sb(pb, "xtmpb%d" % i, [128, D], F32) for i in range(2)]
                hT = [sb(pb, "hTb%d" % i, [128, 8, BB], BF16) for i in range(2)]
                qA_l = [sb(pb, "qA%d" % i, [128, 4, BB], BF16) for i in range(2)]
                qB_l = [sb(pb, "qB%d" % i, [128, 4, BB], BF16) for i in range(2)]
                kT = [sb(pb, "kT%d" % i, [128, 4, BB], BF16) for i in range(4)]
                vpad = [sb(pb, "vpad%d" % i, [128, 8, 128], BF16) for i in range(8)]
                ybT = sb(pb, "ybT", [128, 4, BB], F32)
                ybn = sb(pb, "ybn", [128, 4, BB], BF16)
                sbuf_s = [sb(pb, "sbs%d" % i, [128, 640], F32) for i in range(2)]
                Pm = [sb(pb, "Pm%d" % i, [128, 640], BF16) for i in range(2)]
                Pn = [sb(pb, "Pn%d" % i, [128, 640], BF16) for i in range(2)]
                PT = [sb(pb, "PT%d" % i, [128, 5, 128], BF16) for i in range(2)]
                st = [dict(mx=sb(pb, "a_mx%d" % i, [128, 1], F32), rs=sb(pb, "a_rs%d" % i, [128, 1], F32),
                           ri=sb(pb, "a_ri%d" % i, [128, 1], F32)) for i in range(2)]
                sq4 = [sb(pb, "sqb%d" % i, [128, BB], BF16) for i in range(4)]
                rsn = sb(pb, "rsnb", [128, BB], F32)
                pj = [ps(pb, "pjb%d" % i, [128, 512], F32) for i in range(2)]
                pT = [ps(pb, "pTb%d" % i, [128, 8, 128], BF16) for i in range(1)]
                pSm = [ps(pb, "pSm%d" % i, [128, 512], F32) for i in range(2)]
                pSr = Tn(pb.enter_context(nc.psum_tensor("ps_pSr", [128, 4, 128], F32)), "pSr", nres=4)
                pPT = ps(pb, "pPT", [128, 5, 128], BF16)
                pO = ps(pb, "pO", [128, 4, 128], F32)
                pn = pj[0]
                for v_ in vpad:
                    k.op("pool", lambda e, v_=v_: e.memset(v_[:], 0.0), w=[v_.res])
                for q_ in qA_l + qB_l:
                    k.op("pool", lambda e, q_=q_: e.memset(q_[:], 0.0), w=[q_.res])
                set_gain(SM_MIX)

                nhalo = 512 // BB
                nown = NT // BB
                tcount = 0
                pjc = 0
                ac = 0
                def prep(b):
                    nonlocal tcount, pjc
                    own = b >= nhalo
                    ob = b - nhalo
                    h = hT[b % 2]
                    kcur = kT[b % 4]
                    qA, qB = qA_l[b % 2], qB_l[b % 2]
                    for t in range(2):
                        xt_ = xtmp[tcount % 2]
                        xa = xt_[:]
                        xr = xt_.res
                        if own:
                            r0 = ob * BB + t * 128
                            k.dma("sp", xa, xown[r0:r0 + 128, :], r=[], w=[xr], sres=xr)
                        else:
                            r0 = NPRE - 512 + b * BB + t * 128
                            k.dma("sp", xa, xprev[r0:r0 + 128, :], r=[], w=[xr], sres=xr)
                        yield from norm_T_g(xa, xr, pT[0], h[:, :, t * 128:(t + 1) * 128], h.res)
                        tcount += 1
                        yield
                    for m in range(4):
                        pp = pj[pjc % 2]
                        pjc += 1
                        mm_group(pp[:, 0:BB], [(w_inB[:, kc, 512 + m * 128:512 + (m + 1) * 128], h[:, kc, :]) for kc in range(8)],
                                 r=[w_inB.res, h.res], w=[pp.res])
                        k.op("act", lambda e, pp=pp, m=m, kcur=kcur: e.activation(out=kcur[:, m, :], in_=pp[:, 0:BB], func=AF.Copy),
                             r=[pp.res], w=[kcur.res])
                    yield
                    for t in range(2):
                        yield
                        gt = b * 2 + t
                        vp = vpad[gt % 8]
                        pp = pj[pjc % 2]
                        pjc += 1
                        mm_group(pp[:], [(h[:, kc, t * 128:(t + 1) * 128], w_inB[:, kc, 1024:1536]) for kc in range(8)],
                                 r=[w_inB.res, h.res], w=[pp.res])
                        ppv = pp[:].rearrange("p (h d) -> p h d", h=8)
                        k.op("act", lambda e, vp=vp, ppv=ppv: e.activation(out=vp[:, 0:8:2, 0:64], in_=ppv[:, 0:8:2, :], func=AF.Copy),
                             r=[pp.res], w=[vp.res])
                        k.op("act", lambda e, vp=vp, ppv=ppv: e.activation(out=vp[:, 1:8:2, 64:128], in_=ppv[:, 1:8:2, :], func=AF.Copy),
                             r=[pp.res], w=[vp.res])
                    if not own:
                        return
                    yield
                    for m in range(4):
                        if m == 2:
                            yield
                        pp = pj[pjc % 2]
                        pjc += 1
                        mm_group(pp[:, 0:BB], [(w_inB[:, kc, m * 128:(m + 1) * 128], h[:, kc, :]) for kc in range(8)],
                                 r=[w_inB.res, h.res], w=[pp.res])
                        k.op("act", lambda e, pp=pp, m=m: e.activation(out=qA[0:64, m, :], in_=pp[0:64, 0:BB], func=AF.Copy),
                             r=[pp.res], w=[qA.res])
                        k.op("act", lambda e, pp=pp, m=m: e.activation(out=qB[64:128, m, :], in_=pp[64:128, 0:BB], func=AF.Copy),
                             r=[pp.res], w=[qB.res])
                def attend(b, fillers):
                    nonlocal pjc, ac
                    ob = b - nhalo
                    qA, qB = qA_l[b % 2], qB_l[b % 2]
                    units = []
                    for p in range(2):
                        pieces = []
                        oc = 0
                        remaining = 640
                        pos = 128 * p
                        while remaining > 0:
                            bi = pos // BB
                            c0 = pos % BB
                            n = min(BB - c0, remaining)
                            if oc < 512 and oc + n > 512:
                                n = 512 - oc
                            pieces.append((kT[(b - 2 + bi) % 4], c0, n, oc))
                            oc += n
                            pos += n
                            remaining -= n
                        for hd in range(8):
                            units.append((p, hd, pieces))

                    def S1(u):
                        p, hd, pieces = units[u]
                        m = hd // 2
                        qs = (qA if hd % 2 == 0 else qB)
                        gi = ac0 + u
                        pm, pr = pSm[gi % 2], pSr
                        for (kt_, c0, n, oc) in pieces:
                            if oc < 512:
                                oap = pm[:, oc:oc + n]
                                ores = pm.res
                            else:
                                oap = pr[:, gi % 4, oc - 512:oc - 512 + n]
                                ores = pr.res
                            k.op("pe", lambda e, kt_=kt_, c0=c0, n=n, oap=oap: e.matmul(
                                oap, lhsT=qs[:, m, p * 128:(p + 1) * 128], rhs=kt_[:, m, c0:c0 + n],
                                start=True, stop=True), r=[qs.res, kt_.res], w=[ores])

                    def S2(u, part):
                        p, hd, pieces = units[u]
                        gi = ac0 + u
                        i2 = gi % 2
                        pm, pr = pSm[gi % 2], pSr
                        S, PP, PN, s_ = sbuf_s[i2], Pm[i2], Pn[i2], st[i2]
                        if part == 1:
                            k.op("dve", lambda e: e.reciprocal(out=s_["ri"][:], in_=s_["rs"][:]), r=[s_["rs"].res], w=[s_["ri"].res])
                            k.op("dve", lambda e: e.tensor_scalar(out=PN[:], in0=PP[:], scalar1=s_["ri"][:, 0:1],
                                                                  scalar2=None, op0=ALU.mult),
                                 r=[PP.res, s_["ri"].res], w=[PN.res])
                            return
                        k.op("dve", lambda e: e.scalar_tensor_tensor(
                            out=S[:, 0:512], in0=pm[:], scalar=0.125, in1=abias[:, hd, 0:512], op0=ALU.mult, op1=ALU.add),
                            r=[pm.res, abias.res], w=[S.res])
                        k.op("dve", lambda e: e.scalar_tensor_tensor(
                            out=S[:, 512:640], in0=pr[:, gi % 4, :], scalar=0.125, in1=abias[:, hd, 512:640], op0=ALU.mult, op1=ALU.add),
                            r=[pr.res, abias.res], w=[S.res])
                        cnt = 512 - ob * BB - 128 * p
                        if cnt > 0:
                            hb0 = ob * BB + 128 * p
                            k.op("dve", lambda e: e.tensor_tensor(
                                out=S[:, 0:cnt], in0=S[:, 0:cnt], in1=halob[:, hb0:512], op=ALU.add),
                                r=[S.res, halob.res], w=[S.res])
                        k.op("dve", lambda e: e.tensor_reduce(out=s_["mx"][:], in_=S[:], axis=AX.X, op=ALU.max, negate=True),
                             r=[S.res], w=[s_["mx"].res])
                        k.op("act", lambda e: e.activation(out=PP[:], in_=S[:], func=AF.Exp, bias=s_["mx"][:, 0:1],
                                                           scale=1.0, accum_out=s_["rs"][:]),
                             r=[S.res, s_["mx"].res], w=[PP.res, s_["rs"].res])

                    def S3(u):
                        p, hd, pieces = units[u]
                        m = hd // 2
                        gi = ac0 + u
                        i2 = gi % 2
                        PN, PTs = Pn[i2], PT[i2]
                        for kc in range(5):
                            k.op("pe", lambda e, kc=kc: e.transpose(out=pPT[:, kc, :], in_=PN[:, kc * 128:(kc + 1) * 128],
                                                                    identity=ident_b[:]),
                                 r=[PN.res, ident_b.res], w=[pPT.res])
                        k.op("act", lambda e: e.activation(out=PTs[:], in_=pPT[:], func=AF.Copy), r=[pPT.res], w=[PTs.res])
                        g0 = (b * 2 + p) - 4
                        for kc in range(5):
                            vp = vpad[(g0 + kc) % 8]
                            k.op("pe", lambda e, vp=vp, kc=kc: e.matmul(
                                pO[:, m, :], lhsT=vp[:, hd, :], rhs=PTs[:, kc, :],
                                start=(hd % 2 == 0 and kc == 0), stop=(hd % 2 == 1 and kc == 4)),
                                r=[vp.res, PTs.res], w=[pO.res])
                        if hd == 7:
                            k.op("act", lambda e: e.activation(out=ybT[:, :, p * 128:(p + 1) * 128], in_=pO[:], func=AF.Copy),
                                 r=[pO.res], w=[ybT.res])

                    def advance():
                        while fillers:
                            try:
                                next(fillers[0])
                                return
                            except StopIteration:
                                fillers.pop(0)

                    ac0 = ac
                    S1(0)
                    S1(1)
                    S2(0, 0)
                    for u in range(len(units)):
                        if u + 1 < len(units):
                            S2(u + 1, 0)
                        S2(u, 1)
                        if u + 2 < len(units):
                            S1(u + 2)
                        S3(u)
                        advance()
                    while fillers:
                        advance()
                    ac += len(units)

                def tail(b):
                    nonlocal pjc
                    ob = b - nhalo
                    for cc in range(4):
                        k.op("dve", lambda e, cc=cc: e.tensor_tensor(out=sq4[cc][:], in0=ybT[:, cc, :], in1=ybT[:, cc, :], op=ALU.mult),
                             r=[ybT.res], w=[sq4[cc].res])
                    for cc in range(4):
                        k.op("pe", lambda e, cc=cc: e.matmul(pn[:, 0:BB], lhsT=ones_b[:], rhs=sq4[cc][:], start=(cc == 0), stop=(cc == 3)),
                             r=[ones_b.res, sq4[cc].res], w=[pn.res])
                    yield
                    k.op("act", lambda e: e.activation(out=rsn[:], in_=pn[:, 0:BB], func=AF.Ln, bias=cst[:, 0:1], scale=1.0 / 512),
                         r=[pn.res, cst.res], w=[rsn.res])
                    k.op("act", lambda e: e.activation(out=rsn[:], in_=rsn[:], func=AF.Exp, scale=-0.5), r=[rsn.res], w=[rsn.res])
                    yield
                    for cc in range(4):
                        k.op("dve", lambda e, cc=cc: e.scalar_tensor_tensor(
                            out=ybn[:, cc, :], in0=ybT[:, cc, :], scalar=smalls[:, SM_GB + cc:SM_GB + cc + 1], in1=rsn[:],
                            op0=ALU.mult, op1=ALU.mult), r=[ybT.res, smalls.res, rsn.res], w=[ybn.res])
                    yield
                    for t in range(2):
                        tile = ob * 2 + t
                        tok0 = ob * BB + t * 128
                        for half in range(2):
                            pp = pj[pjc % 2]
                            pjc += 1
                            pairs = [(ybn[:, c, t * 128:(t + 1) * 128], w_out[:, c, half * 512:(half + 1) * 512]) for c in range(4)]
                            mm_group(pp[:], pairs, r=[ybn.res, w_out.res], w=[pp.res])
                            k.op("dve", lambda e, pp=pp, tile=tile, half=half: e.tensor_tensor(
                                out=x_res[:, tile, half * 512:(half + 1) * 512], in0=pp[:], in1=x_res[:, tile, half * 512:(half + 1) * 512],
                                op=ALU.add), r=[pp.res, x_res.rs[tile]], w=[x_res.rs[tile]])
                        yield
                nb_ = nhalo + nown
                for b0 in range(nhalo + 1):
                    for _ in prep(b0):
                        pass
                for b in range(nhalo, nb_):
                    fl = []
                    if b - 1 >= nhalo:
                        fl.append(tail(b - 1))
                    if b + 1 < nb_:
                        fl.append(prep(b + 1))
                    attend(b, fl)
                for _ in tail(nb_ - 1):
                    pass
                k.barrier(release=[w_inB.res, w_out.res, abias.res, halob.res] + [t_.res for t_ in xtmp])

        if debug:
            for tile in range(NTILE):
                k.dma("sp", dbg["dbg1"][tile * 128:(tile + 1) * 128, :], x_res[:, tile, :], r=[x_res.rs[tile]], w=[], sres=x_res.rs[tile])

        with ExitStack() as p2:
            B2 = 512
            w_q = sb(p2, "w_q", [128, 8, D], BF16)
            w_o = sb(p2, "w_o", [128, 8, D], BF16)
            kmT = sb(p2, "kmT", [128, 8, 256], BF16)
            vmem = sb(p2, "vmem", [128, 2, D], BF16)
            pj = [ps(p2, "pj2%d" % i, [128, 512], F32) for i in range(2)]
            pT = [ps(p2, "pT2%d" % i, [128, 8, 128], BF16) for i in range(1)]
            pS2 = [ps(p2, "pS2%d" % i, [128, 4, 256], F32) for i in range(2)]
            pPT2 = ps(p2, "pPT2", [128, 8, 128], BF16)
            pjc = 0
            with ExitStack() as p2s:
                w_kv = sb(p2s, "w_kv", [128, 8, 2048], BF16)
                load_w_bf16(w_kv, w_kv_d[:, :], 2048)
                load_w_bf16(w_q, w_q_d[:, :], D)
                load_w_bf16(w_o, w_o_d[:, :], D)
                memt = [sb(p2s, "memt%d" % i, [128, D], F32) for i in range(2)]
                memT = sb(p2s, "memT", [128, 8, 256], BF16)
                set_gain(SM_MEM)
                for t in range(2):
                    k.dma("sp", memt[t][:], mem_d[t * 128:(t + 1) * 128, :], r=[], w=[memt[t].res], sres=memt[t].res)
                    norm_T(memt[t][:], memt[t].res, pT[0], memT[:, :, t * 128:(t + 1) * 128], memT.res)
                for oc in range(8):
                    pp = pj[pjc % 2]
                    pjc += 1
                    mm_group(pp[:, 0:256], [(w_kv[:, kc, oc * 128:(oc + 1) * 128], memT[:, kc, :]) for kc in range(8)],
                             r=[w_kv.res, memT.res], w=[pp.res])
                    k.op("act", lambda e, pp=pp, oc=oc: e.activation(out=kmT[:, oc, :], in_=pp[:, 0:256], func=AF.Copy),
                         r=[pp.res], w=[kmT.res])
                for t in range(2):
                    for half in range(2):
                        pp = pj[pjc % 2]
                        pjc += 1
                        mm_group(pp[:], [(memT[:, kc, t * 128:(t + 1) * 128], w_kv[:, kc, 1024 + half * 512:1024 + (half + 1) * 512])
                                         for kc in range(8)], r=[w_kv.res, memT.res], w=[pp.res])
                        k.op("act", lambda e, pp=pp, t=t, half=half: e.activation(out=vmem[:, t, half * 512:(half + 1) * 512], in_=pp[:], func=AF.Copy),
                             r=[pp.res], w=[vmem.res])
                k.barrier(release=[w_kv.res] + [t_.res for t_ in memt])
            hT = [sb(p2, "hT2%d" % i, [128, 8, B2], BF16) for i in range(2)]
            qT = sb(p2, "qT2", [128, 8, B2], BF16)
            P2 = [sb(p2, "P2%d" % i, [128, 4, 256], BF16) for i in range(2)]
            P2n = [sb(p2, "P2n%d" % i, [128, 4, 256], BF16) for i in range(2)]
            PT2 = sb(p2, "PT2", [128, 4, 2, B2], BF16)
            oT = sb(p2, "oT2", [128, 8, B2], BF16)
            st2 = [dict(mx=sb(p2, "c_mx%d" % i, [128, 4], F32), rs=sb(p2, "c_rs%d" % i, [128, 4], F32),
                        ri=sb(p2, "c_ri%d" % i, [128, 4], F32)) for i in range(2)]
            set_gain(SM_CROSS)
            tc2 = 0
            for b in range(NT // B2):
                h = hT[b % 2]
                for t in range(4):
                    norm_T(x_res[:, b * 4 + t, :], x_res.rs[b * 4 + t], pT[0], h[:, :, t * 128:(t + 1) * 128], h.res)
                for oc in range(8):
                    pp = pj[pjc % 2]
                    pjc += 1
                    mm_group(pp[:], [(w_q[:, kc, oc * 128:(oc + 1) * 128], h[:, kc, :]) for kc in range(8)],
                             r=[w_q.res, h.res], w=[pp.res])
                    k.op("act", lambda e, pp=pp, oc=oc: e.activation(out=qT[:, oc, :], in_=pp[:], func=AF.Copy), r=[pp.res], w=[qT.res])
                def c_S1(t, i2):
                    pS_ = pS2[i2]
                    for hh_ in range(4):
                        mm_group(pS_[:, hh_, :], [(qT[:, 2 * hh_ + j, t * 128:(t + 1) * 128], kmT[:, 2 * hh_ + j, :]) for j in range(2)],
                                 r=[qT.res, kmT.res], w=[pS_.res])

                def c_S2(t, i2):
                    pS_, P_, Pn_, s_ = pS2[i2], P2[i2], P2n[i2], st2[i2]
                    k.op("dve", lambda e: e.tensor_reduce(out=s_["mx"][:], in_=pS_[:], axis=AX.X, op=ALU.max, negate=True),
                         r=[pS_.res], w=[s_["mx"].res])
                    k.op("dve", lambda e: e.tensor_scalar(out=s_["mx"][:], in0=s_["mx"][:], scalar1=1.0 / 16, scalar2=None, op0=ALU.mult),
                         r=[s_["mx"].res], w=[s_["mx"].res])
                    for hh_ in range(4):
                        k.op("act", lambda e, hh_=hh_: e.activation(
                            out=P_[:, hh_, :], in_=pS_[:, hh_, :], func=AF.Exp, bias=s_["mx"][:, hh_:hh_ + 1], scale=1.0 / 16,
                            accum_out=s_["rs"][:, hh_:hh_ + 1]), r=[pS_.res, s_["mx"].res], w=[P_.res, s_["rs"].res])
                    k.op("dve", lambda e: e.reciprocal(out=s_["ri"][:], in_=s_["rs"][:]), r=[s_["rs"].res], w=[s_["ri"].res])
                    k.op("dve", lambda e: e.tensor_tensor(
                        out=Pn_[:], in0=P_[:], in1=s_["ri"][:, :].unsqueeze(2).to_broadcast([128, 4, 256]), op=ALU.mult),
                        r=[P_.res, s_["ri"].res], w=[Pn_.res])

                def c_S3(t, i2):
                    Pn_ = P2n[i2]
                    for hh_ in range(4):
                        for mc in range(2):
                            k.op("pe", lambda e, hh_=hh_, mc=mc: e.transpose(
                                out=pPT2[:, hh_ * 2 + mc, :], in_=Pn_[:, hh_, mc * 128:(mc + 1) * 128], identity=ident_b[:]),
                                r=[Pn_.res, ident_b.res], w=[pPT2.res])
                    k.op("act", lambda e: e.activation(out=PT2[:, :, :, t * 128:(t + 1) * 128],
                                                       in_=pPT2[:].rearrange("p (h m) t -> p h m t", h=4), func=AF.Copy),
                         r=[pPT2.res], w=[PT2.res])

                c_S1(0, tc2 % 2)
                for t in range(4):
                    if t + 1 < 4:
                        c_S1(t + 1, (tc2 + 1) % 2)
                    c_S2(t, tc2 % 2)
                    c_S3(t, tc2 % 2)
                    tc2 += 1
                for oc in range(8):
                    pp = pj[pjc % 2]
                    pjc += 1
                    mm_group(pp[:], [(vmem[:, mc, oc * 128:(oc + 1) * 128], PT2[:, oc // 2, mc, :]) for mc in range(2)],
                             r=[vmem.res, PT2.res], w=[pp.res])
                    k.op("act", lambda e, pp=pp, oc=oc: e.activation(out=oT[:, oc, :], in_=pp[:], func=AF.Copy), r=[pp.res], w=[oT.res])
                for t in range(4):
                    tile = b * 4 + t
                    for half in range(2):
                        pp = pj[pjc % 2]
                        pjc += 1
                        mm_group(pp[:], [(oT[:, c, t * 128:(t + 1) * 128], w_o[:, c, half * 512:(half + 1) * 512]) for c in range(8)],
                                 r=[oT.res, w_o.res], w=[pp.res])
                        k.op("dve", lambda e, pp=pp, tile=tile, half=half: e.tensor_tensor(
                            out=x_res[:, tile, half * 512:(half + 1) * 512], in0=pp[:], in1=x_res[:, tile, half * 512:(half + 1) * 512],
                            op=ALU.add), r=[pp.res, x_res.rs[tile]], w=[x_res.rs[tile]])
            k.barrier(release=[w_q.res, w_o.res])

        if debug:
            for tile in range(NTILE):
                k.dma("sp", dbg["dbg2"][tile * 128:(tile + 1) * 128, :], x_res[:, tile, :], r=[x_res.rs[tile]], w=[], sres=x_res.rs[tile])

        with ExitStack() as p3:
            p3s = p3.enter_context(ExitStack())
            slotI = sb(p3s, "slotI", [128, NT], F32)
            slotJ = sb(p3s, "slotJ", [128, NT], F32)
            slotG = sb(p3s, "slotG", [128, NT], F32)
            set_gain(SM_FFN)
            with ExitStack() as pa:
                B3 = 256
                w_qry = sb(pa, "w_qry", [128, 8, 2048], BF16)
                load_w_bf16(w_qry, w_qry_d[:, :], 2048)
                skb = sb(pa, "skb", [128, 16, 128], BF16)
                k.dma("pool", skb[:], skT_d.rearrange("p (o n) -> p o n", o=16), r=[], w=[skb.res], sres=skb.res)
                hT = [sb(pa, "hT3%d" % i, [128, 8, B3], BF16) for i in range(2)]
                qpT = sb(pa, "qpT", [128, 16, B3], BF16)
                sc = sb(pa, "sc", [128, 16, 128], F32, nres=4)
                sc2 = sb(pa, "sc2", [128, 16, 128], F32, nres=16)
                tv = sb(pa, "tv", [128, 16, 16], F32, nres=16)
                tiu = sb(pa, "tiu", [128, 16, 16], U32, nres=16)
                tif = sb(pa, "tif", [128, 16, 16], F32)
                cand = sb(pa, "cand", [128, 8, 256], F32)
                cand2 = sb(pa, "cand2", [128, 8, 256], F32, nres=8)
                ts = sb(pa, "ts", [128, 8, 16], F32, nres=8)
                posu = sb(pa, "posu", [128, 8, 16], U32, nres=8)
                au = sb(pa, "au", [128, 8, 16], U32)
                bu = sb(pa, "bu", [128, 8, 16], U32)
                af = sb(pa, "af", [128, 8, 16], F32)
                bf = sb(pa, "bf", [128, 8, 16], F32)
                eq = sb(pa, "eq", [128, 8, 16, 16], F32)
                isel = sb(pa, "isel", [128, 8, 16], F32)
                jsel = sb(pa, "jsel", [128, 8, 16], F32)
                gsel = sb(pa, "gsel", [128, 8, 16], F32)
                sm = sb(pa, "sm", [128, 8], F32)
                pj = [ps(pa, "pj3%d" % i, [128, 512], F32) for i in range(2)]
                pT = [ps(pa, "pT3%d" % i, [128, 8, 128], BF16) for i in range(2)]
                psc = [ps(pa, "psc%d" % i, [128, 4, 128], F32) for i in range(2)]
                pTs = ps(pa, "pTs", [128, 3, 128], F32)
                pjc = 0
                scc = 0
                iota16 = iota_f[:, 0:16]
                for b in range(NT // B3):
                    h = hT[b % 2]
                    norm_pairs([(x_res[:, b * 2 + t, :], x_res.rs[b * 2 + t], pT[t % 2], h[:, :, t * 128:(t + 1) * 128], h.res) for t in range(2)])
                    for oc in range(16):
                        pp = pj[pjc % 2]
                        pjc += 1
                        mm_group(pp[:, 0:B3], [(w_qry[:, kc, oc * 128:(oc + 1) * 128], h[:, kc, :]) for kc in range(8)],
                                 r=[w_qry.res, h.res], w=[pp.res])
                        k.op("act", lambda e, pp=pp, oc=oc: e.activation(out=qpT[:, oc, :], in_=pp[:, 0:B3], func=AF.Copy),
                             r=[pp.res], w=[qpT.res])
                    for t in range(2):
                        tile = b * 2 + t
                        for g4 in range(4):
                            pq = psc[scc % 2]
                            scc += 1
                            for j in range(4):
                                oc = g4 * 4 + j
                                k.op("pe", lambda e, pq=pq, j=j, oc=oc, t=t: e.matmul(
                                    pq[:, j, :], lhsT=qpT[:, oc, t * 128:(t + 1) * 128], rhs=skb[:, oc, :], start=True, stop=True),
                                    r=[qpT.res, skb.res], w=[pq.res])
                            k.op("act", lambda e, pq=pq, g4=g4: e.activation(out=sc[:, g4 * 4:(g4 + 1) * 4, :], in_=pq[:], func=AF.Copy),
                                 r=[pq.res], w=[sc.rs[g4]])
                        for oc in range(16):
                            k.op("dve", lambda e, oc=oc: e.max(out=tv[:, oc, 0:8], in_=sc[:, oc, :]), r=[sc.rs[oc // 4]], w=[tv.rs[oc]])
                        for oc in range(16):
                            k.op("dve", lambda e, oc=oc: e.max_index(out=tiu[:, oc, 0:8], in_max=tv[:, oc, 0:8], in_values=sc[:, oc, :]),
                                 r=[sc.rs[oc // 4], tv.rs[oc]], w=[tiu.rs[oc]])
                        for oc in range(16):
                            k.op("dve", lambda e, oc=oc: e.match_replace(out=sc2[:, oc, :], in_to_replace=tv[:, oc, 0:8], in_values=sc[:, oc, :],
                                                                        imm_value=NEG), r=[sc.rs[oc // 4], tv.rs[oc]], w=[sc2.rs[oc]])
                        for oc in range(16):
                            k.op("dve", lambda e, oc=oc: e.max(out=tv[:, oc, 8:16], in_=sc2[:, oc, :]), r=[sc2.rs[oc]], w=[tv.rs[oc]])
                        for oc in range(16):
                            k.op("dve", lambda e, oc=oc: e.max_index(out=tiu[:, oc, 8:16], in_max=tv[:, oc, 8:16], in_values=sc2[:, oc, :]),
                                 r=[sc2.rs[oc], tv.rs[oc]], w=[tiu.rs[oc]])
                        k.op("dve", lambda e: e.tensor_copy(out=tif[:], in_=tiu[:]), r=tiu.rs, w=[tif.res])
                        tv4 = tv[:].rearrange("p (h two) a -> p h two a", two=2)
                        tif4 = tif[:].rearrange("p (h two) a -> p h two a", two=2)
                        k.op("dve", lambda e, tv4=tv4: e.tensor_tensor(
                            out=cand[:].rearrange("p h (a b) -> p h a b", a=16),
                            in0=tv4[:, :, 0, :].unsqueeze(3).to_broadcast([128, 8, 16, 16]),
                            in1=tv4[:, :, 1, :].unsqueeze(2).to_broadcast([128, 8, 16, 16]), op=ALU.add),
                            r=tv.rs, w=[cand.res])
                        for hd in range(8):
                            k.op("dve", lambda e, hd=hd: e.max(out=ts[:, hd, 0:8], in_=cand[:, hd, :]), r=[cand.res], w=[ts.rs[hd]])
                        for hd in range(8):
                            k.op("dve", lambda e, hd=hd: e.max_index(out=posu[:, hd, 0:8], in_max=ts[:, hd, 0:8], in_values=cand[:, hd, :]),
                                 r=[cand.res, ts.rs[hd]], w=[posu.rs[hd]])
                        for hd in range(8):
                            k.op("dve", lambda e, hd=hd: e.match_replace(out=cand2[:, hd, :], in_to_replace=ts[:, hd, 0:8], in_values=cand[:, hd, :],
                                                                        imm_value=NEG), r=[cand.res, ts.rs[hd]], w=[cand2.rs[hd]])
                        for hd in range(8):
                            k.op("dve", lambda e, hd=hd: e.max(out=ts[:, hd, 8:16], in_=cand2[:, hd, :]), r=[cand2.rs[hd]], w=[ts.rs[hd]])
                        for hd in range(8):
                            k.op("dve", lambda e, hd=hd: e.max_index(out=posu[:, hd, 8:16], in_max=ts[:, hd, 8:16], in_values=cand2[:, hd, :]),
                                 r=[cand2.rs[hd], ts.rs[hd]], w=[posu.rs[hd]])
                        k.op("dve", lambda e: e.tensor_scalar(out=au[:], in0=posu[:], scalar1=4, scalar2=None, op0=ALU.logical_shift_right),
                             r=posu.rs, w=[au.res])
                        k.op("dve", lambda e: e.tensor_scalar(out=bu[:], in0=posu[:], scalar1=15, scalar2=None, op0=ALU.bitwise_and),
                             r=posu.rs, w=[bu.res])
                        k.op("dve", lambda e: e.tensor_copy(out=af[:], in_=au[:]), r=[au.res], w=[af.res])
                        k.op("dve", lambda e: e.tensor_copy(out=bf[:], in_=bu[:]), r=[bu.res], w=[bf.res])
                        for (sel, rk, which) in ((isel, af, 0), (jsel, bf, 1)):
                            k.op("dve", lambda e, rk=rk: e.tensor_tensor(
                                out=eq[:], in0=rk[:].unsqueeze(3).to_broadcast([128, 8, 16, 16]),
                                in1=iota16.unsqueeze(1).unsqueeze(1).to_broadcast([128, 8, 16, 16]), op=ALU.is_equal),
                                r=[rk.res, iota_f.res], w=[eq.res])
                            k.op("dve", lambda e, which=which, tif4=tif4: e.tensor_tensor(
                                out=eq[:], in0=eq[:], in1=tif4[:, :, which, :].unsqueeze(2).to_broadcast([128, 8, 16, 16]), op=ALU.mult),
                                r=[eq.res, tif.res], w=[eq.res])
                            k.op("dve", lambda e, sel=sel: e.tensor_reduce(out=sel[:], in_=eq[:], axis=AX.X, op=ALU.add),
                                 r=[eq.res], w=[sel.res])
                        k.op("dve", lambda e: e.tensor_tensor(out=gsel[:], in0=ts[:], in1=ts[:, :, 0:1].to_broadcast([128, 8, 16]), op=ALU.subtract),
                             r=ts.rs, w=[gsel.res])
                        k.op("act", lambda e: e.activation(out=gsel[:], in_=gsel[:], func=AF.Exp), r=[gsel.res], w=[gsel.res])
                        k.op("dve", lambda e: e.tensor_reduce(out=sm[:], in_=gsel[:], axis=AX.X, op=ALU.add), r=[gsel.res], w=[sm.res])
                        k.op("dve", lambda e: e.reciprocal(out=sm[:], in_=sm[:]), r=[sm.res], w=[sm.res])
                        k.op("dve", lambda e: e.tensor_tensor(out=gsel[:], in0=gsel[:], in1=sm[:, :].unsqueeze(2).to_broadcast([128, 8, 16]), op=ALU.mult),
                             r=[gsel.res, sm.res], w=[gsel.res])
                        for i3, (src, dst) in enumerate(((isel, slotI), (jsel, slotJ), (gsel, slotG))):
                            k.op("pe", lambda e, i3=i3, src=src: e.transpose(out=pTs[:, i3, :], in_=src[:].rearrange("p h r -> p (h r)"),
                                                                            identity=ident_f[:]),
                                 r=[src.res, ident_f.res], w=[pTs.res])
                        for i3, (src, dst) in enumerate(((isel, slotI), (jsel, slotJ), (gsel, slotG))):
                            k.op("act", lambda e, i3=i3, dst=dst, tile=tile: e.activation(out=dst[:, tile * 128:(tile + 1) * 128], in_=pTs[:, i3, :], func=AF.Copy),
                                 r=[pTs.res], w=[dst.res])
                k.barrier(release=[w_qry.res, skb.res])

            with ExitStack() as pb:
                TCH = 8
                ohj = [sb(pb, "ohj%d" % i, [128, TCH, 128], BF16) for i in range(4)]
                eqi = [sb(pb, "eqi%d" % i, [128, TCH, 128], BF16) for i in range(4)]
                rig = [sb(pb, "rig%d" % i, [128, TCH, 128], BF16, nres=TCH) for i in range(4)]
                Wst = [sb(pb, "Wst%d" % i, [128, 128, 128], BF16, nres=32) for i in range(2)]
                pW = [ps(pb, "pW%d" % i, [128, 4, 128], F32) for i in range(4)]
                wc = 0
                chc = 0
                iota_b3 = iota_f[:, :].unsqueeze(1).to_broadcast([128, TCH, 128])
                for tile in range(NTILE):
                    W_ = Wst[tile % 2]
                    for ch in range(128 // TCH):
                        oj, ei, rg = ohj[chc % 4], eqi[chc % 4], rig[chc % 4]
                        chc += 1
                        tok0 = tile * 128 + ch * TCH
                        k.op("dve", lambda e, oj=oj, tok0=tok0: e.tensor_tensor(
                            out=oj[:], in0=iota_b3, in1=slotJ[:, tok0:tok0 + TCH].unsqueeze(2).to_broadcast([128, TCH, 128]), op=ALU.is_equal),
                            r=[iota_f.res, slotJ.res], w=[oj.res])
                        k.op("dve", lambda e, ei=ei, tok0=tok0: e.tensor_tensor(
                            out=ei[:], in0=iota_b3, in1=slotI[:, tok0:tok0 + TCH].unsqueeze(2).to_broadcast([128, TCH, 128]), op=ALU.is_equal),
                            r=[iota_f.res, slotI.res], w=[ei.res])
                        if chc % 9 < 4:
                            for tt_ in range(TCH):
                                k.op("act", lambda e, ei=ei, rg=rg, tt_=tt_, tok0=tok0: e.activation(
                                    out=rg[:, tt_, :], in_=ei[:, tt_, :], func=AF.Copy, scale=slotG[:, tok0 + tt_:tok0 + tt_ + 1]),
                                    r=[ei.res, slotG.res], w=[rg.rs[tt_]])
                        else:
                            k.op("dve", lambda e, ei=ei, rg=rg, tok0=tok0: e.tensor_tensor(
                                out=rg[:], in0=ei[:], in1=slotG[:, tok0:tok0 + TCH].unsqueeze(2).to_broadcast([128, TCH, 128]), op=ALU.mult),
                                r=[ei.res, slotG.res], w=rg.rs)
                        for q4 in range(TCH // 4):
                            pw = pW[wc % 4]
                            for j in range(4):
                                tt_ = q4 * 4 + j
                                k.op("pe", lambda e, pw=pw, j=j, oj=oj, rg=rg, tt_=tt_: e.matmul(
                                    pw[:, j, :], lhsT=oj[:, tt_, :], rhs=rg[:, tt_, :], start=True, stop=True),
                                    r=[oj.res, rg.rs[tt_]], w=[pw.res])
                            t0 = ch * TCH + q4 * 4
                            if True:
                                k.op("act", lambda e, pw=pw, W_=W_, t0=t0: e.activation(
                                    out=W_[:, :, t0:t0 + 4], in_=pw[:].rearrange("p t i -> p i t"), func=AF.Copy),
                                    r=[pw.res], w=[W_.rs[t0 // 4]])
                            else:
                                k.op("dve", lambda e, pw=pw, W_=W_, t0=t0: e.tensor_copy(
                                    out=W_[:, :, t0:t0 + 4], in_=pw[:].rearrange("p t i -> p i t")),
                                    r=[pw.res], w=[W_.rs[t0 // 4]])
                            wc += 1
                    k.dma("sp", wd_d[tile], W_[:].rearrange("p i t -> p (i t)"), r=W_.rs, w=[], sres=W_.res)
                k.barrier(release=[w_.res for w_ in Wst])
            p3s.close()

            with ExitStack() as pc:
                T3 = 256
                GS = 8
                NG = 128 // GS
                hfT = sb(pc, "hfT", [128, 8, NT], BF16)
                ut = [sb(pc, "ut%d" % i, [128, GS, 8, 128], BF16) for i in range(2)]
                vt = [sb(pc, "vt%d" % i, [128, GS, D], BF16) for i in range(2)]
                uT_v = uT_d.rearrange("(i p) (c e) -> p i c e", p=128, c=8)
                ev_v = ev_d.rearrange("(i e) d -> e i d", e=128)
                for i_ in range(GS):
                    k.dma("pool", ut[0][:, i_, :, :], uT_v[:, i_, :, :], r=[], w=[ut[0].res], sres=ut[0].res)
                    k.dma("pool", vt[0][:, i_, :], ev_v[:, i_, :], r=[], w=[vt[0].res], sres=vt[0].res, max_dma_last_dim=4096)
                with ExitStack() as pcn:
                    pTn = [ps(pcn, "pT4%d" % i, [128, 8, 128], BF16) for i in range(2)]
                    norm_pairs([(x_res[:, tile, :], x_res.rs[tile], pTn[tile % 2], hfT[:, :, tile * 128:(tile + 1) * 128], hfT.res)
                                for tile in range(NTILE)])
                    k.barrier()
                pA = [ps(pc, "pA%d" % i, [128, 512], F32) for i in range(3)]
                pO = [ps(pc, "pO4%d" % i, [128, 512], F32) for i in range(4)]
                wsl = [sb(pc, "wsl%d" % i, [128, 2, GS, 128], BF16) for i in range(2)]
                ge = [sb(pc, "ge%d" % i, [128, T3], F32) for i in range(3)]
                gw = [sb(pc, "gw%d" % i, [128, T3], BF16) for i in range(3)]
                wd_v = wd_d.rearrange("t j (i x) -> j t i x", i=128)
                items = [(g, tb, i) for g in range(NG) for tb in range(NT // T3) for i in range(GS)]
                state = {}

                def emitA(n):
                    g, tb, i = items[n]
                    u_, v_ = ut[g % 2], vt[g % 2]
                    if tb == 0 and i == 0 and g > 0:
                        for i_ in range(GS):
                            k.dma("pool", u_[:, i_, :, :], uT_v[:, g * GS + i_, :, :], r=[], w=[u_.res], sres=u_.res)
                            k.dma("pool", v_[:, i_, :], ev_v[:, g * GS + i_, :], r=[], w=[v_.res], sres=v_.res, max_dma_last_dim=4096)
                    if i == 0:
                        ws_ = wsl[(g * (NT // T3) + tb) % 2]
                        k.dma("sp", ws_[:], wd_v[:, 2 * tb:2 * tb + 2, g * GS:(g + 1) * GS, :], r=[], w=[ws_.res], sres=ws_.res)
                    ws_ = wsl[(g * (NT // T3) + tb) % 2]
                    pa_ = pA[n % 3]
                    ge_, gw_ = ge[n % 3], gw[n % 3]
                    mm_group(pa_[:, 0:T3], [(u_[:, i, c, :], hfT[:, c, tb * T3:(tb + 1) * T3]) for c in range(8)],
                             r=[u_.res, hfT.res], w=[pa_.res])
                    k.op("act", lambda e: e.activation(out=ge_[:], in_=pa_[:, 0:T3], func=AF.Gelu_apprx_tanh),
                         r=[pa_.res], w=[ge_.res])
                    k.op("dve", lambda e: e.tensor_tensor(
                        out=gw_[:].rearrange("p (a t) -> p a t", a=2), in0=ge_[:].rearrange("p (a t) -> p a t", a=2),
                        in1=ws_[:, :, i, :], op=ALU.mult), r=[ge_.res, ws_.res], w=[gw_.res])

                def emitV(n):
                    g, tb, i = items[n]
                    v_ = vt[g % 2]
                    gw_ = gw[n % 3]
                    for t in range(2):
                        for half in range(2):
                            po = pO[t * 2 + half]
                            k.op("pe", lambda e, po=po, t=t, half=half: e.matmul(
                                po[:], lhsT=gw_[:, t * 128:(t + 1) * 128], rhs=v_[:, i, half * 512:(half + 1) * 512],
                                start=(i == 0), stop=(i == GS - 1)), r=[gw_.res, v_.res], w=[po.res])
                    if i == GS - 1:
                        for t in range(2):
                            tile = tb * 2 + t
                            for half in range(2):
                                po = pO[t * 2 + half]
                                k.op("dve", lambda e, po=po, tile=tile, half=half: e.tensor_tensor(
                                    out=x_res[:, tile, half * 512:(half + 1) * 512], in0=po[:], in1=x_res[:, tile, half * 512:(half + 1) * 512],
                                    op=ALU.add), r=[po.res, x_res.rs[tile]], w=[x_res.rs[tile]])

                emitA(0)
                emitA(1)
                for n in range(len(items)):
                    if n + 2 < len(items):
                        emitA(n + 2)
                    emitV(n)
                k.barrier(release=[t_.res for t_ in ut + vt + wsl])

        with ExitStack() as p4:
            gfin = sb(p4, "gfin", [128, D], F32)
            k.dma("sp", gfin[:], gfin_d[:, :], r=[], w=[gfin.res], sres=gfin.res)
            ot = [sb(p4, "ot%d" % i, [128, D], F32) for i in range(2)]
            def fin_g(tile):
                n = nrm[tile % 2]
                o_ = ot[tile % 2]
                xa = x_res[:, tile, :]
                xr = x_res.rs[tile]
                k.op("dve", lambda e: e.scalar_tensor_tensor(out=n["junk"][:], in0=xa, scalar=1.0, in1=xa,
                                                             op0=ALU.mult, op1=ALU.mult, accum_out=n["ss"][:]),
                     r=[xr], w=[n["junk"].res, n["ss"].res])
                yield
                k.op("act", lambda e: e.activation(out=n["sd"][:], in_=n["ss"][:], func=AF.Ln, bias=cst[:, 0:1], scale=1.0 / D),
                     r=[n["ss"].res, cst.res], w=[n["sd"].res])
                k.op("act", lambda e: e.activation(out=n["rstd"][:], in_=n["sd"][:], func=AF.Exp, scale=-0.5),
                     r=[n["sd"].res], w=[n["rstd"].res])
                yield
                k.op("dve", lambda e: e.scalar_tensor_tensor(out=o_[:], in0=xa, scalar=n["rstd"][:, 0:1], in1=gfin[:],
                                                             op0=ALU.mult, op1=ALU.mult),
                     r=[xr, n["rstd"].res, gfin.res], w=[o_.res])
                k.dma("sp", y_d[tile * 128:(tile + 1) * 128, :], o_[:], r=[o_.res], w=[], sres=o_.res)

            for t2 in range(0, NTILE, 2):
                for _ in lockstep_g([fin_g(t2), fin_g(t2 + 1)]):
                    pass
            k.barrier()
    return nc, k.n_ins


def prep_shared(inp):
    f = np.float32
    g = lambda a: np.ascontiguousarray(np.asarray(a, dtype=f))
    sm = np.zeros((128, NSM), f)

    def put(col, vec, nch):
        sm[:, col:col + nch] = np.asarray(vec, f).reshape(nch, 128).T

    put(SM_MIX, inp["norm_mix"][0], 8)
    put(SM_CROSS, inp["norm_cross"][0], 8)
    put(SM_MEM, inp["norm_mem"][0], 8)
    put(SM_FFN, inp["norm_ffn"][0], 8)
    put(SM_GA, inp["norm_grp_a"][0], 4)
    put(SM_GB, inp["norm_grp_b"][0], 4)
    put(SM_CB, inp["conv_b"][0], 4)
    put(SM_BA, inp["gate_a_b"][0], 4)
    put(SM_BX, inp["gate_x_b"][0], 4)
    put(SM_LAM, inp["lru_lambda"][0], 4)
    cw = np.asarray(inp["conv_w"][0], f)
    for cc in range(4):
        for j in range(4):
            sm[:, SM_CW + cc * 4 + j] = cw[j, cc * 128:(cc + 1) * 128]
    gbd = np.zeros((128, 8, 128), f)
    for gi, key in enumerate(("gate_a_w", "gate_x_w")):
        w = np.asarray(inp[key][0], f)
        for cc in range(4):
            gbd[0:64, gi * 4 + cc, 0:64] = w[2 * cc]
            gbd[64:128, gi * 4 + cc, 64:128] = w[2 * cc + 1]
    rb = np.asarray(inp["rel_bias"][0], f)
    qi = np.arange(128)[:, None]
    kj = np.arange(640)[None, :]
    idx = np.clip(512 + qi - kj, -128, 128) + 128
    ab = rb[:, idx]
    valid = np.where(qi < 64, kj < 576, kj >= 64)
    ab = np.where(valid[None], ab, f(NEG)).astype(f)
    abias = np.ascontiguousarray(ab.transpose(1, 0, 2)).reshape(128, 8 * 640)
    sk = np.asarray(inp["sub_keys"][0], f)
    skT = np.ascontiguousarray(sk.reshape(16, 128, 128).transpose(2, 0, 1)).reshape(128, 16 * 128)
    u = np.asarray(inp["expert_u"][0], f)
    uT = np.ascontiguousarray(u.reshape(128, 128, 8, 128).transpose(0, 3, 2, 1)).reshape(16384, D)
    shared = {
        "w_in": g(inp["w_in"][0]), "w_out": g(inp["w_out"][0]), "w_q": g(inp["w_q_mem"][0]),
        "w_kv": g(inp["w_kv_mem"][0]), "w_o": g(inp["w_o_mem"][0]), "w_qry": g(inp["w_query"][0]),
        "smalls": sm, "gbd": gbd.reshape(128, 8 * 128), "abias": abias, "skT": skT, "uT": uT,
        "ev": g(inp["expert_v"][0]),
        "gfin": np.ascontiguousarray(np.broadcast_to(np.asarray(inp["norm_final"], f)[None, :], (128, D))),
        "ident": np.eye(128, dtype=f),
        "iota": np.ascontiguousarray(np.broadcast_to(np.arange(128, dtype=f)[None, :], (128, 128))),
    }
    return shared


def make_in_maps(inp, NT):
    x = np.asarray(inp["x"], np.float32)
    mem = np.asarray(inp["mem"], np.float32)
    B, S, _ = x.shape
    per_seq = S // NT
    NPRE = 3 * NT
    shared = prep_shared(inp)
    maps = []
    for c in range(B * per_seq):
        b, q = divmod(c, per_seq)
        xprev = np.zeros((NPRE, D), np.float32)
        if q > 0:
            xprev[NPRE - q * NT:] = x[b, 0:q * NT]
        pflag = np.zeros((128, NPRE // 512), np.float32)
        for j in range(NPRE // 512):
            if j * 512 >= NPRE - q * NT:
                pflag[:, j] = 1.0
        halob = np.full((128, 512), 0.0 if q > 0 else NEG, np.float32)
        m = dict(shared)
        m.update({"xown": np.ascontiguousarray(x[b, q * NT:(q + 1) * NT]), "xprev": xprev, "pflag": pflag,
                  "halob": halob, "mem": np.ascontiguousarray(mem[b])})
        maps.append(m)
    return maps


_CACHE = {}


def kernel(**inputs):
    NT = 2048
    if NT not in _CACHE:
        _CACHE[NT] = build(NT)[0]
    nc = _CACHE[NT]
    maps = make_in_maps(inputs, NT)
    res = run_bass_kernel_spmd(nc, maps, core_ids=list(range(N_CORES)))
    x = np.asarray(inputs["x"])
    B, S, _ = x.shape
    out = np.empty((B, S, D), np.float32)
    per_seq = S // NT
    for c in range(N_CORES):
        b, q = divmod(c, per_seq)
        out[b, q * NT:(q + 1) * NT] = res.results[c]["y"]
    return out
```

```python
import numpy as np
from contextlib import ExitStack
import concourse.bass as bass
import concourse.mybir as mybir
from concourse.bass_utils import run_bass_kernel_spmd

F32 = mybir.dt.float32
BF16 = mybir.dt.bfloat16
U32 = mybir.dt.uint32
AF = mybir.ActivationFunctionType
ALU = mybir.AluOpType
AX = mybir.AxisListType

D = 1024
EPS = 1e-6
NEG = -1e30
N_CORES = 8
SAME_ENG_SYNC = True

SM_MIX, SM_CROSS, SM_MEM, SM_FFN = 0, 8, 16, 24
SM_GA, SM_GB, SM_CB, SM_BA, SM_BX, SM_LAM, SM_CW = 32, 36, 40, 44, 48, 52, 56
NSM = 72


class Res:
    __slots__ = ("name", "w", "r", "ds")

    def __init__(self, name):
        self.name = name
        self.w = None
        self.r = {}
        self.ds = None


class KB:
    def __init__(self, nc, es):
        self.nc = nc
        self.es = es
        self.E = {}
        for nm, e in (("pe", nc.tensor), ("act", nc.scalar), ("dve", nc.vector),
                      ("pool", nc.gpsimd), ("sp", nc.sync)):
            self.E[nm] = dict(e=e, sem=es.enter_context(nc.semaphore("e_" + nm)), cnt=0, waited={}, nm=nm)
        self.free_ds = []
        self.all_ds = []
        self.n_ins = 0

    def _collect(self, r, w):
        deps = {}
        for x in r:
            if x.w is not None:
                s, v = x.w
                if deps.get(s, 0) < v:
                    deps[s] = v
        for x in w:
            if x.w is not None:
                s, v = x.w
                if deps.get(s, 0) < v:
                    deps[s] = v
            for s, v in x.r.items():
                if deps.get(s, 0) < v:
                    deps[s] = v
        return deps

    def _waits(self, E, deps, skip_own):
        for s, v in deps.items():
            if skip_own and s is E["sem"]:
                continue
            if E["waited"].get(s, 0) >= v:
                continue
            E["e"].wait_ge(s, v)
            E["waited"][s] = v

    def op(self, en, fn, r=(), w=()):
        E = self.E[en]
        skip_own = (en == "pe") or (not SAME_ENG_SYNC)
        self._waits(E, self._collect(r, w), skip_own)
        ins = fn(E["e"])
        E["cnt"] += 1
        self.n_ins += 1
        ins.then_inc(E["sem"], 1)
        tag = (E["sem"], E["cnt"])
        for x in w:
            x.w = tag
            x.r = {}
        for x in r:
            if x not in w:
                x.r[E["sem"]] = E["cnt"]
        return ins

    def dma(self, qn, out, in_, r, w, sres, **kw):
        E = self.E[qn]
        self._waits(E, self._collect(r, w), False)
        if sres.ds is None:
            if qn != "pool" and self.free_ds:
                sres.ds = self.free_ds.pop()
            else:
                sres.ds = [self.es.enter_context(self.nc.semaphore("d%d" % len(self.all_ds))), 0, qn]
                self.all_ds.append(sres.ds)
        ins = E["e"].dma_start(out=out, in_=in_, **kw)
        self.n_ins += 1
        sres.ds[1] += 1
        ins.then_inc(sres.ds[0], 16)
        val = sres.ds[1] * 16
        for x in w:
            x.w = (sres.ds[0], val)
            x.r = {}
        for x in r:
            if x not in w:
                x.r[sres.ds[0]] = val

    def barrier(self, release=()):
        deps = {}
        for nm in ("pe", "act", "dve", "pool"):
            E = self.E[nm]
            if E["cnt"] > 0:
                deps[E["sem"]] = E["cnt"]
        for ds in self.all_ds:
            if ds[1] > 0:
                deps[ds[0]] = ds[1] * 16
        for nm in ("pe", "act", "dve", "pool", "sp"):
            self._waits(self.E[nm], deps, True)
        for x in release:
            if x.ds is not None:
                if x.ds[2] != "pool":
                    self.free_ds.append(x.ds)
                x.ds = None


class Tn:
    def __init__(self, h, name, nres=1):
        self.h = h
        self.res = Res(name)
        self.rs = [Res("%s_%d" % (name, i)) for i in range(nres)] if nres > 1 else [self.res]

    def __getitem__(self, k):
        return self.h[k]


def build(NT=2048, debug=False):
    NPRE = 3 * NT
    NTILE = NT // 128
    nc = bass.Bass("TRN2", target_bir_lowering=False)
    dt_in = lambda n, s, d=F32: nc.dram_tensor(n, list(s), d, kind="ExternalInput").ap()
    xown = dt_in("xown", [NT, D])
    xprev = dt_in("xprev", [NPRE, D])
    pflag_d = dt_in("pflag", [128, NPRE // 512])
    halob_d = dt_in("halob", [128, 512])
    mem_d = dt_in("mem", [256, D])
    w_in_d = dt_in("w_in", [D, 2560])
    w_out_d = dt_in("w_out", [D, D])
    w_q_d = dt_in("w_q", [D, D])
    w_kv_d = dt_in("w_kv", [D, 2048])
    w_o_d = dt_in("w_o", [D, D])
    w_qry_d = dt_in("w_qry", [D, 2048])
    smalls_d = dt_in("smalls", [128, NSM])
    gbd_d = dt_in("gbd", [128, 8 * 128])
    abias_d = dt_in("abias", [128, 8 * 640])
    skT_d = dt_in("skT", [128, 16 * 128])
    uT_d = dt_in("uT", [16384, D])
    ev_d = dt_in("ev", [16384, D])
    gfin_d = dt_in("gfin", [128, D])
    ident_d = dt_in("ident", [128, 128])
    iota_d = dt_in("iota", [128, 128])
    y_d = nc.dram_tensor("y", [NT, D], F32, kind="ExternalOutput").ap()
    wd_d = nc.dram_tensor("wd_scratch", [NTILE, 128, 16384], BF16, kind="Internal").ap()
    dbg = {}
    if debug:
        for nm in ("dbg1", "dbg2"):
            dbg[nm] = nc.dram_tensor(nm, [NT, D], F32, kind="ExternalOutput").ap()

    with ExitStack() as es:
        k = KB(nc, es)

        def sb(ctx, name, shape, dt, nres=1):
            return Tn(ctx.enter_context(nc.sbuf_tensor("sb_" + name, list(shape), dt)), name, nres)

        def ps(ctx, name, shape, dt):
            return Tn(ctx.enter_context(nc.psum_tensor("ps_" + name, list(shape), dt)), name)

        x_res = sb(es, "x_res", [128, NTILE, D], F32, nres=NTILE)
        ident_f = sb(es, "ident_f", [128, 128], F32)
        ident_b = sb(es, "ident_b", [128, 128], BF16)
        iota_f = sb(es, "iota_f", [128, 128], F32)
        ones_b = sb(es, "ones_b", [128, 128], BF16)
        smalls = sb(es, "smalls", [128, NSM], F32)
        cst = sb(es, "cst", [128, 4], F32)
        gB = sb(es, "gB", [128, 8, 128], F32)
        nrm = [dict(ss=sb(es, "n_ss%d" % i, [128, 1], F32), sd=sb(es, "n_sd%d" % i, [128, 1], F32),
                    rstd=sb(es, "n_rstd%d" % i, [128, 1], F32), xs=sb(es, "n_xs%d" % i, [128, D], BF16),
                    junk=sb(es, "n_junk%d" % i, [128, D], BF16)) for i in range(2)]
        nrm_i = [0]

        k.dma("sp", ident_f[:], ident_d[:, :], r=[], w=[ident_f.res], sres=ident_f.res)
        k.dma("sp", iota_f[:], iota_d[:, :], r=[], w=[iota_f.res], sres=iota_f.res)
        k.dma("sp", smalls[:], smalls_d[:, :], r=[], w=[smalls.res], sres=smalls.res)
        k.op("dve", lambda e: e.tensor_copy(out=ident_b[:], in_=ident_f[:]), r=[ident_f.res], w=[ident_b.res])
        k.op("pool", lambda e: e.memset(ones_b[:], 1.0), w=[ones_b.res])
        k.op("pool", lambda e: e.memset(cst[:, 0:1], EPS), w=[cst.res])
        k.op("pool", lambda e: e.memset(cst[:, 1:2], 1.0), w=[cst.res])
        k.op("pool", lambda e: e.memset(cst[:, 2:3], 0.0), w=[cst.res])

        def set_gain(col):
            k.op("dve", lambda e: e.tensor_copy(
                out=gB[:], in_=smalls[:, col:col + 8].unsqueeze(2).to_broadcast([128, 8, 128])),
                r=[smalls.res], w=[gB.res])

        def norm_T_g(x_ap, x_r, pT, hT_ap, hT_r):
            n = nrm[nrm_i[0] % 2]
            nrm_i[0] += 1
            k.op("dve", lambda e: e.scalar_tensor_tensor(out=n["junk"][:], in0=x_ap, scalar=1.0, in1=x_ap,
                                                         op0=ALU.mult, op1=ALU.mult, accum_out=n["ss"][:]),
                 r=[x_r], w=[n["junk"].res, n["ss"].res])
            yield
            k.op("act", lambda e: e.activation(out=n["sd"][:], in_=n["ss"][:], func=AF.Ln,
                                               bias=cst[:, 0:1], scale=1.0 / D),
                 r=[n["ss"].res, cst.res], w=[n["sd"].res])
            k.op("act", lambda e: e.activation(out=n["rstd"][:], in_=n["sd"][:], func=AF.Exp, scale=-0.5),
                 r=[n["sd"].res], w=[n["rstd"].res])
            yield
            k.op("dve", lambda e: e.tensor_scalar(out=n["xs"][:], in0=x_ap, scalar1=n["rstd"][:, 0:1], scalar2=None,
                                                  op0=ALU.mult), r=[x_r, n["rstd"].res], w=[n["xs"].res])
            for c in range(8):
                k.op("pe", lambda e, c=c: e.transpose(out=pT[:, c, :], in_=n["xs"][:, c * 128:(c + 1) * 128],
                                                      identity=ident_b[:]),
                     r=[n["xs"].res, ident_b.res], w=[pT.res])
            yield
            k.op("dve", lambda e: e.tensor_tensor(out=hT_ap, in0=pT[:], in1=gB[:], op=ALU.mult),
                 r=[pT.res, gB.res], w=[hT_r])

        def norm_T(*a):
            for _ in norm_T_g(*a):
                pass

        def lockstep_g(gens):
            gens = list(gens)
            while gens:
                for g_ in list(gens):
                    try:
                        next(g_)
                    except StopIteration:
                        gens.remove(g_)
                yield

        def norm_pairs(args_list):
            for i_ in range(0, len(args_list), 2):
                for _ in lockstep_g([norm_T_g(*a) for a in args_list[i_:i_ + 2]]):
                    pass

        def mm_group(out_ap, pairs, r, w):
            n = len(pairs)
            for i, (l, rh) in enumerate(pairs):
                k.op("pe", lambda e, l=l, rh=rh, i=i: e.matmul(out_ap, lhsT=l, rhs=rh, start=(i == 0), stop=(i == n - 1)),
                     r=r, w=w)

        def load_w_bf16(dst, src_ap, ncols):
            for c in range(8):
                k.dma("pool", dst[:, c, :], src_ap[c * 128:(c + 1) * 128, :], r=[], w=[dst.res], sres=dst.res,
                      max_dma_last_dim=4096)

        with ExitStack() as p1:
            with ExitStack() as pa:
                BA = 512
                w_inA = sb(pa, "w_inA", [128, 8, 1024], BF16)
                load_w_bf16(w_inA, w_in_d[:, 0:1024], 1024)
                w_outA = sb(pa, "w_outA", [128, 4, D], BF16)
                for c in range(4):
                    k.dma("pool", w_outA[:, c, :], w_out_d[c * 128:(c + 1) * 128, :], r=[], w=[w_outA.res], sres=w_outA.res,
                          max_dma_last_dim=4096)
                yan = sb(pa, "yan", [128, 4, 512], BF16)
                gbd_f = sb(pa, "gbd_f", [128, 8, 128], F32)
                gbd = sb(pa, "gbd", [128, 8, 128], BF16)
                k.dma("sp", gbd_f[:], gbd_d.rearrange("p (c j) -> p c j", c=8), r=[], w=[gbd_f.res], sres=gbd_f.res)
                k.op("dve", lambda e: e.tensor_copy(out=gbd[:], in_=gbd_f[:]), r=[gbd_f.res], w=[gbd.res])
                pflag = sb(pa, "pflag", [128, NPRE // 512], F32)
                k.dma("sp", pflag[:], pflag_d[:, :], r=[], w=[pflag.res], sres=pflag.res)
                cL = sb(pa, "cL", [128, 4], F32)
                tmp4 = sb(pa, "tmp4", [128, 4], F32)
                k.op("act", lambda e: e.activation(out=tmp4[:], in_=smalls[:, SM_LAM:SM_LAM + 4], func=AF.Exp, scale=-1.0),
                     r=[smalls.res], w=[tmp4.res])
                k.op("act", lambda e: e.activation(out=cL[:], in_=tmp4[:], func=AF.Ln, bias=cst[:, 1:2], scale=1.0),
                     r=[tmp4.res, cst.res], w=[cL.res])
                k.op("dve", lambda e: e.tensor_scalar(out=cL[:], in0=cL[:], scalar1=-8.0, scalar2=None, op0=ALU.mult),
                     r=[cL.res], w=[cL.res])
                set_gain(SM_MIX)
                xtmp = [sb(pa, "xtmp%d" % i, [128, D], F32) for i in range(2)]
                hT = [sb(pa, "hTa%d" % i, [128, 8, BA], BF16) for i in range(2)]
                xl = sb(pa, "xl", [128, 4, 3 + BA], F32, nres=4)
                gg = sb(pa, "gg", [128, 4, BA], F32, nres=4)
                hh = sb(pa, "hh", [128, 4, BA], F32, nres=4)
                hst = sb(pa, "hst", [128, 4], F32, nres=4)
                yaT = sb(pa, "yaT", [128, 4, BA], F32, nres=4)
                LT = [{nm: sb(pa, "l%s%d" % (nm, i), [128, BA], BF16 if nm == "xcb" else F32)
                       for nm in ("xc", "xc2", "xcb", "rr", "ii", "aa", "tt", "bb")} for i in range(2)]
                sq = sb(pa, "sq", [128, BA], BF16)
                rsn = sb(pa, "rsn", [128, BA], F32)
                pj = [ps(pa, "pj%d" % i, [128, 512], F32) for i in range(2)]
                pT = [ps(pa, "pT%d" % i, [128, 8, 128], BF16) for i in range(2)]
                pg = [ps(pa, "pg%d" % i, [128, 512], F32) for i in range(4)]
                pn = pj[0]
                k.op("pool", lambda e: e.memset(xl[:], 0.0), w=xl.rs)
                k.op("pool", lambda e: e.memset(hst[:], 0.0), w=hst.rs)

                nblk_pre = NPRE // BA
                nblk_own = NT // BA
                tcount = 0
                pjc = 0
                def front_norm(b):
                    nonlocal tcount
                    own = b >= nblk_pre
                    ob = b - nblk_pre
                    h = hT[b % 2]

                    def tile_g(t):
                        nonlocal tcount
                        if own:
                            tile = ob * 4 + t
                            xa = x_res[:, tile, :]
                            xr = x_res.rs[tile]
                            k.dma("sp", xa, xown[tile * 128:(tile + 1) * 128, :], r=[], w=[xr], sres=xr)
                        else:
                            xt_ = xtmp[tcount % 2]
                            xa = xt_[:]
                            xr = xt_.res
                            r0 = b * BA + t * 128
                            k.dma("sp", xa, xprev[r0:r0 + 128, :], r=[], w=[xr], sres=xr)
                        tcount += 1
                        yield from norm_T_g(xa, xr, pT[t % 2], h[:, :, t * 128:(t + 1) * 128], h.res)

                    yield from lockstep_g([tile_g(0), tile_g(1)])
                    yield from lockstep_g([tile_g(2), tile_g(3)])

                def front_proj(b):
                    nonlocal pjc
                    own = b >= nblk_pre
                    h = hT[b % 2]
                    for cc in range(8 if own else 4):
                        pp = pj[pjc % 2]
                        pjc += 1
                        mm_group(pp[:], [(w_inA[:, kc, cc * 128:(cc + 1) * 128], h[:, kc, :]) for kc in range(8)],
                                 r=[w_inA.res, h.res], w=[pp.res])
                        if cc < 4:
                            k.op("act", lambda e, pp=pp, cc=cc: e.activation(out=xl[:, cc, 3:3 + BA], in_=pp[:], func=AF.Copy),
                                 r=[pp.res], w=[xl.rs[cc]])
                        else:
                            k.op("act", lambda e, pp=pp, cc=cc: e.activation(out=gg[:, cc - 4, :], in_=pp[:], func=AF.Gelu_apprx_tanh),
                                 r=[pp.res], w=[gg.rs[cc - 4]])

                for _ in front_norm(0):
                    pass
                front_proj(0)
                for b in range(nblk_pre + nblk_own):
                    own = b >= nblk_pre
                    ob = b - nblk_pre
                    fn = front_norm(b + 1) if b + 1 < nblk_pre + nblk_own else iter(())
                    def lru_chain(cc, L, pga, pgb, own=own, b=b):
                            cw = lambda j, cc=cc: smalls[:, SM_CW + cc * 4 + j:SM_CW + cc * 4 + j + 1]
                            k.op("dve", lambda e, cc=cc, L=L, cw=cw: e.tensor_scalar(
                                out=L["xc"][:], in0=xl[:, cc, 3:3 + BA], scalar1=cw(3), scalar2=smalls[:, SM_CB + cc:SM_CB + cc + 1],
                                op0=ALU.mult, op1=ALU.add), r=[xl.rs[cc], smalls.res], w=[L["xc"].res])
                            src, dst = "xc", "xc2"
                            for j in range(3):
                                yield
                                k.op("dve", lambda e, cc=cc, L=L, cw=cw, j=j, src=src, dst=dst: e.scalar_tensor_tensor(
                                    out=L[dst][:], in0=xl[:, cc, j:j + BA], scalar=cw(j), in1=L[src][:], op0=ALU.mult, op1=ALU.add),
                                    r=[xl.rs[cc], smalls.res, L[src].res], w=[L[dst].res])
                                src, dst = dst, src
                            xc = L[src]
                            yield
                            k.op("pool", lambda e, cc=cc: e.tensor_copy(out=xl[:, cc, 0:3], in_=xl[:, cc, BA:BA + 3]),
                                 r=[xl.rs[cc]], w=[xl.rs[cc]])
                            yield
                            k.op("act", lambda e, L=L, xc=xc: e.activation(out=L["xcb"][:], in_=xc[:], func=AF.Copy),
                                 r=[xc.res], w=[L["xcb"].res])
                            yield
                            k.op("pe", lambda e, cc=cc, L=L: e.matmul(pga[:], lhsT=gbd[:, cc, :], rhs=L["xcb"][:], start=True, stop=True),
                                 r=[gbd.res, L["xcb"].res], w=[pga.res])
                            yield
                            k.op("pe", lambda e, cc=cc, L=L: e.matmul(pgb[:], lhsT=gbd[:, 4 + cc, :], rhs=L["xcb"][:], start=True, stop=True),
                                 r=[gbd.res, L["xcb"].res], w=[pgb.res])
                            yield
                            k.op("act", lambda e, cc=cc, L=L: e.activation(out=L["rr"][:], in_=pga[:], func=AF.Sigmoid,
                                                                           bias=smalls[:, SM_BA + cc:SM_BA + cc + 1], scale=1.0),
                                 r=[pga.res, smalls.res], w=[L["rr"].res])
                            yield
                            k.op("act", lambda e, cc=cc, L=L: e.activation(out=L["ii"][:], in_=pgb[:], func=AF.Sigmoid,
                                                                           bias=smalls[:, SM_BX + cc:SM_BX + cc + 1], scale=1.0),
                                 r=[pgb.res, smalls.res], w=[L["ii"].res])
                            yield
                            k.op("act", lambda e, cc=cc, L=L: e.activation(out=L["aa"][:], in_=L["rr"][:], func=AF.Exp,
                                                                           scale=cL[:, cc:cc + 1]),
                                 r=[L["rr"].res, cL.res], w=[L["aa"].res])
                            yield
                            k.op("dve", lambda e, L=L: e.tensor_tensor(out=L["tt"][:], in0=L["aa"][:], in1=L["aa"][:], op=ALU.mult),
                                 r=[L["aa"].res], w=[L["tt"].res])
                            yield
                            k.op("act", lambda e, L=L: e.activation(out=L["tt"][:], in_=L["tt"][:], func=AF.Sqrt,
                                                                    bias=cst[:, 1:2], scale=-1.0),
                                 r=[L["tt"].res, cst.res], w=[L["tt"].res])
                            yield
                            k.op("dve", lambda e, L=L: e.tensor_tensor(out=L["bb"][:], in0=L["tt"][:], in1=L["ii"][:], op=ALU.mult),
                                 r=[L["tt"].res, L["ii"].res], w=[L["bb"].res])
                            yield
                            k.op("dve", lambda e, L=L, xc=xc: e.tensor_tensor(out=L["rr"][:], in0=L["bb"][:], in1=xc[:], op=ALU.mult),
                                 r=[L["bb"].res, xc.res], w=[L["rr"].res])
                            yield
                            k.op("dve", lambda e, cc=cc, L=L: e.tensor_tensor_scan(
                                out=hh[:, cc, :], data0=L["aa"][:], data1=L["rr"][:], initial=hst[:, cc:cc + 1],
                                op0=ALU.mult, op1=ALU.add), r=[L["aa"].res, L["rr"].res, hst.rs[cc]], w=[hh.rs[cc]])
                            if own:
                                yield
                                k.op("dve", lambda e, cc=cc: e.tensor_copy(out=hst[:, cc:cc + 1], in_=hh[:, cc, BA - 1:BA]),
                                     r=[hh.rs[cc]], w=[hst.rs[cc]])
                                yield
                                k.op("dve", lambda e, cc=cc: e.tensor_tensor(out=yaT[:, cc, :], in0=hh[:, cc, :], in1=gg[:, cc, :], op=ALU.mult),
                                     r=[hh.rs[cc], gg.rs[cc]], w=[yaT.rs[cc]])
                            else:
                                yield
                                k.op("dve", lambda e, cc=cc, b=b: e.tensor_tensor(out=hst[:, cc:cc + 1], in0=hh[:, cc, BA - 1:BA],
                                                                                  in1=pflag[:, b:b + 1], op=ALU.mult),
                                     r=[hh.rs[cc], pflag.res], w=[hst.rs[cc]])

                    for pair in range(2):
                        gens = [lru_chain(2 * pair + i_, LT[i_], pg[2 * i_], pg[2 * i_ + 1]) for i_ in range(2)]
                        while gens:
                            for g_ in list(gens):
                                try:
                                    next(g_)
                                except StopIteration:
                                    gens.remove(g_)
                            next(fn, None)
                    for _ in fn:
                        pass
                    if own:
                        for cc in range(4):
                            k.op("dve", lambda e, cc=cc: e.tensor_tensor(out=sq[:], in0=yaT[:, cc, :], in1=yaT[:, cc, :], op=ALU.mult),
                                 r=[yaT.rs[cc]], w=[sq.res])
                            k.op("pe", lambda e, cc=cc: e.matmul(pn[:], lhsT=ones_b[:], rhs=sq[:], start=(cc == 0), stop=(cc == 3)),
                                 r=[ones_b.res, sq.res], w=[pn.res])
                        k.op("act", lambda e: e.activation(out=rsn[:], in_=pn[:], func=AF.Ln, bias=cst[:, 0:1], scale=1.0 / 512),
                             r=[pn.res, cst.res], w=[rsn.res])
                        k.op("act", lambda e: e.activation(out=rsn[:], in_=rsn[:], func=AF.Exp, scale=-0.5), r=[rsn.res], w=[rsn.res])
                        for cc in range(4):
                            k.op("dve", lambda e, cc=cc, ob=ob: e.scalar_tensor_tensor(
                                out=yan[:, cc, :], in0=yaT[:, cc, :],
                                scalar=smalls[:, SM_GA + cc:SM_GA + cc + 1], in1=rsn[:], op0=ALU.mult, op1=ALU.mult),
                                r=[yaT.rs[cc], smalls.res, rsn.res], w=[yan.res])
                        for t in range(4):
                            tile = ob * 4 + t
                            for half in range(2):
                                pp = pj[pjc % 2]
                                pjc += 1
                                mm_group(pp[:], [(yan[:, c, t * 128:(t + 1) * 128], w_outA[:, c, half * 512:(half + 1) * 512]) for c in range(4)],
                                         r=[yan.res, w_outA.res], w=[pp.res])
                                k.op("dve", lambda e, pp=pp, tile=tile, half=half: e.tensor_tensor(
                                    out=x_res[:, tile, half * 512:(half + 1) * 512], in0=pp[:], in1=x_res[:, tile, half * 512:(half + 1) * 512],
                                    op=ALU.add), r=[pp.res, x_res.rs[tile]], w=[x_res.rs[tile]])
                    if b + 1 < nblk_pre + nblk_own:
                        front_proj(b + 1)
                k.barrier(release=[w_inA.res, w_outA.res, gbd_f.res, pflag.res] + [t_.res for t_ in xtmp])

            with ExitStack() as pb:
                BB = 256
                w_inB = sb(pb, "w_inB", [128, 8, 1536], BF16)
                load_w_bf16(w_inB, w_in_d[:, 1024:2560], 1536)
                w_out = sb(pb, "w_outB", [128, 4, D], BF16)
                for c in range(4):
                    k.dma("pool", w_out[:, c, :], w_out_d[512 + c * 128:512 + (c + 1) * 128, :], r=[], w=[w_out.res], sres=w_out.res,
                          max_dma_last_dim=4096)
                abias = sb(pb, "abias", [128, 8, 640], F32)
                k.dma("sp", abias[:], abias_d.rearrange("p (h c) -> p h c", h=8), r=[], w=[abias.res], sres=abias.res)
                halob = sb(pb, "halob", [128, 512], F32)
                k.dma("sp", halob[:], halob_d[:, :], r=[], w=[halob.res], sres=halob.res)
                xtmp = [sb(pb, "xtmpb%d" % i, [128, D], F32) for i in range(2)]
                hT = [sb(pb, "hTb%d" % i, [128, 8, BB], BF16) for i in range(2)]
                qA_l = [sb(pb, "qA%d" % i, [128, 4, BB], BF16) for i in range(2)]
                qB_l = [sb(pb, "qB%d" % i, [128, 4, BB], BF16) for i in range(2)]
                kT = [sb(pb, "kT%d" % i, [128, 4, BB], BF16) for i in range(4)]
                vpad = [sb(pb, "vpad%d" % i, [128, 8, 128], BF16) for i in range(8)]
                ybT = sb(pb, "ybT", [128, 4, BB], F32)
                ybn = sb(pb, "ybn", [128, 4, BB], BF16)
                sbuf_s = [sb(pb, "sbs%d" % i, [128, 640], F32) for i in range(2)]
                Pm = [sb(pb, "Pm%d" % i, [128, 640], BF16) for i in range(2)]
                Pn = [sb(pb, "Pn%d" % i, [128, 640], BF16) for i in range(2)]
                PT = [sb(pb, "PT%d" % i, [128, 5, 128], BF16) for i in range(2)]
                st = [dict(mx=sb(pb, "a_mx%d" % i, [128, 1], F32), rs=sb(pb, "a_rs%d" % i, [128, 1], F32),
                           ri=sb(pb, "a_ri%d" % i, [128, 1], F32)) for i in range(2)]
                sq4 = [sb(pb, "sqb%d" % i, [128, BB], BF16) for i in range(4)]
                rsn = sb(pb, "rsnb", [128, BB], F32)
                pj = [ps(pb, "pjb%d" % i, [128, 512], F32) for i in range(2)]
                pT = [ps(pb, "pTb%d" % i, [128, 8, 128], BF16) for i in range(1)]
                pSm = [ps(pb, "pSm%d" % i, [128, 512], F32) for i in range(2)]
                pSr = Tn(pb.enter_context(nc.psum_tensor("ps_pSr", [128, 4, 128], F32)), "pSr", nres=4)
                pPT = ps(pb, "pPT", [128, 5, 128], BF16)
                pO = ps(pb, "pO", [128, 4, 128], F32)
                pn = pj[0]
                for v_ in vpad:
                    k.op("pool", lambda e, v_=v_: e.memset(v_[:], 0.0), w=[v_.res])
                for q_ in qA_l + qB_l:
                    k.op("pool", lambda e, q_=q_: e.memset(q_[:], 0.0), w=[q_.res])
                set_gain(SM_MIX)

                nhalo = 512 // BB
                nown = NT // BB
                tcount = 0
                pjc = 0
                ac = 0
                def prep(b):
                    nonlocal tcount, pjc
                    own = b >= nhalo
                    ob = b - nhalo
                    h = hT[b % 2]
                    kcur = kT[b % 4]
                    qA, qB = qA_l[b % 2], qB_l[b % 2]
                    for t in range(2):
                        xt_ = xtmp[tcount % 2]
                        xa = xt_[:]
                        xr = xt_.res
                        if own:
                            r0 = ob * BB + t * 128
                            k.dma("sp", xa, xown[r0:r0 + 128, :], r=[], w=[xr], sres=xr)
                        else:
                            r0 = NPRE - 512 + b * BB + t * 128
                            k.dma("sp", xa, xprev[r0:r0 + 128, :], r=[], w=[xr], sres=xr)
                        yield from norm_T_g(xa, xr, pT[0], h[:, :, t * 128:(t + 1) * 128], h.res)
                        tcount += 1
                        yield
                    for m in range(4):
                        pp = pj[pjc % 2]
                        pjc += 1
                        mm_group(pp[:, 0:BB], [(w_inB[:, kc, 512 + m * 128:512 + (m + 1) * 128], h[:, kc, :]) for kc in range(8)],
                                 r=[w_inB.res, h.res], w=[pp.res])
                        k.op("act", lambda e, pp=pp, m=m, kcur=kcur: e.activation(out=kcur[:, m, :], in_=pp[:, 0:BB], func=AF.Copy),
                             r=[pp.res], w=[kcur.res])
                    yield
                    for t in range(2):
                        yield
                        gt = b * 2 + t
                        vp = vpad[gt % 8]
                        pp = pj[pjc % 2]
                        pjc += 1
                        mm_group(pp[:], [(h[:, kc, t * 128:(t + 1) * 128], w_inB[:, kc, 1024:1536]) for kc in range(8)],
                                 r=[w_inB.res, h.res], w=[pp.res])
                        ppv = pp[:].rearrange("p (h d) -> p h d", h=8)
                        k.op("act", lambda e, vp=vp, ppv=ppv: e.activation(out=vp[:, 0:8:2, 0:64], in_=ppv[:, 0:8:2, :], func=AF.Copy),
                             r=[pp.res], w=[vp.res])
                        k.op("act", lambda e, vp=vp, ppv=ppv: e.activation(out=vp[:, 1:8:2, 64:128], in_=ppv[:, 1:8:2, :], func=AF.Copy),
                             r=[pp.res], w=[vp.res])
                    if not own:
                        return
                    yield
                    for m in range(4):
                        if m == 2:
                            yield
                        pp = pj[pjc % 2]
                        pjc += 1
                        mm_group(pp[:, 0:BB], [(w_inB[:, kc, m * 128:(m + 1) * 128], h[:, kc, :]) for kc in range(8)],
                                 r=[w_inB.res, h.res], w=[pp.res])
                        k.op("act", lambda e, pp=pp, m=m: e.activation(out=qA[0:64, m, :], in_=pp[0:64, 0:BB], func=AF.Copy),
                             r=[pp.res], w=[qA.res])
                        k.op("act", lambda e, pp=pp, m=m: e.activation(out=qB[64:128, m, :], in_=pp[64:128, 0:BB], func=AF.Copy),
                             r=[pp.res], w=[qB.res])
                def attend(b, fillers):
                    nonlocal pjc, ac
                    ob = b - nhalo
                    qA, qB = qA_l[b % 2], qB_l[b % 2]
                    units = []
                    for p in range(2):
                        pieces = []
                        oc = 0
                        remaining = 640
                        pos = 128 * p
                        while remaining > 0:
                            bi = pos // BB
                            c0 = pos % BB
                            n = min(BB - c0, remaining)
                            if oc < 512 and oc + n > 512:
                                n = 512 - oc
                            pieces.append((kT[(b - 2 + bi) % 4], c0, n, oc))
                            oc += n
                            pos += n
                            remaining -= n
                        for hd in range(8):
                            units.append((p, hd, pieces))

                    def S1(u):
                        p, hd, pieces = units[u]
                        m = hd // 2
                        qs = (qA if hd % 2 == 0 else qB)
                        gi = ac0 + u
                        pm, pr = pSm[gi % 2], pSr
                        for (kt_, c0, n, oc) in pieces:
                            if oc < 512:
                                oap = pm[:, oc:oc + n]
                                ores = pm.res
                            else:
                                oap = pr[:, gi % 4, oc - 512:oc - 512 + n]
                                ores = pr.res
                            k.op("pe", lambda e, kt_=kt_, c0=c0, n=n, oap=oap: e.matmul(
                                oap, lhsT=qs[:, m, p * 128:(p + 1) * 128], rhs=kt_[:, m, c0:c0 + n],
                                start=True, stop=True), r=[qs.res, kt_.res], w=[ores])

                    def S2(u, part):
                        p, hd, pieces = units[u]
                        gi = ac0 + u
                        i2 = gi % 2
                        pm, pr = pSm[gi % 2], pSr
                        S, PP, PN, s_ = sbuf_s[i2], Pm[i2], Pn[i2], st[i2]
                        if part == 1:
                            k.op("dve", lambda e: e.reciprocal(out=s_["ri"][:], in_=s_["rs"][:]), r=[s_["rs"].res], w=[s_["ri"].res])
                            k.op("dve", lambda e: e.tensor_scalar(out=PN[:], in0=PP[:], scalar1=s_["ri"][:, 0:1],
                                                                  scalar2=None, op0=ALU.mult),
                                 r=[PP.res, s_["ri"].res], w=[PN.res])
                            return
                        k.op("dve", lambda e: e.scalar_tensor_tensor(
                            out=S[:, 0:512], in0=pm[:], scalar=0.125, in1=abias[:, hd, 0:512], op0=ALU.mult, op1=ALU.add),
                            r=[pm.res, abias.res], w=[S.res])
                        k.op("dve", lambda e: e.scalar_tensor_tensor(
                            out=S[:, 512:640], in0=pr[:, gi % 4, :], scalar=0.125, in1=abias[:, hd, 512:640], op0=ALU.mult, op1=ALU.add),
                            r=[pr.res, abias.res], w=[S.res])
                        cnt = 512 - ob * BB - 128 * p
                        if cnt > 0:
                            hb0 = ob * BB + 128 * p
                            k.op("dve", lambda e: e.tensor_tensor(
                                out=S[:, 0:cnt], in0=S[:, 0:cnt], in1=halob[:, hb0:512], op=ALU.add),
                                r=[S.res, halob.res], w=[S.res])
                        k.op("dve", lambda e: e.tensor_reduce(out=s_["mx"][:], in_=S[:], axis=AX.X, op=ALU.max, negate=True),
                             r=[S.res], w=[s_["mx"].res])
                        k.op("act", lambda e: e.activation(out=PP[:], in_=S[:], func=AF.Exp, bias=s_["mx"][:, 0:1],
                                                           scale=1.0, accum_out=s_["rs"][:]),
                             r=[S.res, s_["mx"].res], w=[PP.res, s_["rs"].res])

                    def S3(u):
                        p, hd, pieces = units[u]
                        m = hd // 2
                        gi = ac0 + u
                        i2 = gi % 2
                        PN, PTs = Pn[i2], PT[i2]
                        for kc in range(5):
                            k.op("pe", lambda e, kc=kc: e.transpose(out=pPT[:, kc, :], in_=PN[:, kc * 128:(kc + 1) * 128],
                                                                    identity=ident_b[:]),
                                 r=[PN.res, ident_b.res], w=[pPT.res])
                        k.op("act", lambda e: e.activation(out=PTs[:], in_=pPT[:], func=AF.Copy), r=[pPT.res], w=[PTs.res])
                        g0 = (b * 2 + p) - 4
                        for kc in range(5):
                            vp = vpad[(g0 + kc) % 8]
                            k.op("pe", lambda e, vp=vp, kc=kc: e.matmul(
                                pO[:, m, :], lhsT=vp[:, hd, :], rhs=PTs[:, kc, :],
                                start=(hd % 2 == 0 and kc == 0), stop=(hd % 2 == 1 and kc == 4)),
                                r=[vp.res, PTs.res], w=[pO.res])
                        if hd == 7:
                            k.op("act", lambda e: e.activation(out=ybT[:, :, p * 128:(p + 1) * 128], in_=pO[:], func=AF.Copy),
                                 r=[pO.res], w=[ybT.res])

                    def advance():
                        while fillers:
                            try:
                                next(fillers[0])
                                return
                            except StopIteration:
                                fillers.pop(0)

                    ac0 = ac
                    S1(0)
                    S1(1)
                    S2(0, 0)
                    for u in range(len(units)):
                        if u + 1 < len(units):
                            S2(u + 1, 0)
                        S2(u, 1)
                        if u + 2 < len(units):
                            S1(u + 2)
                        S3(u)
                        advance()
                    while fillers:
                        advance()
                    ac += len(units)

                def tail(b):
                    nonlocal pjc
                    ob = b - nhalo
                    for cc in range(4):
                        k.op("dve", lambda e, cc=cc: e.tensor_tensor(out=sq4[cc][:], in0=ybT[:, cc, :], in1=ybT[:, cc, :], op=ALU.mult),
                             r=[ybT.res], w=[sq4[cc].res])
                    for cc in range(4):
                        k.op("pe", lambda e, cc=cc: e.matmul(pn[:, 0:BB], lhsT=ones_b[:], rhs=sq4[cc][:], start=(cc == 0), stop=(cc == 3)),
                             r=[ones_b.res, sq4[cc].res], w=[pn.res])
                    yield
                    k.op("act", lambda e: e.activation(out=rsn[:], in_=pn[:, 0:BB], func=AF.Ln, bias=cst[:, 0:1], scale=1.0 / 512),
                         r=[pn.res, cst.res], w=[rsn.res])
                    k.op("act", lambda e: e.activation(out=rsn[:], in_=rsn[:], func=AF.Exp, scale=-0.5), r=[rsn.res], w=[rsn.res])
                    yield
                    for cc in range(4):
                        k.op("dve", lambda e, cc=cc: e.scalar_tensor_tensor(
                            out=ybn[:, cc, :], in0=ybT[:, cc, :], scalar=smalls[:, SM_GB + cc:SM_GB + cc + 1], in1=rsn[:],
                            op0=ALU.mult, op1=ALU.mult), r=[ybT.res, smalls.res, rsn.res], w=[ybn.res])
                    yield
                    for t in range(2):
                        tile = ob * 2 + t
                        tok0 = ob * BB + t * 128
                        for half in range(2):
                            pp = pj[pjc % 2]
                            pjc += 1
                            pairs = [(ybn[:, c, t * 128:(t + 1) * 128], w_out[:, c, half * 512:(half + 1) * 512]) for c in range(4)]
                            mm_group(pp[:], pairs, r=[ybn.res, w_out.res], w=[pp.res])
                            k.op("dve", lambda e, pp=pp, tile=tile, half=half: e.tensor_tensor(
                                out=x_res[:, tile, half * 512:(half + 1) * 512], in0=pp[:], in1=x_res[:, tile, half * 512:(half + 1) * 512],
                                op=ALU.add), r=[pp.res, x_res.rs[tile]], w=[x_res.rs[tile]])
                        yield
                nb_ = nhalo + nown
                for b0 in range(nhalo + 1):
                    for _ in prep(b0):
                        pass
                for b in range(nhalo, nb_):
                    fl = []
                    if b - 1 >= nhalo:
                        fl.append(tail(b - 1))
                    if b + 1 < nb_:
                        fl.append(prep(b + 1))
                    attend(b, fl)
                for _ in tail(nb_ - 1):
                    pass
                k.barrier(release=[w_inB.res, w_out.res, abias.res, halob.res] + [t_.res for t_ in xtmp])

        if debug:
            for tile in range(NTILE):
                k.dma("sp", dbg["dbg1"][tile * 128:(tile + 1) * 128, :], x_res[:, tile, :], r=[x_res.rs[tile]], w=[], sres=x_res.rs[tile])

        with ExitStack() as p2:
            B2 = 512
            w_q = sb(p2, "w_q", [128, 8, D], BF16)
            w_o = sb(p2, "w_o", [128, 8, D], BF16)
            kmT = sb(p2, "kmT", [128, 8, 256], BF16)
            vmem = sb(p2, "vmem", [128, 2, D], BF16)
            pj = [ps(p2, "pj2%d" % i, [128, 512], F32) for i in range(2)]
            pT = [ps(p2, "pT2%d" % i, [128, 8, 128], BF16) for i in range(1)]
            pS2 = [ps(p2, "pS2%d" % i, [128, 4, 256], F32) for i in range(2)]
            pPT2 = ps(p2, "pPT2", [128, 8, 128], BF16)
            pjc = 0
            with ExitStack() as p2s:
                w_kv = sb(p2s, "w_kv", [128, 8, 2048], BF16)
                load_w_bf16(w_kv, w_kv_d[:, :], 2048)
                load_w_bf16(w_q, w_q_d[:, :], D)
                load_w_bf16(w_o, w_o_d[:, :], D)
                memt = [sb(p2s, "memt%d" % i, [128, D], F32) for i in range(2)]
                memT = sb(p2s, "memT", [128, 8, 256], BF16)
                set_gain(SM_MEM)
                for t in range(2):
                    k.dma("sp", memt[t][:], mem_d[t * 128:(t + 1) * 128, :], r=[], w=[memt[t].res], sres=memt[t].res)
                    norm_T(memt[t][:], memt[t].res, pT[0], memT[:, :, t * 128:(t + 1) * 128], memT.res)
                for oc in range(8):
                    pp = pj[pjc % 2]
                    pjc += 1
                    mm_group(pp[:, 0:256], [(w_kv[:, kc, oc * 128:(oc + 1) * 128], memT[:, kc, :]) for kc in range(8)],
                             r=[w_kv.res, memT.res], w=[pp.res])
                    k.op("act", lambda e, pp=pp, oc=oc: e.activation(out=kmT[:, oc, :], in_=pp[:, 0:256], func=AF.Copy),
                         r=[pp.res], w=[kmT.res])
                for t in range(2):
                    for half in range(2):
                        pp = pj[pjc % 2]
                        pjc += 1
                        mm_group(pp[:], [(memT[:, kc, t * 128:(t + 1) * 128], w_kv[:, kc, 1024 + half * 512:1024 + (half + 1) * 512])
                                         for kc in range(8)], r=[w_kv.res, memT.res], w=[pp.res])
                        k.op("act", lambda e, pp=pp, t=t, half=half: e.activation(out=vmem[:, t, half * 512:(half + 1) * 512], in_=pp[:], func=AF.Copy),
                             r=[pp.res], w=[vmem.res])
                k.barrier(release=[w_kv.res] + [t_.res for t_ in memt])
            hT = [sb(p2, "hT2%d" % i, [128, 8, B2], BF16) for i in range(2)]
            qT = sb(p2, "qT2", [128, 8, B2], BF16)
            P2 = [sb(p2, "P2%d" % i, [128, 4, 256], BF16) for i in range(2)]
            P2n = [sb(p2, "P2n%d" % i, [128, 4, 256], BF16) for i in range(2)]
            PT2 = sb(p2, "PT2", [128, 4, 2, B2], BF16)
            oT = sb(p2, "oT2", [128, 8, B2], BF16)
            st2 = [dict(mx=sb(p2, "c_mx%d" % i, [128, 4], F32), rs=sb(p2, "c_rs%d" % i, [128, 4], F32),
                        ri=sb(p2, "c_ri%d" % i, [128, 4], F32)) for i in range(2)]
            set_gain(SM_CROSS)
            tc2 = 0
            for b in range(NT // B2):
                h = hT[b % 2]
                for t in range(4):
                    norm_T(x_res[:, b * 4 + t, :], x_res.rs[b * 4 + t], pT[0], h[:, :, t * 128:(t + 1) * 128], h.res)
                for oc in range(8):
                    pp = pj[pjc % 2]
                    pjc += 1
                    mm_group(pp[:], [(w_q[:, kc, oc * 128:(oc + 1) * 128], h[:, kc, :]) for kc in range(8)],
                             r=[w_q.res, h.res], w=[pp.res])
                    k.op("act", lambda e, pp=pp, oc=oc: e.activation(out=qT[:, oc, :], in_=pp[:], func=AF.Copy), r=[pp.res], w=[qT.res])
                def c_S1(t, i2):
                    pS_ = pS2[i2]
                    for hh_ in range(4):
                        mm_group(pS_[:, hh_, :], [(qT[:, 2 * hh_ + j, t * 128:(t + 1) * 128], kmT[:, 2 * hh_ + j, :]) for j in range(2)],
                                 r=[qT.res, kmT.res], w=[pS_.res])

                def c_S2(t, i2):
                    pS_, P_, Pn_, s_ = pS2[i2], P2[i2], P2n[i2], st2[i2]
                    k.op("dve", lambda e: e.tensor_reduce(out=s_["mx"][:], in_=pS_[:], axis=AX.X, op=ALU.max, negate=True),
                         r=[pS_.res], w=[s_["mx"].res])
                    k.op("dve", lambda e: e.tensor_scalar(out=s_["mx"][:], in0=s_["mx"][:], scalar1=1.0 / 16, scalar2=None, op0=ALU.mult),
                         r=[s_["mx"].res], w=[s_["mx"].res])
                    for hh_ in range(4):
                        k.op("act", lambda e, hh_=hh_: e.activation(
                            out=P_[:, hh_, :], in_=pS_[:, hh_, :], func=AF.Exp, bias=s_["mx"][:, hh_:hh_ + 1], scale=1.0 / 16,
                            accum_out=s_["rs"][:, hh_:hh_ + 1]), r=[pS_.res, s_["mx"].res], w=[P_.res, s_["rs"].res])
                    k.op("dve", lambda e: e.reciprocal(out=s_["ri"][:], in_=s_["rs"][:]), r=[s_["rs"].res], w=[s_["ri"].res])
                    k.op("dve", lambda e: e.tensor_tensor(
                        out=Pn_[:], in0=P_[:], in1=s_["ri"][:, :].unsqueeze(2).to_broadcast([128, 4, 256]), op=ALU.mult),
                        r=[P_.res, s_["ri"].res], w=[Pn_.res])

                def c_S3(t, i2):
                    Pn_ = P2n[i2]
                    for hh_ in range(4):
                        for mc in range(2):
                            k.op("pe", lambda e, hh_=hh_, mc=mc: e.transpose(
                                out=pPT2[:, hh_ * 2 + mc, :], in_=Pn_[:, hh_, mc * 128:(mc + 1) * 128], identity=ident_b[:]),
                                r=[Pn_.res, ident_b.res], w=[pPT2.res])
                    k.op("act", lambda e: e.activation(out=PT2[:, :, :, t * 128:(t + 1) * 128],
                                                       in_=pPT2[:].rearrange("p (h m) t -> p h m t", h=4), func=AF.Copy),
                         r=[pPT2.res], w=[PT2.res])

                c_S1(0, tc2 % 2)
                for t in range(4):
                    if t + 1 < 4:
                        c_S1(t + 1, (tc2 + 1) % 2)
                    c_S2(t, tc2 % 2)
                    c_S3(t, tc2 % 2)
                    tc2 += 1
                for oc in range(8):
                    pp = pj[pjc % 2]
                    pjc += 1
                    mm_group(pp[:], [(vmem[:, mc, oc * 128:(oc + 1) * 128], PT2[:, oc // 2, mc, :]) for mc in range(2)],
                             r=[vmem.res, PT2.res], w=[pp.res])
                    k.op("act", lambda e, pp=pp, oc=oc: e.activation(out=oT[:, oc, :], in_=pp[:], func=AF.Copy), r=[pp.res], w=[oT.res])
                for t in range(4):
                    tile = b * 4 + t
                    for half in range(2):
                        pp = pj[pjc % 2]
                        pjc += 1
                        mm_group(pp[:], [(oT[:, c, t * 128:(t + 1) * 128], w_o[:, c, half * 512:(half + 1) * 512]) for c in range(8)],
                                 r=[oT.res, w_o.res], w=[pp.res])
                        k.op("dve", lambda e, pp=pp, tile=tile, half=half: e.tensor_tensor(
                            out=x_res[:, tile, half * 512:(half + 1) * 512], in0=pp[:], in1=x_res[:, tile, half * 512:(half + 1) * 512],
                            op=ALU.add), r=[pp.res, x_res.rs[tile]], w=[x_res.rs[tile]])
            k.barrier(release=[w_q.res, w_o.res])

        if debug:
            for tile in range(NTILE):
                k.dma("sp", dbg["dbg2"][tile * 128:(tile + 1) * 128, :], x_res[:, tile, :], r=[x_res.rs[tile]], w=[], sres=x_res.rs[tile])

        with ExitStack() as p3:
            p3s = p3.enter_context(ExitStack())
            slotI = sb(p3s, "slotI", [128, NT], F32)
            slotJ = sb(p3s, "slotJ", [128, NT], F32)
            slotG = sb(p3s, "slotG", [128, NT], F32)
            set_gain(SM_FFN)
            with ExitStack() as pa:
                B3 = 256
                w_qry = sb(pa, "w_qry", [128, 8, 2048], BF16)
                load_w_bf16(w_qry, w_qry_d[:, :], 2048)
                skb = sb(pa, "skb", [128, 16, 128], BF16)
                k.dma("pool", skb[:], skT_d.rearrange("p (o n) -> p o n", o=16), r=[], w=[skb.res], sres=skb.res)
                hT = [sb(pa, "hT3%d" % i, [128, 8, B3], BF16) for i in range(2)]
                qpT = sb(pa, "qpT", [128, 16, B3], BF16)
                sc = sb(pa, "sc", [128, 16, 128], F32, nres=4)
                sc2 = sb(pa, "sc2", [128, 16, 128], F32, nres=16)
                tv = sb(pa, "tv", [128, 16, 16], F32, nres=16)
                tiu = sb(pa, "tiu", [128, 16, 16], U32, nres=16)
                tif = sb(pa, "tif", [128, 16, 16], F32)
                cand = sb(pa, "cand", [128, 8, 256], F32)
                cand2 = sb(pa, "cand2", [128, 8, 256], F32, nres=8)
                ts = sb(pa, "ts", [128, 8, 16], F32, nres=8)
                posu = sb(pa, "posu", [128, 8, 16], U32, nres=8)
                au = sb(pa, "au", [128, 8, 16], U32)
                bu = sb(pa, "bu", [128, 8, 16], U32)
                af = sb(pa, "af", [128, 8, 16], F32)
                bf = sb(pa, "bf", [128, 8, 16], F32)
                eq = sb(pa, "eq", [128, 8, 16, 16], F32)
                isel = sb(pa, "isel", [128, 8, 16], F32)
                jsel = sb(pa, "jsel", [128, 8, 16], F32)
                gsel = sb(pa, "gsel", [128, 8, 16], F32)
                sm = sb(pa, "sm", [128, 8], F32)
                pj = [ps(pa, "pj3%d" % i, [128, 512], F32) for i in range(2)]
                pT = [ps(pa, "pT3%d" % i, [128, 8, 128], BF16) for i in range(2)]
                psc = [ps(pa, "psc%d" % i, [128, 4, 128], F32) for i in range(2)]
                pTs = ps(pa, "pTs", [128, 3, 128], F32)
                pjc = 0
                scc = 0
                iota16 = iota_f[:, 0:16]
                for b in range(NT // B3):
                    h = hT[b % 2]
                    norm_pairs([(x_res[:, b * 2 + t, :], x_res.rs[b * 2 + t], pT[t % 2], h[:, :, t * 128:(t + 1) * 128], h.res) for t in range(2)])
                    for oc in range(16):
                        pp = pj[pjc % 2]
                        pjc += 1
                        mm_group(pp[:, 0:B3], [(w_qry[:, kc, oc * 128:(oc + 1) * 128], h[:, kc, :]) for kc in range(8)],
                                 r=[w_qry.res, h.res], w=[pp.res])
                        k.op("act", lambda e, pp=pp, oc=oc: e.activation(out=qpT[:, oc, :], in_=pp[:, 0:B3], func=AF.Copy),
                             r=[pp.res], w=[qpT.res])
                    for t in range(2):
                        tile = b * 2 + t
                        for g4 in range(4):
                            pq = psc[scc % 2]
                            scc += 1
                            for j in range(4):
                                oc = g4 * 4 + j
                                k.op("pe", lambda e, pq=pq, j=j, oc=oc, t=t: e.matmul(
                                    pq[:, j, :], lhsT=qpT[:, oc, t * 128:(t + 1) * 128], rhs=skb[:, oc, :], start=True, stop=True),
                                    r=[qpT.res, skb.res], w=[pq.res])
                            k.op("act", lambda e, pq=pq, g4=g4: e.activation(out=sc[:, g4 * 4:(g4 + 1) * 4, :], in_=pq[:], func=AF.Copy),
                                 r=[pq.res], w=[sc.rs[g4]])
                        for oc in range(16):
                            k.op("dve", lambda e, oc=oc: e.max(out=tv[:, oc, 0:8], in_=sc[:, oc, :]), r=[sc.rs[oc // 4]], w=[tv.rs[oc]])
                        for oc in range(16):
                            k.op("dve", lambda e, oc=oc: e.max_index(out=tiu[:, oc, 0:8], in_max=tv[:, oc, 0:8], in_values=sc[:, oc, :]),
                                 r=[sc.rs[oc // 4], tv.rs[oc]], w=[tiu.rs[oc]])
                        for oc in range(16):
                            k.op("dve", lambda e, oc=oc: e.match_replace(out=sc2[:, oc, :], in_to_replace=tv[:, oc, 0:8], in_values=sc[:, oc, :],
                                                                        imm_value=NEG), r=[sc.rs[oc // 4], tv.rs[oc]], w=[sc2.rs[oc]])
                        for oc in range(16):
                            k.op("dve", lambda e, oc=oc: e.max(out=tv[:, oc, 8:16], in_=sc2[:, oc, :]), r=[sc2.rs[oc]], w=[tv.rs[oc]])
                        for oc in range(16):
                            k.op("dve", lambda e, oc=oc: e.max_index(out=tiu[:, oc, 8:16], in_max=tv[:, oc, 8:16], in_values=sc2[:, oc, :]),
                                 r=[sc2.rs[oc], tv.rs[oc]], w=[tiu.rs[oc]])
                        k.op("dve", lambda e: e.tensor_copy(out=tif[:], in_=tiu[:]), r=tiu.rs, w=[tif.res])
                        tv4 = tv[:].rearrange("p (h two) a -> p h two a", two=2)
                        tif4 = tif[:].rearrange("p (h two) a -> p h two a", two=2)
                        k.op("dve", lambda e, tv4=tv4: e.tensor_tensor(
                            out=cand[:].rearrange("p h (a b) -> p h a b", a=16),
                            in0=tv4[:, :, 0, :].unsqueeze(3).to_broadcast([128, 8, 16, 16]),
                            in1=tv4[:, :, 1, :].unsqueeze(2).to_broadcast([128, 8, 16, 16]), op=ALU.add),
                            r=tv.rs, w=[cand.res])
                        for hd in range(8):
                            k.op("dve", lambda e, hd=hd: e.max(out=ts[:, hd, 0:8], in_=cand[:, hd, :]), r=[cand.res], w=[ts.rs[hd]])
                        for hd in range(8):
                            k.op("dve", lambda e, hd=hd: e.max_index(out=posu[:, hd, 0:8], in_max=ts[:, hd, 0:8], in_values=cand[:, hd, :]),
                                 r=[cand.res, ts.rs[hd]], w=[posu.rs[hd]])
                        for hd in range(8):
                            k.op("dve", lambda e, hd=hd: e.match_replace(out=cand2[:, hd, :], in_to_replace=ts[:, hd, 0:8], in_values=cand[:, hd, :],
                                                                        imm_value=NEG), r=[cand.res, ts.rs[hd]], w=[cand2.rs[hd]])
                        for hd in range(8):
                            k.op("dve", lambda e, hd=hd: e.max(out=ts[:, hd, 8:16], in_=cand2[:, hd, :]), r=[cand2.rs[hd]], w=[ts.rs[hd]])
                        for hd in range(8):
                            k.op("dve", lambda e, hd=hd: e.max_index(out=posu[:, hd, 8:16], in_max=ts[:, hd, 8:16], in_values=cand2[:, hd, :]),
                                 r=[cand2.rs[hd], ts.rs[hd]], w=[posu.rs[hd]])
                        k.op("dve", lambda e: e.tensor_scalar(out=au[:], in0=posu[:], scalar1=4, scalar2=None, op0=ALU.logical_shift_right),
                             r=posu.rs, w=[au.res])
                        k.op("dve", lambda e: e.tensor_scalar(out=bu[:], in0=posu[:], scalar1=15, scalar2=None, op0=ALU.bitwise_and),
                             r=posu.rs, w=[bu.res])
                        k.op("dve", lambda e: e.tensor_copy(out=af[:], in_=au[:]), r=[au.res], w=[af.res])
                        k.op("dve", lambda e: e.tensor_copy(out=bf[:], in_=bu[:]), r=[bu.res], w=[bf.res])
                        for (sel, rk, which) in ((isel, af, 0), (jsel, bf, 1)):
                            k.op("dve", lambda e, rk=rk: e.tensor_tensor(
                                out=eq[:], in0=rk[:].unsqueeze(3).to_broadcast([128, 8, 16, 16]),
                                in1=iota16.unsqueeze(1).unsqueeze(1).to_broadcast([128, 8, 16, 16]), op=ALU.is_equal),
                                r=[rk.res, iota_f.res], w=[eq.res])
                            k.op("dve", lambda e, which=which, tif4=tif4: e.tensor_tensor(
                                out=eq[:], in0=eq[:], in1=tif4[:, :, which, :].unsqueeze(2).to_broadcast([128, 8, 16, 16]), op=ALU.mult),
                                r=[eq.res, tif.res], w=[eq.res])
                            k.op("dve", lambda e, sel=sel: e.tensor_reduce(out=sel[:], in_=eq[:], axis=AX.X, op=ALU.add),
                                 r=[eq.res], w=[sel.res])
                        k.op("dve", lambda e: e.tensor_tensor(out=gsel[:], in0=ts[:], in1=ts[:, :, 0:1].to_broadcast([128, 8, 16]), op=ALU.subtract),
                             r=ts.rs, w=[gsel.res])
                        k.op("act", lambda e: e.activation(out=gsel[:], in_=gsel[:], func=AF.Exp), r=[gsel.res], w=[gsel.res])
                        k.op("dve", lambda e: e.tensor_reduce(out=sm[:], in_=gsel[:], axis=AX.X, op=ALU.add), r=[gsel.res], w=[sm.res])
                        k.op("dve", lambda e: e.reciprocal(out=sm[:], in_=sm[:]), r=[sm.res], w=[sm.res])
                        k.op("dve", lambda e: e.tensor_tensor(out=gsel[:], in0=gsel[:], in1=sm[:, :].unsqueeze(2).to_broadcast([128, 8, 16]), op=ALU.mult),
                             r=[gsel.res, sm.res], w=[gsel.res])
                        for i3, (src, dst) in enumerate(((isel, slotI), (jsel, slotJ), (gsel, slotG))):
                            k.op("pe", lambda e, i3=i3, src=src: e.transpose(out=pTs[:, i3, :], in_=src[:].rearrange("p h r -> p (h r)"),
                                                                            identity=ident_f[:]),
                                 r=[src.res, ident_f.res], w=[pTs.res])
                        for i3, (src, dst) in enumerate(((isel, slotI), (jsel, slotJ), (gsel, slotG))):
                            k.op("act", lambda e, i3=i3, dst=dst, tile=tile: e.activation(out=dst[:, tile * 128:(tile + 1) * 128], in_=pTs[:, i3, :], func=AF.Copy),
                                 r=[pTs.res], w=[dst.res])
                k.barrier(release=[w_qry.res, skb.res])

            with ExitStack() as pb:
                TCH = 8
                ohj = [sb(pb, "ohj%d" % i, [128, TCH, 128], BF16) for i in range(4)]
                eqi = [sb(pb, "eqi%d" % i, [128, TCH, 128], BF16) for i in range(4)]
                rig = [sb(pb, "rig%d" % i, [128, TCH, 128], BF16, nres=TCH) for i in range(4)]
                Wst = [sb(pb, "Wst%d" % i, [128, 128, 128], BF16, nres=32) for i in range(2)]
                pW = [ps(pb, "pW%d" % i, [128, 4, 128], F32) for i in range(4)]
                wc = 0
                chc = 0
                iota_b3 = iota_f[:, :].unsqueeze(1).to_broadcast([128, TCH, 128])
                for tile in range(NTILE):
                    W_ = Wst[tile % 2]
                    for ch in range(128 // TCH):
                        oj, ei, rg = ohj[chc % 4], eqi[chc % 4], rig[chc % 4]
                        chc += 1
                        tok0 = tile * 128 + ch * TCH
                        k.op("dve", lambda e, oj=oj, tok0=tok0: e.tensor_tensor(
                            out=oj[:], in0=iota_b3, in1=slotJ[:, tok0:tok0 + TCH].unsqueeze(2).to_broadcast([128, TCH, 128]), op=ALU.is_equal),
                            r=[iota_f.res, slotJ.res], w=[oj.res])
                        k.op("dve", lambda e, ei=ei, tok0=tok0: e.tensor_tensor(
                            out=ei[:], in0=iota_b3, in1=slotI[:, tok0:tok0 + TCH].unsqueeze(2).to_broadcast([128, TCH, 128]), op=ALU.is_equal),
                            r=[iota_f.res, slotI.res], w=[ei.res])
                        if chc % 9 < 4:
                            for tt_ in range(TCH):
                                k.op("act", lambda e, ei=ei, rg=rg, tt_=tt_, tok0=tok0: e.activation(
                                    out=rg[:, tt_, :], in_=ei[:, tt_, :], func=AF.Copy, scale=slotG[:, tok0 + tt_:tok0 + tt_ + 1]),
                                    r=[ei.res, slotG.res], w=[rg.rs[tt_]])
                        else:
                            k.op("dve", lambda e, ei=ei, rg=rg, tok0=tok0: e.tensor_tensor(
                                out=rg[:], in0=ei[:], in1=slotG[:, tok0:tok0 + TCH].unsqueeze(2).to_broadcast([128, TCH, 128]), op=ALU.mult),
                                r=[ei.res, slotG.res], w=rg.rs)
                        for q4 in range(TCH // 4):
                            pw = pW[wc % 4]
                            for j in range(4):
                                tt_ = q4 * 4 + j
                                k.op("pe", lambda e, pw=pw, j=j, oj=oj, rg=rg, tt_=tt_: e.matmul(
                                    pw[:, j, :], lhsT=oj[:, tt_, :], rhs=rg[:, tt_, :], start=True, stop=True),
                                    r=[oj.res, rg.rs[tt_]], w=[pw.res])
                            t0 = ch * TCH + q4 * 4
                            if True:
                                k.op("act", lambda e, pw=pw, W_=W_, t0=t0: e.activation(
                                    out=W_[:, :, t0:t0 + 4], in_=pw[:].rearrange("p t i -> p i t"), func=AF.Copy),
                                    r=[pw.res], w=[W_.rs[t0 // 4]])
                            else:
                                k.op("dve", lambda e, pw=pw, W_=W_, t0=t0: e.tensor_copy(
                                    out=W_[:, :, t0:t0 + 4], in_=pw[:].rearrange("p t i -> p i t")),
                                    r=[pw.res], w=[W_.rs[t0 // 4]])
                            wc += 1
                    k.dma("sp", wd_d[tile], W_[:].rearrange("p i t -> p (i t)"), r=W_.rs, w=[], sres=W_.res)
                k.barrier(release=[w_.res for w_ in Wst])
            p3s.close()

            with ExitStack() as pc:
                T3 = 256
                GS = 8
                NG = 128 // GS
                hfT = sb(pc, "hfT", [128, 8, NT], BF16)
                ut = [sb(pc, "ut%d" % i, [128, GS, 8, 128], BF16) for i in range(2)]
                vt = [sb(pc, "vt%d" % i, [128, GS, D], BF16) for i in range(2)]
                uT_v = uT_d.rearrange("(i p) (c e) -> p i c e", p=128, c=8)
                ev_v = ev_d.rearrange("(i e) d -> e i d", e=128)
                for i_ in range(GS):
                    k.dma("pool", ut[0][:, i_, :, :], uT_v[:, i_, :, :], r=[], w=[ut[0].res], sres=ut[0].res)
                    k.dma("pool", vt[0][:, i_, :], ev_v[:, i_, :], r=[], w=[vt[0].res], sres=vt[0].res, max_dma_last_dim=4096)
                with ExitStack() as pcn:
                    pTn = [ps(pcn, "pT4%d" % i, [128, 8, 128], BF16) for i in range(2)]
                    norm_pairs([(x_res[:, tile, :], x_res.rs[tile], pTn[tile % 2], hfT[:, :, tile * 128:(tile + 1) * 128], hfT.res)
                                for tile in range(NTILE)])
                    k.barrier()
                pA = [ps(pc, "pA%d" % i, [128, 512], F32) for i in range(3)]
                pO = [ps(pc, "pO4%d" % i, [128, 512], F32) for i in range(4)]
                wsl = [sb(pc, "wsl%d" % i, [128, 2, GS, 128], BF16) for i in range(2)]
                ge = [sb(pc, "ge%d" % i, [128, T3], F32) for i in range(3)]
                gw = [sb(pc, "gw%d" % i, [128, T3], BF16) for i in range(3)]
                stage = [sb(pc, "stage%d" % i, [128, 2, D], F32, nres=4) for i in range(2)]
                wd_v = wd_d.rearrange("t j (i x) -> j t i x", i=128)
                items = [(g, tb, i) for g in range(NG) for tb in range(NT // T3) for i in range(GS)]
                state = {}

                def emitA(n):
                    g, tb, i = items[n]
                    u_, v_ = ut[g % 2], vt[g % 2]
                    if tb == 0 and i == 0 and g > 0:
                        for i_ in range(GS):
                            k.dma("pool", u_[:, i_, :, :], uT_v[:, g * GS + i_, :, :], r=[], w=[u_.res], sres=u_.res)
                            k.dma("pool", v_[:, i_, :], ev_v[:, g * GS + i_, :], r=[], w=[v_.res], sres=v_.res, max_dma_last_dim=4096)
                    if i == 0:
                        ws_ = wsl[(g * (NT // T3) + tb) % 2]
                        k.dma("sp", ws_[:], wd_v[:, 2 * tb:2 * tb + 2, g * GS:(g + 1) * GS, :], r=[], w=[ws_.res], sres=ws_.res)
                    ws_ = wsl[(g * (NT // T3) + tb) % 2]
                    pa_ = pA[n % 3]
                    ge_, gw_ = ge[n % 3], gw[n % 3]
                    mm_group(pa_[:, 0:T3], [(u_[:, i, c, :], hfT[:, c, tb * T3:(tb + 1) * T3]) for c in range(8)],
                             r=[u_.res, hfT.res], w=[pa_.res])
                    k.op("act", lambda e: e.activation(out=ge_[:], in_=pa_[:, 0:T3], func=AF.Gelu_apprx_tanh),
                         r=[pa_.res], w=[ge_.res])
                    k.op("dve", lambda e: e.tensor_tensor(
                        out=gw_[:].rearrange("p (a t) -> p a t", a=2), in0=ge_[:].rearrange("p (a t) -> p a t", a=2),
                        in1=ws_[:, :, i, :], op=ALU.mult), r=[ge_.res, ws_.res], w=[gw_.res])

                def emitV(n):
                    g, tb, i = items[n]
                    v_ = vt[g % 2]
                    gw_ = gw[n % 3]
                    for t in range(2):
                        for half in range(2):
                            po = pO[t * 2 + half]
                            k.op("pe", lambda e, po=po, t=t, half=half: e.matmul(
                                po[:], lhsT=gw_[:, t * 128:(t + 1) * 128], rhs=v_[:, i, half * 512:(half + 1) * 512],
                                start=(i == 0), stop=(i == GS - 1)), r=[gw_.res, v_.res], w=[po.res])
                    if i == GS - 1:
                        stg = stage[(g * (NT // T3) + tb) % 2]
                        for t in range(2):
                            for half in range(2):
                                po = pO[t * 2 + half]
                                k.op("act", lambda e, po=po, t=t, half=half: e.activation(
                                    out=stg[:, t, half * 512:(half + 1) * 512], in_=po[:], func=AF.Copy),
                                    r=[po.res], w=[stg.rs[t * 2 + half]])
                        for t in range(2):
                            tile = tb * 2 + t
                            for half in range(2):
                                k.op("dve", lambda e, t=t, tile=tile, half=half: e.tensor_tensor(
                                    out=x_res[:, tile, half * 512:(half + 1) * 512], in0=stg[:, t, half * 512:(half + 1) * 512],
                                    in1=x_res[:, tile, half * 512:(half + 1) * 512],
                                    op=ALU.add), r=[stg.rs[t * 2 + half], x_res.rs[tile]], w=[x_res.rs[tile]])

                emitA(0)
                emitA(1)
                for n in range(len(items)):
                    if n + 2 < len(items):
                        emitA(n + 2)
                    emitV(n)
                k.barrier(release=[t_.res for t_ in ut + vt + wsl])

        with ExitStack() as p4:
            gfin = sb(p4, "gfin", [128, D], F32)
            k.dma("sp", gfin[:], gfin_d[:, :], r=[], w=[gfin.res], sres=gfin.res)
            ot = [sb(p4, "ot%d" % i, [128, D], F32) for i in range(2)]
            def fin_g(tile):
                n = nrm[tile % 2]
                o_ = ot[tile % 2]
                xa = x_res[:, tile, :]
                xr = x_res.rs[tile]
                k.op("dve", lambda e: e.scalar_tensor_tensor(out=n["junk"][:], in0=xa, scalar=1.0, in1=xa,
                                                             op0=ALU.mult, op1=ALU.mult, accum_out=n["ss"][:]),
                     r=[xr], w=[n["junk"].res, n["ss"].res])
                yield
                k.op("act", lambda e: e.activation(out=n["sd"][:], in_=n["ss"][:], func=AF.Ln, bias=cst[:, 0:1], scale=1.0 / D),
                     r=[n["ss"].res, cst.res], w=[n["sd"].res])
                k.op("act", lambda e: e.activation(out=n["rstd"][:], in_=n["sd"][:], func=AF.Exp, scale=-0.5),
                     r=[n["sd"].res], w=[n["rstd"].res])
                yield
                k.op("dve", lambda e: e.scalar_tensor_tensor(out=o_[:], in0=xa, scalar=n["rstd"][:, 0:1], in1=gfin[:],
                                                             op0=ALU.mult, op1=ALU.mult),
                     r=[xr, n["rstd"].res, gfin.res], w=[o_.res])
                k.dma("sp", y_d[tile * 128:(tile + 1) * 128, :], o_[:], r=[o_.res], w=[], sres=o_.res)

            for t2 in range(0, NTILE, 2):
                for _ in lockstep_g([fin_g(t2), fin_g(t2 + 1)]):
                    pass
            k.barrier()
    return nc, k.n_ins


def prep_shared(inp):
    f = np.float32
    g = lambda a: np.ascontiguousarray(np.asarray(a, dtype=f))
    sm = np.zeros((128, NSM), f)

    def put(col, vec, nch):
        sm[:, col:col + nch] = np.asarray(vec, f).reshape(nch, 128).T

    put(SM_MIX, inp["norm_mix"][0], 8)
    put(SM_CROSS, inp["norm_cross"][0], 8)
    put(SM_MEM, inp["norm_mem"][0], 8)
    put(SM_FFN, inp["norm_ffn"][0], 8)
    put(SM_GA, inp["norm_grp_a"][0], 4)
    put(SM_GB, inp["norm_grp_b"][0], 4)
    put(SM_CB, inp["conv_b"][0], 4)
    put(SM_BA, inp["gate_a_b"][0], 4)
    put(SM_BX, inp["gate_x_b"][0], 4)
    put(SM_LAM, inp["lru_lambda"][0], 4)
    cw = np.asarray(inp["conv_w"][0], f)
    for cc in range(4):
        for j in range(4):
            sm[:, SM_CW + cc * 4 + j] = cw[j, cc * 128:(cc + 1) * 128]
    gbd = np.zeros((128, 8, 128), f)
    for gi, key in enumerate(("gate_a_w", "gate_x_w")):
        w = np.asarray(inp[key][0], f)
        for cc in range(4):
            gbd[0:64, gi * 4 + cc, 0:64] = w[2 * cc]
            gbd[64:128, gi * 4 + cc, 64:128] = w[2 * cc + 1]
    rb = np.asarray(inp["rel_bias"][0], f)
    qi = np.arange(128)[:, None]
    kj = np.arange(640)[None, :]
    idx = np.clip(512 + qi - kj, -128, 128) + 128
    ab = rb[:, idx]
    valid = np.where(qi < 64, kj < 576, kj >= 64)
    ab = np.where(valid[None], ab, f(NEG)).astype(f)
    abias = np.ascontiguousarray(ab.transpose(1, 0, 2)).reshape(128, 8 * 640)
    sk = np.asarray(inp["sub_keys"][0], f)
    skT = np.ascontiguousarray(sk.reshape(16, 128, 128).transpose(2, 0, 1)).reshape(128, 16 * 128)
    u = np.asarray(inp["expert_u"][0], f)
    uT = np.ascontiguousarray(u.reshape(128, 128, 8, 128).transpose(0, 3, 2, 1)).reshape(16384, D)
    shared = {
        "w_in": g(inp["w_in"][0]), "w_out": g(inp["w_out"][0]), "w_q": g(inp["w_q_mem"][0]),
        "w_kv": g(inp["w_kv_mem"][0]), "w_o": g(inp["w_o_mem"][0]), "w_qry": g(inp["w_query"][0]),
        "smalls": sm, "gbd": gbd.reshape(128, 8 * 128), "abias": abias, "skT": skT, "uT": uT,
        "ev": g(inp["expert_v"][0]),
        "gfin": np.ascontiguousarray(np.broadcast_to(np.asarray(inp["norm_final"], f)[None, :], (128, D))),
        "ident": np.eye(128, dtype=f),
        "iota": np.ascontiguousarray(np.broadcast_to(np.arange(128, dtype=f)[None, :], (128, 128))),
    }
    return shared


def make_in_maps(inp, NT):
    x = np.asarray(inp["x"], np.float32)
    mem = np.asarray(inp["mem"], np.float32)
    B, S, _ = x.shape
    per_seq = S // NT
    NPRE = 3 * NT
    shared = prep_shared(inp)
    maps = []
    for c in range(B * per_seq):
        b, q = divmod(c, per_seq)
        xprev = np.zeros((NPRE, D), np.float32)
        if q > 0:
            xprev[NPRE - q * NT:] = x[b, 0:q * NT]
        pflag = np.zeros((128, NPRE // 512), np.float32)
        for j in range(NPRE // 512):
            if j * 512 >= NPRE - q * NT:
                pflag[:, j] = 1.0
        halob = np.full((128, 512), 0.0 if q > 0 else NEG, np.float32)
        m = dict(shared)
        m.update({"xown": np.ascontiguousarray(x[b, q * NT:(q + 1) * NT]), "xprev": xprev, "pflag": pflag,
                  "halob": halob, "mem": np.ascontiguousarray(mem[b])})
        maps.append(m)
    return maps


_CACHE = {}


def kernel(**inputs):
    NT = 2048
    if NT not in _CACHE:
        _CACHE[NT] = build(NT)[0]
    nc = _CACHE[NT]
    maps = make_in_maps(inputs, NT)
    res = run_bass_kernel_spmd(nc, maps, core_ids=list(range(N_CORES)))
    x = np.asarray(inputs["x"])
    B, S, _ = x.shape
    out = np.empty((B, S, D), np.float32)
    per_seq = S // NT
    for c in range(N_CORES):
        b, q = divmod(c, per_seq)
        out[b, q * NT:(q + 1) * NT] = res.results[c]["y"]
    return out
```

```python
import numpy as np
from contextlib import ExitStack
import concourse.bass as bass
import concourse.mybir as mybir
from concourse.bass_utils import run_bass_kernel_spmd

F32 = mybir.dt.float32
BF16 = mybir.dt.bfloat16
U32 = mybir.dt.uint32
AF = mybir.ActivationFunctionType
ALU = mybir.AluOpType
AX = mybir.AxisListType

D = 1024
EPS = 1e-6
NEG = -1e30
N_CORES = 8
SAME_ENG_SYNC = True

SM_MIX, SM_CROSS, SM_MEM, SM_FFN = 0, 8, 16, 24
SM_GA, SM_GB, SM_CB, SM_BA, SM_BX, SM_LAM, SM_CW = 32, 36, 40, 44, 48, 52, 56
NSM = 72


class Res:
    __slots__ = ("name", "w", "r", "ds")

    def __init__(self, name):
        self.name = name
        self.w = None
        self.r = {}
        self.ds = None


class KB:
    def __init__(self, nc, es):
        self.nc = nc
        self.es = es
        self.E = {}
        for nm, e in (("pe", nc.tensor), ("act", nc.scalar), ("dve", nc.vector),
                      ("pool", nc.gpsimd), ("sp", nc.sync)):
            self.E[nm] = dict(e=e, sem=es.enter_context(nc.semaphore("e_" + nm)), cnt=0, waited={}, nm=nm)
        self.free_ds = []
        self.all_ds = []
        self.n_ins = 0

    def _collect(self, r, w):
        deps = {}
        for x in r:
            if x.w is not None:
                s, v = x.w
                if deps.get(s, 0) < v:
                    deps[s] = v
        for x in w:
            if x.w is not None:
                s, v = x.w
                if deps.get(s, 0) < v:
                    deps[s] = v
            for s, v in x.r.items():
                if deps.get(s, 0) < v:
                    deps[s] = v
        return deps

    def _waits(self, E, deps, skip_own):
        for s, v in deps.items():
            if skip_own and s is E["sem"]:
                continue
            if E["waited"].get(s, 0) >= v:
                continue
            E["e"].wait_ge(s, v)
            E["waited"][s] = v

    def op(self, en, fn, r=(), w=()):
        E = self.E[en]
        skip_own = (en == "pe") or (not SAME_ENG_SYNC)
        self._waits(E, self._collect(r, w), skip_own)
        ins = fn(E["e"])
        E["cnt"] += 1
        self.n_ins += 1
        ins.then_inc(E["sem"], 1)
        tag = (E["sem"], E["cnt"])
        for x in w:
            x.w = tag
            x.r = {}
        for x in r:
            if x not in w:
                x.r[E["sem"]] = E["cnt"]
        return ins

    def dma(self, qn, out, in_, r, w, sres, **kw):
        E = self.E[qn]
        self._waits(E, self._collect(r, w), False)
        if sres.ds is None:
            if qn != "pool" and self.free_ds:
                sres.ds = self.free_ds.pop()
            else:
                sres.ds = [self.es.enter_context(self.nc.semaphore("d%d" % len(self.all_ds))), 0, qn]
                self.all_ds.append(sres.ds)
        ins = E["e"].dma_start(out=out, in_=in_, **kw)
        self.n_ins += 1
        sres.ds[1] += 1
        ins.then_inc(sres.ds[0], 16)
        val = sres.ds[1] * 16
        for x in w:
            x.w = (sres.ds[0], val)
            x.r = {}
        for x in r:
            if x not in w:
                x.r[sres.ds[0]] = val

    def barrier(self, release=()):
        deps = {}
        for nm in ("pe", "act", "dve", "pool"):
            E = self.E[nm]
            if E["cnt"] > 0:
                deps[E["sem"]] = E["cnt"]
        for ds in self.all_ds:
            if ds[1] > 0:
                deps[ds[0]] = ds[1] * 16
        for nm in ("pe", "act", "dve", "pool", "sp"):
            self._waits(self.E[nm], deps, True)
        for x in release:
            if x.ds is not None:
                if x.ds[2] != "pool":
                    self.free_ds.append(x.ds)
                x.ds = None


class Tn:
    def __init__(self, h, name, nres=1):
        self.h = h
        self.res = Res(name)
        self.rs = [Res("%s_%d" % (name, i)) for i in range(nres)] if nres > 1 else [self.res]

    def __getitem__(self, k):
        return self.h[k]


def build(NT=2048, debug=False):
    NPRE = 3 * NT
    NTILE = NT // 128
    nc = bass.Bass("TRN2", target_bir_lowering=False)
    dt_in = lambda n, s, d=F32: nc.dram_tensor(n, list(s), d, kind="ExternalInput").ap()
    xown = dt_in("xown", [NT, D])
    xprev = dt_in("xprev", [NPRE, D])
    pflag_d = dt_in("pflag", [128, NPRE // 512])
    halob_d = dt_in("halob", [128, 512])
    mem_d = dt_in("mem", [256, D])
    w_in_d = dt_in("w_in", [D, 2560])
    w_out_d = dt_in("w_out", [D, D])
    w_q_d = dt_in("w_q", [D, D])
    w_kv_d = dt_in("w_kv", [D, 2048])
    w_o_d = dt_in("w_o", [D, D])
    w_qry_d = dt_in("w_qry", [D, 2048])
    smalls_d = dt_in("smalls", [128, NSM])
    gbd_d = dt_in("gbd", [128, 8 * 128])
    abias_d = dt_in("abias", [128, 8 * 640])
    skT_d = dt_in("skT", [128, 16 * 128])
    uT_d = dt_in("uT", [16384, D])
    ev_d = dt_in("ev", [16384, D])
    gfin_d = dt_in("gfin", [128, D])
    ident_d = dt_in("ident", [128, 128])
    iota_d = dt_in("iota", [128, 128])
    y_d = nc.dram_tensor("y", [NT, D], F32, kind="ExternalOutput").ap()
    wd_d = nc.dram_tensor("wd_scratch", [NTILE, 128, 16384], BF16, kind="Internal").ap()
    dbg = {}
    if debug:
        for nm in ("dbg1", "dbg2"):
            dbg[nm] = nc.dram_tensor(nm, [NT, D], F32, kind="ExternalOutput").ap()

    with ExitStack() as es:
        k = KB(nc, es)

        def sb(ctx, name, shape, dt, nres=1):
            return Tn(ctx.enter_context(nc.sbuf_tensor("sb_" + name, list(shape), dt)), name, nres)

        def ps(ctx, name, shape, dt):
            return Tn(ctx.enter_context(nc.psum_tensor("ps_" + name, list(shape), dt)), name)

        x_res = sb(es, "x_res", [128, NTILE, D], F32, nres=NTILE)
        ident_f = sb(es, "ident_f", [128, 128], F32)
        ident_b = sb(es, "ident_b", [128, 128], BF16)
        iota_f = sb(es, "iota_f", [128, 128], F32)
        ones_b = sb(es, "ones_b", [128, 128], BF16)
        smalls = sb(es, "smalls", [128, NSM], F32)
        cst = sb(es, "cst", [128, 4], F32)
        gB = sb(es, "gB", [128, 8, 128], F32)
        nrm = [dict(ss=sb(es, "n_ss%d" % i, [128, 1], F32), sd=sb(es, "n_sd%d" % i, [128, 1], F32),
                    rstd=sb(es, "n_rstd%d" % i, [128, 1], F32), xs=sb(es, "n_xs%d" % i, [128, D], BF16),
                    junk=sb(es, "n_junk%d" % i, [128, D], BF16)) for i in range(2)]
        nrm_i = [0]

        k.dma("sp", ident_f[:], ident_d[:, :], r=[], w=[ident_f.res], sres=ident_f.res)
        k.dma("sp", iota_f[:], iota_d[:, :], r=[], w=[iota_f.res], sres=iota_f.res)
        k.dma("sp", smalls[:], smalls_d[:, :], r=[], w=[smalls.res], sres=smalls.res)
        k.op("dve", lambda e: e.tensor_copy(out=ident_b[:], in_=ident_f[:]), r=[ident_f.res], w=[ident_b.res])
        k.op("pool", lambda e: e.memset(ones_b[:], 1.0), w=[ones_b.res])
        k.op("pool", lambda e: e.memset(cst[:, 0:1], EPS), w=[cst.res])
        k.op("pool", lambda e: e.memset(cst[:, 1:2], 1.0), w=[cst.res])
        k.op("pool", lambda e: e.memset(cst[:, 2:3], 0.0), w=[cst.res])

        def set_gain(col):
            k.op("dve", lambda e: e.tensor_copy(
                out=gB[:], in_=smalls[:, col:col + 8].unsqueeze(2).to_broadcast([128, 8, 128])),
                r=[smalls.res], w=[gB.res])

        def norm_T_g(x_ap, x_r, pT, hT_ap, hT_r):
            n = nrm[nrm_i[0] % 2]
            nrm_i[0] += 1
            k.op("dve", lambda e: e.scalar_tensor_tensor(out=n["junk"][:], in0=x_ap, scalar=1.0, in1=x_ap,
                                                         op0=ALU.mult, op1=ALU.mult, accum_out=n["ss"][:]),
                 r=[x_r], w=[n["junk"].res, n["ss"].res])
            yield
            k.op("act", lambda e: e.activation(out=n["sd"][:], in_=n["ss"][:], func=AF.Ln,
                                               bias=cst[:, 0:1], scale=1.0 / D),
                 r=[n["ss"].res, cst.res], w=[n["sd"].res])
            k.op("act", lambda e: e.activation(out=n["rstd"][:], in_=n["sd"][:], func=AF.Exp, scale=-0.5),
                 r=[n["sd"].res], w=[n["rstd"].res])
            yield
            k.op("dve", lambda e: e.tensor_scalar(out=n["xs"][:], in0=x_ap, scalar1=n["rstd"][:, 0:1], scalar2=None,
                                                  op0=ALU.mult), r=[x_r, n["rstd"].res], w=[n["xs"].res])
            for c in range(8):
                k.op("pe", lambda e, c=c: e.transpose(out=pT[:, c, :], in_=n["xs"][:, c * 128:(c + 1) * 128],
                                                      identity=ident_b[:]),
                     r=[n["xs"].res, ident_b.res], w=[pT.res])
            yield
            k.op("dve", lambda e: e.tensor_tensor(out=hT_ap, in0=pT[:], in1=gB[:], op=ALU.mult),
                 r=[pT.res, gB.res], w=[hT_r])

        def norm_T(*a):
            for _ in norm_T_g(*a):
                pass

        def lockstep_g(gens):
            gens = list(gens)
            while gens:
                for g_ in list(gens):
                    try:
                        next(g_)
                    except StopIteration:
                        gens.remove(g_)
                yield

        def norm_pairs(args_list):
            for i_ in range(0, len(args_list), 2):
                for _ in lockstep_g([norm_T_g(*a) for a in args_list[i_:i_ + 2]]):
                    pass

        def mm_group(out_ap, pairs, r, w):
            n = len(pairs)
            for i, (l, rh) in enumerate(pairs):
                k.op("pe", lambda e, l=l, rh=rh, i=i: e.matmul(out_ap, lhsT=l, rhs=rh, start=(i == 0), stop=(i == n - 1)),
                     r=r, w=w)

        def load_w_bf16(dst, src_ap, ncols):
            for c in range(8):
                k.dma("pool", dst[:, c, :], src_ap[c * 128:(c + 1) * 128, :], r=[], w=[dst.res], sres=dst.res,
                      max_dma_last_dim=4096)

        with ExitStack() as p1:
            with ExitStack() as pa:
                BA = 512
                w_inA = sb(pa, "w_inA", [128, 8, 1024], BF16)
                load_w_bf16(w_inA, w_in_d[:, 0:1024], 1024)
                w_outA = sb(pa, "w_outA", [128, 4, D], BF16)
                for c in range(4):
                    k.dma("pool", w_outA[:, c, :], w_out_d[c * 128:(c + 1) * 128, :], r=[], w=[w_outA.res], sres=w_outA.res,
                          max_dma_last_dim=4096)
                yan = sb(pa, "yan", [128, 4, 512], BF16)
                gbd_f = sb(pa, "gbd_f", [128, 8, 128], F32)
                gbd = sb(pa, "gbd", [128, 8, 128], BF16)
                k.dma("sp", gbd_f[:], gbd_d.rearrange("p (c j) -> p c j", c=8), r=[], w=[gbd_f.res], sres=gbd_f.res)
                k.op("dve", lambda e: e.tensor_copy(out=gbd[:], in_=gbd_f[:]), r=[gbd_f.res], w=[gbd.res])
                pflag = sb(pa, "pflag", [128, NPRE // 512], F32)
                k.dma("sp", pflag[:], pflag_d[:, :], r=[], w=[pflag.res], sres=pflag.res)
                cL = sb(pa, "cL", [128, 4], F32)
                tmp4 = sb(pa, "tmp4", [128, 4], F32)
                k.op("act", lambda e: e.activation(out=tmp4[:], in_=smalls[:, SM_LAM:SM_LAM + 4], func=AF.Exp, scale=-1.0),
                     r=[smalls.res], w=[tmp4.res])
                k.op("act", lambda e: e.activation(out=cL[:], in_=tmp4[:], func=AF.Ln, bias=cst[:, 1:2], scale=1.0),
                     r=[tmp4.res, cst.res], w=[cL.res])
                k.op("dve", lambda e: e.tensor_scalar(out=cL[:], in0=cL[:], scalar1=-8.0, scalar2=None, op0=ALU.mult),
                     r=[cL.res], w=[cL.res])
                set_gain(SM_MIX)
                xtmp = [sb(pa, "xtmp%d" % i, [128, D], F32) for i in range(2)]
                hT = [sb(pa, "hTa%d" % i, [128, 8, BA], BF16) for i in range(2)]
                xl = sb(pa, "xl", [128, 4, 3 + BA], F32, nres=4)
                gg = sb(pa, "gg", [128, 4, BA], F32, nres=4)
                hh = sb(pa, "hh", [128, 4, BA], F32, nres=4)
                hst = sb(pa, "hst", [128, 4], F32, nres=4)
                yaT = sb(pa, "yaT", [128, 4, BA], F32, nres=4)
                LT = [{nm: sb(pa, "l%s%d" % (nm, i), [128, BA], BF16 if nm == "xcb" else F32)
                       for nm in ("xc", "xc2", "xcb", "rr", "ii", "aa", "tt", "bb")} for i in range(2)]
                sq = sb(pa, "sq", [128, BA], BF16)
                rsn = sb(pa, "rsn", [128, BA], F32)
                pj = [ps(pa, "pj%d" % i, [128, 512], F32) for i in range(2)]
                pT = [ps(pa, "pT%d" % i, [128, 8, 128], BF16) for i in range(2)]
                pg = [ps(pa, "pg%d" % i, [128, 512], F32) for i in range(4)]
                pn = pj[0]
                k.op("pool", lambda e: e.memset(xl[:], 0.0), w=xl.rs)
                k.op("pool", lambda e: e.memset(hst[:], 0.0), w=hst.rs)

                nblk_pre = NPRE // BA
                nblk_own = NT // BA
                tcount = 0
                pjc = 0
                def front_norm(b):
                    nonlocal tcount
                    own = b >= nblk_pre
                    ob = b - nblk_pre
                    h = hT[b % 2]

                    def tile_g(t):
                        nonlocal tcount
                        if own:
                            tile = ob * 4 + t
                            xa = x_res[:, tile, :]
                            xr = x_res.rs[tile]
                            k.dma("sp", xa, xown[tile * 128:(tile + 1) * 128, :], r=[], w=[xr], sres=xr)
                        else:
                            xt_ = xtmp[tcount % 2]
                            xa = xt_[:]
                            xr = xt_.res
                            r0 = b * BA + t * 128
                            k.dma("sp", xa, xprev[r0:r0 + 128, :], r=[], w=[xr], sres=xr)
                        tcount += 1
                        yield from norm_T_g(xa, xr, pT[t % 2], h[:, :, t * 128:(t + 1) * 128], h.res)

                    yield from lockstep_g([tile_g(0), tile_g(1)])
                    yield from lockstep_g([tile_g(2), tile_g(3)])

                def front_proj(b):
                    nonlocal pjc
                    own = b >= nblk_pre
                    h = hT[b % 2]
                    for cc in range(8 if own else 4):
                        pp = pj[pjc % 2]
                        pjc += 1
                        mm_group(pp[:], [(w_inA[:, kc, cc * 128:(cc + 1) * 128], h[:, kc, :]) for kc in range(8)],
                                 r=[w_inA.res, h.res], w=[pp.res])
                        if cc < 4:
                            k.op("act", lambda e, pp=pp, cc=cc: e.activation(out=xl[:, cc, 3:3 + BA], in_=pp[:], func=AF.Copy),
                                 r=[pp.res], w=[xl.rs[cc]])
                        else:
                            k.op("act", lambda e, pp=pp, cc=cc: e.activation(out=gg[:, cc - 4, :], in_=pp[:], func=AF.Gelu_apprx_tanh),
                                 r=[pp.res], w=[gg.rs[cc - 4]])

                for _ in front_norm(0):
                    pass
                front_proj(0)
                for b in range(nblk_pre + nblk_own):
                    own = b >= nblk_pre
                    ob = b - nblk_pre
                    fn = front_norm(b + 1) if b + 1 < nblk_pre + nblk_own else iter(())
                    def lru_chain(cc, L, pga, pgb, own=own, b=b):
                            cw = lambda j, cc=cc: smalls[:, SM_CW + cc * 4 + j:SM_CW + cc * 4 + j + 1]
                            k.op("dve", lambda e, cc=cc, L=L, cw=cw: e.tensor_scalar(
                                out=L["xc"][:], in0=xl[:, cc, 3:3 + BA], scalar1=cw(3), scalar2=smalls[:, SM_CB + cc:SM_CB + cc + 1],
                                op0=ALU.mult, op1=ALU.add), r=[xl.rs[cc], smalls.res], w=[L["xc"].res])
                            src, dst = "xc", "xc2"
                            for j in range(3):
                                yield
                                k.op("dve", lambda e, cc=cc, L=L, cw=cw, j=j, src=src, dst=dst: e.scalar_tensor_tensor(
                                    out=L[dst][:], in0=xl[:, cc, j:j + BA], scalar=cw(j), in1=L[src][:], op0=ALU.mult, op1=ALU.add),
                                    r=[xl.rs[cc], smalls.res, L[src].res], w=[L[dst].res])
                                src, dst = dst, src
                            xc = L[src]
                            yield
                            k.op("pool", lambda e, cc=cc: e.tensor_copy(out=xl[:, cc, 0:3], in_=xl[:, cc, BA:BA + 3]),
                                 r=[xl.rs[cc]], w=[xl.rs[cc]])
                            yield
                            k.op("act", lambda e, L=L, xc=xc: e.activation(out=L["xcb"][:], in_=xc[:], func=AF.Copy),
                                 r=[xc.res], w=[L["xcb"].res])
                            yield
                            k.op("pe", lambda e, cc=cc, L=L: e.matmul(pga[:], lhsT=gbd[:, cc, :], rhs=L["xcb"][:], start=True, stop=True),
                                 r=[gbd.res, L["xcb"].res], w=[pga.res])
                            yield
                            k.op("pe", lambda e, cc=cc, L=L: e.matmul(pgb[:], lhsT=gbd[:, 4 + cc, :], rhs=L["xcb"][:], start=True, stop=True),
                                 r=[gbd.res, L["xcb"].res], w=[pgb.res])
                            yield
                            k.op("act", lambda e, cc=cc, L=L: e.activation(out=L["rr"][:], in_=pga[:], func=AF.Sigmoid,
                                                                           bias=smalls[:, SM_BA + cc:SM_BA + cc + 1], scale=1.0),
                                 r=[pga.res, smalls.res], w=[L["rr"].res])
                            yield
                            k.op("act", lambda e, cc=cc, L=L: e.activation(out=L["ii"][:], in_=pgb[:], func=AF.Sigmoid,
                                                                           bias=smalls[:, SM_BX + cc:SM_BX + cc + 1], scale=1.0),
                                 r=[pgb.res, smalls.res], w=[L["ii"].res])
                            yield
                            k.op("act", lambda e, cc=cc, L=L: e.activation(out=L["aa"][:], in_=L["rr"][:], func=AF.Exp,
                                                                           scale=cL[:, cc:cc + 1]),
                                 r=[L["rr"].res, cL.res], w=[L["aa"].res])
                            yield
                            k.op("dve", lambda e, L=L: e.tensor_tensor(out=L["tt"][:], in0=L["aa"][:], in1=L["aa"][:], op=ALU.mult),
                                 r=[L["aa"].res], w=[L["tt"].res])
                            yield
                            k.op("act", lambda e, L=L: e.activation(out=L["tt"][:], in_=L["tt"][:], func=AF.Sqrt,
                                                                    bias=cst[:, 1:2], scale=-1.0),
                                 r=[L["tt"].res, cst.res], w=[L["tt"].res])
                            yield
                            k.op("dve", lambda e, L=L: e.tensor_tensor(out=L["bb"][:], in0=L["tt"][:], in1=L["ii"][:], op=ALU.mult),
                                 r=[L["tt"].res, L["ii"].res], w=[L["bb"].res])
                            yield
                            k.op("dve", lambda e, L=L, xc=xc: e.tensor_tensor(out=L["rr"][:], in0=L["bb"][:], in1=xc[:], op=ALU.mult),
                                 r=[L["bb"].res, xc.res], w=[L["rr"].res])
                            yield
                            k.op("dve", lambda e, cc=cc, L=L: e.tensor_tensor_scan(
                                out=hh[:, cc, :], data0=L["aa"][:], data1=L["rr"][:], initial=hst[:, cc:cc + 1],
                                op0=ALU.mult, op1=ALU.add), r=[L["aa"].res, L["rr"].res, hst.rs[cc]], w=[hh.rs[cc]])
                            if own:
                                yield
                                k.op("dve", lambda e, cc=cc: e.tensor_copy(out=hst[:, cc:cc + 1], in_=hh[:, cc, BA - 1:BA]),
                                     r=[hh.rs[cc]], w=[hst.rs[cc]])
                                yield
                                k.op("dve", lambda e, cc=cc: e.tensor_tensor(out=yaT[:, cc, :], in0=hh[:, cc, :], in1=gg[:, cc, :], op=ALU.mult),
                                     r=[hh.rs[cc], gg.rs[cc]], w=[yaT.rs[cc]])
                            else:
                                yield
                                k.op("dve", lambda e, cc=cc, b=b: e.tensor_tensor(out=hst[:, cc:cc + 1], in0=hh[:, cc, BA - 1:BA],
                                                                                  in1=pflag[:, b:b + 1], op=ALU.mult),
                                     r=[hh.rs[cc], pflag.res], w=[hst.rs[cc]])

                    for pair in range(2):
                        gens = [lru_chain(2 * pair + i_, LT[i_], pg[2 * i_], pg[2 * i_ + 1]) for i_ in range(2)]
                        while gens:
                            for g_ in list(gens):
                                try:
                                    next(g_)
                                except StopIteration:
                                    gens.remove(g_)
                            next(fn, None)
                    for _ in fn:
                        pass
                    if own:
                        for cc in range(4):
                            k.op("dve", lambda e, cc=cc: e.tensor_tensor(out=sq[:], in0=yaT[:, cc, :], in1=yaT[:, cc, :], op=ALU.mult),
                                 r=[yaT.rs[cc]], w=[sq.res])
                            k.op("pe", lambda e, cc=cc: e.matmul(pn[:], lhsT=ones_b[:], rhs=sq[:], start=(cc == 0), stop=(cc == 3)),
                                 r=[ones_b.res, sq.res], w=[pn.res])
                        k.op("act", lambda e: e.activation(out=rsn[:], in_=pn[:], func=AF.Ln, bias=cst[:, 0:1], scale=1.0 / 512),
                             r=[pn.res, cst.res], w=[rsn.res])
                        k.op("act", lambda e: e.activation(out=rsn[:], in_=rsn[:], func=AF.Exp, scale=-0.5), r=[rsn.res], w=[rsn.res])
                        for cc in range(4):
                            k.op("dve", lambda e, cc=cc, ob=ob: e.scalar_tensor_tensor(
                                out=yan[:, cc, :], in0=yaT[:, cc, :],
                                scalar=smalls[:, SM_GA + cc:SM_GA + cc + 1], in1=rsn[:], op0=ALU.mult, op1=ALU.mult),
                                r=[yaT.rs[cc], smalls.res, rsn.res], w=[yan.res])
                        for t in range(4):
                            tile = ob * 4 + t
                            for half in range(2):
                                pp = pj[pjc % 2]
                                pjc += 1
                                mm_group(pp[:], [(yan[:, c, t * 128:(t + 1) * 128], w_outA[:, c, half * 512:(half + 1) * 512]) for c in range(4)],
                                         r=[yan.res, w_outA.res], w=[pp.res])
                                k.op("dve", lambda e, pp=pp, tile=tile, half=half: e.tensor_tensor(
                                    out=x_res[:, tile, half * 512:(half + 1) * 512], in0=pp[:], in1=x_res[:, tile, half * 512:(half + 1) * 512],
                                    op=ALU.add), r=[pp.res, x_res.rs[tile]], w=[x_res.rs[tile]])
                    if b + 1 < nblk_pre + nblk_own:
                        front_proj(b + 1)
                k.barrier(release=[w_inA.res, w_outA.res, gbd_f.res, pflag.res] + [t_.res for t_ in xtmp])

            with ExitStack() as pb:
                BB = 256
                w_inB = sb(pb, "w_inB", [128, 8, 1536], BF16)
                load_w_bf16(w_inB, w_in_d[:, 1024:2560], 1536)
                w_out = sb(pb, "w_outB", [128, 4, D], BF16)
                for c in range(4):
                    k.dma("pool", w_out[:, c, :], w_out_d[512 + c * 128:512 + (c + 1) * 128, :], r=[], w=[w_out.res], sres=w_out.res,
                          max_dma_last_dim=4096)
                abias = sb(pb, "abias", [128, 8, 640], F32)
                k.dma("sp", abias[:], abias_d.rearrange("p (h c) -> p h c", h=8), r=[], w=[abias.res], sres=abias.res)
                halob = sb(pb, "halob", [128, 512], F32)
                k.dma("sp", halob[:], halob_d[:, :], r=[], w=[halob.res], sres=halob.res)
                xtmp = [sb(pb, "xtmpb%d" % i, [128, D], F32) for i in range(2)]
                hT = [sb(pb, "hTb%d" % i, [128, 8, BB], BF16) for i in range(2)]
                qA_l = [sb(pb, "qA%d" % i, [128, 4, BB], BF16) for i in range(2)]
                qB_l = [sb(pb, "qB%d" % i, [128, 4, BB], BF16) for i in range(2)]
                kT = [sb(pb, "kT%d" % i, [128, 4, BB], BF16) for i in range(4)]
                vpad = [sb(pb, "vpad%d" % i, [128, 8, 128], BF16) for i in range(8)]
                ybT = sb(pb, "ybT", [128, 4, BB], F32)
                ybn = sb(pb, "ybn", [128, 4, BB], BF16)
                sbuf_s = [sb(pb, "sbs%d" % i, [128, 640], F32) for i in range(2)]
                Pm = [sb(pb, "Pm%d" % i, [128, 640], BF16) for i in range(2)]
                Pn = [sb(pb, "Pn%d" % i, [128, 640], BF16) for i in range(2)]
                PT = [sb(pb, "PT%d" % i, [128, 5, 128], BF16) for i in range(2)]
                st = [dict(mx=sb(pb, "a_mx%d" % i, [128, 1], F32), rs=sb(pb, "a_rs%d" % i, [128, 1], F32),
                           ri=sb(pb, "a_ri%d" % i, [128, 1], F32)) for i in range(2)]
                sq4 = [sb(pb, "sqb%d" % i, [128, BB], BF16) for i in range(4)]
                rsn = sb(pb, "rsnb", [128, BB], F32)
                pj = [ps(pb, "pjb%d" % i, [128, 512], F32) for i in range(2)]
                pT = [ps(pb, "pTb%d" % i, [128, 8, 128], BF16) for i in range(1)]
                pSm = [ps(pb, "pSm%d" % i, [128, 512], F32) for i in range(2)]
                pSr = Tn(pb.enter_context(nc.psum_tensor("ps_pSr", [128, 4, 128], F32)), "pSr", nres=4)
                pPT = ps(pb, "pPT", [128, 5, 128], BF16)
                pO = ps(pb, "pO", [128, 4, 128], F32)
                pn = pj[0]
                for v_ in vpad:
                    k.op("pool", lambda e, v_=v_: e.memset(v_[:], 0.0), w=[v_.res])
                for q_ in qA_l + qB_l:
                    k.op("pool", lambda e, q_=q_: e.memset(q_[:], 0.0), w=[q_.res])
                set_gain(SM_MIX)

                nhalo = 512 // BB
                nown = NT // BB
                tcount = 0
                pjc = 0
                ac = 0
                def prep(b):
                    nonlocal tcount, pjc
                    own = b >= nhalo
                    ob = b - nhalo
                    h = hT[b % 2]
                    kcur = kT[b % 4]
                    qA, qB = qA_l[b % 2], qB_l[b % 2]
                    for t in range(2):
                        xt_ = xtmp[tcount % 2]
                        xa = xt_[:]
                        xr = xt_.res
                        if own:
                            r0 = ob * BB + t * 128
                            k.dma("sp", xa, xown[r0:r0 + 128, :], r=[], w=[xr], sres=xr)
                        else:
                            r0 = NPRE - 512 + b * BB + t * 128
                            k.dma("sp", xa, xprev[r0:r0 + 128, :], r=[], w=[xr], sres=xr)
                        yield from norm_T_g(xa, xr, pT[0], h[:, :, t * 128:(t + 1) * 128], h.res)
                        tcount += 1
                        yield
                    for m in range(4):
                        pp = pj[pjc % 2]
                        pjc += 1
                        mm_group(pp[:, 0:BB], [(w_inB[:, kc, 512 + m * 128:512 + (m + 1) * 128], h[:, kc, :]) for kc in range(8)],
                                 r=[w_inB.res, h.res], w=[pp.res])
                        k.op("act", lambda e, pp=pp, m=m, kcur=kcur: e.activation(out=kcur[:, m, :], in_=pp[:, 0:BB], func=AF.Copy),
                             r=[pp.res], w=[kcur.res])
                    yield
                    for t in range(2):
                        yield
                        gt = b * 2 + t
                        vp = vpad[gt % 8]
                        pp = pj[pjc % 2]
                        pjc += 1
                        mm_group(pp[:], [(h[:, kc, t * 128:(t + 1) * 128], w_inB[:, kc, 1024:1536]) for kc in range(8)],
                                 r=[w_inB.res, h.res], w=[pp.res])
                        ppv = pp[:].rearrange("p (h d) -> p h d", h=8)
                        k.op("act", lambda e, vp=vp, ppv=ppv: e.activation(out=vp[:, 0:8:2, 0:64], in_=ppv[:, 0:8:2, :], func=AF.Copy),
                             r=[pp.res], w=[vp.res])
                        k.op("act", lambda e, vp=vp, ppv=ppv: e.activation(out=vp[:, 1:8:2, 64:128], in_=ppv[:, 1:8:2, :], func=AF.Copy),
                             r=[pp.res], w=[vp.res])
                    if not own:
                        return
                    yield
                    for m in range(4):
                        if m == 2:
                            yield
                        pp = pj[pjc % 2]
                        pjc += 1
                        mm_group(pp[:, 0:BB], [(w_inB[:, kc, m * 128:(m + 1) * 128], h[:, kc, :]) for kc in range(8)],
                                 r=[w_inB.res, h.res], w=[pp.res])
                        k.op("act", lambda e, pp=pp, m=m: e.activation(out=qA[0:64, m, :], in_=pp[0:64, 0:BB], func=AF.Copy),
                             r=[pp.res], w=[qA.res])
                        k.op("act", lambda e, pp=pp, m=m: e.activation(out=qB[64:128, m, :], in_=pp[64:128, 0:BB], func=AF.Copy),
                             r=[pp.res], w=[qB.res])
                def attend(b, fillers):
                    nonlocal pjc, ac
                    ob = b - nhalo
                    qA, qB = qA_l[b % 2], qB_l[b % 2]
                    units = []
                    for p in range(2):
                        pieces = []
                        oc = 0
                        remaining = 640
                        pos = 128 * p
                        while remaining > 0:
                            bi = pos // BB
                            c0 = pos % BB
                            n = min(BB - c0, remaining)
                            if oc < 512 and oc + n > 512:
                                n = 512 - oc
                            pieces.append((kT[(b - 2 + bi) % 4], c0, n, oc))
                            oc += n
                            pos += n
                            remaining -= n
                        for hd in range(8):
                            units.append((p, hd, pieces))

                    def S1(u):
                        p, hd, pieces = units[u]
                        m = hd // 2
                        qs = (qA if hd % 2 == 0 else qB)
                        gi = ac0 + u
                        pm, pr = pSm[gi % 2], pSr
                        for (kt_, c0, n, oc) in pieces:
                            if oc < 512:
                                oap = pm[:, oc:oc + n]
                                ores = pm.res
                            else:
                                oap = pr[:, gi % 4, oc - 512:oc - 512 + n]
                                ores = pr.res
                            k.op("pe", lambda e, kt_=kt_, c0=c0, n=n, oap=oap: e.matmul(
                                oap, lhsT=qs[:, m, p * 128:(p + 1) * 128], rhs=kt_[:, m, c0:c0 + n],
                                start=True, stop=True), r=[qs.res, kt_.res], w=[ores])

                    def S2(u, part):
                        p, hd, pieces = units[u]
                        gi = ac0 + u
                        i2 = gi % 2
                        pm, pr = pSm[gi % 2], pSr
                        S, PP, PN, s_ = sbuf_s[i2], Pm[i2], Pn[i2], st[i2]
                        if part == 1:
                            k.op("dve", lambda e: e.reciprocal(out=s_["ri"][:], in_=s_["rs"][:]), r=[s_["rs"].res], w=[s_["ri"].res])
                            k.op("dve", lambda e: e.tensor_scalar(out=PN[:], in0=PP[:], scalar1=s_["ri"][:, 0:1],
                                                                  scalar2=None, op0=ALU.mult),
                                 r=[PP.res, s_["ri"].res], w=[PN.res])
                            return
                        k.op("dve", lambda e: e.scalar_tensor_tensor(
                            out=S[:, 0:512], in0=pm[:], scalar=0.125, in1=abias[:, hd, 0:512], op0=ALU.mult, op1=ALU.add),
                            r=[pm.res, abias.res], w=[S.res])
                        k.op("dve", lambda e: e.scalar_tensor_tensor(
                            out=S[:, 512:640], in0=pr[:, gi % 4, :], scalar=0.125, in1=abias[:, hd, 512:640], op0=ALU.mult, op1=ALU.add),
                            r=[pr.res, abias.res], w=[S.res])
                        cnt = 512 - ob * BB - 128 * p
                        if cnt > 0:
                            hb0 = ob * BB + 128 * p
                            k.op("dve", lambda e: e.tensor_tensor(
                                out=S[:, 0:cnt], in0=S[:, 0:cnt], in1=halob[:, hb0:512], op=ALU.add),
                                r=[S.res, halob.res], w=[S.res])
                        k.op("dve", lambda e: e.tensor_reduce(out=s_["mx"][:], in_=S[:], axis=AX.X, op=ALU.max, negate=True),
                             r=[S.res], w=[s_["mx"].res])
                        k.op("act", lambda e: e.activation(out=PP[:], in_=S[:], func=AF.Exp, bias=s_["mx"][:, 0:1],
                                                           scale=1.0, accum_out=s_["rs"][:]),
                             r=[S.res, s_["mx"].res], w=[PP.res, s_["rs"].res])

                    def S3(u):
                        p, hd, pieces = units[u]
                        m = hd // 2
                        gi = ac0 + u
                        i2 = gi % 2
                        PN, PTs = Pn[i2], PT[i2]
                        for kc in range(5):
                            k.op("pe", lambda e, kc=kc: e.transpose(out=pPT[:, kc, :], in_=PN[:, kc * 128:(kc + 1) * 128],
                                                                    identity=ident_b[:]),
                                 r=[PN.res, ident_b.res], w=[pPT.res])
                        k.op("act", lambda e: e.activation(out=PTs[:], in_=pPT[:], func=AF.Copy), r=[pPT.res], w=[PTs.res])
                        g0 = (b * 2 + p) - 4
                        for kc in range(5):
                            vp = vpad[(g0 + kc) % 8]
                            k.op("pe", lambda e, vp=vp, kc=kc: e.matmul(
                                pO[:, m, :], lhsT=vp[:, hd, :], rhs=PTs[:, kc, :],
                                start=(hd % 2 == 0 and kc == 0), stop=(hd % 2 == 1 and kc == 4)),
                                r=[vp.res, PTs.res], w=[pO.res])
                        if hd == 7:
                            k.op("act", lambda e: e.activation(out=ybT[:, :, p * 128:(p + 1) * 128], in_=pO[:], func=AF.Copy),
                                 r=[pO.res], w=[ybT.res])

                    def advance():
                        while fillers:
                            try:
                                next(fillers[0])
                                return
                            except StopIteration:
                                fillers.pop(0)

                    ac0 = ac
                    S1(0)
                    S1(1)
                    S2(0, 0)
                    for u in range(len(units)):
                        if u + 1 < len(units):
                            S2(u + 1, 0)
                        S2(u, 1)
                        if u + 2 < len(units):
                            S1(u + 2)
                        S3(u)
                        advance()
                    while fillers:
                        advance()
                    ac += len(units)

                def tail(b):
                    nonlocal pjc
                    ob = b - nhalo
                    for cc in range(4):
                        k.op("dve", lambda e, cc=cc: e.tensor_tensor(out=sq4[cc][:], in0=ybT[:, cc, :], in1=ybT[:, cc, :], op=ALU.mult),
                             r=[ybT.res], w=[sq4[cc].res])
                    for cc in range(4):
                        k.op("pe", lambda e, cc=cc: e.matmul(pn[:, 0:BB], lhsT=ones_b[:], rhs=sq4[cc][:], start=(cc == 0), stop=(cc == 3)),
                             r=[ones_b.res, sq4[cc].res], w=[pn.res])
                    yield
                    k.op("act", lambda e: e.activation(out=rsn[:], in_=pn[:, 0:BB], func=AF.Ln, bias=cst[:, 0:1], scale=1.0 / 512),
                         r=[pn.res, cst.res], w=[rsn.res])
                    k.op("act", lambda e: e.activation(out=rsn[:], in_=rsn[:], func=AF.Exp, scale=-0.5), r=[rsn.res], w=[rsn.res])
                    yield
                    for cc in range(4):
                        k.op("dve", lambda e, cc=cc: e.scalar_tensor_tensor(
                            out=ybn[:, cc, :], in0=ybT[:, cc, :], scalar=smalls[:, SM_GB + cc:SM_GB + cc + 1], in1=rsn[:],
                            op0=ALU.mult, op1=ALU.mult), r=[ybT.res, smalls.res, rsn.res], w=[ybn.res])
                    yield
                    for t in range(2):
                        tile = ob * 2 + t
                        tok0 = ob * BB + t * 128
                        for half in range(2):
                            pp = pj[pjc % 2]
                            pjc += 1
                            pairs = [(ybn[:, c, t * 128:(t + 1) * 128], w_out[:, c, half * 512:(half + 1) * 512]) for c in range(4)]
                            mm_group(pp[:], pairs, r=[ybn.res, w_out.res], w=[pp.res])
                            k.op("dve", lambda e, pp=pp, tile=tile, half=half: e.tensor_tensor(
                                out=x_res[:, tile, half * 512:(half + 1) * 512], in0=pp[:], in1=x_res[:, tile, half * 512:(half + 1) * 512],
                                op=ALU.add), r=[pp.res, x_res.rs[tile]], w=[x_res.rs[tile]])
                        yield
                nb_ = nhalo + nown
                for b0 in range(nhalo + 1):
                    for _ in prep(b0):
                        pass
                for b in range(nhalo, nb_):
                    fl = []
                    if b - 1 >= nhalo:
                        fl.append(tail(b - 1))
                    if b + 1 < nb_:
                        fl.append(prep(b + 1))
                    attend(b, fl)
                for _ in tail(nb_ - 1):
                    pass
                k.barrier(release=[w_inB.res, w_out.res, abias.res, halob.res] + [t_.res for t_ in xtmp])

        if debug:
            for tile in range(NTILE):
                k.dma("sp", dbg["dbg1"][tile * 128:(tile + 1) * 128, :], x_res[:, tile, :], r=[x_res.rs[tile]], w=[], sres=x_res.rs[tile])

        with ExitStack() as p2:
            B2 = 512
            w_q = sb(p2, "w_q", [128, 8, D], BF16)
            w_o = sb(p2, "w_o", [128, 8, D], BF16)
            kmT = sb(p2, "kmT", [128, 8, 256], BF16)
            vmem = sb(p2, "vmem", [128, 2, D], BF16)
            pj = [ps(p2, "pj2%d" % i, [128, 512], F32) for i in range(2)]
            pT = [ps(p2, "pT2%d" % i, [128, 8, 128], BF16) for i in range(1)]
            pS2 = [ps(p2, "pS2%d" % i, [128, 4, 256], F32) for i in range(2)]
            pPT2 = ps(p2, "pPT2", [128, 8, 128], BF16)
            pjc = 0
            with ExitStack() as p2s:
                w_kv = sb(p2s, "w_kv", [128, 8, 2048], BF16)
                load_w_bf16(w_kv, w_kv_d[:, :], 2048)
                load_w_bf16(w_q, w_q_d[:, :], D)
                load_w_bf16(w_o, w_o_d[:, :], D)
                memt = [sb(p2s, "memt%d" % i, [128, D], F32) for i in range(2)]
                memT = sb(p2s, "memT", [128, 8, 256], BF16)
                set_gain(SM_MEM)
                for t in range(2):
                    k.dma("sp", memt[t][:], mem_d[t * 128:(t + 1) * 128, :], r=[], w=[memt[t].res], sres=memt[t].res)
                    norm_T(memt[t][:], memt[t].res, pT[0], memT[:, :, t * 128:(t + 1) * 128], memT.res)
                for oc in range(8):
                    pp = pj[pjc % 2]
                    pjc += 1
                    mm_group(pp[:, 0:256], [(w_kv[:, kc, oc * 128:(oc + 1) * 128], memT[:, kc, :]) for kc in range(8)],
                             r=[w_kv.res, memT.res], w=[pp.res])
                    k.op("act", lambda e, pp=pp, oc=oc: e.activation(out=kmT[:, oc, :], in_=pp[:, 0:256], func=AF.Copy),
                         r=[pp.res], w=[kmT.res])
                for t in range(2):
                    for half in range(2):
                        pp = pj[pjc % 2]
                        pjc += 1
                        mm_group(pp[:], [(memT[:, kc, t * 128:(t + 1) * 128], w_kv[:, kc, 1024 + half * 512:1024 + (half + 1) * 512])
                                         for kc in range(8)], r=[w_kv.res, memT.res], w=[pp.res])
                        k.op("act", lambda e, pp=pp, t=t, half=half: e.activation(out=vmem[:, t, half * 512:(half + 1) * 512], in_=pp[:], func=AF.Copy),
                             r=[pp.res], w=[vmem.res])
                k.barrier(release=[w_kv.res] + [t_.res for t_ in memt])
            hT = [sb(p2, "hT2%d" % i, [128, 8, B2], BF16) for i in range(2)]
            qT = sb(p2, "qT2", [128, 8, B2], BF16)
            P2 = [sb(p2, "P2%d" % i, [128, 4, 256], BF16) for i in range(2)]
            P2n = [sb(p2, "P2n%d" % i, [128, 4, 256], BF16) for i in range(2)]
            PT2 = sb(p2, "PT2", [128, 4, 2, B2], BF16)
            oT = sb(p2, "oT2", [128, 8, B2], BF16)
            st2 = [dict(mx=sb(p2, "c_mx%d" % i, [128, 4], F32), rs=sb(p2, "c_rs%d" % i, [128, 4], F32),
                        ri=sb(p2, "c_ri%d" % i, [128, 4], F32)) for i in range(2)]
            set_gain(SM_CROSS)
            tc2 = 0
            qT_l = [qT, sb(p2, "qT2b", [128, 8, B2], BF16)]
            NB2 = NT // B2

            def front2(b):
                nonlocal pjc
                h = hT[b % 2]
                q_ = qT_l[b % 2]
                for t in range(4):
                    yield from norm_T_g(x_res[:, b * 4 + t, :], x_res.rs[b * 4 + t], pT[0], h[:, :, t * 128:(t + 1) * 128], h.res)
                    yield
                for oc in range(8):
                    pp = pj[pjc % 2]
                    pjc += 1
                    mm_group(pp[:], [(w_q[:, kc, oc * 128:(oc + 1) * 128], h[:, kc, :]) for kc in range(8)],
                             r=[w_q.res, h.res], w=[pp.res])
                    k.op("act", lambda e, pp=pp, oc=oc: e.activation(out=q_[:, oc, :], in_=pp[:], func=AF.Copy), r=[pp.res], w=[q_.res])
                    yield

            for _ in front2(0):
                pass
            for b in range(NB2):
                qT = qT_l[b % 2]
                fn = front2(b + 1) if b + 1 < NB2 else iter(())
                def c_S1(t, i2):
                    pS_ = pS2[i2]
                    for hh_ in range(4):
                        mm_group(pS_[:, hh_, :], [(qT[:, 2 * hh_ + j, t * 128:(t + 1) * 128], kmT[:, 2 * hh_ + j, :]) for j in range(2)],
                                 r=[qT.res, kmT.res], w=[pS_.res])

                def c_S2(t, i2):
                    pS_, P_, Pn_, s_ = pS2[i2], P2[i2], P2n[i2], st2[i2]
                    k.op("dve", lambda e: e.tensor_reduce(out=s_["mx"][:], in_=pS_[:], axis=AX.X, op=ALU.max, negate=True),
                         r=[pS_.res], w=[s_["mx"].res])
                    k.op("dve", lambda e: e.tensor_scalar(out=s_["mx"][:], in0=s_["mx"][:], scalar1=1.0 / 16, scalar2=None, op0=ALU.mult),
                         r=[s_["mx"].res], w=[s_["mx"].res])
                    for hh_ in range(4):
                        k.op("act", lambda e, hh_=hh_: e.activation(
                            out=P_[:, hh_, :], in_=pS_[:, hh_, :], func=AF.Exp, bias=s_["mx"][:, hh_:hh_ + 1], scale=1.0 / 16,
                            accum_out=s_["rs"][:, hh_:hh_ + 1]), r=[pS_.res, s_["mx"].res], w=[P_.res, s_["rs"].res])
                    k.op("dve", lambda e: e.reciprocal(out=s_["ri"][:], in_=s_["rs"][:]), r=[s_["rs"].res], w=[s_["ri"].res])
                    k.op("dve", lambda e: e.tensor_tensor(
                        out=Pn_[:], in0=P_[:], in1=s_["ri"][:, :].unsqueeze(2).to_broadcast([128, 4, 256]), op=ALU.mult),
                        r=[P_.res, s_["ri"].res], w=[Pn_.res])

                def c_S3(t, i2):
                    Pn_ = P2n[i2]
                    for hh_ in range(4):
                        for mc in range(2):
                            k.op("pe", lambda e, hh_=hh_, mc=mc: e.transpose(
                                out=pPT2[:, hh_ * 2 + mc, :], in_=Pn_[:, hh_, mc * 128:(mc + 1) * 128], identity=ident_b[:]),
                                r=[Pn_.res, ident_b.res], w=[pPT2.res])
                    k.op("act", lambda e: e.activation(out=PT2[:, :, :, t * 128:(t + 1) * 128],
                                                       in_=pPT2[:].rearrange("p (h m) t -> p h m t", h=4), func=AF.Copy),
                         r=[pPT2.res], w=[PT2.res])

                c_S1(0, tc2 % 2)
                for t in range(4):
                    if t + 1 < 4:
                        c_S1(t + 1, (tc2 + 1) % 2)
                    c_S2(t, tc2 % 2)
                    next(fn, None)
                    c_S3(t, tc2 % 2)
                    next(fn, None)
                    tc2 += 1
                for oc in range(8):
                    pp = pj[pjc % 2]
                    pjc += 1
                    mm_group(pp[:], [(vmem[:, mc, oc * 128:(oc + 1) * 128], PT2[:, oc // 2, mc, :]) for mc in range(2)],
                             r=[vmem.res, PT2.res], w=[pp.res])
                    k.op("act", lambda e, pp=pp, oc=oc: e.activation(out=oT[:, oc, :], in_=pp[:], func=AF.Copy), r=[pp.res], w=[oT.res])
                    next(fn, None)
                for t in range(4):
                    tile = b * 4 + t
                    for half in range(2):
                        pp = pj[pjc % 2]
                        pjc += 1
                        mm_group(pp[:], [(oT[:, c, t * 128:(t + 1) * 128], w_o[:, c, half * 512:(half + 1) * 512]) for c in range(8)],
                                 r=[oT.res, w_o.res], w=[pp.res])
                        k.op("dve", lambda e, pp=pp, tile=tile, half=half: e.tensor_tensor(
                            out=x_res[:, tile, half * 512:(half + 1) * 512], in0=pp[:], in1=x_res[:, tile, half * 512:(half + 1) * 512],
                            op=ALU.add), r=[pp.res, x_res.rs[tile]], w=[x_res.rs[tile]])
                        next(fn, None)
                for _ in fn:
                    pass
            k.barrier(release=[w_q.res, w_o.res])

        if debug:
            for tile in range(NTILE):
                k.dma("sp", dbg["dbg2"][tile * 128:(tile + 1) * 128, :], x_res[:, tile, :], r=[x_res.rs[tile]], w=[], sres=x_res.rs[tile])

        with ExitStack() as p3:
            p3s = p3.enter_context(ExitStack())
            slotI = sb(p3s, "slotI", [128, NT], F32)
            slotJ = sb(p3s, "slotJ", [128, NT], F32)
            slotG = sb(p3s, "slotG", [128, NT], F32)
            set_gain(SM_FFN)
            with ExitStack() as pa:
                B3 = 256
                w_qry = sb(pa, "w_qry", [128, 8, 2048], BF16)
                load_w_bf16(w_qry, w_qry_d[:, :], 2048)
                skb = sb(pa, "skb", [128, 16, 128], BF16)
                k.dma("pool", skb[:], skT_d.rearrange("p (o n) -> p o n", o=16), r=[], w=[skb.res], sres=skb.res)
                hT = [sb(pa, "hT3%d" % i, [128, 8, B3], BF16) for i in range(2)]
                qpT = sb(pa, "qpT", [128, 16, B3], BF16)
                sc = sb(pa, "sc", [128, 16, 128], F32, nres=4)
                sc2 = sb(pa, "sc2", [128, 16, 128], F32, nres=16)
                tv = sb(pa, "tv", [128, 16, 16], F32, nres=16)
                tiu = sb(pa, "tiu", [128, 16, 16], U32, nres=16)
                tif = sb(pa, "tif", [128, 16, 16], F32)
                cand = sb(pa, "cand", [128, 8, 256], F32)
                cand2 = sb(pa, "cand2", [128, 8, 256], F32, nres=8)
                ts = sb(pa, "ts", [128, 8, 16], F32, nres=8)
                posu = sb(pa, "posu", [128, 8, 16], U32, nres=8)
                au = sb(pa, "au", [128, 8, 16], U32)
                bu = sb(pa, "bu", [128, 8, 16], U32)
                af = sb(pa, "af", [128, 8, 16], F32)
                bf = sb(pa, "bf", [128, 8, 16], F32)
                eq = sb(pa, "eq", [128, 8, 16, 16], F32)
                isel = sb(pa, "isel", [128, 8, 16], F32)
                jsel = sb(pa, "jsel", [128, 8, 16], F32)
                gsel = sb(pa, "gsel", [128, 8, 16], F32)
                sm = sb(pa, "sm", [128, 8], F32)
                pj = [ps(pa, "pj3%d" % i, [128, 512], F32) for i in range(2)]
                pT = [ps(pa, "pT3%d" % i, [128, 8, 128], BF16) for i in range(2)]
                psc = [ps(pa, "psc%d" % i, [128, 4, 128], F32) for i in range(2)]
                pTs = ps(pa, "pTs", [128, 3, 128], F32)
                pjc = 0
                scc = 0
                iota16 = iota_f[:, 0:16]
                for b in range(NT // B3):
                    h = hT[b % 2]
                    norm_pairs([(x_res[:, b * 2 + t, :], x_res.rs[b * 2 + t], pT[t % 2], h[:, :, t * 128:(t + 1) * 128], h.res) for t in range(2)])
                    for oc in range(16):
                        pp = pj[pjc % 2]
                        pjc += 1
                        mm_group(pp[:, 0:B3], [(w_qry[:, kc, oc * 128:(oc + 1) * 128], h[:, kc, :]) for kc in range(8)],
                                 r=[w_qry.res, h.res], w=[pp.res])
                        k.op("act", lambda e, pp=pp, oc=oc: e.activation(out=qpT[:, oc, :], in_=pp[:, 0:B3], func=AF.Copy),
                             r=[pp.res], w=[qpT.res])
                    for t in range(2):
                        tile = b * 2 + t
                        for g4 in range(4):
                            pq = psc[scc % 2]
                            scc += 1
                            for j in range(4):
                                oc = g4 * 4 + j
                                k.op("pe", lambda e, pq=pq, j=j, oc=oc, t=t: e.matmul(
                                    pq[:, j, :], lhsT=qpT[:, oc, t * 128:(t + 1) * 128], rhs=skb[:, oc, :], start=True, stop=True),
                                    r=[qpT.res, skb.res], w=[pq.res])
                            k.op("act", lambda e, pq=pq, g4=g4: e.activation(out=sc[:, g4 * 4:(g4 + 1) * 4, :], in_=pq[:], func=AF.Copy),
                                 r=[pq.res], w=[sc.rs[g4]])
                        for oc in range(16):
                            k.op("dve", lambda e, oc=oc: e.max(out=tv[:, oc, 0:8], in_=sc[:, oc, :]), r=[sc.rs[oc // 4]], w=[tv.rs[oc]])
                        for oc in range(16):
                            k.op("dve", lambda e, oc=oc: e.max_index(out=tiu[:, oc, 0:8], in_max=tv[:, oc, 0:8], in_values=sc[:, oc, :]),
                                 r=[sc.rs[oc // 4], tv.rs[oc]], w=[tiu.rs[oc]])
                        for oc in range(16):
                            k.op("dve", lambda e, oc=oc: e.match_replace(out=sc2[:, oc, :], in_to_replace=tv[:, oc, 0:8], in_values=sc[:, oc, :],
                                                                        imm_value=NEG), r=[sc.rs[oc // 4], tv.rs[oc]], w=[sc2.rs[oc]])
                        for oc in range(16):
                            k.op("dve", lambda e, oc=oc: e.max(out=tv[:, oc, 8:16], in_=sc2[:, oc, :]), r=[sc2.rs[oc]], w=[tv.rs[oc]])
                        for oc in range(16):
                            k.op("dve", lambda e, oc=oc: e.max_index(out=tiu[:, oc, 8:16], in_max=tv[:, oc, 8:16], in_values=sc2[:, oc, :]),
                                 r=[sc2.rs[oc], tv.rs[oc]], w=[tiu.rs[oc]])
                        k.op("dve", lambda e: e.tensor_copy(out=tif[:], in_=tiu[:]), r=tiu.rs, w=[tif.res])
                        tv4 = tv[:].rearrange("p (h two) a -> p h two a", two=2)
                        tif4 = tif[:].rearrange("p (h two) a -> p h two a", two=2)
                        k.op("dve", lambda e, tv4=tv4: e.tensor_tensor(
                            out=cand[:].rearrange("p h (a b) -> p h a b", a=16),
                            in0=tv4[:, :, 0, :].unsqueeze(3).to_broadcast([128, 8, 16, 16]),
                            in1=tv4[:, :, 1, :].unsqueeze(2).to_broadcast([128, 8, 16, 16]), op=ALU.add),
                            r=tv.rs, w=[cand.res])
                        for hd in range(8):
                            k.op("dve", lambda e, hd=hd: e.max(out=ts[:, hd, 0:8], in_=cand[:, hd, :]), r=[cand.res], w=[ts.rs[hd]])
                        for hd in range(8):
                            k.op("dve", lambda e, hd=hd: e.max_index(out=posu[:, hd, 0:8], in_max=ts[:, hd, 0:8], in_values=cand[:, hd, :]),
                                 r=[cand.res, ts.rs[hd]], w=[posu.rs[hd]])
                        for hd in range(8):
                            k.op("dve", lambda e, hd=hd: e.match_replace(out=cand2[:, hd, :], in_to_replace=ts[:, hd, 0:8], in_values=cand[:, hd, :],
                                                                        imm_value=NEG), r=[cand.res, ts.rs[hd]], w=[cand2.rs[hd]])
                        for hd in range(8):
                            k.op("dve", lambda e, hd=hd: e.max(out=ts[:, hd, 8:16], in_=cand2[:, hd, :]), r=[cand2.rs[hd]], w=[ts.rs[hd]])
                        for hd in range(8):
                            k.op("dve", lambda e, hd=hd: e.max_index(out=posu[:, hd, 8:16], in_max=ts[:, hd, 8:16], in_values=cand2[:, hd, :]),
                                 r=[cand2.rs[hd], ts.rs[hd]], w=[posu.rs[hd]])
                        k.op("dve", lambda e: e.tensor_scalar(out=au[:], in0=posu[:], scalar1=4, scalar2=None, op0=ALU.logical_shift_right),
                             r=posu.rs, w=[au.res])
                        k.op("dve", lambda e: e.tensor_scalar(out=bu[:], in0=posu[:], scalar1=15, scalar2=None, op0=ALU.bitwise_and),
                             r=posu.rs, w=[bu.res])
                        k.op("dve", lambda e: e.tensor_copy(out=af[:], in_=au[:]), r=[au.res], w=[af.res])
                        k.op("dve", lambda e: e.tensor_copy(out=bf[:], in_=bu[:]), r=[bu.res], w=[bf.res])
                        for (sel, rk, which) in ((isel, af, 0), (jsel, bf, 1)):
                            k.op("dve", lambda e, rk=rk: e.tensor_tensor(
                                out=eq[:], in0=rk[:].unsqueeze(3).to_broadcast([128, 8, 16, 16]),
                                in1=iota16.unsqueeze(1).unsqueeze(1).to_broadcast([128, 8, 16, 16]), op=ALU.is_equal),
                                r=[rk.res, iota_f.res], w=[eq.res])
                            k.op("dve", lambda e, which=which, tif4=tif4: e.tensor_tensor(
                                out=eq[:], in0=eq[:], in1=tif4[:, :, which, :].unsqueeze(2).to_broadcast([128, 8, 16, 16]), op=ALU.mult),
                                r=[eq.res, tif.res], w=[eq.res])
                            k.op("dve", lambda e, sel=sel: e.tensor_reduce(out=sel[:], in_=eq[:], axis=AX.X, op=ALU.add),
                                 r=[eq.res], w=[sel.res])
                        k.op("dve", lambda e: e.tensor_tensor(out=gsel[:], in0=ts[:], in1=ts[:, :, 0:1].to_broadcast([128, 8, 16]), op=ALU.subtract),
                             r=ts.rs, w=[gsel.res])
                        k.op("act", lambda e: e.activation(out=gsel[:], in_=gsel[:], func=AF.Exp), r=[gsel.res], w=[gsel.res])
                        k.op("dve", lambda e: e.tensor_reduce(out=sm[:], in_=gsel[:], axis=AX.X, op=ALU.add), r=[gsel.res], w=[sm.res])
                        k.op("dve", lambda e: e.reciprocal(out=sm[:], in_=sm[:]), r=[sm.res], w=[sm.res])
                        k.op("dve", lambda e: e.tensor_tensor(out=gsel[:], in0=gsel[:], in1=sm[:, :].unsqueeze(2).to_broadcast([128, 8, 16]), op=ALU.mult),
                             r=[gsel.res, sm.res], w=[gsel.res])
                        for i3, (src, dst) in enumerate(((isel, slotI), (jsel, slotJ), (gsel, slotG))):
                            k.op("pe", lambda e, i3=i3, src=src: e.transpose(out=pTs[:, i3, :], in_=src[:].rearrange("p h r -> p (h r)"),
                                                                            identity=ident_f[:]),
                                 r=[src.res, ident_f.res], w=[pTs.res])
                        for i3, (src, dst) in enumerate(((isel, slotI), (jsel, slotJ), (gsel, slotG))):
                            k.op("act", lambda e, i3=i3, dst=dst, tile=tile: e.activation(out=dst[:, tile * 128:(tile + 1) * 128], in_=pTs[:, i3, :], func=AF.Copy),
                                 r=[pTs.res], w=[dst.res])
                k.barrier(release=[w_qry.res, skb.res])

            with ExitStack() as pb:
                TCH = 8
                ohj = [sb(pb, "ohj%d" % i, [128, TCH, 128], BF16) for i in range(4)]
                eqi = [sb(pb, "eqi%d" % i, [128, TCH, 128], BF16) for i in range(4)]
                rig = [sb(pb, "rig%d" % i, [128, TCH, 128], BF16, nres=TCH) for i in range(4)]
                Wst = [sb(pb, "Wst%d" % i, [128, 128, 128], BF16, nres=32) for i in range(2)]
                pW = [ps(pb, "pW%d" % i, [128, 4, 128], F32) for i in range(4)]
                wc = 0
                chc = 0
                iota_b3 = iota_f[:, :].unsqueeze(1).to_broadcast([128, TCH, 128])
                for tile in range(NTILE):
                    W_ = Wst[tile % 2]
                    for ch in range(128 // TCH):
                        oj, ei, rg = ohj[chc % 4], eqi[chc % 4], rig[chc % 4]
                        chc += 1
                        tok0 = tile * 128 + ch * TCH
                        k.op("dve", lambda e, oj=oj, tok0=tok0: e.tensor_tensor(
                            out=oj[:], in0=iota_b3, in1=slotJ[:, tok0:tok0 + TCH].unsqueeze(2).to_broadcast([128, TCH, 128]), op=ALU.is_equal),
                            r=[iota_f.res, slotJ.res], w=[oj.res])
                        k.op("dve", lambda e, ei=ei, tok0=tok0: e.tensor_tensor(
                            out=ei[:], in0=iota_b3, in1=slotI[:, tok0:tok0 + TCH].unsqueeze(2).to_broadcast([128, TCH, 128]), op=ALU.is_equal),
                            r=[iota_f.res, slotI.res], w=[ei.res])
                        if chc % 9 < 4:
                            for tt_ in range(TCH):
                                k.op("act", lambda e, ei=ei, rg=rg, tt_=tt_, tok0=tok0: e.activation(
                                    out=rg[:, tt_, :], in_=ei[:, tt_, :], func=AF.Copy, scale=slotG[:, tok0 + tt_:tok0 + tt_ + 1]),
                                    r=[ei.res, slotG.res], w=[rg.rs[tt_]])
                        else:
                            k.op("dve", lambda e, ei=ei, rg=rg, tok0=tok0: e.tensor_tensor(
                                out=rg[:], in0=ei[:], in1=slotG[:, tok0:tok0 + TCH].unsqueeze(2).to_broadcast([128, TCH, 128]), op=ALU.mult),
                                r=[ei.res, slotG.res], w=rg.rs)
                        for q4 in range(TCH // 4):
                            pw = pW[wc % 4]
                            for j in range(4):
                                tt_ = q4 * 4 + j
                                k.op("pe", lambda e, pw=pw, j=j, oj=oj, rg=rg, tt_=tt_: e.matmul(
                                    pw[:, j, :], lhsT=oj[:, tt_, :], rhs=rg[:, tt_, :], start=True, stop=True),
                                    r=[oj.res, rg.rs[tt_]], w=[pw.res])
                            t0 = ch * TCH + q4 * 4
                            if True:
                                k.op("act", lambda e, pw=pw, W_=W_, t0=t0: e.activation(
                                    out=W_[:, :, t0:t0 + 4], in_=pw[:].rearrange("p t i -> p i t"), func=AF.Copy),
                                    r=[pw.res], w=[W_.rs[t0 // 4]])
                            else:
                                k.op("dve", lambda e, pw=pw, W_=W_, t0=t0: e.tensor_copy(
                                    out=W_[:, :, t0:t0 + 4], in_=pw[:].rearrange("p t i -> p i t")),
                                    r=[pw.res], w=[W_.rs[t0 // 4]])
                            wc += 1
                    k.dma("sp", wd_d[tile], W_[:].rearrange("p i t -> p (i t)"), r=W_.rs, w=[], sres=W_.res)
                k.barrier(release=[w_.res for w_ in Wst])
            p3s.close()

            with ExitStack() as pc:
                T3 = 256
                GS = 8
                NG = 128 // GS
                hfT = sb(pc, "hfT", [128, 8, NT], BF16)
                ut = [sb(pc, "ut%d" % i, [128, GS, 8, 128], BF16) for i in range(2)]
                vt = [sb(pc, "vt%d" % i, [128, GS, D], BF16) for i in range(2)]
                uT_v = uT_d.rearrange("(i p) (c e) -> p i c e", p=128, c=8)
                ev_v = ev_d.rearrange("(i e) d -> e i d", e=128)
                for i_ in range(GS):
                    k.dma("pool", ut[0][:, i_, :, :], uT_v[:, i_, :, :], r=[], w=[ut[0].res], sres=ut[0].res)
                    k.dma("pool", vt[0][:, i_, :], ev_v[:, i_, :], r=[], w=[vt[0].res], sres=vt[0].res, max_dma_last_dim=4096)
                with ExitStack() as pcn:
                    pTn = [ps(pcn, "pT4%d" % i, [128, 8, 128], BF16) for i in range(2)]
                    norm_pairs([(x_res[:, tile, :], x_res.rs[tile], pTn[tile % 2], hfT[:, :, tile * 128:(tile + 1) * 128], hfT.res)
                                for tile in range(NTILE)])
                    k.barrier()
                pA = [ps(pc, "pA%d" % i, [128, 512], F32) for i in range(3)]
                pO = [ps(pc, "pO4%d" % i, [128, 512], F32) for i in range(4)]
                wsl = [sb(pc, "wsl%d" % i, [128, 2, GS, 128], BF16) for i in range(2)]
                ge = [sb(pc, "ge%d" % i, [128, T3], F32) for i in range(3)]
                gw = [sb(pc, "gw%d" % i, [128, T3], BF16) for i in range(3)]
                stage = [sb(pc, "stage%d" % i, [128, 2, D], F32, nres=4) for i in range(2)]
                wd_v = wd_d.rearrange("t j (i x) -> j t i x", i=128)
                items = [(g, tb, i) for g in range(NG) for tb in range(NT // T3) for i in range(GS)]
                state = {}

                def emitA(n):
                    g, tb, i = items[n]
                    u_, v_ = ut[g % 2], vt[g % 2]
                    if tb == 0 and i == 0 and g > 0:
                        for i_ in range(GS):
                            k.dma("pool", u_[:, i_, :, :], uT_v[:, g * GS + i_, :, :], r=[], w=[u_.res], sres=u_.res)
                            k.dma("pool", v_[:, i_, :], ev_v[:, g * GS + i_, :], r=[], w=[v_.res], sres=v_.res, max_dma_last_dim=4096)
                    if i == 0:
                        ws_ = wsl[(g * (NT // T3) + tb) % 2]
                        k.dma("sp", ws_[:], wd_v[:, 2 * tb:2 * tb + 2, g * GS:(g + 1) * GS, :], r=[], w=[ws_.res], sres=ws_.res)
                    ws_ = wsl[(g * (NT // T3) + tb) % 2]
                    pa_ = pA[n % 3]
                    ge_, gw_ = ge[n % 3], gw[n % 3]
                    mm_group(pa_[:, 0:T3], [(u_[:, i, c, :], hfT[:, c, tb * T3:(tb + 1) * T3]) for c in range(8)],
                             r=[u_.res, hfT.res], w=[pa_.res])
                    k.op("act", lambda e: e.activation(out=ge_[:], in_=pa_[:, 0:T3], func=AF.Gelu_apprx_tanh),
                         r=[pa_.res], w=[ge_.res])
                    k.op("dve", lambda e: e.tensor_tensor(
                        out=gw_[:].rearrange("p (a t) -> p a t", a=2), in0=ge_[:].rearrange("p (a t) -> p a t", a=2),
                        in1=ws_[:, :, i, :], op=ALU.mult), r=[ge_.res, ws_.res], w=[gw_.res])

                def emitV(n):
                    g, tb, i = items[n]
                    v_ = vt[g % 2]
                    gw_ = gw[n % 3]
                    for t in range(2):
                        for half in range(2):
                            po = pO[t * 2 + half]
                            k.op("pe", lambda e, po=po, t=t, half=half: e.matmul(
                                po[:], lhsT=gw_[:, t * 128:(t + 1) * 128], rhs=v_[:, i, half * 512:(half + 1) * 512],
                                start=(i == 0), stop=(i == GS - 1)), r=[gw_.res, v_.res], w=[po.res])
                    if i == GS - 1:
                        stg = stage[(g * (NT // T3) + tb) % 2]
                        for t in range(2):
                            for half in range(2):
                                po = pO[t * 2 + half]
                                k.op("act", lambda e, po=po, t=t, half=half: e.activation(
                                    out=stg[:, t, half * 512:(half + 1) * 512], in_=po[:], func=AF.Copy),
                                    r=[po.res], w=[stg.rs[t * 2 + half]])
                        for t in range(2):
                            tile = tb * 2 + t
                            for half in range(2):
                                k.op("dve", lambda e, t=t, tile=tile, half=half: e.tensor_tensor(
                                    out=x_res[:, tile, half * 512:(half + 1) * 512], in0=stg[:, t, half * 512:(half + 1) * 512],
                                    in1=x_res[:, tile, half * 512:(half + 1) * 512],
                                    op=ALU.add), r=[stg.rs[t * 2 + half], x_res.rs[tile]], w=[x_res.rs[tile]])

                emitA(0)
                emitA(1)
                for n in range(len(items)):
                    if n + 2 < len(items):
                        emitA(n + 2)
                    emitV(n)
                k.barrier(release=[t_.res for t_ in ut + vt + wsl])

        with ExitStack() as p4:
            gfin = sb(p4, "gfin", [128, D], F32)
            k.dma("sp", gfin[:], gfin_d[:, :], r=[], w=[gfin.res], sres=gfin.res)
            ot = [sb(p4, "ot%d" % i, [128, D], F32) for i in range(2)]
            def fin_g(tile):
                n = nrm[tile % 2]
                o_ = ot[tile % 2]
                xa = x_res[:, tile, :]
                xr = x_res.rs[tile]
                k.op("dve", lambda e: e.scalar_tensor_tensor(out=n["junk"][:], in0=xa, scalar=1.0, in1=xa,
                                                             op0=ALU.mult, op1=ALU.mult, accum_out=n["ss"][:]),
                     r=[xr], w=[n["junk"].res, n["ss"].res])
                yield
                k.op("act", lambda e: e.activation(out=n["sd"][:], in_=n["ss"][:], func=AF.Ln, bias=cst[:, 0:1], scale=1.0 / D),
                     r=[n["ss"].res, cst.res], w=[n["sd"].res])
                k.op("act", lambda e: e.activation(out=n["rstd"][:], in_=n["sd"][:], func=AF.Exp, scale=-0.5),
                     r=[n["sd"].res], w=[n["rstd"].res])
                yield
                k.op("dve", lambda e: e.scalar_tensor_tensor(out=o_[:], in0=xa, scalar=n["rstd"][:, 0:1], in1=gfin[:],
                                                             op0=ALU.mult, op1=ALU.mult),
                     r=[xr, n["rstd"].res, gfin.res], w=[o_.res])
                k.dma("sp", y_d[tile * 128:(tile + 1) * 128, :], o_[:], r=[o_.res], w=[], sres=o_.res)

            for t2 in range(0, NTILE, 2):
                for _ in lockstep_g([fin_g(t2), fin_g(t2 + 1)]):
                    pass
            k.barrier()
    return nc, k.n_ins


def prep_shared(inp):
    f = np.float32
    g = lambda a: np.ascontiguousarray(np.asarray(a, dtype=f))
    sm = np.zeros((128, NSM), f)

    def put(col, vec, nch):
        sm[:, col:col + nch] = np.asarray(vec, f).reshape(nch, 128).T

    put(SM_MIX, inp["norm_mix"][0], 8)
    put(SM_CROSS, inp["norm_cross"][0], 8)
    put(SM_MEM, inp["norm_mem"][0], 8)
    put(SM_FFN, inp["norm_ffn"][0], 8)
    put(SM_GA, inp["norm_grp_a"][0], 4)
    put(SM_GB, inp["norm_grp_b"][0], 4)
    put(SM_CB, inp["conv_b"][0], 4)
    put(SM_BA, inp["gate_a_b"][0], 4)
    put(SM_BX, inp["gate_x_b"][0], 4)
    put(SM_LAM, inp["lru_lambda"][0], 4)
    cw = np.asarray(inp["conv_w"][0], f)
    for cc in range(4):
        for j in range(4):
            sm[:, SM_CW + cc * 4 + j] = cw[j, cc * 128:(cc + 1) * 128]
    gbd = np.zeros((128, 8, 128), f)
    for gi, key in enumerate(("gate_a_w", "gate_x_w")):
        w = np.asarray(inp[key][0], f)
        for cc in range(4):
            gbd[0:64, gi * 4 + cc, 0:64] = w[2 * cc]
            gbd[64:128, gi * 4 + cc, 64:128] = w[2 * cc + 1]
    rb = np.asarray(inp["rel_bias"][0], f)
    qi = np.arange(128)[:, None]
    kj = np.arange(640)[None, :]
    idx = np.clip(512 + qi - kj, -128, 128) + 128
    ab = rb[:, idx]
    valid = np.where(qi < 64, kj < 576, kj >= 64)
    ab = np.where(valid[None], ab, f(NEG)).astype(f)
    abias = np.ascontiguousarray(ab.transpose(1, 0, 2)).reshape(128, 8 * 640)
    sk = np.asarray(inp["sub_keys"][0], f)
    skT = np.ascontiguousarray(sk.reshape(16, 128, 128).transpose(2, 0, 1)).reshape(128, 16 * 128)
    u = np.asarray(inp["expert_u"][0], f)
    uT = np.ascontiguousarray(u.reshape(128, 128, 8, 128).transpose(0, 3, 2, 1)).reshape(16384, D)
    shared = {
        "w_in": g(inp["w_in"][0]), "w_out": g(inp["w_out"][0]), "w_q": g(inp["w_q_mem"][0]),
        "w_kv": g(inp["w_kv_mem"][0]), "w_o": g(inp["w_o_mem"][0]), "w_qry": g(inp["w_query"][0]),
        "smalls": sm, "gbd": gbd.reshape(128, 8 * 128), "abias": abias, "skT": skT, "uT": uT,
        "ev": g(inp["expert_v"][0]),
        "gfin": np.ascontiguousarray(np.broadcast_to(np.asarray(inp["norm_final"], f)[None, :], (128, D))),
        "ident": np.eye(128, dtype=f),
        "iota": np.ascontiguousarray(np.broadcast_to(np.arange(128, dtype=f)[None, :], (128, 128))),
    }
    return shared


def make_in_maps(inp, NT):
    x = np.asarray(inp["x"], np.float32)
    mem = np.asarray(inp["mem"], np.float32)
    B, S, _ = x.shape
    per_seq = S // NT
    NPRE = 3 * NT
    shared = prep_shared(inp)
    maps = []
    for c in range(B * per_seq):
        b, q = divmod(c, per_seq)
        xprev = np.zeros((NPRE, D), np.float32)
        if q > 0:
            xprev[NPRE - q * NT:] = x[b, 0:q * NT]
        pflag = np.zeros((128, NPRE // 512), np.float32)
        for j in range(NPRE // 512):
            if j * 512 >= NPRE - q * NT:
                pflag[:, j] = 1.0
        halob = np.full((128, 512), 0.0 if q > 0 else NEG, np.float32)
        m = dict(shared)
        m.update({"xown": np.ascontiguousarray(x[b, q * NT:(q + 1) * NT]), "xprev": xprev, "pflag": pflag,
                  "halob": halob, "mem": np.ascontiguousarray(mem[b])})
        maps.append(m)
    return maps


_CACHE = {}


def kernel(**inputs):
    NT = 2048
    if NT not in _CACHE:
        _CACHE[NT] = build(NT)[0]
    nc = _CACHE[NT]
    maps = make_in_maps(inputs, NT)
    res = run_bass_kernel_spmd(nc, maps, core_ids=list(range(N_CORES)))
    x = np.asarray(inputs["x"])
    B, S, _ = x.shape
    out = np.empty((B, S, D), np.float32)
    per_seq = S // NT
    for c in range(N_CORES):
        b, q = divmod(c, per_seq)
        out[b, q * NT:(q + 1) * NT] = res.results[c]["y"]
    return out
```
